# Optimizing a Trainium2 kernel written in Bass

```python
import jax
import jax.numpy as jnp
from jax import lax
import numpy as np

D_MODEL = 1024
BATCH = 8
SEQ = 4096
DEPTH = 2

CTX_LEN = 256
GRID_W = 64
EPS = 1e-6
N_MOD = 6

N_HEADS = 8
N_KV_HEADS = 2
GQA_GROUP = N_HEADS // N_KV_HEADS
HEAD_DIM = 64
WINDOW = 128
ATTN_BLOCK = 128
ATTN_W = N_HEADS * HEAD_DIM
KV_W = N_KV_HEADS * HEAD_DIM
ROPE_BASE = 10000.0
ROPE_FREQS = HEAD_DIM // 4

POOL_SIZES = (2, 4, 8, 16)
POOL_GROUP_W = 64
POOL_W = len(POOL_SIZES) * POOL_GROUP_W

SGU_CHUNK = 128
SGU_GROUPS = 4
SGU_W = 256
SGU_GROUP_W = SGU_W // SGU_GROUPS

N_BRANCH = 3
Q_OFF = 0
K_OFF = Q_OFF + ATTN_W
V_OFF = K_OFF + KV_W
POOL_OFF = V_OFF + KV_W
SGU_U_OFF = POOL_OFF + POOL_W
SGU_V_OFF = SGU_U_OFF + SGU_W
GATE_OFF = SGU_V_OFF + SGU_W
IN_W = GATE_OFF + N_BRANCH * D_MODEL

PEER_HEADS = 8
PEER_KEYS = 128
PEER_EXPERTS = PEER_KEYS * PEER_KEYS
PEER_KEY_DIM = 256
PEER_HALF = PEER_KEY_DIM // 2
PEER_TOPK = 16
PEER_BLOCK = 128

kernel_name = "hybrid_dit_window_attn_pool_sgu_peer"


def rms_norm(x, g):
    xf = x.astype(jnp.float32)
    y = xf * lax.rsqrt(jnp.mean(xf * xf, axis=-1, keepdims=True) + EPS)
    return (y * g.astype(jnp.float32)).astype(x.dtype)


def adaln_params(cond, w_mod, b_mod):
    m = jax.nn.silu(cond) @ w_mod + b_mod
    return jnp.split(m[:, None, :], N_MOD, axis=-1)


def modulate(x, g, shift, scale):
    return rms_norm(x, g) * (1.0 + scale) + shift


def axial_rope(length):
    rows = length // GRID_W
    row = jnp.repeat(jnp.arange(rows, dtype=jnp.float32), GRID_W)
    col = jnp.tile(jnp.arange(GRID_W, dtype=jnp.float32), rows)
    inv = ROPE_BASE ** (-jnp.arange(ROPE_FREQS, dtype=jnp.float32) / ROPE_FREQS)
    ang = jnp.stack([row[:, None] * inv, col[:, None] * inv], axis=1)
    return jnp.cos(ang), jnp.sin(ang)


def apply_rope(x, cos, sin):
    b, l, h, _ = x.shape
    xs = x.astype(jnp.float32).reshape(b, l, h, 2, 2, ROPE_FREQS)
    x1, x2 = xs[..., 0, :], xs[..., 1, :]
    c, s = cos[None, :, None], sin[None, :, None]
    out = jnp.stack([x1 * c - x2 * s, x2 * c + x1 * s], axis=-2)
    return out.reshape(b, l, h, HEAD_DIM).astype(x.dtype)


def attention_latent(q, k, v, k_ctx, v_ctx, sink):
    b, l = q.shape[:2]
    n_blocks = l // ATTN_BLOCK
    span = ATTN_BLOCK + 2 * WINDOW
    pad = ((0, 0), (WINDOW, WINDOW), (0, 0), (0, 0))
    kp, vp = jnp.pad(k, pad), jnp.pad(v, pad)
    qb = q.reshape(b, n_blocks, ATTN_BLOCK, N_KV_HEADS, GQA_GROUP, HEAD_DIM)
    qb = jnp.moveaxis(qb, 1, 0)
    scale = HEAD_DIM ** -0.5
    qi = jnp.arange(ATTN_BLOCK)[:, None]
    kj = jnp.arange(span)[None, :]
    in_window = jnp.abs(kj - WINDOW - qi) <= WINDOW
    sink_logit = jnp.broadcast_to(
        sink.astype(jnp.float32).reshape(N_KV_HEADS, GQA_GROUP)[None, :, :, None, None],
        (b, N_KV_HEADS, GQA_GROUP, ATTN_BLOCK, 1))
    n_ctx = k_ctx.shape[1]

    def one_block(args):
        n, q_blk = args
        start = n * ATTN_BLOCK
        k_win = lax.dynamic_slice_in_dim(kp, start, span, axis=1)
        v_win = lax.dynamic_slice_in_dim(vp, start, span, axis=1)
        kpos = start - WINDOW + kj
        valid = in_window & (kpos >= 0) & (kpos < l)
        s_win = jnp.einsum("bqhgd,bkhd->bhgqk", q_blk, k_win, preferred_element_type=jnp.float32) * scale
        s_win = jnp.where(valid, s_win, -jnp.inf)
        s_ctx = jnp.einsum("bqhgd,bkhd->bhgqk", q_blk, k_ctx, preferred_element_type=jnp.float32) * scale
        p = jax.nn.softmax(jnp.concatenate([s_win, s_ctx, sink_logit], axis=-1), axis=-1)
        o = jnp.einsum("bhgqk,bkhd->bqhgd", p[..., :span].astype(v.dtype), v_win)
        o = o + jnp.einsum("bhgqk,bkhd->bqhgd", p[..., span:span + n_ctx].astype(v.dtype), v_ctx)
        return o

    out = lax.map(one_block, (jnp.arange(n_blocks), qb))
    return jnp.moveaxis(out, 0, 1).reshape(b, l, ATTN_W)


def attention_context(q, k, v, sink):
    b, n_ctx = q.shape[:2]
    qg = q.reshape(b, n_ctx, N_KV_HEADS, GQA_GROUP, HEAD_DIM)
    s = jnp.einsum("bqhgd,bkhd->bhgqk", qg, k, preferred_element_type=jnp.float32) * HEAD_DIM ** -0.5
    sink_logit = jnp.broadcast_to(
        sink.astype(jnp.float32).reshape(N_KV_HEADS, GQA_GROUP)[None, :, :, None, None],
        (b, N_KV_HEADS, GQA_GROUP, n_ctx, 1))
    p = jax.nn.softmax(jnp.concatenate([s, sink_logit], axis=-1), axis=-1)
    o = jnp.einsum("bhgqk,bkhd->bqhgd", p[..., :n_ctx].astype(v.dtype), v)
    return o.reshape(b, n_ctx, ATTN_W)


def pool_mixer(z, pool_w, pool_scale):
    b, l, _ = z.shape
    zf = z.astype(jnp.float32)
    cs = jnp.pad(jnp.cumsum(zf, axis=1), ((0, 0), (1, 0), (0, 0)))
    t = jnp.arange(l)
    groups = []
    for g, size in enumerate(POOL_SIZES):
        lo = jnp.clip(t - size // 2, 0, l)
        hi = jnp.clip(t + size // 2, 0, l)
        sl = slice(g * POOL_GROUP_W, (g + 1) * POOL_GROUP_W)
        csg = cs[..., sl]
        mean = (csg[:, hi] - csg[:, lo]) / (hi - lo).astype(jnp.float32)[None, :, None]
        groups.append(mean - zf[..., sl])
    d = jnp.stack(groups, axis=2).astype(z.dtype)
    y = jnp.einsum("blgc,gce->blge", d, pool_w).reshape(b, l, POOL_W)
    return y * pool_scale


def sgu_mixer(u, v, sgu_w, sgu_b):
    b, l, _ = u.shape
    n_chunks = l // SGU_CHUNK
    vf = v.astype(jnp.float32)
    vn = (vf * lax.rsqrt(jnp.mean(vf * vf, axis=-1, keepdims=True) + EPS)).astype(v.dtype)
    vc = vn.reshape(b, n_chunks, SGU_CHUNK, SGU_GROUPS, SGU_GROUP_W)
    mixed = jnp.einsum("hpq,bnqhc->bnphc", sgu_w, vc) + sgu_b.T[None, None, :, :, None]
    return u * mixed.reshape(b, l, SGU_W)


def merge_branches(proj, y_attn, pool_w, pool_scale, sgu_w, sgu_b, w_br_attn, w_br_pool, w_br_sgu, w_out):
    y_pool = pool_mixer(proj[..., POOL_OFF:SGU_U_OFF], pool_w, pool_scale)
    y_sgu = sgu_mixer(jax.nn.gelu(proj[..., SGU_U_OFF:SGU_V_OFF]),
                      jax.nn.gelu(proj[..., SGU_V_OFF:GATE_OFF]), sgu_w, sgu_b)
    gates = jax.nn.sigmoid(proj[..., GATE_OFF:].reshape(*proj.shape[:-1], N_BRANCH, D_MODEL))
    merged = (gates[..., 0, :] * (y_attn @ w_br_attn)
              + gates[..., 1, :] * (y_pool @ w_br_pool)
              + gates[..., 2, :] * (y_sgu @ w_br_sgu))
    return merged @ w_out


def peer_ffn(h, w_q, subkeys, u_tab, v_tab):
    t = h.shape[0]
    blocks = h.reshape(t // PEER_BLOCK, PEER_BLOCK, D_MODEL)

    def one_block(xb):
        q = (xb @ w_q).reshape(PEER_BLOCK, PEER_HEADS, 2, PEER_HALF)
        s = jnp.einsum("thpd,hpkd->thpk", q, subkeys, preferred_element_type=jnp.float32)
        sv, si = lax.top_k(s, PEER_TOPK)
        n_cand = PEER_TOPK * PEER_TOPK
        cand_s = (sv[..., 0, :, None] + sv[..., 1, None, :]).reshape(PEER_BLOCK, PEER_HEADS, n_cand)
        cand_e = (si[..., 0, :, None] * PEER_KEYS + si[..., 1, None, :]).reshape(PEER_BLOCK, PEER_HEADS, n_cand)
        top_s, top_pos = lax.top_k(cand_s, PEER_TOPK)
        experts = jnp.take_along_axis(cand_e, top_pos, axis=-1)
        gate = jax.nn.softmax(top_s, axis=-1)
        u_sel = u_tab[experts]
        v_sel = v_tab[experts]
        act = jax.nn.gelu(jnp.einsum("td,thkd->thk", xb, u_sel, preferred_element_type=jnp.float32))
        return jnp.einsum("thk,thkd->td", (gate * act).astype(xb.dtype), v_sel)

    return lax.map(one_block, blocks).reshape(t, D_MODEL)


def setup_inputs(seed: int = 0) -> dict:
    key = jax.random.key(seed)
    ks = jax.random.split(key, 24)
    nrm = jax.random.normal
    f32 = jnp.float32
    L, D = DEPTH, D_MODEL
    return {
        "x": nrm(ks[0], (BATCH, SEQ, D), f32),
        "c": nrm(ks[1], (BATCH, D), f32),
        "ctx": nrm(ks[2], (BATCH, CTX_LEN, D), f32),
        "c_ctx": nrm(ks[3], (D,), f32),
        "w_mod": nrm(ks[4], (L, D, N_MOD * D), f32) * (0.5 * D ** -0.5),
        "b_mod": 0.02 * nrm(ks[5], (L, N_MOD * D), f32),
        "g_mix": 1.0 + 0.02 * nrm(ks[6], (L, D), f32),
        "g_ffn": 1.0 + 0.02 * nrm(ks[7], (L, D), f32),
        "w_in": nrm(ks[8], (L, D, IN_W), f32) * D ** -0.5,
        "attn_sink": 0.5 * nrm(ks[9], (L, N_HEADS), f32),
        "pool_w": nrm(ks[10], (L, len(POOL_SIZES), POOL_GROUP_W, POOL_GROUP_W), f32) * POOL_GROUP_W ** -0.5,
        "pool_scale": 1.0 + 0.02 * nrm(ks[11], (L, POOL_W), f32),
        "sgu_w": nrm(ks[12], (L, SGU_GROUPS, SGU_CHUNK, SGU_CHUNK), f32) * SGU_CHUNK ** -0.5,
        "sgu_b": 1.0 + 0.02 * nrm(ks[13], (L, SGU_GROUPS, SGU_CHUNK), f32),
        "w_br_attn": nrm(ks[14], (L, ATTN_W, D), f32) * ATTN_W ** -0.5,
        "w_br_pool": nrm(ks[15], (L, POOL_W, D), f32) * POOL_W ** -0.5,
        "w_br_sgu": nrm(ks[16], (L, SGU_W, D), f32) * SGU_W ** -0.5,
        "w_out": nrm(ks[17], (L, D, D), f32) * D ** -0.5,
        "peer_wq": nrm(ks[18], (L, D, PEER_HEADS * PEER_KEY_DIM), f32) * D ** -0.5,
        "peer_subkeys": nrm(ks[19], (L, PEER_HEADS, 2, PEER_KEYS, PEER_HALF), f32) * PEER_HALF ** -0.5,
        "peer_u": nrm(ks[20], (L, PEER_EXPERTS, D), f32) * D ** -0.5,
        "peer_v": 0.5 * nrm(ks[21], (L, PEER_EXPERTS, D), f32),
        "g_final": 1.0 + 0.02 * nrm(ks[22], (D,), f32),
    }


def reference(x, c, ctx, c_ctx, w_mod, b_mod, g_mix, g_ffn, w_in, attn_sink, pool_w, pool_scale,
              sgu_w, sgu_b, w_br_attn, w_br_pool, w_br_sgu, w_out, peer_wq, peer_subkeys,
              peer_u, peer_v, g_final):
    b, l, _ = x.shape
    n_ctx = ctx.shape[1]
    cos, sin = axial_rope(l)
    xc = ctx
    for layer in range(DEPTH):
        last = layer == DEPTH - 1
        sh1, sc1, gt1, sh2, sc2, gt2 = adaln_params(c, w_mod[layer], b_mod[layer])
        csh1, csc1, cgt1, csh2, csc2, cgt2 = adaln_params(c_ctx[None], w_mod[layer], b_mod[layer])

        h = modulate(x, g_mix[layer], sh1, sc1)
        hc = modulate(xc, g_mix[layer], csh1, csc1)
        proj = h @ w_in[layer]
        if last:
            kv_c = hc @ w_in[layer][:, K_OFF:POOL_OFF]
        else:
            proj_c = hc @ w_in[layer]
            kv_c = proj_c[..., K_OFF:POOL_OFF]
        k_c = kv_c[..., :KV_W].reshape(b, n_ctx, N_KV_HEADS, HEAD_DIM)
        v_c = kv_c[..., KV_W:].reshape(b, n_ctx, N_KV_HEADS, HEAD_DIM)

        q = apply_rope(proj[..., Q_OFF:K_OFF].reshape(b, l, N_HEADS, HEAD_DIM), cos, sin)
        k = apply_rope(proj[..., K_OFF:V_OFF].reshape(b, l, N_KV_HEADS, HEAD_DIM), cos, sin)
        v = proj[..., V_OFF:POOL_OFF].reshape(b, l, N_KV_HEADS, HEAD_DIM)
        y_attn = attention_latent(q, k, v, k_c, v_c, attn_sink[layer])
        x = x + gt1 * merge_branches(proj, y_attn, pool_w[layer], pool_scale[layer], sgu_w[layer],
                                     sgu_b[layer], w_br_attn[layer], w_br_pool[layer],
                                     w_br_sgu[layer], w_out[layer])
        if not last:
            q_c = proj_c[..., Q_OFF:K_OFF].reshape(b, n_ctx, N_HEADS, HEAD_DIM)
            y_attn_c = attention_context(q_c, k_c, v_c, attn_sink[layer])
            xc = xc + cgt1 * merge_branches(proj_c, y_attn_c, pool_w[layer], pool_scale[layer],
                                            sgu_w[layer], sgu_b[layer], w_br_attn[layer],
                                            w_br_pool[layer], w_br_sgu[layer], w_out[layer])

        h = modulate(x, g_ffn[layer], sh2, sc2)
        x = x + gt2 * peer_ffn(h.reshape(b * l, D_MODEL), peer_wq[layer], peer_subkeys[layer],
                               peer_u[layer], peer_v[layer]).reshape(b, l, D_MODEL)
        if not last:
            hc = modulate(xc, g_ffn[layer], csh2, csc2)
            xc = xc + cgt2 * peer_ffn(hc.reshape(b * n_ctx, D_MODEL), peer_wq[layer], peer_subkeys[layer],
                                      peer_u[layer], peer_v[layer]).reshape(b, n_ctx, D_MODEL)
    return rms_norm(x, g_final)
```

```python
import numpy as np
import concourse.bass as bass
import concourse.mybir as mybir
from concourse.bass_utils import run_bass_kernel_spmd
from contextlib import ExitStack

F32 = mybir.dt.float32
BF16 = mybir.dt.bfloat16
U32 = mybir.dt.uint32
AF = mybir.ActivationFunctionType
ALU = mybir.AluOpType
AX = mybir.AxisListType

D = 1024
CTX = 256
EPS = 1e-6
NEG = -30000.0
POOL_SIZES = (2, 4, 8, 16)
ND_SEM = 40
NGB = 16


class Prog:
    def __init__(self, nc, es):
        self.nc = nc
        self.eng = {"pe": nc.tensor, "dve": nc.vector, "act": nc.scalar, "pool": nc.gpsimd, "sp": nc.sync}
        self.sem = {}
        for e in self.eng:
            self.sem[e] = es.enter_context(nc.semaphore("s_" + e))
        for i in range(ND_SEM):
            self.sem[("d", i)] = es.enter_context(nc.semaphore("s_d%d" % i))
        self.cnt = {e: 0 for e in self.eng}
        self.seen = {e: {} for e in self.eng}
        self.lw = {}
        self.rd = {}
        self.dval = [0] * ND_SEM
        self.rr_rng = {"sp": (0, 24), "pool": (24, ND_SEM)}
        self.rr = {"sp": 0, "pool": 24}
        self.out_tokens = []
        self.n_ins = 0

    def _deps(self, e, r, w, extra=()):
        deps = {}

        def add(tok):
            sk, v = tok
            if sk == "pe" and e == "pe":
                return
            if self.seen[e].get(sk, 0) < v:
                if deps.get(sk, 0) < v:
                    deps[sk] = v

        for b in r:
            if b in self.lw:
                add(self.lw[b])
        for b in w:
            if b in self.lw:
                add(self.lw[b])
            for sk, v in self.rd.get(b, {}).items():
                add((sk, v))
        for t in extra:
            add(t)
        eng = self.eng[e]
        for sk, v in deps.items():
            eng.wait_ge(self.sem[sk], v)
            self.seen[e][sk] = v
            self.n_ins += 1

    def _commit(self, tok, r, w):
        for b in w:
            self.lw[b] = tok
            self.rd[b] = {}
        for b in r:
            d = self.rd.setdefault(b, {})
            if d.get(tok[0], 0) < tok[1]:
                d[tok[0]] = tok[1]

    def op(self, e, fn, r=(), w=()):
        self._deps(e, r, w)
        ins = fn()
        self.cnt[e] += 1
        ins.then_inc(self.sem[e], 1)
        self.n_ins += 1
        tok = (e, self.cnt[e])
        self._commit(tok, r, w)
        return tok

    def dma(self, e, fn, r=(), w=(), is_out=False):
        lo_, hi_ = self.rr_rng[e]
        idx = self.rr[e]
        self.rr[e] = lo_ + (idx + 1 - lo_) % (hi_ - lo_)
        sk = ("d", idx)
        extra = [(sk, self.dval[idx])] if self.dval[idx] > 0 else []
        self._deps(e, r, w, extra)
        ins = fn()
        self.dval[idx] += 16
        ins.then_inc(self.sem[sk], 16)
        self.n_ins += 1
        tok = (sk, self.dval[idx])
        self._commit(tok, r, w)
        if is_out:
            self.out_tokens.append(tok)
        return tok

    def barrier(self):
        cur = {e: self.cnt[e] for e in self.eng}
        for i in range(ND_SEM):
            cur[("d", i)] = self.dval[i]
        for e in self.eng:
            for sk, v in cur.items():
                if v > self.seen[e].get(sk, 0):
                    self.eng[e].wait_ge(self.sem[sk], v)
                    self.seen[e][sk] = v
                    self.n_ins += 1

    def finish(self):
        e = "sp"
        for sk, v in self.out_tokens:
            if self.seen[e].get(sk, 0) < v:
                self.eng[e].wait_ge(self.sem[sk], v)
                self.seen[e][sk] = v


def build(NTL=32, n_layers=2, dbg=False, peer_slots=128):
    T = NTL * 128
    TT = T + CTX
    NTILES = NTL + 2
    NGL = NTL // 4
    L = TT + 64
    groups = [(g, list(range(4 * g, 4 * g + 4))) for g in range(NGL)] + [(NGL, [NTL, NTL + 1])]

    def ppos(tok):
        return tok + 16 if tok < T else tok + 48

    nc = bass.Bass("TRN2", target_bir_lowering=False)

    def din(name, shape, dt=F32):
        return nc.dram_tensor(name, list(shape), dt, kind="ExternalInput").ap()

    def dscr(name, shape, dt=F32):
        kind = "ExternalOutput" if (dbg and name == "xs") else "Internal"
        return nc.dram_tensor(name, list(shape), dt, kind=kind).ap()

    x_in = din("x", [T, D])
    ctx_in = din("ctx", [CTX, D])
    cvec = din("cvec", [128, 2, 8])
    w_mod = din("w_mod", [2, D, 6 * D])
    b_mod_r = din("b_mod_r", [2, 128, 6 * D])
    g_mix_r = din("g_mix_r", [2, 128, D])
    g_ffn_r = din("g_ffn_r", [2, 128, D])
    g_fin_r = din("g_fin_r", [128, D])
    w1_d = din("w1", [2, D, 640])
    w2a_d = din("w2a", [2, D, 1536])
    w2g_d = din("w2g", [2, D, 3072])
    ropeC = din("ropeC", [128, TT])
    ropeS = din("ropeS", [128, TT])
    sink_r = din("sink_r", [2, 1, 2, 512])
    pwbd_d = din("pwbd", [2, 128, 2, 128])
    pscale_d = din("pscale", [2, 128, 2])
    rc_d = din("rc", [128, 2, TT])
    sguwT_d = din("sgu_wT", [2, 128, 4, 128])
    sgub_d = din("sgu_b_r", [2, 64, 4, 128])
    wbrA_d = din("wbrA", [2, 64, 8, D])
    wbrP_d = din("wbrP", [2, 128, 2, D])
    wbrS_d = din("wbrS", [2, 64, 4, D])
    wout_d = din("wout", [2, 128, 8, D])
    wqT_d = din("wqT", [2, 128, 16, D])
    subkT_d = din("subkT", [2, 128, 16, 128])
    peer_u = [din("peer_u%d" % l, [16384, D]) for l in range(2)]
    peer_v = [din("peer_v%d" % l, [16384, D]) for l in range(2)]
    mask_d = din("maskAB", [128, 2, 512])
    ident_d = din("ident", [128, 128])
    iota_d = din("iota16", [128, 16])
    y_out = nc.dram_tensor("y", [T, D], F32, kind="ExternalOutput").ap()

    xs = dscr("xs", [TT, D])
    hTd = dscr("hTd", [NGL + 1, 128, 8, 512], BF16)
    yTd = dscr("yTd", [NGL + 1, 64, 8, 512], BF16)
    ysTd = dscr("ysTd", [NGL + 1, 64, 4, 512], BF16)
    ypTd = dscr("ypTd", [NGL + 1, 128, 2, 512], BF16)
    uvb = [dscr("uvb%d" % l, [16384, 2 * D], BF16) for l in range(2)]

    with ExitStack() as es:
        P = Prog(nc, es)
        V, A, G, PE, SP = nc.vector, nc.scalar, nc.gpsimd, nc.tensor, nc.sync

        uid = [0]

        def sb(es_, name, shape, dt=F32):
            uid[0] += 1
            return es_.enter_context(nc.sbuf_tensor("sb%d_%s" % (uid[0], name), list(shape), dt))

        PS = es.enter_context(nc.psum_tensor("ps", [128, 8, 512], F32))
        bank_ptr = [0]

        def bank(n=1):
            if n > 1 and bank_ptr[0] % n:
                bank_ptr[0] += n - bank_ptr[0] % n
            b = bank_ptr[0] % 8
            bank_ptr[0] = (bank_ptr[0] + n)
            return b

        identf = sb(es, "identf", [128, 128])
        identb = sb(es, "identb", [128, 128], BF16)
        maskb = sb(es, "maskb", [128, 2, 512], BF16)
        ones_b = sb(es, "ones_b", [128, 64], BF16)
        iota16 = sb(es, "iota16", [128, 16])
        screp = sb(es, "screp", [128, 2, 8, 128])
        with ExitStack() as e0:
            maskf = sb(e0, "maskf", [128, 2, 512])
            cv = sb(e0, "cv", [128, 2, 8])
            cs = sb(e0, "cs", [128, 2, 8])
            P.dma("sp", lambda: SP.dma_start(out=identf[:], in_=ident_d[:, :]), w=["identf"])
            P.dma("sp", lambda: SP.dma_start(out=maskf[:], in_=mask_d[:, :, :]), w=["maskf"])
            P.dma("sp", lambda: SP.dma_start(out=iota16[:], in_=iota_d[:, :]), w=["iota16"])
            P.dma("sp", lambda: SP.dma_start(out=cv[:], in_=cvec[:, :, :]), w=["cv"])
            P.dma("sp", lambda: SP.dma_start(out=xs[0:T, :], in_=x_in[:, :]), w=[("xs", t) for t in range(NTL)])
            P.dma("sp", lambda: SP.dma_start(out=xs[T:TT, :], in_=ctx_in[:, :]), w=[("xs", NTL), ("xs", NTL + 1)])
            P.op("dve", lambda: V.tensor_copy(out=identb[:], in_=identf[:]), r=["identf"], w=["identb"])
            P.op("dve", lambda: V.tensor_copy(out=maskb[:], in_=maskf[:]), r=["maskf"], w=["maskb"])
            P.op("dve", lambda: V.memset(ones_b[:], 1.0), w=["ones_b"])
            P.op("act", lambda: A.activation(out=cs[:], in_=cv[:], func=AF.Silu), r=["cv"], w=["cs"])
            P.op("dve", lambda: V.tensor_copy(out=screp[:], in_=cs[:].unsqueeze(3).broadcast_to([128, 2, 8, 128])),
                 r=["cs"], w=["screp"])
            P.barrier()

        def modulation(layer, which, mod, g_r):
            base = which * 3 * D
            with ExitStack() as em:
                wm = [sb(em, "wm%d" % i, [128, 8, 512]) for i in range(2)]
                bm = [sb(em, "bm%d" % i, [128, 512]) for i in range(2)]
                gs = [sb(em, "gs%d" % i, [128, 512]) for i in range(2)]
                tm = [sb(em, "tm%d" % i, [128, 512]) for i in range(2)]
                for ci in range(6):
                    kind, half = ci // 2, ci % 2
                    c0 = base + ci * 512
                    j = ci % 2
                    P.dma("sp", lambda: SP.dma_start(
                        out=wm[j][:], in_=w_mod[layer, :, c0:c0 + 512].rearrange("(k p) n -> p k n", p=128)),
                        w=[("wm", j)])
                    P.dma("sp", lambda: SP.dma_start(out=bm[j][:], in_=b_mod_r[layer, :, c0:c0 + 512]), w=[("bm", j)])
                    if kind == 1:
                        P.dma("sp", lambda: SP.dma_start(out=gs[j][:], in_=g_r[layer, :, half * 512:(half + 1) * 512]),
                              w=[("gs", j)])
                    for s in range(2):
                        b = bank()

                        def mm():
                            for k in range(8):
                                ins = PE.matmul(PS[:, b, :], lhsT=screp[:, s, k, :], rhs=wm[j][:, k, :],
                                                start=(k == 0), stop=(k == 7))
                            return ins
                        P.op("pe", mm, r=["screp", ("wm", j)], w=[("ps", b)])
                        dst_kind = {0: 1, 1: 0, 2: 2}[kind]
                        dst = mod[s][dst_kind][:, half * 512:(half + 1) * 512]
                        dname = ("mod", which, s, dst_kind, half)
                        if kind == 1:
                            P.op("dve", lambda: V.tensor_tensor(out=tm[s][:], in0=PS[:, b, :], in1=bm[j][:], op=ALU.add),
                                 r=[("ps", b), ("bm", j)], w=[("tm", s)])
                            P.op("dve", lambda: V.scalar_tensor_tensor(out=dst, in0=tm[s][:], scalar=1.0, in1=gs[j][:],
                                                                       op0=ALU.add, op1=ALU.mult),
                                 r=[("tm", s), ("gs", j)], w=[dname])
                        else:
                            P.op("dve", lambda: V.tensor_tensor(out=dst, in0=PS[:, b, :], in1=bm[j][:], op=ALU.add),
                                 r=[("ps", b), ("bm", j)], w=[dname])
                P.barrier()

        def modnames(which, s):
            return [("mod", which, s, k, h) for k in range(3) for h in range(2)]

        def load_cast(es_, name, dram_ap, shape, stage_bufs, dst=None, eng_cycle=("act", "pool")):
            raise NotImplementedError

        def norm_mod(xt, xname, Amod, Bmod, modr, junk, ss, hf_out, hb_out, hname, add_eng="pool"):
            P.op("dve", lambda: V.scalar_tensor_tensor(out=junk[:], in0=xt[:], scalar=1.0, in1=xt[:], op0=ALU.mult, op1=ALU.mult, accum_out=ss[:, 0:1]),
                 r=[xname], w=["junk", "ss0"])
            P.op("dve", lambda: V.tensor_scalar(out=ss[:, 1:2], in0=ss[:, 0:1], scalar1=1.0 / D, scalar2=EPS,
                                                op0=ALU.mult, op1=ALU.add), r=["ss0"], w=["ss1"])
            P.op("act", lambda: A.activation(out=ss[:, 2:3], in_=ss[:, 1:2], func=AF.Ln), r=["ss1"], w=["ss2"])
            P.op("act", lambda: A.activation(out=ss[:, 3:4], in_=ss[:, 2:3], func=AF.Exp, scale=-0.5), r=["ss2"], w=["ss3"])
            P.op("dve", lambda: V.scalar_tensor_tensor(out=junk[:], in0=xt[:], scalar=ss[:, 3:4], in1=Amod[:],
                                                       op0=ALU.mult, op1=ALU.mult),
                 r=[xname, "ss3"] + modr, w=["junk"])
            if hf_out is not None:
                P.op("dve", lambda: V.tensor_tensor(out=hf_out[:], in0=junk[:], in1=Bmod[:], op=ALU.add),
                     r=["junk"] + modr, w=[hname + "f"])
                P.op("act", lambda: A.copy(out=hb_out[:], in_=hf_out[:]), r=[hname + "f"], w=[hname])
            elif add_eng == "dve":
                P.op("dve", lambda: V.tensor_tensor(out=hb_out[:], in0=junk[:], in1=Bmod[:], op=ALU.add),
                     r=["junk"] + modr, w=[hname])
            else:
                P.op("pool", lambda: G.tensor_tensor(out=hb_out[:], in0=junk[:], in1=Bmod[:], op=ALU.add),
                     r=["junk"] + modr, w=[hname])

        def transpose8(hb, hname, dst_ap, dname, evac="act", b=None):
            if b is None:
                b = bank()
            psb = PS[:, b, :].bitcast(BF16)

            def tr():
                for k in range(8):
                    ins = PE.transpose(out=psb[:, k * 128:(k + 1) * 128], in_=hb[:, k * 128:(k + 1) * 128],
                                       identity=identb[:])
                return ins
            P.op("pe", tr, r=[hname, "identb"], w=[("ps", b)])
            src = psb.rearrange("p (k t) -> p k t", k=8)
            if evac == "act":
                P.op("act", lambda: A.copy(out=dst_ap, in_=src), r=[("ps", b)], w=[dname])
            else:
                P.op("dve", lambda: V.tensor_copy(out=dst_ap, in_=src), r=[("ps", b)], w=[dname])

        def cast_load(dst_tile_ap, dname, src_ap, stg, sname, ceng):
            P.dma("sp", lambda: SP.dma_start(out=stg, in_=src_ap), w=[sname])
            if ceng == "act":
                P.op("act", lambda: A.copy(out=dst_tile_ap, in_=stg), r=[sname], w=[dname])
            elif ceng == "pool":
                P.op("pool", lambda: G.tensor_copy(out=dst_tile_ap, in_=stg), r=[sname], w=[dname])
            else:
                P.op("dve", lambda: V.tensor_copy(out=dst_tile_ap, in_=stg), r=[sname], w=[dname])

        NQ = 16

        def tabnames(l):
            return [("uvb", l, c0, q) for c0 in (0, D) for q in range(NQ)]

        def conv_gen(l):
            rpq = 16384 // NQ
            for q in range(NQ):
                for (src_t, c0) in ((peer_u[l], 0), (peer_v[l], D)):
                    rows = slice(q * rpq, (q + 1) * rpq)
                    P.dma("pool", lambda: G.dma_start(out=uvb[l][rows, c0:c0 + D], in_=src_t[rows, :]), w=[("uvb", l, c0, q)])
                    yield
        convs = [conv_gen(l) for l in range(n_layers)]

        def pump(l, k):
            if l < n_layers:
                for _ in range(k):
                    next(convs[l], None)

        for layer in range(n_layers):
            last = layer == n_layers - 1
            with ExitStack() as eL:
                modm = [[sb(eL, "modm%d%d" % (s, k), [128, D]) for k in range(3)] for s in range(2)]
                eKV = ExitStack()
                kT = sb(eKV, "kT", [128, TT], BF16)
                vtok = sb(eKV, "vtok", [128, NTILES, 128], BF16)
                dT = sb(eKV, "dT", [128, 2, TT], BF16)
                sinkrow = sb(eKV, "sinkrow", [1, 2, 512], BF16)
                with ExitStack() as e1:
                    sk_f = sb(e1, "sk_f", [1, 2, 512])
                    sk_e = sb(e1, "sk_e", [1, 2, 512])
                    P.dma("sp", lambda: SP.dma_start(out=sk_f[:], in_=sink_r[layer, :, :, :]), w=["sk_f"])
                    P.op("act", lambda: A.activation(out=sk_e[:], in_=sk_f[:], func=AF.Exp), r=["sk_f"], w=["sk_e"])
                    P.op("act", lambda: A.copy(out=sinkrow[:], in_=sk_e[:]), r=["sk_e"], w=["sinkrow"])
                    P.barrier()
                modulation(layer, 0, modm, g_mix_r)

                with ExitStack() as e1:
                    zpad = sb(e1, "zpad", [128, 2, L])
                    e1w = ExitStack()
                    w1 = sb(e1w, "w1", [128, 8, 640], BF16)
                    stg = [sb(e1w, "stg%d" % i, [128, 2, 640]) for i in range(2)]
                    xt_ = [sb(e1w, "xt%d" % i, [128, D]) for i in range(2)]
                    junk = sb(e1w, "junk", [128, D])
                    ss = sb(e1w, "ss", [128, 4])
                    hb_ = [sb(e1w, "hb%d" % i, [128, D], BF16) for i in range(2)]
                    hTg = [sb(e1w, "hTg%d" % i, [128, 8, 512], BF16) for i in range(2)]
                    cS = [sb(e1w, "cS%d" % i, [128, 2, 512]) for i in range(2)]
                    t1 = sb(e1w, "t1", [128, 512])
                    t2 = sb(e1w, "t2", [128, 512])
                    for i in range(4):
                        cast_load(w1[:, 2 * i:2 * i + 2, :], ("w1", i),
                                  w1_d[layer, 256 * i:256 * (i + 1), :].rearrange("(k p) n -> p k n", p=128),
                                  stg[i % 2][:], ("stg", i % 2), "act")
                    P.op("dve", lambda: V.memset(zpad[:], 0.0), w=["zpad"])
                    for gi, tiles in groups:
                        j = gi % 2
                        ng = len(tiles) * 128
                        s = 0 if tiles[0] < NTL else 1
                        tok0 = tiles[0] * 128
                        for ti, tile in enumerate(tiles):
                            jj = tile % 2
                            P.dma("sp", lambda: SP.dma_start(out=xt_[jj][:], in_=xs[tile * 128:(tile + 1) * 128, :]),
                                  r=[("xs", tile)], w=[("xt", jj)])
                            norm_mod(xt_[jj], ("xt", jj), modm[s][0], modm[s][1], modnames(0, s), junk, ss, None,
                                     hb_[jj], ("hb", jj), add_eng="dve")
                            transpose8(hb_[jj], ("hb", jj), hTg[j][:, :, ti * 128:(ti + 1) * 128], ("hTg", j, ti))
                        hr = [("hTg", j, ti) for ti in range(len(tiles))]
                        P.dma("sp", lambda: SP.dma_start(out=cS[j][:, 0, 0:ng], in_=ropeC[:, tok0:tok0 + ng]), w=[("cS", j, 0)])
                        P.dma("sp", lambda: SP.dma_start(out=cS[j][:, 1, 0:ng], in_=ropeS[:, tok0:tok0 + ng]), w=[("cS", j, 1)])
                        if layer == 0:
                            pump(0, 4)
                        bk, bkr = bank(), bank()
                        for (bb, c0) in ((bk, 0), (bkr, 128)):
                            def mm(bb=bb, c0=c0):
                                for k in range(8):
                                    ins = PE.matmul(PS[:, bb, 0:ng], lhsT=w1[:, k, c0:c0 + 128], rhs=hTg[j][:, k, 0:ng],
                                                    start=(k == 0), stop=(k == 7))
                                return ins
                            P.op("pe", mm, r=hr + [("w1", i_) for i_ in range(4)], w=[("ps", bb)])
                        P.op("dve", lambda: V.tensor_tensor(out=t1[:, 0:ng], in0=PS[:, bkr, 0:ng], in1=cS[j][:, 1, 0:ng], op=ALU.mult),
                             r=[("ps", bkr), ("cS", j, 1)], w=["t1"])
                        P.op("dve", lambda: V.tensor_tensor(out=t2[:, 0:ng], in0=PS[:, bk, 0:ng], in1=cS[j][:, 0, 0:ng], op=ALU.mult),
                             r=[("ps", bk), ("cS", j, 0)], w=["t2"])
                        P.op("dve", lambda: V.tensor_tensor(out=kT[:, tok0:tok0 + ng], in0=t1[:, 0:ng], in1=t2[:, 0:ng], op=ALU.add),
                             r=["t1", "t2"], w=[("kT", gi)])
                        for c in range(2):
                            bz = bank()

                            def mm(bz=bz, c=c):
                                for k in range(8):
                                    ins = PE.matmul(PS[:, bz, 0:ng], lhsT=w1[:, k, 384 + c * 128:512 + c * 128],
                                                    rhs=hTg[j][:, k, 0:ng], start=(k == 0), stop=(k == 7))
                                return ins
                            P.op("pe", mm, r=hr + [("w1", i_) for i_ in range(4)], w=[("ps", bz)])
                            p0 = ppos(tok0)
                            P.op("act", lambda: A.copy(out=zpad[:, c, p0:p0 + ng], in_=PS[:, bz, 0:ng]),
                                 r=[("ps", bz)], w=["zpad"])
                        bv = bank()

                        def mm():
                            for ti in range(len(tiles)):
                                for k in range(8):
                                    ins = PE.matmul(PS[:, bv, ti * 128:(ti + 1) * 128], lhsT=hTg[j][:, k, ti * 128:(ti + 1) * 128],
                                                    rhs=w1[:, k, 256:384], start=(k == 0), stop=(k == 7))
                            return ins
                        P.op("pe", mm, r=hr + [("w1", i_) for i_ in range(4)], w=[("ps", bv)])
                        nt = len(tiles)
                        P.op("act", lambda: A.copy(out=vtok[:, tiles[0]:tiles[0] + nt, :],
                                                   in_=PS[:, bv, 0:ng].rearrange("p (a b) -> p a b", a=nt)),
                             r=[("ps", bv)], w=[("vtok", gi)])

                    P.barrier()
                    e1w.close()
                    PA = sb(e1, "PA", [128, L])
                    PB = sb(e1, "PB", [128, L])
                    P.op("dve", lambda: V.memset(PA[:], 0.0), w=["PA"])
                    P.op("dve", lambda: V.memset(PB[:], 0.0), w=["PB"])
                    rc = sb(e1, "rc", [128, 2, TT])
                    P.dma("sp", lambda: SP.dma_start(out=rc[:], in_=rc_d[:, :, :]), w=["rc"])
                    lo, hi = 8, L - 8

                    def shadd(dst, src, s1, s2, p0=0, p1=128):
                        P.op("dve", lambda: V.tensor_tensor(out=dst[p0:p1, lo:hi], in0=src[p0:p1, lo + s1:hi + s1],
                                                            in1=src[p0:p1, lo + s2:hi + s2], op=ALU.add),
                             r=["PA", "PB", "zpad"], w=["PA", "PB"])
                    segs = [(0, T, 16), (T, TT, 48)]

                    def dfin(src, c, p0, p1):
                        for (a, b_, off) in segs:
                            P.op("dve", lambda: V.tensor_tensor(out=src[p0:p1, a + off:b_ + off], in0=src[p0:p1, a + off:b_ + off],
                                                                in1=rc[p0:p1, c, a:b_], op=ALU.mult),
                                 r=["PA", "PB", "rc"], w=["PA", "PB"])
                            P.op("dve", lambda: V.tensor_tensor(out=dT[p0:p1, c, a:b_], in0=src[p0:p1, a + off:b_ + off],
                                                                in1=zpad[p0:p1, c, a + off:b_ + off], op=ALU.subtract),
                                 r=["PA", "PB", "zpad"], w=["dT"])
                    shadd(PA, zpad[:, 0, :], -1, 0)
                    shadd(PB, PA, -1, 1, 64, 128)
                    dfin(PA, 0, 0, 64)
                    dfin(PB, 0, 64, 128)
                    shadd(PA, zpad[:, 1, :], -1, 0)
                    shadd(PB, PA, -1, 1)
                    shadd(PA, PB, -2, 2)
                    shadd(PB, PA, -4, 4, 64, 128)
                    dfin(PA, 1, 0, 64)
                    dfin(PB, 1, 64, 128)
                    P.barrier()

                allk = [("kT", gi) for gi, _ in groups]
                allv = [("vtok", gi) for gi, _ in groups]
                m2_groups = groups if not last else groups[:NGL]
                with ExitStack() as e2:
                    w2a = sb(e2, "w2a", [128, 8, 1536], BF16)
                    stg = [sb(e2, "stgA%d" % i, [128, 1, 1536]) for i in range(2)]
                    pwbd = sb(e2, "pwbd", [128, 2, 128], BF16)
                    pwf = sb(e2, "pwf", [128, 2, 128])
                    pscale = sb(e2, "pscale", [128, 2])
                    sguwT = sb(e2, "sguwT", [128, 4, 128], BF16)
                    sguf = sb(e2, "sguf", [128, 4, 128])
                    sgub = sb(e2, "sgub", [64, 4, 128])
                    xt_ = [sb(e2, "xtA%d" % i, [128, D]) for i in range(1)] * 2
                    junk = sb(e2, "junkA", [128, D])
                    ss = sb(e2, "ssA", [128, 4])
                    hb_ = [sb(e2, "hbA%d" % i, [128, D], BF16) for i in range(1)] * 2
                    hTg = [sb(e2, "hTgA%d" % i, [128, 8, 512], BF16) for i in range(1)] * 2
                    cS = [sb(e2, "cSA%d" % i, [128, 2, 512]) for i in range(1)] * 2
                    t1 = sb(e2, "t1A", [128, 512])
                    t2 = sb(e2, "t2A", [128, 512])
                    qT = sb(e2, "qT", [128, 4, 512], BF16)
                    pT = [sb(e2, "pT%d" % i, [128, 512], BF16) for i in range(6)]
                    lnd = sb(e2, "lnd", [64, 512])
                    rec = sb(e2, "rec", [64, 512])
                    yT = sb(e2, "yT", [64, 8, 512], BF16)
                    uT = sb(e2, "uT", [64, 4, 512], BF16)
                    vg = sb(e2, "vg", [128, 256])
                    vn = sb(e2, "vn", [128, 256], BF16)
                    sv = sb(e2, "svA", [128, 4])
                    ysT = sb(e2, "ysT", [64, 4, 512], BF16)
                    ypT = sb(e2, "ypT", [128, 2, 512], BF16)
                    tsg = sb(e2, "tsg", [64, 512])
                    for i in range(8):
                        cast_load(w2a[:, i:i + 1, :], ("w2a", i),
                                  w2a_d[layer, 128 * i:128 * (i + 1), :].rearrange("(k p) n -> p k n", p=128),
                                  stg[i % 2][:], ("stgA", i % 2), "act" if i % 2 == 0 else "pool")
                    w2r = [("w2a", i) for i in range(8)]
                    cast_load(pwbd[:], "pwbd", pwbd_d[layer, :, :, :], pwf[:], "pwf", "dve")
                    cast_load(sguwT[:], "sguwT", sguwT_d[layer, :, :, :], sguf[:], "sguf", "dve")
                    P.dma("sp", lambda: SP.dma_start(out=pscale[:], in_=pscale_d[layer, :, :]), w=["pscale"])
                    P.dma("sp", lambda: SP.dma_start(out=sgub[:], in_=sgub_d[layer, :, :, :]), w=["sgub"])
                    pti = [0]
                    for gi, tiles in m2_groups:
                        j = 0
                        nt = len(tiles)
                        ng = nt * 128
                        s = 0 if tiles[0] < NTL else 1
                        tok0 = tiles[0] * 128
                        for ti, tile in enumerate(tiles):
                            jj = 0
                            P.dma("sp", lambda: SP.dma_start(out=xt_[jj][:], in_=xs[tile * 128:(tile + 1) * 128, :]),
                                  r=[("xs", tile)], w=[("xt", jj)])
                            norm_mod(xt_[jj], ("xt", jj), modm[s][0], modm[s][1], modnames(0, s), junk, ss, None,
                                     hb_[jj], ("hb", jj))
                            transpose8(hb_[jj], ("hb", jj), hTg[j][:, :, ti * 128:(ti + 1) * 128], ("hTg", j, ti))
                        hr = [("hTg", j, ti) for ti in range(nt)]
                        P.dma("sp", lambda: SP.dma_start(out=hTd[gi, :, :, 0:ng], in_=hTg[j][:, :, 0:ng]), r=hr, w=[("hTd", gi)])
                        P.dma("sp", lambda: SP.dma_start(out=cS[j][:, 0, 0:ng], in_=ropeC[:, tok0:tok0 + ng]), w=[("cS", j, 0)])
                        P.dma("sp", lambda: SP.dma_start(out=cS[j][:, 1, 0:ng], in_=ropeS[:, tok0:tok0 + ng]), w=[("cS", j, 1)])
                        for c in range(4):
                            bq, bqr = bank(), bank()
                            for (bb, c0) in ((bq, c * 128), (bqr, 512 + c * 128)):
                                def mm(bb=bb, c0=c0):
                                    for k in range(8):
                                        ins = PE.matmul(PS[:, bb, 0:ng], lhsT=w2a[:, k, c0:c0 + 128], rhs=hTg[j][:, k, 0:ng],
                                                        start=(k == 0), stop=(k == 7))
                                    return ins
                                P.op("pe", mm, r=hr + w2r, w=[("ps", bb)])
                            P.op("dve", lambda: V.tensor_tensor(out=t1[:, 0:ng], in0=PS[:, bqr, 0:ng], in1=cS[j][:, 1, 0:ng], op=ALU.mult),
                                 r=[("ps", bqr), ("cS", j, 1)], w=["t1"])
                            P.op("dve", lambda: V.tensor_tensor(out=t2[:, 0:ng], in0=PS[:, bq, 0:ng], in1=cS[j][:, 0, 0:ng], op=ALU.mult),
                                 r=[("ps", bq), ("cS", j, 0)], w=["t2"])
                            P.op("dve", lambda: V.tensor_tensor(out=qT[:, c, 0:ng], in0=t1[:, 0:ng], in1=t2[:, 0:ng], op=ALU.add),
                                 r=["t1", "t2"], w=[("qT", c)])
                        qr_ = [("qT", c) for c in range(4)]
                        if layer == 0:
                            pump(1, 4)
                        for ti, tile in enumerate(tiles):
                            if tile < NTL:
                                keys = []
                                if tile - 1 >= 0:
                                    keys.append((tile - 1, 0))
                                keys.append((tile, None))
                                if tile + 1 < NTL:
                                    keys.append((tile + 1, 1))
                                keys += [(NTL, None), (NTL + 1, None)]
                            else:
                                keys = [(NTL, None), (NTL + 1, None)]
                            for hk in range(2):
                                h0, h1 = hk * 64, hk * 64 + 64
                                bnum, bden = bank(), bank()
                                sbanks = []
                                for (kt, mk) in keys:
                                    bs = bank()
                                    sbanks.append(bs)

                                    def mm(bs=bs, kt=kt, mk=mk):
                                        ins = PE.matmul(PS[:, bs, :], lhsT=kT[h0:h1, kt * 128:(kt + 1) * 128],
                                                        rhs=qT[h0:h1, :, ti * 128:(ti + 1) * 128],
                                                        start=True, stop=(mk is None))
                                        if mk is not None:
                                            ins = PE.matmul(PS[:, bs, :], lhsT=identb[:], rhs=maskb[:, mk, :], start=False, stop=True)
                                        return ins
                                    P.op("pe", mm, r=allk + qr_ + ["identb", "maskb"], w=[("ps", bs)])
                                pts = []
                                for bs in sbanks:
                                    pi = pti[0] % 6
                                    pti[0] += 1
                                    pts.append(pi)
                                    P.op("act", lambda bs=bs, pi=pi: A.activation(out=pT[pi][:], in_=PS[:, bs, :], func=AF.Exp, scale=0.125),
                                         r=[("ps", bs)], w=[("pT", pi)])
                                nk = len(keys)
                                for ki, ((kt, mk), pi) in enumerate(zip(keys, pts)):
                                    def mm(ki=ki, kt=kt, pi=pi):
                                        PE.matmul(PS[0:64, bnum, :], lhsT=vtok[:, kt, h0:h1], rhs=pT[pi][:],
                                                  start=(ki == 0), stop=(ki == nk - 1))
                                        ins = PE.matmul(PS[0:64, bden, :], lhsT=ones_b[:, 0:64], rhs=pT[pi][:],
                                                        start=(ki == 0), stop=False)
                                        if ki == nk - 1:
                                            ins = PE.matmul(PS[0:64, bden, :], lhsT=ones_b[0:1, 0:64], rhs=sinkrow[0:1, hk, :],
                                                            start=False, stop=True)
                                        return ins
                                    P.op("pe", mm, r=allv + [("pT", pi), "ones_b", "sinkrow"], w=[("ps", bnum), ("ps", bden)])
                                P.op("act", lambda: A.activation(out=lnd[:], in_=PS[0:64, bden, :], func=AF.Ln),
                                     r=[("ps", bden)], w=["lnd"])
                                P.op("act", lambda: A.activation(out=rec[:], in_=lnd[:], func=AF.Exp, scale=-1.0),
                                     r=["lnd"], w=["rec"])
                                P.op("dve", lambda: V.tensor_tensor(
                                    out=yT[:, hk * 4:hk * 4 + 4, ti * 128:(ti + 1) * 128],
                                    in0=PS[0:64, bnum, :].rearrange("p (g q) -> p g q", g=4),
                                    in1=rec[:].rearrange("p (g q) -> p g q", g=4), op=ALU.mult),
                                    r=[("ps", bnum), "rec"], w=[("yT", ti, hk)])
                        for h4 in range(4):
                            bu = bank()

                            def mm():
                                for k in range(8):
                                    ins = PE.matmul(PS[0:64, bu, 0:ng], lhsT=w2a[:, k, 1024 + h4 * 64:1088 + h4 * 64],
                                                    rhs=hTg[j][:, k, 0:ng], start=(k == 0), stop=(k == 7))
                                return ins
                            P.op("pe", mm, r=hr + w2r, w=[("ps", bu)])
                            P.op("act", lambda: A.activation(out=uT[:, h4, 0:ng], in_=PS[0:64, bu, 0:ng], func=AF.Gelu_apprx_tanh),
                                 r=[("ps", bu)], w=[("uT", h4)])
                        for ti, tile in enumerate(tiles):
                            bvs = bank()

                            def mm():
                                for k in range(8):
                                    ins = PE.matmul(PS[:, bvs, 0:256], lhsT=hTg[j][:, k, ti * 128:(ti + 1) * 128],
                                                    rhs=w2a[:, k, 1280:1536], start=(k == 0), stop=(k == 7))
                                return ins
                            P.op("pe", mm, r=hr + w2r, w=[("ps", bvs)])
                            P.op("act", lambda: A.activation(out=vg[:], in_=PS[:, bvs, 0:256], func=AF.Gelu_apprx_tanh),
                                 r=[("ps", bvs)], w=["vg"])
                            P.op("dve", lambda: V.scalar_tensor_tensor(out=junk[:, 0:256], in0=vg[:], scalar=1.0, in1=vg[:],
                                                                        op0=ALU.mult, op1=ALU.mult, accum_out=sv[:, 0:1]),
                                 r=["vg"], w=["junk", "sv0"])
                            P.op("dve", lambda: V.tensor_scalar(out=sv[:, 1:2], in0=sv[:, 0:1], scalar1=1.0 / 256, scalar2=EPS,
                                                                op0=ALU.mult, op1=ALU.add), r=["sv0"], w=["sv1"])
                            P.op("act", lambda: A.activation(out=sv[:, 2:3], in_=sv[:, 1:2], func=AF.Ln), r=["sv1"], w=["sv2"])
                            P.op("act", lambda: A.activation(out=sv[:, 3:4], in_=sv[:, 2:3], func=AF.Exp, scale=-0.5), r=["sv2"], w=["sv3"])
                            P.op("dve", lambda: V.tensor_scalar(out=vn[:], in0=vg[:], scalar1=sv[:, 3:4], scalar2=None, op0=ALU.mult),
                                 r=["vg", "sv3"], w=["vn"])
                            bm_ = bank()

                            def mm():
                                for h4 in range(4):
                                    ins = PE.matmul(PS[0:64, bm_, h4 * 128:(h4 + 1) * 128], lhsT=vn[:, h4 * 64:(h4 + 1) * 64],
                                                    rhs=sguwT[:, h4, :], start=True, stop=True)
                                return ins
                            P.op("pe", mm, r=["vn", "sguwT"], w=[("ps", bm_)])
                            P.op("dve", lambda: V.tensor_tensor(out=tsg[:], in0=PS[0:64, bm_, :],
                                                                in1=sgub[:].rearrange("p a b -> p (a b)"), op=ALU.add),
                                 r=[("ps", bm_), "sgub"], w=["tsg"])
                            P.op("dve", lambda: V.tensor_tensor(out=ysT[:, :, ti * 128:(ti + 1) * 128],
                                                                in0=tsg[:].rearrange("p (a b) -> p a b", a=4),
                                                                in1=uT[:, :, ti * 128:(ti + 1) * 128], op=ALU.mult),
                                 r=["tsg"] + [("uT", h4) for h4 in range(4)], w=[("ysT", ti)])
                        for c in range(2):
                            bp = bank()
                            P.op("pe", lambda: PE.matmul(PS[:, bp, 0:ng], lhsT=pwbd[:, c, :], rhs=dT[:, c, tok0:tok0 + ng],
                                                         start=True, stop=True), r=["pwbd", "dT"], w=[("ps", bp)])
                            P.op("dve", lambda: V.tensor_scalar(out=ypT[:, c, 0:ng], in0=PS[:, bp, 0:ng], scalar1=pscale[:, c:c + 1],
                                                                scalar2=None, op0=ALU.mult),
                                 r=[("ps", bp), "pscale"], w=[("ypT", c)])
                        yr = [("yT", ti, hk) for ti in range(nt) for hk in range(2)]
                        P.dma("sp", lambda: SP.dma_start(out=yTd[gi, :, :, 0:ng], in_=yT[:, :, 0:ng]), r=yr, w=[("yTd", gi)])
                        P.dma("sp", lambda: SP.dma_start(out=ysTd[gi, :, :, 0:ng], in_=ysT[:, :, 0:ng]),
                              r=[("ysT", ti) for ti in range(nt)], w=[("ysTd", gi)])
                        P.dma("sp", lambda: SP.dma_start(out=ypTd[gi, :, :, 0:ng], in_=ypT[:, :, 0:ng]),
                              r=[("ypT", 0), ("ypT", 1)], w=[("ypTd", gi)])
                    P.barrier()

                P.barrier()
                eKV.close()
                with ExitStack() as e3:
                    w2g = sb(e3, "w2g", [128, 8, 3072], BF16)
                    wbrA = sb(e3, "wbrA", [64, 8, D], BF16)
                    wbrP = sb(e3, "wbrP", [128, 2, D], BF16)
                    wbrS = sb(e3, "wbrS", [64, 4, D], BF16)
                    wout = sb(e3, "wout", [128, 8, D], BF16)
                    stg = [sb(e3, "stgB%d" % i, [128, 2048]) for i in range(2)]
                    hTl = [sb(e3, "hTl%d" % i, [128, 8, 512], BF16) for i in range(1)] * 2
                    yTl = sb(e3, "yTl", [64, 8, 512], BF16)
                    ysTl = sb(e3, "ysTl", [64, 4, 512], BF16)
                    ypTl = sb(e3, "ypTl", [128, 2, 512], BF16)
                    sig = [sb(e3, "sig%d" % i, [128, 512], BF16) for i in range(3)]
                    tt = [sb(e3, "tt%d" % i, [128, 512]) for i in range(4)]
                    mT = sb(e3, "mT", [128, 8, 512], BF16)
                    xt_ = [sb(e3, "xtB%d" % i, [128, D]) for i in range(1)] * 2
                    xo_ = [sb(e3, "xoB%d" % i, [128, D]) for i in range(1)] * 2
                    ci = [0]

                    def cl(dst, dname, src, width):
                        i = ci[0] % 2
                        ci[0] += 1
                        cast_load(dst, dname, src, stg[i][:, 0:width] if True else None, ("stgB", i),
                                  ("act", "pool", "dve")[ci[0] % 3])
                    for k in range(8):
                        for hh in range(2):
                            cl(w2g[:, k, hh * 1536:(hh + 1) * 1536], ("w2g", k, hh),
                               w2g_d[layer, k * 128:(k + 1) * 128, hh * 1536:(hh + 1) * 1536], 1536)
                    for h in range(0, 8, 2):
                        i = ci[0] % 2
                        ci[0] += 1
                        cast_load(wbrA[:, h:h + 2, :], ("wbrA", h), wbrA_d[layer, :, h:h + 2, :],
                                  stg[i][0:64, 0:2048].rearrange("p (a b) -> p a b", a=2), ("stgB", i), "act")
                    i = ci[0] % 2
                    ci[0] += 1
                    cast_load(wbrP[:], "wbrP", wbrP_d[layer, :, :, :], stg[i][:, 0:2048].rearrange("p (a b) -> p a b", a=2),
                              ("stgB", i), "pool")
                    for h in range(0, 4, 2):
                        i = ci[0] % 2
                        ci[0] += 1
                        cast_load(wbrS[:, h:h + 2, :], ("wbrS", h), wbrS_d[layer, :, h:h + 2, :],
                                  stg[i][0:64, 0:2048].rearrange("p (a b) -> p a b", a=2), ("stgB", i), "dve")
                    for k in range(0, 8, 2):
                        i = ci[0] % 2
                        ci[0] += 1
                        cast_load(wout[:, k:k + 2, :], ("wout", k), wout_d[layer, :, k:k + 2, :],
                                  stg[i][:, 0:2048].rearrange("p (a b) -> p a b", a=2), ("stgB", i), "act")
                    wr_g = [("w2g", k, hh) for k in range(8) for hh in range(2)]
                    wr_b = [("wbrA", h) for h in range(0, 8, 2)] + ["wbrP"] + [("wbrS", h) for h in range(0, 4, 2)]
                    wr_o = [("wout", k) for k in range(0, 8, 2)]
                    for gi, tiles in m2_groups:
                        j = 0
                        nt = len(tiles)
                        ng = nt * 128
                        s = 0 if tiles[0] < NTL else 1
                        P.dma("sp", lambda: SP.dma_start(out=hTl[j][:, :, 0:ng], in_=hTd[gi, :, :, 0:ng]), r=[("hTd", gi)], w=[("hTl", j)])
                        P.dma("sp", lambda: SP.dma_start(out=yTl[:, :, 0:ng], in_=yTd[gi, :, :, 0:ng]), r=[("yTd", gi)], w=["yTl"])
                        P.dma("sp", lambda: SP.dma_start(out=ysTl[:, :, 0:ng], in_=ysTd[gi, :, :, 0:ng]), r=[("ysTd", gi)], w=["ysTl"])
                        P.dma("sp", lambda: SP.dma_start(out=ypTl[:, :, 0:ng], in_=ypTd[gi, :, :, 0:ng]), r=[("ypTd", gi)], w=["ypTl"])
                        for m in range(8):
                            mc = slice(m * 128, (m + 1) * 128)
                            bA, bP, bS = bank(), bank(), bank()

                            def mmA():
                                for h in range(8):
                                    ins = PE.matmul(PS[:, bA, 0:ng], lhsT=wbrA[:, h, mc], rhs=yTl[:, h, 0:ng], start=(h == 0), stop=(h == 7))
                                return ins

                            def mmP():
                                for c in range(2):
                                    ins = PE.matmul(PS[:, bP, 0:ng], lhsT=wbrP[:, c, mc], rhs=ypTl[:, c, 0:ng], start=(c == 0), stop=(c == 1))
                                return ins

                            def mmS():
                                for h in range(4):
                                    ins = PE.matmul(PS[:, bS, 0:ng], lhsT=wbrS[:, h, mc], rhs=ysTl[:, h, 0:ng], start=(h == 0), stop=(h == 3))
                                return ins
                            P.op("pe", mmA, r=wr_b + ["yTl"], w=[("ps", bA)])
                            P.op("pe", mmP, r=wr_b + ["ypTl"], w=[("ps", bP)])
                            P.op("pe", mmS, r=wr_b + ["ysTl"], w=[("ps", bS)])
                            bgs = []
                            for i3 in range(3):
                                bg = bank()
                                bgs.append(bg)

                                def mmg(bg=bg, i3=i3):
                                    for k in range(8):
                                        ins = PE.matmul(PS[:, bg, 0:ng], lhsT=w2g[:, k, i3 * D + m * 128:i3 * D + (m + 1) * 128],
                                                        rhs=hTl[j][:, k, 0:ng], start=(k == 0), stop=(k == 7))
                                    return ins
                                P.op("pe", mmg, r=wr_g + [("hTl", j)], w=[("ps", bg)])
                                P.op("act", lambda bg=bg, i3=i3: A.activation(out=sig[i3][:, 0:ng], in_=PS[:, bg, 0:ng], func=AF.Sigmoid),
                                     r=[("ps", bg)], w=[("sig", i3)])
                            for i3, bb in enumerate((bA, bP, bS)):
                                P.op("dve", lambda i3=i3, bb=bb: V.tensor_tensor(out=tt[i3][:, 0:ng], in0=PS[:, bb, 0:ng], in1=sig[i3][:, 0:ng], op=ALU.mult),
                                     r=[("ps", bb), ("sig", i3)], w=[("tt", i3)])
                            P.op("pool", lambda: G.tensor_tensor(out=tt[3][:, 0:ng], in0=tt[0][:, 0:ng], in1=tt[1][:, 0:ng], op=ALU.add),
                                 r=[("tt", 0), ("tt", 1)], w=[("tt", 3)])
                            P.op("pool", lambda: G.tensor_tensor(out=mT[:, m, 0:ng], in0=tt[3][:, 0:ng], in1=tt[2][:, 0:ng], op=ALU.add),
                                 r=[("tt", 3), ("tt", 2)], w=[("mT", m)])
                        mr = [("mT", m) for m in range(8)]
                        for ti, tile in enumerate(tiles):
                            jj = 0
                            P.dma("sp", lambda: SP.dma_start(out=xt_[jj][:], in_=xs[tile * 128:(tile + 1) * 128, :]),
                                  r=[("xs", tile)], w=[("xt", jj)])
                            for half in range(2):
                                hc = slice(half * 512, (half + 1) * 512)
                                bo = bank()

                                def mmo():
                                    for k in range(8):
                                        ins = PE.matmul(PS[:, bo, :], lhsT=mT[:, k, ti * 128:(ti + 1) * 128], rhs=wout[:, k, hc],
                                                        start=(k == 0), stop=(k == 7))
                                    return ins
                                P.op("pe", mmo, r=mr + wr_o, w=[("ps", bo)])
                                P.op("dve", lambda: V.tensor_tensor(out=tt[half][:], in0=PS[:, bo, :], in1=modm[s][2][:, hc], op=ALU.mult),
                                     r=[("ps", bo)] + modnames(0, s), w=[("tt", half)])
                                P.op("pool", lambda: G.tensor_tensor(out=xo_[jj][:, hc], in0=tt[half][:], in1=xt_[jj][:, hc], op=ALU.add),
                                     r=[("tt", half), ("xt", jj)], w=[("xo", jj, half)])
                            P.dma("sp", lambda: SP.dma_start(out=xs[tile * 128:(tile + 1) * 128, :], in_=xo_[jj][:]),
                                  r=[("xo", jj, 0), ("xo", jj, 1)], w=[("xs", tile)])
                    P.barrier()
                P.barrier()

            for _ in convs[layer]:
                pass
            with ExitStack() as eP:
                modp = [[sb(eP, "modp%d%d" % (s, k), [128, D]) for k in range(3)] for s in range(2)]
                modulation(layer, 1, modp, g_ffn_r)
                wf = sb(eP, "wfold", [128, 8, 2048], BF16)
                with ExitStack() as ef:
                    wqT = sb(ef, "wqT", [128, 16, D])
                    subkT = sb(ef, "subkT", [128, 16, 128])
                    for c in range(0, 16, 4):
                        P.dma("sp", lambda: SP.dma_start(out=wqT[:, c:c + 4, :], in_=wqT_d[layer, :, c:c + 4, :]), w=[("wqT", c)])
                    P.dma("sp", lambda: SP.dma_start(out=subkT[:], in_=subkT_d[layer, :, :, :]), w=["subkT"])
                    for k in range(8):
                        for c4 in range(4):
                            b = bank()

                            def mm():
                                for cc in range(4):
                                    c = c4 * 4 + cc
                                    ins = PE.matmul(PS[:, b, cc * 128:(cc + 1) * 128], lhsT=wqT[:, c, k * 128:(k + 1) * 128],
                                                    rhs=subkT[:, c, :], start=True, stop=True)
                                return ins
                            P.op("pe", mm, r=[("wqT", c4 * 4), "subkT"], w=[("ps", b)])
                            P.op("act", lambda: A.copy(out=wf[:, k, c4 * 512:(c4 + 1) * 512], in_=PS[:, b, :]),
                                 r=[("ps", b)], w=[("wf", k, c4)])
                    P.barrier()
                wfr = [("wf", k, c4) for k in range(8) for c4 in range(4)]
                xt_ = [sb(eP, "xtP%d" % i, [128, D]) for i in range(3)]
                junk = sb(eP, "junkP", [128, D])
                ss = sb(eP, "ssP", [128, 4])
                h2b_ = [sb(eP, "h2b%d" % i, [128, D], BF16) for i in range(2)]
                h2T = sb(eP, "h2T", [128, 8, 128], BF16)
                sS = sb(eP, "sS", [128, 16, 128])
                sS2 = sb(eP, "sS2", [128, 16, 128])
                svt = sb(eP, "svt", [128, 8, 2, 16])
                sit = sb(eP, "sit", [128, 8, 2, 16], U32)
                sif = sb(eP, "sif", [128, 8, 2, 16])
                cand = sS[:].rearrange("p a b -> p (a b)").rearrange("p (h c) -> p h c", h=8)
                cand2 = sS2[:].rearrange("p a b -> p (a b)").rearrange("p (h c) -> p h c", h=8)
                ts = sb(eP, "ts", [128, 8, 16])
                tp = sb(eP, "tp", [128, 8, 16], U32)
                ta = sb(eP, "ta", [128, 8, 16], U32)
                tb_ = sb(eP, "tb", [128, 8, 16], U32)
                taf = sb(eP, "taf", [128, 8, 16])
                tbf = sb(eP, "tbf", [128, 8, 16])
                eq = sS2[:].rearrange("p a b -> p (a b)").rearrange("p (h a b) -> p h a b", h=8, a=16)
                If = sb(eP, "If", [128, 8, 16])
                Jf = sb(eP, "Jf", [128, 8, 16])
                ef_ = sb(eP, "ef", [128, 128])
                eu_ = [sb(eP, "eu%d" % i, [128, 128], U32) for i in range(2)]
                tsc = sb(eP, "tsc", [128, 8, 16])
                ex = sb(eP, "ex", [128, 8, 16])
                zz = sb(eP, "zz", [128, 8])
                rz = sb(eP, "rz", [128, 8])
                gate_ = [sb(eP, "gate%d" % i, [128, 128]) for i in range(2)]
                actv = sb(eP, "actv", [128, 128])
                gact = sb(eP, "gact", [128, 128])
                wgt = sb(eP, "wgt", [128, 128])
                guv = [sb(eP, "guv%d" % i, [128, 2 * D], BF16) for i in range(NGB)]
                prod = [sb(eP, "prod%d" % i, [128, D], BF16) for i in range(3)]
                junkb = sb(eP, "junkb", [128, D], BF16)
                GS = 4
                diag = [sb(eP, "diag%d" % i, [128, GS, 128], BF16) for i in range(3)]
                tmpo = [sb(eP, "tmpo%d" % i, [128, 512]) for i in range(2)]
                xo = sb(eP, "xoP", [128, D])
                if last:
                    gfin = sb(eP, "gfin", [128, D])
                    P.dma("sp", lambda: SP.dma_start(out=gfin[:], in_=g_fin_r[:, :]), w=["gfin"])
                cnt_u, cnt_v, cnt_p = [0], [0], [0]
                ptiles = list(range(NTILES)) if not last else list(range(NTL))
                pidx = {t_: i_ for i_, t_ in enumerate(ptiles)}

                def stage_A(tile):
                    jj = tile % 2
                    x3 = pidx[tile] % 3
                    s = 0 if tile < NTL else 1
                    P.dma("sp", lambda: SP.dma_start(out=xt_[x3][:], in_=xs[tile * 128:(tile + 1) * 128, :]),
                          r=[("xs", tile)], w=[("xt", x3)])
                    norm_mod(xt_[x3], ("xt", x3), modp[s][0], modp[s][1], modnames(1, s), junk, ss, None, h2b_[jj], ("h2", jj),
                             add_eng="dve")
                    transpose8(h2b_[jj], ("h2", jj), h2T[:], "h2T", b=0)

                    def mm():
                        for n in range(4):
                            for k in range(8):
                                ins = PE.matmul(PS[:, n, :], lhsT=h2T[:, k, :], rhs=wf[:, k, n * 512:(n + 1) * 512],
                                                start=(k == 0), stop=(k == 7))
                        return ins
                    psn = [("ps", n) for n in range(4)]
                    P.op("pe", mm, r=["h2T"] + wfr, w=psn)
                    P.op("act", lambda: A.copy(out=sS[:].rearrange("p a b -> p (a b)"),
                                               in_=PS[:, 0:4, :].rearrange("p a b -> p (a b)")), r=psn, w=["sS"])

                def stage_B(tile):
                    eu = eu_[tile % 2]
                    eun = ("eu", tile % 2)
                    gate = gate_[tile % 2]
                    gaten = ("gate", tile % 2)
                    for c in range(16):
                        h, p_ = c // 2, c % 2
                        P.op("dve", lambda: V.max(out=svt[:, h, p_, 0:8], in_=sS[:, c, :]), r=["sS"], w=[("sv", c, 0)])
                        yield
                    for c in range(16):
                        h, p_ = c // 2, c % 2
                        P.op("dve", lambda: V.max_index(out=sit[:, h, p_, 0:8], in_max=svt[:, h, p_, 0:8], in_values=sS[:, c, :]),
                             r=["sS", ("sv", c, 0)], w=[("si", c, 0)])
                        yield
                    for c in range(16):
                        h, p_ = c // 2, c % 2
                        P.op("dve", lambda: V.match_replace(out=sS2[:, c, :], in_to_replace=svt[:, h, p_, 0:8], in_values=sS[:, c, :],
                                                            imm_value=-1e30), r=["sS", ("sv", c, 0)], w=[("sS2", c)])
                        yield
                    for c in range(16):
                        h, p_ = c // 2, c % 2
                        P.op("dve", lambda: V.max(out=svt[:, h, p_, 8:16], in_=sS2[:, c, :]), r=[("sS2", c)], w=[("sv", c, 1)])
                        yield
                    for c in range(16):
                        h, p_ = c // 2, c % 2
                        P.op("dve", lambda: V.max_index(out=sit[:, h, p_, 8:16], in_max=svt[:, h, p_, 8:16], in_values=sS2[:, c, :]),
                             r=[("sS2", c), ("sv", c, 1)], w=[("si", c, 1)])
                        yield
                    svr = [("sv", c, q) for c in range(16) for q in range(2)]
                    sir = [("si", c, q) for c in range(16) for q in range(2)]
                    s2r = [("sS2", c) for c in range(16)]
                    P.op("dve", lambda: V.tensor_copy(out=sif[:], in_=sit[:]), r=sir, w=["sif"])
                    yield
                    P.op("dve", lambda: V.tensor_tensor(
                        out=cand.rearrange("p h (a b) -> p h a b", a=16),
                        in0=svt[:, :, 0, :].unsqueeze(3).broadcast_to([128, 8, 16, 16]),
                        in1=svt[:, :, 1, :].unsqueeze(2).broadcast_to([128, 8, 16, 16]), op=ALU.add), r=svr, w=["sS"])
                    yield
                    for h in range(8):
                        P.op("dve", lambda: V.max(out=ts[:, h, 0:8], in_=cand[:, h, :]), r=["sS"], w=[("ts", h, 0)])
                        yield
                    for h in range(8):
                        P.op("dve", lambda: V.max_index(out=tp[:, h, 0:8], in_max=ts[:, h, 0:8], in_values=cand[:, h, :]),
                             r=["sS", ("ts", h, 0)], w=[("tp", h, 0)])
                        yield
                    for h in range(8):
                        P.op("dve", lambda: V.match_replace(out=cand2[:, h, :], in_to_replace=ts[:, h, 0:8], in_values=cand[:, h, :],
                                                            imm_value=-1e30), r=["sS", ("ts", h, 0)] + s2r, w=[("cand2", h)])
                        yield
                    for h in range(8):
                        P.op("dve", lambda: V.max(out=ts[:, h, 8:16], in_=cand2[:, h, :]), r=[("cand2", h)], w=[("ts", h, 1)])
                        yield
                    for h in range(8):
                        P.op("dve", lambda: V.max_index(out=tp[:, h, 8:16], in_max=ts[:, h, 8:16], in_values=cand2[:, h, :]),
                             r=[("cand2", h), ("ts", h, 1)], w=[("tp", h, 1)])
                        yield
                    tsr = [("ts", h, q) for h in range(8) for q in range(2)]
                    tpr = [("tp", h, q) for h in range(8) for q in range(2)]
                    c2r = [("cand2", h) for h in range(8)]
                    P.op("dve", lambda: V.tensor_scalar(out=ta[:], in0=tp[:], scalar1=4, scalar2=None, op0=ALU.logical_shift_right),
                         r=tpr, w=["ta"])
                    yield
                    P.op("dve", lambda: V.tensor_scalar(out=tb_[:], in0=tp[:], scalar1=15, scalar2=None, op0=ALU.bitwise_and),
                         r=tpr, w=["tb"])
                    yield
                    P.op("dve", lambda: V.tensor_copy(out=taf[:], in_=ta[:]), r=["ta"], w=["taf"])
                    yield
                    P.op("dve", lambda: V.tensor_copy(out=tbf[:], in_=tb_[:]), r=["tb"], w=["tbf"])
                    yield
                    io4 = iota16[:].unsqueeze(1).unsqueeze(1).broadcast_to([128, 8, 16, 16])
                    for (src, pp, dst, dn) in ((taf, 0, If, "If"), (tbf, 1, Jf, "Jf")):
                        P.op("dve", lambda: V.tensor_tensor(out=eq, in0=src[:].unsqueeze(3).broadcast_to([128, 8, 16, 16]),
                                                            in1=io4, op=ALU.is_equal), r=["taf", "tbf", "iota16"] + c2r + s2r, w=["eqb"])
                        yield
                        P.op("dve", lambda: V.tensor_tensor(out=eq, in0=eq,
                                                            in1=sif[:, :, pp, :].unsqueeze(2).broadcast_to([128, 8, 16, 16]),
                                                            op=ALU.mult), r=["eqb", "sif"], w=["eqb"])
                        yield
                        P.op("dve", lambda: V.tensor_reduce(out=dst[:], in_=eq, axis=AX.X, op=ALU.add), r=["eqb"], w=[dn])
                        yield
                    P.op("dve", lambda: V.scalar_tensor_tensor(out=ef_[:], in0=If[:].rearrange("p a b -> p (a b)"), scalar=128.0,
                                                               in1=Jf[:].rearrange("p a b -> p (a b)"), op0=ALU.mult, op1=ALU.add),
                         r=["If", "Jf"], w=["ef"])
                    yield
                    P.op("dve", lambda: V.tensor_copy(out=eu[:], in_=ef_[:]), r=["ef"], w=[eun])
                    yield
                    P.op("dve", lambda: V.tensor_tensor(out=tsc[:], in0=ts[:], in1=ts[:, :, 0:1].broadcast_to([128, 8, 16]), op=ALU.subtract),
                         r=tsr, w=["tsc"])
                    yield
                    P.op("act", lambda: A.activation(out=ex[:], in_=tsc[:], func=AF.Exp), r=["tsc"], w=["ex"])
                    yield
                    P.op("dve", lambda: V.tensor_reduce(out=zz[:], in_=ex[:], axis=AX.X, op=ALU.add), r=["ex"], w=["zz"])
                    yield
                    P.op("dve", lambda: V.reciprocal(out=rz[:], in_=zz[:]), r=["zz"], w=["rz"])
                    yield
                    P.op("dve", lambda: V.tensor_tensor(out=gate[:].rearrange("p (a b) -> p a b", a=8), in0=ex[:],
                                                        in1=rz[:].unsqueeze(2).broadcast_to([128, 8, 16]), op=ALU.mult),
                         r=["ex", "rz"], w=[gaten])
                    yield

                def stage_CD(tile, genB, hook=None):
                    jj = tile % 2
                    eu = eu_[jj]
                    eun = ("eu", jj)
                    gate = gate_[jj]
                    h2b = h2b_[jj]
                    ngrp = peer_slots // GS
                    ab = 4 + 2 * (pidx[tile] % 2)
                    bis_of = {}
                    di_of = {}

                    def fin_act(g):
                        gs = slice(g * GS, (g + 1) * GS)
                        P.op("act", lambda: A.activation(out=gact[:, gs], in_=actv[:, gs], func=AF.Gelu_apprx_tanh),
                             r=[("actv", g * GS + q) for q in range(GS)], w=[("gact", g)])

                    def fin_rest(g):
                        gs = slice(g * GS, (g + 1) * GS)
                        di = cnt_v[0] % 3
                        cnt_v[0] += 1
                        P.op("dve", lambda: V.tensor_tensor(out=wgt[:, gs], in0=gate[:, gs], in1=gact[:, gs], op=ALU.mult),
                             r=[("gate", jj), ("gact", g)], w=[("wgt", g)])
                        P.op("dve", lambda: V.tensor_tensor(
                            out=diag[di][:],
                            in0=identb[:].unsqueeze(1).broadcast_to([128, GS, 128]),
                            in1=wgt[:, gs].unsqueeze(2).broadcast_to([128, GS, 128]), op=ALU.mult),
                            r=["identb", ("wgt", g)], w=[("diag", di)])
                        for q in range(GS):
                            sl = g * GS + q
                            bi = bis_of[g][q]

                            def mm():
                                for half in range(2):
                                    ins = PE.matmul(PS[:, ab + half, :], lhsT=diag[di][:, q, :],
                                                    rhs=guv[bi][:, D + half * 512:D + (half + 1) * 512],
                                                    start=(sl == 0), stop=(sl == peer_slots - 1))
                                return ins
                            P.op("pe", mm, r=[("guv", bi), ("diag", di)], w=[("ps", ab), ("ps", ab + 1)])

                    for g in range(ngrp):
                        if g >= 1:
                            fin_act(g - 1)
                        bis = []
                        for q in range(GS):
                            sl = g * GS + q
                            bi = cnt_u[0] % NGB
                            cnt_u[0] += 1
                            bis.append(bi)
                            pi = cnt_p[0] % 3
                            cnt_p[0] += 1
                            P.dma("pool", lambda: G.indirect_dma_start(
                                out=guv[bi][:], out_offset=None, in_=uvb[layer][:, :],
                                in_offset=bass.IndirectOffsetOnAxis(ap=eu[:, sl:sl + 1], axis=0)), r=[eun] + tabnames(layer), w=[("guv", bi)])
                            P.op("dve", lambda: V.tensor_tensor(out=prod[pi][:], in0=guv[bi][:, 0:D], in1=h2b[:], op=ALU.mult),
                                 r=[("guv", bi), ("h2", jj)], w=[("prod", pi)])
                            P.op("act", lambda: A.activation(out=junkb[:], in_=prod[pi][:], func=AF.Copy, accum_out=actv[:, sl:sl + 1]),
                                 r=[("prod", pi)], w=[("actv", sl)])
                            if genB is not None:
                                for _ in range(2):
                                    next(genB, None)
                        bis_of[g] = bis
                        if g >= 1:
                            fin_rest(g - 1)
                        if g == 2 and hook is not None:
                            hook()
                    fin_act(ngrp - 1)
                    fin_rest(ngrp - 1)
                    if genB is not None:
                        for _ in genB:
                            pass

                def stage_E(tile):
                    jj = pidx[tile] % 3
                    ab = 4 + 2 * (pidx[tile] % 2)
                    s = 0 if tile < NTL else 1
                    for half in range(2):
                        hc = slice(half * 512, (half + 1) * 512)
                        P.op("dve", lambda: V.tensor_tensor(out=tmpo[half][:], in0=PS[:, ab + half, :], in1=modp[s][2][:, hc], op=ALU.mult),
                             r=[("ps", ab + half)] + modnames(1, s), w=[("tmpo", half)])
                        P.op("dve", lambda: V.tensor_tensor(out=xo[:, hc], in0=tmpo[half][:], in1=xt_[jj][:, hc], op=ALU.add),
                             r=[("tmpo", half), ("xt", jj)], w=[("xo", half)])
                    xor_ = [("xo", 0), ("xo", 1)]
                    if not last:
                        P.dma("sp", lambda: SP.dma_start(out=xs[tile * 128:(tile + 1) * 128, :], in_=xo[:]), r=xor_, w=[("xs", tile)],
                              is_out=dbg)
                    else:
                        P.op("dve", lambda: V.scalar_tensor_tensor(out=junk[:], in0=xo[:], scalar=1.0, in1=xo[:],
                                                                   op0=ALU.mult, op1=ALU.mult, accum_out=ss[:, 0:1]),
                             r=xor_, w=["junk", "ss0"])
                        P.op("dve", lambda: V.tensor_scalar(out=ss[:, 1:2], in0=ss[:, 0:1], scalar1=1.0 / D, scalar2=EPS,
                                                            op0=ALU.mult, op1=ALU.add), r=["ss0"], w=["ss1"])
                        P.op("act", lambda: A.activation(out=ss[:, 2:3], in_=ss[:, 1:2], func=AF.Ln), r=["ss1"], w=["ss2"])
                        P.op("act", lambda: A.activation(out=ss[:, 3:4], in_=ss[:, 2:3], func=AF.Exp, scale=-0.5), r=["ss2"], w=["ss3"])
                        P.op("dve", lambda: V.scalar_tensor_tensor(out=junk[:], in0=xo[:], scalar=ss[:, 3:4], in1=gfin[:],
                                                                   op0=ALU.mult, op1=ALU.mult),
                             r=xor_ + ["ss3", "gfin"], w=["junk"])
                        P.dma("sp", lambda: SP.dma_start(out=y_out[tile * 128:(tile + 1) * 128, :], in_=junk[:]), r=["junk"],
                              w=[("yout", tile)], is_out=True)

                stage_A(ptiles[0])
                for _ in stage_B(ptiles[0]):
                    pass
                for i_, tile in enumerate(ptiles):
                    nxt = ptiles[i_ + 1] if i_ + 1 < len(ptiles) else None
                    genB = None
                    if nxt is not None:
                        stage_A(nxt)
                        genB = stage_B(nxt)
                    prev = ptiles[i_ - 1] if i_ >= 1 else None
                    stage_CD(tile, genB, hook=(lambda: stage_E(prev)) if prev is not None else None)
                stage_E(ptiles[-1])
                P.barrier()
        P.finish()
        print("instructions:", P.n_ins)
    return nc


def prep_inputs(inp, NTL=32, n_cores=8):
    f = np.float32
    T = NTL * 128
    TT = T + CTX
    x = np.asarray(inp["x"], f)
    c = np.asarray(inp["c"], f)
    ctx = np.asarray(inp["ctx"], f)
    c_ctx = np.asarray(inp["c_ctx"], f)
    w_in = np.asarray(inp["w_in"], f)
    rp = np.concatenate([np.arange(16, 32), np.arange(0, 16), np.arange(48, 64), np.arange(32, 48)])
    d64 = np.arange(64)
    qcols = np.concatenate([np.concatenate([j * 64 + d64, (4 + j) * 64 + d64]) for j in range(4)])
    qrcols = np.concatenate([np.concatenate([j * 64 + rp, (4 + j) * 64 + rp]) for j in range(4)])
    kcols = 512 + np.arange(128)
    krcols = 512 + np.concatenate([rp, 64 + rp])
    vcols = 640 + np.arange(128)
    pcols = 768 + np.arange(256)
    ucols = 1024 + np.arange(256)
    vscols = 1280 + np.arange(256)
    w1 = np.ascontiguousarray(w_in[:, :, np.concatenate([kcols, krcols, vcols, pcols])])
    w2a = np.ascontiguousarray(w_in[:, :, np.concatenate([qcols, qrcols, ucols, vscols])])
    w2g = np.ascontiguousarray(w_in[:, :, 1536:4608])
    rows = T // 64
    row = np.repeat(np.arange(rows, dtype=f), 64)
    col = np.tile(np.arange(64, dtype=f), rows)
    inv = (np.float32(10000.0) ** (-np.arange(16, dtype=f) / np.float32(16))).astype(f)
    ar, ac = (row[:, None] * inv).astype(f), (col[:, None] * inv).astype(f)
    cos_d = np.concatenate([np.cos(ar), np.cos(ar), np.cos(ac), np.cos(ac)], axis=1).astype(f)
    sin_d = np.concatenate([-np.sin(ar), np.sin(ar), -np.sin(ac), np.sin(ac)], axis=1).astype(f)
    cos_d = np.concatenate([cos_d, np.ones((CTX, 64), f)], axis=0)
    sin_d = np.concatenate([sin_d, np.zeros((CTX, 64), f)], axis=0)
    ropeC = np.ascontiguousarray(np.concatenate([cos_d.T, cos_d.T], axis=0))
    ropeS = np.ascontiguousarray(np.concatenate([sin_d.T, sin_d.T], axis=0))
    sink = np.asarray(inp["attn_sink"], f)
    sink_r = np.ascontiguousarray(np.repeat(sink.reshape(2, 1, 2, 4, 1), 128, axis=4).reshape(2, 1, 2, 512))
    pool_w = np.asarray(inp["pool_w"], f)
    pwbd = np.zeros((2, 128, 2, 128), f)
    for g in range(4):
        o = (g % 2) * 64
        pwbd[:, o:o + 64, g // 2, o:o + 64] = pool_w[:, g]
    pscale = np.ascontiguousarray(np.asarray(inp["pool_scale"], f).reshape(2, 2, 128).transpose(0, 2, 1))
    rc = np.zeros((128, 2, TT), f)
    for g, size in enumerate(POOL_SIZES):
        for (a, l) in ((0, T), (T, CTX)):
            t = np.arange(l)
            lo = np.clip(t - size // 2, 0, l)
            hi = np.clip(t + size // 2, 0, l)
            o = (g % 2) * 64
            rc[o:o + 64, g // 2, a:a + l] = (1.0 / (hi - lo).astype(f))[None, :]
    sgu_wT = np.ascontiguousarray(np.asarray(inp["sgu_w"], f).transpose(0, 3, 1, 2))
    sgu_b_r = np.ascontiguousarray(np.broadcast_to(np.asarray(inp["sgu_b"], f)[:, None], (2, 64, 4, 128)))
    wbrA = np.ascontiguousarray(np.asarray(inp["w_br_attn"], f).reshape(2, 8, 64, D).transpose(0, 2, 1, 3))
    wbrP = np.ascontiguousarray(np.asarray(inp["w_br_pool"], f).reshape(2, 2, 128, D).transpose(0, 2, 1, 3))
    wbrS = np.ascontiguousarray(np.asarray(inp["w_br_sgu"], f).reshape(2, 4, 64, D).transpose(0, 2, 1, 3))
    wout = np.ascontiguousarray(np.asarray(inp["w_out"], f).reshape(2, 8, 128, D).transpose(0, 2, 1, 3))
    wqT = np.ascontiguousarray(np.asarray(inp["peer_wq"], f).reshape(2, D, 16, 128).transpose(0, 3, 2, 1))
    subkT = np.ascontiguousarray(np.asarray(inp["peer_subkeys"], f).reshape(2, 16, 128, 128).transpose(0, 3, 1, 2))
    jj, ii = np.meshgrid(np.arange(128), np.arange(128), indexing="ij")
    mA = np.where(jj >= ii, 0.0, NEG).astype(f)
    mB = np.where(jj <= ii, 0.0, NEG).astype(f)
    maskAB = np.ascontiguousarray(np.stack([np.tile(mA, (1, 4)), np.tile(mB, (1, 4))], axis=1))
    shared = {
        "w_mod": np.asarray(inp["w_mod"], f),
        "b_mod_r": np.ascontiguousarray(np.broadcast_to(np.asarray(inp["b_mod"], f)[:, None], (2, 128, 6 * D))),
        "g_mix_r": np.ascontiguousarray(np.broadcast_to(np.asarray(inp["g_mix"], f)[:, None], (2, 128, D))),
        "g_ffn_r": np.ascontiguousarray(np.broadcast_to(np.asarray(inp["g_ffn"], f)[:, None], (2, 128, D))),
        "g_fin_r": np.ascontiguousarray(np.broadcast_to(np.asarray(inp["g_final"], f)[None], (128, D))),
        "w1": w1, "w2a": w2a, "w2g": w2g, "ropeC": ropeC, "ropeS": ropeS, "sink_r": sink_r, "pwbd": pwbd,
        "pscale": pscale, "rc": rc, "sgu_wT": sgu_wT, "sgu_b_r": sgu_b_r, "wbrA": wbrA, "wbrP": wbrP, "wbrS": wbrS,
        "wout": wout, "wqT": wqT, "subkT": subkT,
        "peer_u0": np.ascontiguousarray(np.asarray(inp["peer_u"], f)[0]), "peer_u1": np.ascontiguousarray(np.asarray(inp["peer_u"], f)[1]),
        "peer_v0": np.ascontiguousarray(np.asarray(inp["peer_v"], f)[0]), "peer_v1": np.ascontiguousarray(np.asarray(inp["peer_v"], f)[1]),
        "maskAB": maskAB, "ident": np.eye(128, dtype=f),
        "iota16": np.ascontiguousarray(np.broadcast_to(np.arange(16, dtype=f)[None], (128, 16))),
    }
    maps = []
    for b in range(n_cores):
        m = dict(shared)
        m["x"] = np.ascontiguousarray(x[b, :T])
        m["ctx"] = np.ascontiguousarray(ctx[b])
        cv = np.stack([c[b].reshape(8, 128).T, c_ctx.reshape(8, 128).T], axis=1)
        m["cvec"] = np.ascontiguousarray(cv.astype(f))
        maps.append(m)
    return maps


def kernel(**inputs):
    n = 8
    nc = build(NTL=32, n_layers=2)
    maps = prep_inputs(inputs, NTL=32, n_cores=n)
    res = run_bass_kernel_spmd(nc, maps, core_ids=list(range(n)))
    return np.stack([np.asarray(r["y"], np.float32) for r in res.results], axis=0)
```

```python
import numpy as np
import concourse.bass as bass
import concourse.mybir as mybir
from concourse.bass_utils import run_bass_kernel_spmd
from contextlib import ExitStack

F32 = mybir.dt.float32
BF16 = mybir.dt.bfloat16
U32 = mybir.dt.uint32
AF = mybir.ActivationFunctionType
ALU = mybir.AluOpType
AX = mybir.AxisListType

D = 1024
CTX = 256
EPS = 1e-6
NEG = -30000.0
POOL_SIZES = (2, 4, 8, 16)
ND_SEM = 40
NGB = 16


class Prog:
    def __init__(self, nc, es):
        self.nc = nc
        self.eng = {"pe": nc.tensor, "dve": nc.vector, "act": nc.scalar, "pool": nc.gpsimd, "sp": nc.sync}
        self.sem = {}
        for e in self.eng:
            self.sem[e] = es.enter_context(nc.semaphore("s_" + e))
        for i in range(ND_SEM):
            self.sem[("d", i)] = es.enter_context(nc.semaphore("s_d%d" % i))
        self.cnt = {e: 0 for e in self.eng}
        self.seen = {e: {} for e in self.eng}
        self.lw = {}
        self.rd = {}
        self.dval = [0] * ND_SEM
        self.rr_rng = {"sp": (0, 24), "pool": (24, ND_SEM)}
        self.rr = {"sp": 0, "pool": 24}
        self.out_tokens = []
        self.n_ins = 0

    def _deps(self, e, r, w, extra=()):
        deps = {}

        def add(tok):
            sk, v = tok
            if sk == "pe" and e == "pe":
                return
            if self.seen[e].get(sk, 0) < v:
                if deps.get(sk, 0) < v:
                    deps[sk] = v

        for b in r:
            if b in self.lw:
                add(self.lw[b])
        for b in w:
            if b in self.lw:
                add(self.lw[b])
            for sk, v in self.rd.get(b, {}).items():
                add((sk, v))
        for t in extra:
            add(t)
        eng = self.eng[e]
        for sk, v in deps.items():
            eng.wait_ge(self.sem[sk], v)
            self.seen[e][sk] = v
            self.n_ins += 1

    def _commit(self, tok, r, w):
        for b in w:
            self.lw[b] = tok
            self.rd[b] = {}
        for b in r:
            d = self.rd.setdefault(b, {})
            if d.get(tok[0], 0) < tok[1]:
                d[tok[0]] = tok[1]

    def op(self, e, fn, r=(), w=()):
        self._deps(e, r, w)
        ins = fn()
        self.cnt[e] += 1
        ins.then_inc(self.sem[e], 1)
        self.n_ins += 1
        tok = (e, self.cnt[e])
        self._commit(tok, r, w)
        return tok

    def dma(self, e, fn, r=(), w=(), is_out=False):
        lo_, hi_ = self.rr_rng[e]
        idx = self.rr[e]
        self.rr[e] = lo_ + (idx + 1 - lo_) % (hi_ - lo_)
        sk = ("d", idx)
        extra = [(sk, self.dval[idx])] if self.dval[idx] > 0 else []
        self._deps(e, r, w, extra)
        ins = fn()
        self.dval[idx] += 16
        ins.then_inc(self.sem[sk], 16)
        self.n_ins += 1
        tok = (sk, self.dval[idx])
        self._commit(tok, r, w)
        if is_out:
            self.out_tokens.append(tok)
        return tok

    def barrier(self):
        cur = {e: self.cnt[e] for e in self.eng}
        for i in range(ND_SEM):
            cur[("d", i)] = self.dval[i]
        for e in self.eng:
            for sk, v in cur.items():
                if v > self.seen[e].get(sk, 0):
                    self.eng[e].wait_ge(self.sem[sk], v)
                    self.seen[e][sk] = v
                    self.n_ins += 1

    def finish(self):
        e = "sp"
        for sk, v in self.out_tokens:
            if self.seen[e].get(sk, 0) < v:
                self.eng[e].wait_ge(self.sem[sk], v)
                self.seen[e][sk] = v


def build(NTL=32, n_layers=2, dbg=False, peer_slots=128):
    T = NTL * 128
    TT = T + CTX
    NTILES = NTL + 2
    NGL = NTL // 4
    L = TT + 64
    groups = [(g, list(range(4 * g, 4 * g + 4))) for g in range(NGL)] + [(NGL, [NTL, NTL + 1])]

    def ppos(tok):
        return tok + 16 if tok < T else tok + 48

    nc = bass.Bass("TRN2", target_bir_lowering=False)

    def din(name, shape, dt=F32):
        return nc.dram_tensor(name, list(shape), dt, kind="ExternalInput").ap()

    def dscr(name, shape, dt=F32):
        kind = "ExternalOutput" if (dbg and name == "xs") else "Internal"
        return nc.dram_tensor(name, list(shape), dt, kind=kind).ap()

    x_in = din("x", [T, D])
    ctx_in = din("ctx", [CTX, D])
    cvec = din("cvec", [128, 2, 8])
    w_mod = din("w_mod", [2, D, 6 * D])
    b_mod_r = din("b_mod_r", [2, 128, 6 * D])
    g_mix_r = din("g_mix_r", [2, 128, D])
    g_ffn_r = din("g_ffn_r", [2, 128, D])
    g_fin_r = din("g_fin_r", [128, D])
    w1_d = din("w1", [2, D, 640])
    w2a_d = din("w2a", [2, D, 1536])
    w2g_d = din("w2g", [2, D, 3072])
    ropeC = din("ropeC", [128, TT])
    ropeS = din("ropeS", [128, TT])
    sink_r = din("sink_r", [2, 1, 2, 512])
    pwbd_d = din("pwbd", [2, 128, 2, 128])
    pscale_d = din("pscale", [2, 128, 2])
    rc_d = din("rc", [128, 2, TT])
    sguwT_d = din("sgu_wT", [2, 128, 4, 128])
    sgub_d = din("sgu_b_r", [2, 64, 4, 128])
    wbrA_d = din("wbrA", [2, 64, 8, D])
    wbrP_d = din("wbrP", [2, 128, 2, D])
    wbrS_d = din("wbrS", [2, 64, 4, D])
    wout_d = din("wout", [2, 128, 8, D])
    wqT_d = din("wqT", [2, 128, 16, D])
    subkT_d = din("subkT", [2, 128, 16, 128])
    peer_u = [din("peer_u%d" % l, [16384, D]) for l in range(2)]
    peer_v = [din("peer_v%d" % l, [16384, D]) for l in range(2)]
    mask_d = din("maskAB", [128, 2, 512])
    ident_d = din("ident", [128, 128])
    iota_d = din("iota16", [128, 16])
    y_out = nc.dram_tensor("y", [T, D], F32, kind="ExternalOutput").ap()

    xs = dscr("xs", [TT, D])
    hTd = dscr("hTd", [NGL + 1, 128, 8, 512], BF16)
    yTd = dscr("yTd", [NGL + 1, 64, 8, 512], BF16)
    ysTd = dscr("ysTd", [NGL + 1, 64, 4, 512], BF16)
    ypTd = dscr("ypTd", [NGL + 1, 128, 2, 512], BF16)
    uvb = [dscr("uvb%d" % l, [16384, 2 * D], BF16) for l in range(2)]

    with ExitStack() as es:
        P = Prog(nc, es)
        V, A, G, PE, SP = nc.vector, nc.scalar, nc.gpsimd, nc.tensor, nc.sync

        uid = [0]

        def sb(es_, name, shape, dt=F32):
            uid[0] += 1
            return es_.enter_context(nc.sbuf_tensor("sb%d_%s" % (uid[0], name), list(shape), dt))

        PS = es.enter_context(nc.psum_tensor("ps", [128, 8, 512], F32))
        bank_ptr = [0]

        def bank(n=1):
            if n > 1 and bank_ptr[0] % n:
                bank_ptr[0] += n - bank_ptr[0] % n
            b = bank_ptr[0] % 8
            bank_ptr[0] = (bank_ptr[0] + n)
            return b

        identf = sb(es, "identf", [128, 128])
        identb = sb(es, "identb", [128, 128], BF16)
        maskb = sb(es, "maskb", [128, 2, 512], BF16)
        ones_b = sb(es, "ones_b", [128, 64], BF16)
        iota16 = sb(es, "iota16", [128, 16])
        screp = sb(es, "screp", [128, 2, 8, 128])
        with ExitStack() as e0:
            maskf = sb(e0, "maskf", [128, 2, 512])
            cv = sb(e0, "cv", [128, 2, 8])
            cs = sb(e0, "cs", [128, 2, 8])
            P.dma("sp", lambda: SP.dma_start(out=identf[:], in_=ident_d[:, :]), w=["identf"])
            P.dma("sp", lambda: SP.dma_start(out=maskf[:], in_=mask_d[:, :, :]), w=["maskf"])
            P.dma("sp", lambda: SP.dma_start(out=iota16[:], in_=iota_d[:, :]), w=["iota16"])
            P.dma("sp", lambda: SP.dma_start(out=cv[:], in_=cvec[:, :, :]), w=["cv"])
            P.dma("sp", lambda: SP.dma_start(out=xs[0:T, :], in_=x_in[:, :]), w=[("xs", t) for t in range(NTL)])
            P.dma("sp", lambda: SP.dma_start(out=xs[T:TT, :], in_=ctx_in[:, :]), w=[("xs", NTL), ("xs", NTL + 1)])
            P.op("dve", lambda: V.tensor_copy(out=identb[:], in_=identf[:]), r=["identf"], w=["identb"])
            P.op("dve", lambda: V.tensor_copy(out=maskb[:], in_=maskf[:]), r=["maskf"], w=["maskb"])
            P.op("dve", lambda: V.memset(ones_b[:], 1.0), w=["ones_b"])
            P.op("act", lambda: A.activation(out=cs[:], in_=cv[:], func=AF.Silu), r=["cv"], w=["cs"])
            P.op("dve", lambda: V.tensor_copy(out=screp[:], in_=cs[:].unsqueeze(3).broadcast_to([128, 2, 8, 128])),
                 r=["cs"], w=["screp"])
            P.barrier()

        def modulation(layer, which, mod, g_r):
            base = which * 3 * D
            with ExitStack() as em:
                wm = [sb(em, "wm%d" % i, [128, 8, 512]) for i in range(2)]
                bm = [sb(em, "bm%d" % i, [128, 512]) for i in range(2)]
                gs = [sb(em, "gs%d" % i, [128, 512]) for i in range(2)]
                tm = [sb(em, "tm%d" % i, [128, 512]) for i in range(2)]
                for ci in range(6):
                    kind, half = ci // 2, ci % 2
                    c0 = base + ci * 512
                    j = ci % 2
                    P.dma("sp", lambda: SP.dma_start(
                        out=wm[j][:], in_=w_mod[layer, :, c0:c0 + 512].rearrange("(k p) n -> p k n", p=128)),
                        w=[("wm", j)])
                    P.dma("sp", lambda: SP.dma_start(out=bm[j][:], in_=b_mod_r[layer, :, c0:c0 + 512]), w=[("bm", j)])
                    if kind == 1:
                        P.dma("sp", lambda: SP.dma_start(out=gs[j][:], in_=g_r[layer, :, half * 512:(half + 1) * 512]),
                              w=[("gs", j)])
                    for s in range(2):
                        b = bank()

                        def mm():
                            for k in range(8):
                                ins = PE.matmul(PS[:, b, :], lhsT=screp[:, s, k, :], rhs=wm[j][:, k, :],
                                                start=(k == 0), stop=(k == 7))
                            return ins
                        P.op("pe", mm, r=["screp", ("wm", j)], w=[("ps", b)])
                        dst_kind = {0: 1, 1: 0, 2: 2}[kind]
                        dst = mod[s][dst_kind][:, half * 512:(half + 1) * 512]
                        dname = ("mod", which, s, dst_kind, half)
                        if kind == 1:
                            P.op("dve", lambda: V.tensor_tensor(out=tm[s][:], in0=PS[:, b, :], in1=bm[j][:], op=ALU.add),
                                 r=[("ps", b), ("bm", j)], w=[("tm", s)])
                            P.op("dve", lambda: V.scalar_tensor_tensor(out=dst, in0=tm[s][:], scalar=1.0, in1=gs[j][:],
                                                                       op0=ALU.add, op1=ALU.mult),
                                 r=[("tm", s), ("gs", j)], w=[dname])
                        else:
                            P.op("dve", lambda: V.tensor_tensor(out=dst, in0=PS[:, b, :], in1=bm[j][:], op=ALU.add),
                                 r=[("ps", b), ("bm", j)], w=[dname])
                P.barrier()

        def modnames(which, s):
            return [("mod", which, s, k, h) for k in range(3) for h in range(2)]

        def load_cast(es_, name, dram_ap, shape, stage_bufs, dst=None, eng_cycle=("act", "pool")):
            raise NotImplementedError

        def norm_mod(xt, xname, Amod, Bmod, modr, junk, ss, hf_out, hb_out, hname, add_eng="pool"):
            P.op("dve", lambda: V.scalar_tensor_tensor(out=junk[:], in0=xt[:], scalar=1.0, in1=xt[:], op0=ALU.mult, op1=ALU.mult, accum_out=ss[:, 0:1]),
                 r=[xname], w=["junk", "ss0"])
            P.op("dve", lambda: V.tensor_scalar(out=ss[:, 1:2], in0=ss[:, 0:1], scalar1=1.0 / D, scalar2=EPS,
                                                op0=ALU.mult, op1=ALU.add), r=["ss0"], w=["ss1"])
            P.op("act", lambda: A.activation(out=ss[:, 2:3], in_=ss[:, 1:2], func=AF.Ln), r=["ss1"], w=["ss2"])
            P.op("act", lambda: A.activation(out=ss[:, 3:4], in_=ss[:, 2:3], func=AF.Exp, scale=-0.5), r=["ss2"], w=["ss3"])
            P.op("dve", lambda: V.scalar_tensor_tensor(out=junk[:], in0=xt[:], scalar=ss[:, 3:4], in1=Amod[:],
                                                       op0=ALU.mult, op1=ALU.mult),
                 r=[xname, "ss3"] + modr, w=["junk"])
            if hf_out is not None:
                P.op("dve", lambda: V.tensor_tensor(out=hf_out[:], in0=junk[:], in1=Bmod[:], op=ALU.add),
                     r=["junk"] + modr, w=[hname + "f"])
                P.op("act", lambda: A.copy(out=hb_out[:], in_=hf_out[:]), r=[hname + "f"], w=[hname])
            elif add_eng == "dve":
                P.op("dve", lambda: V.tensor_tensor(out=hb_out[:], in0=junk[:], in1=Bmod[:], op=ALU.add),
                     r=["junk"] + modr, w=[hname])
            else:
                P.op("pool", lambda: G.tensor_tensor(out=hb_out[:], in0=junk[:], in1=Bmod[:], op=ALU.add),
                     r=["junk"] + modr, w=[hname])

        def transpose8(hb, hname, dst_ap, dname, evac="act", b=None):
            if b is None:
                b = bank()
            psb = PS[:, b, :].bitcast(BF16)

            def tr():
                for k in range(8):
                    ins = PE.transpose(out=psb[:, k * 128:(k + 1) * 128], in_=hb[:, k * 128:(k + 1) * 128],
                                       identity=identb[:])
                return ins
            P.op("pe", tr, r=[hname, "identb"], w=[("ps", b)])
            src = psb.rearrange("p (k t) -> p k t", k=8)
            if evac == "act":
                P.op("act", lambda: A.copy(out=dst_ap, in_=src), r=[("ps", b)], w=[dname])
            else:
                P.op("dve", lambda: V.tensor_copy(out=dst_ap, in_=src), r=[("ps", b)], w=[dname])

        def cast_load(dst_tile_ap, dname, src_ap, stg, sname, ceng):
            P.dma("sp", lambda: SP.dma_start(out=stg, in_=src_ap), w=[sname])
            if ceng == "act":
                P.op("act", lambda: A.copy(out=dst_tile_ap, in_=stg), r=[sname], w=[dname])
            elif ceng == "pool":
                P.op("pool", lambda: G.tensor_copy(out=dst_tile_ap, in_=stg), r=[sname], w=[dname])
            else:
                P.op("dve", lambda: V.tensor_copy(out=dst_tile_ap, in_=stg), r=[sname], w=[dname])

        NQ = 16

        def tabnames(l):
            return [("uvb", l, c0, q) for c0 in (0, D) for q in range(NQ)]

        def conv_gen(l):
            rpq = 16384 // NQ
            for q in range(NQ):
                for (src_t, c0) in ((peer_u[l], 0), (peer_v[l], D)):
                    rows = slice(q * rpq, (q + 1) * rpq)
                    P.dma("pool", lambda: G.dma_start(out=uvb[l][rows, c0:c0 + D], in_=src_t[rows, :]), w=[("uvb", l, c0, q)])
                    yield
        convs = [conv_gen(l) for l in range(n_layers)]

        def pump(l, k):
            if l < n_layers:
                for _ in range(k):
                    next(convs[l], None)

        for layer in range(n_layers):
            last = layer == n_layers - 1
            with ExitStack() as eL:
                modm = [[sb(eL, "modm%d%d" % (s, k), [128, D]) for k in range(3)] for s in range(2)]
                eKV = ExitStack()
                kT = sb(eKV, "kT", [128, TT], BF16)
                vtok = sb(eKV, "vtok", [128, NTILES, 128], BF16)
                dT = sb(eKV, "dT", [128, 2, TT], BF16)
                sinkrow = sb(eKV, "sinkrow", [1, 2, 512], BF16)
                with ExitStack() as e1:
                    sk_f = sb(e1, "sk_f", [1, 2, 512])
                    sk_e = sb(e1, "sk_e", [1, 2, 512])
                    P.dma("sp", lambda: SP.dma_start(out=sk_f[:], in_=sink_r[layer, :, :, :]), w=["sk_f"])
                    P.op("act", lambda: A.activation(out=sk_e[:], in_=sk_f[:], func=AF.Exp), r=["sk_f"], w=["sk_e"])
                    P.op("act", lambda: A.copy(out=sinkrow[:], in_=sk_e[:]), r=["sk_e"], w=["sinkrow"])
                    P.barrier()
                modulation(layer, 0, modm, g_mix_r)

                with ExitStack() as e1:
                    zpad = sb(e1, "zpad", [128, 2, L])
                    e1w = ExitStack()
                    w1 = sb(e1w, "w1", [128, 8, 640], BF16)
                    stg = [sb(e1w, "stg%d" % i, [128, 2, 640]) for i in range(2)]
                    xt_ = [sb(e1w, "xt%d" % i, [128, D]) for i in range(2)]
                    junk = sb(e1w, "junk", [128, D])
                    ss = sb(e1w, "ss", [128, 4])
                    hb_ = [sb(e1w, "hb%d" % i, [128, D], BF16) for i in range(2)]
                    hTg = [sb(e1w, "hTg%d" % i, [128, 8, 512], BF16) for i in range(2)]
                    cS = [sb(e1w, "cS%d" % i, [128, 2, 512]) for i in range(2)]
                    t1 = sb(e1w, "t1", [128, 512])
                    t2 = sb(e1w, "t2", [128, 512])
                    for i in range(4):
                        cast_load(w1[:, 2 * i:2 * i + 2, :], ("w1", i),
                                  w1_d[layer, 256 * i:256 * (i + 1), :].rearrange("(k p) n -> p k n", p=128),
                                  stg[i % 2][:], ("stg", i % 2), "act")
                    P.op("dve", lambda: V.memset(zpad[:], 0.0), w=["zpad"])
                    for gi, tiles in groups:
                        j = gi % 2
                        ng = len(tiles) * 128
                        s = 0 if tiles[0] < NTL else 1
                        tok0 = tiles[0] * 128
                        for ti, tile in enumerate(tiles):
                            jj = tile % 2
                            P.dma("sp", lambda: SP.dma_start(out=xt_[jj][:], in_=xs[tile * 128:(tile + 1) * 128, :]),
                                  r=[("xs", tile)], w=[("xt", jj)])
                            norm_mod(xt_[jj], ("xt", jj), modm[s][0], modm[s][1], modnames(0, s), junk, ss, None,
                                     hb_[jj], ("hb", jj), add_eng="dve")
                            transpose8(hb_[jj], ("hb", jj), hTg[j][:, :, ti * 128:(ti + 1) * 128], ("hTg", j, ti))
                        hr = [("hTg", j, ti) for ti in range(len(tiles))]
                        P.dma("sp", lambda: SP.dma_start(out=hTd[gi, :, :, 0:ng], in_=hTg[j][:, :, 0:ng]), r=hr, w=[("hTd", gi)])
                        P.dma("sp", lambda: SP.dma_start(out=cS[j][:, 0, 0:ng], in_=ropeC[:, tok0:tok0 + ng]), w=[("cS", j, 0)])
                        P.dma("sp", lambda: SP.dma_start(out=cS[j][:, 1, 0:ng], in_=ropeS[:, tok0:tok0 + ng]), w=[("cS", j, 1)])
                        if layer == 0:
                            pump(0, 4)
                        bk, bkr = bank(), bank()
                        for (bb, c0) in ((bk, 0), (bkr, 128)):
                            def mm(bb=bb, c0=c0):
                                for k in range(8):
                                    ins = PE.matmul(PS[:, bb, 0:ng], lhsT=w1[:, k, c0:c0 + 128], rhs=hTg[j][:, k, 0:ng],
                                                    start=(k == 0), stop=(k == 7))
                                return ins
                            P.op("pe", mm, r=hr + [("w1", i_) for i_ in range(4)], w=[("ps", bb)])
                        P.op("dve", lambda: V.tensor_tensor(out=t1[:, 0:ng], in0=PS[:, bkr, 0:ng], in1=cS[j][:, 1, 0:ng], op=ALU.mult),
                             r=[("ps", bkr), ("cS", j, 1)], w=["t1"])
                        P.op("dve", lambda: V.tensor_tensor(out=t2[:, 0:ng], in0=PS[:, bk, 0:ng], in1=cS[j][:, 0, 0:ng], op=ALU.mult),
                             r=[("ps", bk), ("cS", j, 0)], w=["t2"])
                        P.op("dve", lambda: V.tensor_tensor(out=kT[:, tok0:tok0 + ng], in0=t1[:, 0:ng], in1=t2[:, 0:ng], op=ALU.add),
                             r=["t1", "t2"], w=[("kT", gi)])
                        for c in range(2):
                            bz = bank()

                            def mm(bz=bz, c=c):
                                for k in range(8):
                                    ins = PE.matmul(PS[:, bz, 0:ng], lhsT=w1[:, k, 384 + c * 128:512 + c * 128],
                                                    rhs=hTg[j][:, k, 0:ng], start=(k == 0), stop=(k == 7))
                                return ins
                            P.op("pe", mm, r=hr + [("w1", i_) for i_ in range(4)], w=[("ps", bz)])
                            p0 = ppos(tok0)
                            P.op("act", lambda: A.copy(out=zpad[:, c, p0:p0 + ng], in_=PS[:, bz, 0:ng]),
                                 r=[("ps", bz)], w=["zpad"])
                        bv = bank()

                        def mm():
                            for ti in range(len(tiles)):
                                for k in range(8):
                                    ins = PE.matmul(PS[:, bv, ti * 128:(ti + 1) * 128], lhsT=hTg[j][:, k, ti * 128:(ti + 1) * 128],
                                                    rhs=w1[:, k, 256:384], start=(k == 0), stop=(k == 7))
                            return ins
                        P.op("pe", mm, r=hr + [("w1", i_) for i_ in range(4)], w=[("ps", bv)])
                        nt = len(tiles)
                        P.op("act", lambda: A.copy(out=vtok[:, tiles[0]:tiles[0] + nt, :],
                                                   in_=PS[:, bv, 0:ng].rearrange("p (a b) -> p a b", a=nt)),
                             r=[("ps", bv)], w=[("vtok", gi)])

                    P.barrier()
                    e1w.close()
                    PA = sb(e1, "PA", [128, L])
                    PB = sb(e1, "PB", [128, L])
                    P.op("dve", lambda: V.memset(PA[:], 0.0), w=["PA"])
                    P.op("dve", lambda: V.memset(PB[:], 0.0), w=["PB"])
                    rc = sb(e1, "rc", [128, 2, TT])
                    P.dma("sp", lambda: SP.dma_start(out=rc[:], in_=rc_d[:, :, :]), w=["rc"])
                    lo, hi = 8, L - 8

                    def shadd(dst, src, s1, s2, p0=0, p1=128):
                        P.op("dve", lambda: V.tensor_tensor(out=dst[p0:p1, lo:hi], in0=src[p0:p1, lo + s1:hi + s1],
                                                            in1=src[p0:p1, lo + s2:hi + s2], op=ALU.add),
                             r=["PA", "PB", "zpad"], w=["PA", "PB"])
                    segs = [(0, T, 16), (T, TT, 48)]

                    def dfin(src, c, p0, p1):
                        for (a, b_, off) in segs:
                            P.op("dve", lambda: V.tensor_tensor(out=src[p0:p1, a + off:b_ + off], in0=src[p0:p1, a + off:b_ + off],
                                                                in1=rc[p0:p1, c, a:b_], op=ALU.mult),
                                 r=["PA", "PB", "rc"], w=["PA", "PB"])
                            P.op("dve", lambda: V.tensor_tensor(out=dT[p0:p1, c, a:b_], in0=src[p0:p1, a + off:b_ + off],
                                                                in1=zpad[p0:p1, c, a + off:b_ + off], op=ALU.subtract),
                                 r=["PA", "PB", "zpad"], w=["dT"])
                    shadd(PA, zpad[:, 0, :], -1, 0)
                    shadd(PB, PA, -1, 1, 64, 128)
                    dfin(PA, 0, 0, 64)
                    dfin(PB, 0, 64, 128)
                    shadd(PA, zpad[:, 1, :], -1, 0)
                    shadd(PB, PA, -1, 1)
                    shadd(PA, PB, -2, 2)
                    shadd(PB, PA, -4, 4, 64, 128)
                    dfin(PA, 1, 0, 64)
                    dfin(PB, 1, 64, 128)
                    P.barrier()

                allk = [("kT", gi) for gi, _ in groups]
                allv = [("vtok", gi) for gi, _ in groups]
                m2_groups = groups if not last else groups[:NGL]
                with ExitStack() as e2:
                    w2a = sb(e2, "w2a", [128, 8, 1536], BF16)
                    stg = [sb(e2, "stgA%d" % i, [128, 1, 1536]) for i in range(2)]
                    pwbd = sb(e2, "pwbd", [128, 2, 128], BF16)
                    pwf = sb(e2, "pwf", [128, 2, 128])
                    pscale = sb(e2, "pscale", [128, 2])
                    sguwT = sb(e2, "sguwT", [128, 4, 128], BF16)
                    sguf = sb(e2, "sguf", [128, 4, 128])
                    sgub = sb(e2, "sgub", [64, 4, 128])
                    junk = sb(e2, "junkA", [128, D])
                    ss = sb(e2, "ssA", [128, 4])
                    hTg = [sb(e2, "hTgA%d" % i, [128, 8, 512], BF16) for i in range(2)]
                    cS = [sb(e2, "cSA%d" % i, [128, 2, 512]) for i in range(1)] * 2
                    t1 = sb(e2, "t1A", [128, 512])
                    t2 = sb(e2, "t2A", [128, 512])
                    qT = sb(e2, "qT", [128, 4, 512], BF16)
                    pT = [sb(e2, "pT%d" % i, [128, 512], BF16) for i in range(6)]
                    lnd = sb(e2, "lnd", [64, 512])
                    rec = sb(e2, "rec", [64, 512])
                    yT = sb(e2, "yT", [64, 8, 512], BF16)
                    uT = sb(e2, "uT", [64, 4, 512], BF16)
                    vg = sb(e2, "vg", [128, 256])
                    vn = sb(e2, "vn", [128, 256], BF16)
                    sv = sb(e2, "svA", [128, 4])
                    ysT = sb(e2, "ysT", [64, 4, 512], BF16)
                    ypT = sb(e2, "ypT", [128, 2, 512], BF16)
                    tsg = sb(e2, "tsg", [64, 512])
                    for i in range(8):
                        cast_load(w2a[:, i:i + 1, :], ("w2a", i),
                                  w2a_d[layer, 128 * i:128 * (i + 1), :].rearrange("(k p) n -> p k n", p=128),
                                  stg[i % 2][:], ("stgA", i % 2), "act" if i % 2 == 0 else "pool")
                    w2r = [("w2a", i) for i in range(8)]
                    cast_load(pwbd[:], "pwbd", pwbd_d[layer, :, :, :], pwf[:], "pwf", "dve")
                    cast_load(sguwT[:], "sguwT", sguwT_d[layer, :, :, :], sguf[:], "sguf", "dve")
                    P.dma("sp", lambda: SP.dma_start(out=pscale[:], in_=pscale_d[layer, :, :]), w=["pscale"])
                    P.dma("sp", lambda: SP.dma_start(out=sgub[:], in_=sgub_d[layer, :, :, :]), w=["sgub"])
                    pti = [0]
                    def load_h(gidx):
                        gi2, tiles2 = m2_groups[gidx]
                        ng2 = len(tiles2) * 128
                        j2 = gidx % 2
                        P.dma("sp", lambda: SP.dma_start(out=hTg[j2][:, :, 0:ng2], in_=hTd[gi2, :, :, 0:ng2]),
                              r=[("hTd", gi2)], w=[("hTgA", j2)])
                    load_h(0)
                    for gidx, (gi, tiles) in enumerate(m2_groups):
                        j = gidx % 2
                        nt = len(tiles)
                        ng = nt * 128
                        s = 0 if tiles[0] < NTL else 1
                        tok0 = tiles[0] * 128
                        if gidx + 1 < len(m2_groups):
                            load_h(gidx + 1)
                        hr = [("hTgA", j)]
                        P.dma("sp", lambda: SP.dma_start(out=cS[0][:, 0, 0:ng], in_=ropeC[:, tok0:tok0 + ng]), w=[("cS", 0, 0)])
                        P.dma("sp", lambda: SP.dma_start(out=cS[0][:, 1, 0:ng], in_=ropeS[:, tok0:tok0 + ng]), w=[("cS", 0, 1)])
                        for c in range(4):
                            bq, bqr = bank(), bank()
                            for (bb, c0) in ((bq, c * 128), (bqr, 512 + c * 128)):
                                def mm(bb=bb, c0=c0):
                                    for k in range(8):
                                        ins = PE.matmul(PS[:, bb, 0:ng], lhsT=w2a[:, k, c0:c0 + 128], rhs=hTg[j][:, k, 0:ng],
                                                        start=(k == 0), stop=(k == 7))
                                    return ins
                                P.op("pe", mm, r=hr + w2r, w=[("ps", bb)])
                            P.op("dve", lambda: V.tensor_tensor(out=t1[:, 0:ng], in0=PS[:, bqr, 0:ng], in1=cS[0][:, 1, 0:ng], op=ALU.mult),
                                 r=[("ps", bqr), ("cS", 0, 1)], w=["t1"])
                            P.op("dve", lambda: V.tensor_tensor(out=t2[:, 0:ng], in0=PS[:, bq, 0:ng], in1=cS[0][:, 0, 0:ng], op=ALU.mult),
                                 r=[("ps", bq), ("cS", 0, 0)], w=["t2"])
                            P.op("dve", lambda: V.tensor_tensor(out=qT[:, c, 0:ng], in0=t1[:, 0:ng], in1=t2[:, 0:ng], op=ALU.add),
                                 r=["t1", "t2"], w=[("qT", c)])
                        qr_ = [("qT", c) for c in range(4)]
                        if layer == 0:
                            pump(1, 4)
                        for ti, tile in enumerate(tiles):
                            if tile < NTL:
                                keys = []
                                if tile - 1 >= 0:
                                    keys.append((tile - 1, 0))
                                keys.append((tile, None))
                                if tile + 1 < NTL:
                                    keys.append((tile + 1, 1))
                                keys += [(NTL, None), (NTL + 1, None)]
                            else:
                                keys = [(NTL, None), (NTL + 1, None)]
                            for hk in range(2):
                                h0, h1 = hk * 64, hk * 64 + 64
                                bnum, bden = bank(), bank()
                                sbanks = []
                                for (kt, mk) in keys:
                                    bs = bank()
                                    sbanks.append(bs)

                                    def mm(bs=bs, kt=kt, mk=mk):
                                        ins = PE.matmul(PS[:, bs, :], lhsT=kT[h0:h1, kt * 128:(kt + 1) * 128],
                                                        rhs=qT[h0:h1, :, ti * 128:(ti + 1) * 128],
                                                        start=True, stop=(mk is None))
                                        if mk is not None:
                                            ins = PE.matmul(PS[:, bs, :], lhsT=identb[:], rhs=maskb[:, mk, :], start=False, stop=True)
                                        return ins
                                    P.op("pe", mm, r=allk + qr_ + ["identb", "maskb"], w=[("ps", bs)])
                                pts = []
                                for bs in sbanks:
                                    pi = pti[0] % 6
                                    pti[0] += 1
                                    pts.append(pi)
                                    P.op("act", lambda bs=bs, pi=pi: A.activation(out=pT[pi][:], in_=PS[:, bs, :], func=AF.Exp, scale=0.125),
                                         r=[("ps", bs)], w=[("pT", pi)])
                                nk = len(keys)
                                for ki, ((kt, mk), pi) in enumerate(zip(keys, pts)):
                                    def mm(ki=ki, kt=kt, pi=pi):
                                        PE.matmul(PS[0:64, bnum, :], lhsT=vtok[:, kt, h0:h1], rhs=pT[pi][:],
                                                  start=(ki == 0), stop=(ki == nk - 1))
                                        ins = PE.matmul(PS[0:64, bden, :], lhsT=ones_b[:, 0:64], rhs=pT[pi][:],
                                                        start=(ki == 0), stop=False)
                                        if ki == nk - 1:
                                            ins = PE.matmul(PS[0:64, bden, :], lhsT=ones_b[0:1, 0:64], rhs=sinkrow[0:1, hk, :],
                                                            start=False, stop=True)
                                        return ins
                                    P.op("pe", mm, r=allv + [("pT", pi), "ones_b", "sinkrow"], w=[("ps", bnum), ("ps", bden)])
                                P.op("act", lambda: A.activation(out=lnd[:], in_=PS[0:64, bden, :], func=AF.Ln),
                                     r=[("ps", bden)], w=["lnd"])
                                P.op("act", lambda: A.activation(out=rec[:], in_=lnd[:], func=AF.Exp, scale=-1.0),
                                     r=["lnd"], w=["rec"])
                                P.op("dve", lambda: V.tensor_tensor(
                                    out=yT[:, hk * 4:hk * 4 + 4, ti * 128:(ti + 1) * 128],
                                    in0=PS[0:64, bnum, :].rearrange("p (g q) -> p g q", g=4),
                                    in1=rec[:].rearrange("p (g q) -> p g q", g=4), op=ALU.mult),
                                    r=[("ps", bnum), "rec"], w=[("yT", ti, hk)])
                        for h4 in range(4):
                            bu = bank()

                            def mm():
                                for k in range(8):
                                    ins = PE.matmul(PS[0:64, bu, 0:ng], lhsT=w2a[:, k, 1024 + h4 * 64:1088 + h4 * 64],
                                                    rhs=hTg[j][:, k, 0:ng], start=(k == 0), stop=(k == 7))
                                return ins
                            P.op("pe", mm, r=hr + w2r, w=[("ps", bu)])
                            P.op("act", lambda: A.activation(out=uT[:, h4, 0:ng], in_=PS[0:64, bu, 0:ng], func=AF.Gelu_apprx_tanh),
                                 r=[("ps", bu)], w=[("uT", h4)])
                        for ti, tile in enumerate(tiles):
                            bvs = bank()

                            def mm():
                                for k in range(8):
                                    ins = PE.matmul(PS[:, bvs, 0:256], lhsT=hTg[j][:, k, ti * 128:(ti + 1) * 128],
                                                    rhs=w2a[:, k, 1280:1536], start=(k == 0), stop=(k == 7))
                                return ins
                            P.op("pe", mm, r=hr + w2r, w=[("ps", bvs)])
                            P.op("act", lambda: A.activation(out=vg[:], in_=PS[:, bvs, 0:256], func=AF.Gelu_apprx_tanh),
                                 r=[("ps", bvs)], w=["vg"])
                            P.op("dve", lambda: V.scalar_tensor_tensor(out=junk[:, 0:256], in0=vg[:], scalar=1.0, in1=vg[:],
                                                                        op0=ALU.mult, op1=ALU.mult, accum_out=sv[:, 0:1]),
                                 r=["vg"], w=["junk", "sv0"])
                            P.op("dve", lambda: V.tensor_scalar(out=sv[:, 1:2], in0=sv[:, 0:1], scalar1=1.0 / 256, scalar2=EPS,
                                                                op0=ALU.mult, op1=ALU.add), r=["sv0"], w=["sv1"])
                            P.op("act", lambda: A.activation(out=sv[:, 2:3], in_=sv[:, 1:2], func=AF.Ln), r=["sv1"], w=["sv2"])
                            P.op("act", lambda: A.activation(out=sv[:, 3:4], in_=sv[:, 2:3], func=AF.Exp, scale=-0.5), r=["sv2"], w=["sv3"])
                            P.op("dve", lambda: V.tensor_scalar(out=vn[:], in0=vg[:], scalar1=sv[:, 3:4], scalar2=None, op0=ALU.mult),
                                 r=["vg", "sv3"], w=["vn"])
                            bm_ = bank()

                            def mm():
                                for h4 in range(4):
                                    ins = PE.matmul(PS[0:64, bm_, h4 * 128:(h4 + 1) * 128], lhsT=vn[:, h4 * 64:(h4 + 1) * 64],
                                                    rhs=sguwT[:, h4, :], start=True, stop=True)
                                return ins
                            P.op("pe", mm, r=["vn", "sguwT"], w=[("ps", bm_)])
                            P.op("dve", lambda: V.tensor_tensor(out=tsg[:], in0=PS[0:64, bm_, :],
                                                                in1=sgub[:].rearrange("p a b -> p (a b)"), op=ALU.add),
                                 r=[("ps", bm_), "sgub"], w=["tsg"])
                            P.op("dve", lambda: V.tensor_tensor(out=ysT[:, :, ti * 128:(ti + 1) * 128],
                                                                in0=tsg[:].rearrange("p (a b) -> p a b", a=4),
                                                                in1=uT[:, :, ti * 128:(ti + 1) * 128], op=ALU.mult),
                                 r=["tsg"] + [("uT", h4) for h4 in range(4)], w=[("ysT", ti)])
                        for c in range(2):
                            bp = bank()
                            P.op("pe", lambda: PE.matmul(PS[:, bp, 0:ng], lhsT=pwbd[:, c, :], rhs=dT[:, c, tok0:tok0 + ng],
                                                         start=True, stop=True), r=["pwbd", "dT"], w=[("ps", bp)])
                            P.op("dve", lambda: V.tensor_scalar(out=ypT[:, c, 0:ng], in0=PS[:, bp, 0:ng], scalar1=pscale[:, c:c + 1],
                                                                scalar2=None, op0=ALU.mult),
                                 r=[("ps", bp), "pscale"], w=[("ypT", c)])
                        yr = [("yT", ti, hk) for ti in range(nt) for hk in range(2)]
                        P.dma("sp", lambda: SP.dma_start(out=yTd[gi, :, :, 0:ng], in_=yT[:, :, 0:ng]), r=yr, w=[("yTd", gi)])
                        P.dma("sp", lambda: SP.dma_start(out=ysTd[gi, :, :, 0:ng], in_=ysT[:, :, 0:ng]),
                              r=[("ysT", ti) for ti in range(nt)], w=[("ysTd", gi)])
                        P.dma("sp", lambda: SP.dma_start(out=ypTd[gi, :, :, 0:ng], in_=ypT[:, :, 0:ng]),
                              r=[("ypT", 0), ("ypT", 1)], w=[("ypTd", gi)])
                    P.barrier()

                P.barrier()
                eKV.close()
                with ExitStack() as e3:
                    w2g = sb(e3, "w2g", [128, 8, 3072], BF16)
                    wbrA = sb(e3, "wbrA", [64, 8, D], BF16)
                    wbrP = sb(e3, "wbrP", [128, 2, D], BF16)
                    wbrS = sb(e3, "wbrS", [64, 4, D], BF16)
                    wout = sb(e3, "wout", [128, 8, D], BF16)
                    stg = [sb(e3, "stgB%d" % i, [128, 2048]) for i in range(2)]
                    hTl = [sb(e3, "hTl%d" % i, [128, 8, 512], BF16) for i in range(1)] * 2
                    yTl = sb(e3, "yTl", [64, 8, 512], BF16)
                    ysTl = sb(e3, "ysTl", [64, 4, 512], BF16)
                    ypTl = sb(e3, "ypTl", [128, 2, 512], BF16)
                    sig = [sb(e3, "sig%d" % i, [128, 512], BF16) for i in range(3)]
                    tt = [sb(e3, "tt%d" % i, [128, 512]) for i in range(4)]
                    mT = sb(e3, "mT", [128, 8, 512], BF16)
                    xt_ = [sb(e3, "xtB%d" % i, [128, D]) for i in range(1)] * 2
                    xo_ = [sb(e3, "xoB%d" % i, [128, D]) for i in range(1)] * 2
                    ci = [0]

                    def cl(dst, dname, src, width):
                        i = ci[0] % 2
                        ci[0] += 1
                        cast_load(dst, dname, src, stg[i][:, 0:width] if True else None, ("stgB", i),
                                  ("act", "pool", "dve")[ci[0] % 3])
                    for k in range(8):
                        for hh in range(2):
                            cl(w2g[:, k, hh * 1536:(hh + 1) * 1536], ("w2g", k, hh),
                               w2g_d[layer, k * 128:(k + 1) * 128, hh * 1536:(hh + 1) * 1536], 1536)
                    for h in range(0, 8, 2):
                        i = ci[0] % 2
                        ci[0] += 1
                        cast_load(wbrA[:, h:h + 2, :], ("wbrA", h), wbrA_d[layer, :, h:h + 2, :],
                                  stg[i][0:64, 0:2048].rearrange("p (a b) -> p a b", a=2), ("stgB", i), "act")
                    i = ci[0] % 2
                    ci[0] += 1
                    cast_load(wbrP[:], "wbrP", wbrP_d[layer, :, :, :], stg[i][:, 0:2048].rearrange("p (a b) -> p a b", a=2),
                              ("stgB", i), "pool")
                    for h in range(0, 4, 2):
                        i = ci[0] % 2
                        ci[0] += 1
                        cast_load(wbrS[:, h:h + 2, :], ("wbrS", h), wbrS_d[layer, :, h:h + 2, :],
                                  stg[i][0:64, 0:2048].rearrange("p (a b) -> p a b", a=2), ("stgB", i), "dve")
                    for k in range(0, 8, 2):
                        i = ci[0] % 2
                        ci[0] += 1
                        cast_load(wout[:, k:k + 2, :], ("wout", k), wout_d[layer, :, k:k + 2, :],
                                  stg[i][:, 0:2048].rearrange("p (a b) -> p a b", a=2), ("stgB", i), "act")
                    wr_g = [("w2g", k, hh) for k in range(8) for hh in range(2)]
                    wr_b = [("wbrA", h) for h in range(0, 8, 2)] + ["wbrP"] + [("wbrS", h) for h in range(0, 4, 2)]
                    wr_o = [("wout", k) for k in range(0, 8, 2)]
                    for gi, tiles in m2_groups:
                        j = 0
                        nt = len(tiles)
                        ng = nt * 128
                        s = 0 if tiles[0] < NTL else 1
                        P.dma("sp", lambda: SP.dma_start(out=hTl[j][:, :, 0:ng], in_=hTd[gi, :, :, 0:ng]), r=[("hTd", gi)], w=[("hTl", j)])
                        P.dma("sp", lambda: SP.dma_start(out=yTl[:, :, 0:ng], in_=yTd[gi, :, :, 0:ng]), r=[("yTd", gi)], w=["yTl"])
                        P.dma("sp", lambda: SP.dma_start(out=ysTl[:, :, 0:ng], in_=ysTd[gi, :, :, 0:ng]), r=[("ysTd", gi)], w=["ysTl"])
                        P.dma("sp", lambda: SP.dma_start(out=ypTl[:, :, 0:ng], in_=ypTd[gi, :, :, 0:ng]), r=[("ypTd", gi)], w=["ypTl"])
                        for m in range(8):
                            mc = slice(m * 128, (m + 1) * 128)
                            bA, bP, bS = bank(), bank(), bank()

                            def mmA():
                                for h in range(8):
                                    ins = PE.matmul(PS[:, bA, 0:ng], lhsT=wbrA[:, h, mc], rhs=yTl[:, h, 0:ng], start=(h == 0), stop=(h == 7))
                                return ins

                            def mmP():
                                for c in range(2):
                                    ins = PE.matmul(PS[:, bP, 0:ng], lhsT=wbrP[:, c, mc], rhs=ypTl[:, c, 0:ng], start=(c == 0), stop=(c == 1))
                                return ins

                            def mmS():
                                for h in range(4):
                                    ins = PE.matmul(PS[:, bS, 0:ng], lhsT=wbrS[:, h, mc], rhs=ysTl[:, h, 0:ng], start=(h == 0), stop=(h == 3))
                                return ins
                            P.op("pe", mmA, r=wr_b + ["yTl"], w=[("ps", bA)])
                            P.op("pe", mmP, r=wr_b + ["ypTl"], w=[("ps", bP)])
                            P.op("pe", mmS, r=wr_b + ["ysTl"], w=[("ps", bS)])
                            bgs = []
                            for i3 in range(3):
                                bg = bank()
                                bgs.append(bg)

                                def mmg(bg=bg, i3=i3):
                                    for k in range(8):
                                        ins = PE.matmul(PS[:, bg, 0:ng], lhsT=w2g[:, k, i3 * D + m * 128:i3 * D + (m + 1) * 128],
                                                        rhs=hTl[j][:, k, 0:ng], start=(k == 0), stop=(k == 7))
                                    return ins
                                P.op("pe", mmg, r=wr_g + [("hTl", j)], w=[("ps", bg)])
                                P.op("act", lambda bg=bg, i3=i3: A.activation(out=sig[i3][:, 0:ng], in_=PS[:, bg, 0:ng], func=AF.Sigmoid),
                                     r=[("ps", bg)], w=[("sig", i3)])
                            for i3, bb in enumerate((bA, bP, bS)):
                                P.op("dve", lambda i3=i3, bb=bb: V.tensor_tensor(out=tt[i3][:, 0:ng], in0=PS[:, bb, 0:ng], in1=sig[i3][:, 0:ng], op=ALU.mult),
                                     r=[("ps", bb), ("sig", i3)], w=[("tt", i3)])
                            P.op("pool", lambda: G.tensor_tensor(out=tt[3][:, 0:ng], in0=tt[0][:, 0:ng], in1=tt[1][:, 0:ng], op=ALU.add),
                                 r=[("tt", 0), ("tt", 1)], w=[("tt", 3)])
                            P.op("pool", lambda: G.tensor_tensor(out=mT[:, m, 0:ng], in0=tt[3][:, 0:ng], in1=tt[2][:, 0:ng], op=ALU.add),
                                 r=[("tt", 3), ("tt", 2)], w=[("mT", m)])
                        mr = [("mT", m) for m in range(8)]
                        for ti, tile in enumerate(tiles):
                            jj = 0
                            P.dma("sp", lambda: SP.dma_start(out=xt_[jj][:], in_=xs[tile * 128:(tile + 1) * 128, :]),
                                  r=[("xs", tile)], w=[("xt", jj)])
                            for half in range(2):
                                hc = slice(half * 512, (half + 1) * 512)
                                bo = bank()

                                def mmo():
                                    for k in range(8):
                                        ins = PE.matmul(PS[:, bo, :], lhsT=mT[:, k, ti * 128:(ti + 1) * 128], rhs=wout[:, k, hc],
                                                        start=(k == 0), stop=(k == 7))
                                    return ins
                                P.op("pe", mmo, r=mr + wr_o, w=[("ps", bo)])
                                P.op("dve", lambda: V.tensor_tensor(out=tt[half][:], in0=PS[:, bo, :], in1=modm[s][2][:, hc], op=ALU.mult),
                                     r=[("ps", bo)] + modnames(0, s), w=[("tt", half)])
                                P.op("pool", lambda: G.tensor_tensor(out=xo_[jj][:, hc], in0=tt[half][:], in1=xt_[jj][:, hc], op=ALU.add),
                                     r=[("tt", half), ("xt", jj)], w=[("xo", jj, half)])
                            P.dma("sp", lambda: SP.dma_start(out=xs[tile * 128:(tile + 1) * 128, :], in_=xo_[jj][:]),
                                  r=[("xo", jj, 0), ("xo", jj, 1)], w=[("xs", tile)])
                    P.barrier()
                P.barrier()

            for _ in convs[layer]:
                pass
            with ExitStack() as eP:
                modp = [[sb(eP, "modp%d%d" % (s, k), [128, D]) for k in range(3)] for s in range(2)]
                modulation(layer, 1, modp, g_ffn_r)
                wf = sb(eP, "wfold", [128, 8, 2048], BF16)
                with ExitStack() as ef:
                    wqT = sb(ef, "wqT", [128, 16, D])
                    subkT = sb(ef, "subkT", [128, 16, 128])
                    for c in range(0, 16, 4):
                        P.dma("sp", lambda: SP.dma_start(out=wqT[:, c:c + 4, :], in_=wqT_d[layer, :, c:c + 4, :]), w=[("wqT", c)])
                    P.dma("sp", lambda: SP.dma_start(out=subkT[:], in_=subkT_d[layer, :, :, :]), w=["subkT"])
                    for k in range(8):
                        for c4 in range(4):
                            b = bank()

                            def mm():
                                for cc in range(4):
                                    c = c4 * 4 + cc
                                    ins = PE.matmul(PS[:, b, cc * 128:(cc + 1) * 128], lhsT=wqT[:, c, k * 128:(k + 1) * 128],
                                                    rhs=subkT[:, c, :], start=True, stop=True)
                                return ins
                            P.op("pe", mm, r=[("wqT", c4 * 4), "subkT"], w=[("ps", b)])
                            P.op("act", lambda: A.copy(out=wf[:, k, c4 * 512:(c4 + 1) * 512], in_=PS[:, b, :]),
                                 r=[("ps", b)], w=[("wf", k, c4)])
                    P.barrier()
                wfr = [("wf", k, c4) for k in range(8) for c4 in range(4)]
                xt_ = [sb(eP, "xtP%d" % i, [128, D]) for i in range(3)]
                junk = sb(eP, "junkP", [128, D])
                ss = sb(eP, "ssP", [128, 4])
                h2b_ = [sb(eP, "h2b%d" % i, [128, D], BF16) for i in range(2)]
                h2T = sb(eP, "h2T", [128, 8, 128], BF16)
                sS = sb(eP, "sS", [128, 16, 128])
                sS2 = sb(eP, "sS2", [128, 16, 128])
                svt = sb(eP, "svt", [128, 8, 2, 16])
                sit = sb(eP, "sit", [128, 8, 2, 16], U32)
                sif = sb(eP, "sif", [128, 8, 2, 16])
                cand = sS[:].rearrange("p a b -> p (a b)").rearrange("p (h c) -> p h c", h=8)
                cand2 = sS2[:].rearrange("p a b -> p (a b)").rearrange("p (h c) -> p h c", h=8)
                ts = sb(eP, "ts", [128, 8, 16])
                tp = sb(eP, "tp", [128, 8, 16], U32)
                ta = sb(eP, "ta", [128, 8, 16], U32)
                tb_ = sb(eP, "tb", [128, 8, 16], U32)
                taf = sb(eP, "taf", [128, 8, 16])
                tbf = sb(eP, "tbf", [128, 8, 16])
                eq = sS2[:].rearrange("p a b -> p (a b)").rearrange("p (h a b) -> p h a b", h=8, a=16)
                If = sb(eP, "If", [128, 8, 16])
                Jf = sb(eP, "Jf", [128, 8, 16])
                ef_ = sb(eP, "ef", [128, 128])
                eu_ = [sb(eP, "eu%d" % i, [128, 128], U32) for i in range(2)]
                tsc = sb(eP, "tsc", [128, 8, 16])
                ex = sb(eP, "ex", [128, 8, 16])
                zz = sb(eP, "zz", [128, 8])
                rz = sb(eP, "rz", [128, 8])
                gate_ = [sb(eP, "gate%d" % i, [128, 128]) for i in range(2)]
                actv = sb(eP, "actv", [128, 128])
                gact = sb(eP, "gact", [128, 128])
                wgt = sb(eP, "wgt", [128, 128])
                guv = [sb(eP, "guv%d" % i, [128, 2 * D], BF16) for i in range(NGB)]
                prod = [sb(eP, "prod%d" % i, [128, D], BF16) for i in range(3)]
                junkb = sb(eP, "junkb", [128, D], BF16)
                GS = 4
                diag = [sb(eP, "diag%d" % i, [128, GS, 128], BF16) for i in range(3)]
                tmpo = [sb(eP, "tmpo%d" % i, [128, 512]) for i in range(2)]
                xo = sb(eP, "xoP", [128, D])
                if last:
                    gfin = sb(eP, "gfin", [128, D])
                    P.dma("sp", lambda: SP.dma_start(out=gfin[:], in_=g_fin_r[:, :]), w=["gfin"])
                cnt_u, cnt_v, cnt_p = [0], [0], [0]
                ptiles = list(range(NTILES)) if not last else list(range(NTL))
                pidx = {t_: i_ for i_, t_ in enumerate(ptiles)}

                def stage_A(tile):
                    jj = tile % 2
                    x3 = pidx[tile] % 3
                    s = 0 if tile < NTL else 1
                    P.dma("sp", lambda: SP.dma_start(out=xt_[x3][:], in_=xs[tile * 128:(tile + 1) * 128, :]),
                          r=[("xs", tile)], w=[("xt", x3)])
                    norm_mod(xt_[x3], ("xt", x3), modp[s][0], modp[s][1], modnames(1, s), junk, ss, None, h2b_[jj], ("h2", jj),
                             add_eng="dve")
                    transpose8(h2b_[jj], ("h2", jj), h2T[:], "h2T", b=0)

                    def mm():
                        for n in range(4):
                            for k in range(8):
                                ins = PE.matmul(PS[:, n, :], lhsT=h2T[:, k, :], rhs=wf[:, k, n * 512:(n + 1) * 512],
                                                start=(k == 0), stop=(k == 7))
                        return ins
                    psn = [("ps", n) for n in range(4)]
                    P.op("pe", mm, r=["h2T"] + wfr, w=psn)
                    P.op("act", lambda: A.copy(out=sS[:].rearrange("p a b -> p (a b)"),
                                               in_=PS[:, 0:4, :].rearrange("p a b -> p (a b)")), r=psn, w=["sS"])

                def stage_B(tile):
                    eu = eu_[tile % 2]
                    eun = ("eu", tile % 2)
                    gate = gate_[tile % 2]
                    gaten = ("gate", tile % 2)
                    for c in range(16):
                        h, p_ = c // 2, c % 2
                        P.op("dve", lambda: V.max(out=svt[:, h, p_, 0:8], in_=sS[:, c, :]), r=["sS"], w=[("sv", c, 0)])
                        yield
                    for c in range(16):
                        h, p_ = c // 2, c % 2
                        P.op("dve", lambda: V.max_index(out=sit[:, h, p_, 0:8], in_max=svt[:, h, p_, 0:8], in_values=sS[:, c, :]),
                             r=["sS", ("sv", c, 0)], w=[("si", c, 0)])
                        yield
                    for c in range(16):
                        h, p_ = c // 2, c % 2
                        P.op("dve", lambda: V.match_replace(out=sS2[:, c, :], in_to_replace=svt[:, h, p_, 0:8], in_values=sS[:, c, :],
                                                            imm_value=-1e30), r=["sS", ("sv", c, 0)], w=[("sS2", c)])
                        yield
                    for c in range(16):
                        h, p_ = c // 2, c % 2
                        P.op("dve", lambda: V.max(out=svt[:, h, p_, 8:16], in_=sS2[:, c, :]), r=[("sS2", c)], w=[("sv", c, 1)])
                        yield
                    for c in range(16):
                        h, p_ = c // 2, c % 2
                        P.op("dve", lambda: V.max_index(out=sit[:, h, p_, 8:16], in_max=svt[:, h, p_, 8:16], in_values=sS2[:, c, :]),
                             r=[("sS2", c), ("sv", c, 1)], w=[("si", c, 1)])
                        yield
                    svr = [("sv", c, q) for c in range(16) for q in range(2)]
                    sir = [("si", c, q) for c in range(16) for q in range(2)]
                    s2r = [("sS2", c) for c in range(16)]
                    P.op("dve", lambda: V.tensor_copy(out=sif[:], in_=sit[:]), r=sir, w=["sif"])
                    yield
                    P.op("dve", lambda: V.tensor_tensor(
                        out=cand.rearrange("p h (a b) -> p h a b", a=16),
                        in0=svt[:, :, 0, :].unsqueeze(3).broadcast_to([128, 8, 16, 16]),
                        in1=svt[:, :, 1, :].unsqueeze(2).broadcast_to([128, 8, 16, 16]), op=ALU.add), r=svr, w=["sS"])
                    yield
                    for h in range(8):
                        P.op("dve", lambda: V.max(out=ts[:, h, 0:8], in_=cand[:, h, :]), r=["sS"], w=[("ts", h, 0)])
                        yield
                    for h in range(8):
                        P.op("dve", lambda: V.max_index(out=tp[:, h, 0:8], in_max=ts[:, h, 0:8], in_values=cand[:, h, :]),
                             r=["sS", ("ts", h, 0)], w=[("tp", h, 0)])
                        yield
                    for h in range(8):
                        P.op("dve", lambda: V.match_replace(out=cand2[:, h, :], in_to_replace=ts[:, h, 0:8], in_values=cand[:, h, :],
                                                            imm_value=-1e30), r=["sS", ("ts", h, 0)] + s2r, w=[("cand2", h)])
                        yield
                    for h in range(8):
                        P.op("dve", lambda: V.max(out=ts[:, h, 8:16], in_=cand2[:, h, :]), r=[("cand2", h)], w=[("ts", h, 1)])
                        yield
                    for h in range(8):
                        P.op("dve", lambda: V.max_index(out=tp[:, h, 8:16], in_max=ts[:, h, 8:16], in_values=cand2[:, h, :]),
                             r=[("cand2", h), ("ts", h, 1)], w=[("tp", h, 1)])
                        yield
                    tsr = [("ts", h, q) for h in range(8) for q in range(2)]
                    tpr = [("tp", h, q) for h in range(8) for q in range(2)]
                    c2r = [("cand2", h) for h in range(8)]
                    P.op("dve", lambda: V.tensor_scalar(out=ta[:], in0=tp[:], scalar1=4, scalar2=None, op0=ALU.logical_shift_right),
                         r=tpr, w=["ta"])
                    yield
                    P.op("dve", lambda: V.tensor_scalar(out=tb_[:], in0=tp[:], scalar1=15, scalar2=None, op0=ALU.bitwise_and),
                         r=tpr, w=["tb"])
                    yield
                    P.op("dve", lambda: V.tensor_copy(out=taf[:], in_=ta[:]), r=["ta"], w=["taf"])
                    yield
                    P.op("dve", lambda: V.tensor_copy(out=tbf[:], in_=tb_[:]), r=["tb"], w=["tbf"])
                    yield
                    io4 = iota16[:].unsqueeze(1).unsqueeze(1).broadcast_to([128, 8, 16, 16])
                    for (src, pp, dst, dn) in ((taf, 0, If, "If"), (tbf, 1, Jf, "Jf")):
                        P.op("dve", lambda: V.tensor_tensor(out=eq, in0=src[:].unsqueeze(3).broadcast_to([128, 8, 16, 16]),
                                                            in1=io4, op=ALU.is_equal), r=["taf", "tbf", "iota16"] + c2r + s2r, w=["eqb"])
                        yield
                        P.op("dve", lambda: V.tensor_tensor(out=eq, in0=eq,
                                                            in1=sif[:, :, pp, :].unsqueeze(2).broadcast_to([128, 8, 16, 16]),
                                                            op=ALU.mult), r=["eqb", "sif"], w=["eqb"])
                        yield
                        P.op("dve", lambda: V.tensor_reduce(out=dst[:], in_=eq, axis=AX.X, op=ALU.add), r=["eqb"], w=[dn])
                        yield
                    P.op("dve", lambda: V.scalar_tensor_tensor(out=ef_[:], in0=If[:].rearrange("p a b -> p (a b)"), scalar=128.0,
                                                               in1=Jf[:].rearrange("p a b -> p (a b)"), op0=ALU.mult, op1=ALU.add),
                         r=["If", "Jf"], w=["ef"])
                    yield
                    P.op("dve", lambda: V.tensor_copy(out=eu[:], in_=ef_[:]), r=["ef"], w=[eun])
                    yield
                    P.op("dve", lambda: V.tensor_tensor(out=tsc[:], in0=ts[:], in1=ts[:, :, 0:1].broadcast_to([128, 8, 16]), op=ALU.subtract),
                         r=tsr, w=["tsc"])
                    yield
                    P.op("act", lambda: A.activation(out=ex[:], in_=tsc[:], func=AF.Exp), r=["tsc"], w=["ex"])
                    yield
                    P.op("dve", lambda: V.tensor_reduce(out=zz[:], in_=ex[:], axis=AX.X, op=ALU.add), r=["ex"], w=["zz"])
                    yield
                    P.op("dve", lambda: V.reciprocal(out=rz[:], in_=zz[:]), r=["zz"], w=["rz"])
                    yield
                    P.op("dve", lambda: V.tensor_tensor(out=gate[:].rearrange("p (a b) -> p a b", a=8), in0=ex[:],
                                                        in1=rz[:].unsqueeze(2).broadcast_to([128, 8, 16]), op=ALU.mult),
                         r=["ex", "rz"], w=[gaten])
                    yield

                def stage_CD(tile, genB, hook=None):
                    jj = tile % 2
                    eu = eu_[jj]
                    eun = ("eu", jj)
                    gate = gate_[jj]
                    h2b = h2b_[jj]
                    ngrp = peer_slots // GS
                    ab = 4 + 2 * (pidx[tile] % 2)
                    bis_of = {}
                    di_of = {}

                    def fin_act(g):
                        gs = slice(g * GS, (g + 1) * GS)
                        P.op("act", lambda: A.activation(out=gact[:, gs], in_=actv[:, gs], func=AF.Gelu_apprx_tanh),
                             r=[("actv", g * GS + q) for q in range(GS)], w=[("gact", g)])

                    def fin_rest(g):
                        gs = slice(g * GS, (g + 1) * GS)
                        di = cnt_v[0] % 3
                        cnt_v[0] += 1
                        P.op("dve", lambda: V.tensor_tensor(out=wgt[:, gs], in0=gate[:, gs], in1=gact[:, gs], op=ALU.mult),
                             r=[("gate", jj), ("gact", g)], w=[("wgt", g)])
                        P.op("dve", lambda: V.tensor_tensor(
                            out=diag[di][:],
                            in0=identb[:].unsqueeze(1).broadcast_to([128, GS, 128]),
                            in1=wgt[:, gs].unsqueeze(2).broadcast_to([128, GS, 128]), op=ALU.mult),
                            r=["identb", ("wgt", g)], w=[("diag", di)])
                        for q in range(GS):
                            sl = g * GS + q
                            bi = bis_of[g][q]

                            def mm():
                                for half in range(2):
                                    ins = PE.matmul(PS[:, ab + half, :], lhsT=diag[di][:, q, :],
                                                    rhs=guv[bi][:, D + half * 512:D + (half + 1) * 512],
                                                    start=(sl == 0), stop=(sl == peer_slots - 1))
                                return ins
                            P.op("pe", mm, r=[("guv", bi), ("diag", di)], w=[("ps", ab), ("ps", ab + 1)])

                    for g in range(ngrp):
                        if g >= 1:
                            fin_act(g - 1)
                        bis = []
                        for q in range(GS):
                            sl = g * GS + q
                            bi = cnt_u[0] % NGB
                            cnt_u[0] += 1
                            bis.append(bi)
                            pi = cnt_p[0] % 3
                            cnt_p[0] += 1
                            P.dma("pool", lambda: G.indirect_dma_start(
                                out=guv[bi][:], out_offset=None, in_=uvb[layer][:, :],
                                in_offset=bass.IndirectOffsetOnAxis(ap=eu[:, sl:sl + 1], axis=0)), r=[eun] + tabnames(layer), w=[("guv", bi)])
                            P.op("dve", lambda: V.tensor_tensor(out=prod[pi][:], in0=guv[bi][:, 0:D], in1=h2b[:], op=ALU.mult),
                                 r=[("guv", bi), ("h2", jj)], w=[("prod", pi)])
                            P.op("act", lambda: A.activation(out=junkb[:], in_=prod[pi][:], func=AF.Copy, accum_out=actv[:, sl:sl + 1]),
                                 r=[("prod", pi)], w=["junkb", ("actv", sl)])
                            if genB is not None:
                                for _ in range(2):
                                    next(genB, None)
                        bis_of[g] = bis
                        if g >= 1:
                            fin_rest(g - 1)
                        if g == 2 and hook is not None:
                            hook()
                    fin_act(ngrp - 1)
                    fin_rest(ngrp - 1)
                    if genB is not None:
                        for _ in genB:
                            pass

                def stage_E(tile):
                    jj = pidx[tile] % 3
                    ab = 4 + 2 * (pidx[tile] % 2)
                    s = 0 if tile < NTL else 1
                    for half in range(2):
                        hc = slice(half * 512, (half + 1) * 512)
                        P.op("dve", lambda: V.tensor_tensor(out=tmpo[half][:], in0=PS[:, ab + half, :], in1=modp[s][2][:, hc], op=ALU.mult),
                             r=[("ps", ab + half)] + modnames(1, s), w=[("tmpo", half)])
                        P.op("dve", lambda: V.tensor_tensor(out=xo[:, hc], in0=tmpo[half][:], in1=xt_[jj][:, hc], op=ALU.add),
                             r=[("tmpo", half), ("xt", jj)], w=[("xo", half)])
                    xor_ = [("xo", 0), ("xo", 1)]
                    if not last:
                        P.dma("sp", lambda: SP.dma_start(out=xs[tile * 128:(tile + 1) * 128, :], in_=xo[:]), r=xor_, w=[("xs", tile)],
                              is_out=dbg)
                    else:
                        P.op("dve", lambda: V.scalar_tensor_tensor(out=junk[:], in0=xo[:], scalar=1.0, in1=xo[:],
                                                                   op0=ALU.mult, op1=ALU.mult, accum_out=ss[:, 0:1]),
                             r=xor_, w=["junk", "ss0"])
                        P.op("dve", lambda: V.tensor_scalar(out=ss[:, 1:2], in0=ss[:, 0:1], scalar1=1.0 / D, scalar2=EPS,
                                                            op0=ALU.mult, op1=ALU.add), r=["ss0"], w=["ss1"])
                        P.op("act", lambda: A.activation(out=ss[:, 2:3], in_=ss[:, 1:2], func=AF.Ln), r=["ss1"], w=["ss2"])
                        P.op("act", lambda: A.activation(out=ss[:, 3:4], in_=ss[:, 2:3], func=AF.Exp, scale=-0.5), r=["ss2"], w=["ss3"])
                        P.op("dve", lambda: V.scalar_tensor_tensor(out=junk[:], in0=xo[:], scalar=ss[:, 3:4], in1=gfin[:],
                                                                   op0=ALU.mult, op1=ALU.mult),
                             r=xor_ + ["ss3", "gfin"], w=["junk"])
                        P.dma("sp", lambda: SP.dma_start(out=y_out[tile * 128:(tile + 1) * 128, :], in_=junk[:]), r=["junk"],
                              w=[("yout", tile)], is_out=True)

                stage_A(ptiles[0])
                for _ in stage_B(ptiles[0]):
                    pass
                for i_, tile in enumerate(ptiles):
                    nxt = ptiles[i_ + 1] if i_ + 1 < len(ptiles) else None
                    genB = None
                    if nxt is not None:
                        stage_A(nxt)
                        genB = stage_B(nxt)
                    prev = ptiles[i_ - 1] if i_ >= 1 else None
                    stage_CD(tile, genB, hook=(lambda: stage_E(prev)) if prev is not None else None)
                stage_E(ptiles[-1])
                P.barrier()
        P.finish()
        print("instructions:", P.n_ins)
    return nc


def prep_inputs(inp, NTL=32, n_cores=8):
    f = np.float32
    T = NTL * 128
    TT = T + CTX
    x = np.asarray(inp["x"], f)
    c = np.asarray(inp["c"], f)
    ctx = np.asarray(inp["ctx"], f)
    c_ctx = np.asarray(inp["c_ctx"], f)
    w_in = np.asarray(inp["w_in"], f)
    rp = np.concatenate([np.arange(16, 32), np.arange(0, 16), np.arange(48, 64), np.arange(32, 48)])
    d64 = np.arange(64)
    qcols = np.concatenate([np.concatenate([j * 64 + d64, (4 + j) * 64 + d64]) for j in range(4)])
    qrcols = np.concatenate([np.concatenate([j * 64 + rp, (4 + j) * 64 + rp]) for j in range(4)])
    kcols = 512 + np.arange(128)
    krcols = 512 + np.concatenate([rp, 64 + rp])
    vcols = 640 + np.arange(128)
    pcols = 768 + np.arange(256)
    ucols = 1024 + np.arange(256)
    vscols = 1280 + np.arange(256)
    w1 = np.ascontiguousarray(w_in[:, :, np.concatenate([kcols, krcols, vcols, pcols])])
    w2a = np.ascontiguousarray(w_in[:, :, np.concatenate([qcols, qrcols, ucols, vscols])])
    w2g = np.ascontiguousarray(w_in[:, :, 1536:4608])
    rows = T // 64
    row = np.repeat(np.arange(rows, dtype=f), 64)
    col = np.tile(np.arange(64, dtype=f), rows)
    inv = (np.float32(10000.0) ** (-np.arange(16, dtype=f) / np.float32(16))).astype(f)
    ar, ac = (row[:, None] * inv).astype(f), (col[:, None] * inv).astype(f)
    cos_d = np.concatenate([np.cos(ar), np.cos(ar), np.cos(ac), np.cos(ac)], axis=1).astype(f)
    sin_d = np.concatenate([-np.sin(ar), np.sin(ar), -np.sin(ac), np.sin(ac)], axis=1).astype(f)
    cos_d = np.concatenate([cos_d, np.ones((CTX, 64), f)], axis=0)
    sin_d = np.concatenate([sin_d, np.zeros((CTX, 64), f)], axis=0)
    ropeC = np.ascontiguousarray(np.concatenate([cos_d.T, cos_d.T], axis=0))
    ropeS = np.ascontiguousarray(np.concatenate([sin_d.T, sin_d.T], axis=0))
    sink = np.asarray(inp["attn_sink"], f)
    sink_r = np.ascontiguousarray(np.repeat(sink.reshape(2, 1, 2, 4, 1), 128, axis=4).reshape(2, 1, 2, 512))
    pool_w = np.asarray(inp["pool_w"], f)
    pwbd = np.zeros((2, 128, 2, 128), f)
    for g in range(4):
        o = (g % 2) * 64
        pwbd[:, o:o + 64, g // 2, o:o + 64] = pool_w[:, g]
    pscale = np.ascontiguousarray(np.asarray(inp["pool_scale"], f).reshape(2, 2, 128).transpose(0, 2, 1))
    rc = np.zeros((128, 2, TT), f)
    for g, size in enumerate(POOL_SIZES):
        for (a, l) in ((0, T), (T, CTX)):
            t = np.arange(l)
            lo = np.clip(t - size // 2, 0, l)
            hi = np.clip(t + size // 2, 0, l)
            o = (g % 2) * 64
            rc[o:o + 64, g // 2, a:a + l] = (1.0 / (hi - lo).astype(f))[None, :]
    sgu_wT = np.ascontiguousarray(np.asarray(inp["sgu_w"], f).transpose(0, 3, 1, 2))
    sgu_b_r = np.ascontiguousarray(np.broadcast_to(np.asarray(inp["sgu_b"], f)[:, None], (2, 64, 4, 128)))
    wbrA = np.ascontiguousarray(np.asarray(inp["w_br_attn"], f).reshape(2, 8, 64, D).transpose(0, 2, 1, 3))
    wbrP = np.ascontiguousarray(np.asarray(inp["w_br_pool"], f).reshape(2, 2, 128, D).transpose(0, 2, 1, 3))
    wbrS = np.ascontiguousarray(np.asarray(inp["w_br_sgu"], f).reshape(2, 4, 64, D).transpose(0, 2, 1, 3))
    wout = np.ascontiguousarray(np.asarray(inp["w_out"], f).reshape(2, 8, 128, D).transpose(0, 2, 1, 3))
    wqT = np.ascontiguousarray(np.asarray(inp["peer_wq"], f).reshape(2, D, 16, 128).transpose(0, 3, 2, 1))
    subkT = np.ascontiguousarray(np.asarray(inp["peer_subkeys"], f).reshape(2, 16, 128, 128).transpose(0, 3, 1, 2))
    jj, ii = np.meshgrid(np.arange(128), np.arange(128), indexing="ij")
    mA = np.where(jj >= ii, 0.0, NEG).astype(f)
    mB = np.where(jj <= ii, 0.0, NEG).astype(f)
    maskAB = np.ascontiguousarray(np.stack([np.tile(mA, (1, 4)), np.tile(mB, (1, 4))], axis=1))
    shared = {
        "w_mod": np.asarray(inp["w_mod"], f),
        "b_mod_r": np.ascontiguousarray(np.broadcast_to(np.asarray(inp["b_mod"], f)[:, None], (2, 128, 6 * D))),
        "g_mix_r": np.ascontiguousarray(np.broadcast_to(np.asarray(inp["g_mix"], f)[:, None], (2, 128, D))),
        "g_ffn_r": np.ascontiguousarray(np.broadcast_to(np.asarray(inp["g_ffn"], f)[:, None], (2, 128, D))),
        "g_fin_r": np.ascontiguousarray(np.broadcast_to(np.asarray(inp["g_final"], f)[None], (128, D))),
        "w1": w1, "w2a": w2a, "w2g": w2g, "ropeC": ropeC, "ropeS": ropeS, "sink_r": sink_r, "pwbd": pwbd,
        "pscale": pscale, "rc": rc, "sgu_wT": sgu_wT, "sgu_b_r": sgu_b_r, "wbrA": wbrA, "wbrP": wbrP, "wbrS": wbrS,
        "wout": wout, "wqT": wqT, "subkT": subkT,
        "peer_u0": np.ascontiguousarray(np.asarray(inp["peer_u"], f)[0]), "peer_u1": np.ascontiguousarray(np.asarray(inp["peer_u"], f)[1]),
        "peer_v0": np.ascontiguousarray(np.asarray(inp["peer_v"], f)[0]), "peer_v1": np.ascontiguousarray(np.asarray(inp["peer_v"], f)[1]),
        "maskAB": maskAB, "ident": np.eye(128, dtype=f),
        "iota16": np.ascontiguousarray(np.broadcast_to(np.arange(16, dtype=f)[None], (128, 16))),
    }
    maps = []
    for b in range(n_cores):
        m = dict(shared)
        m["x"] = np.ascontiguousarray(x[b, :T])
        m["ctx"] = np.ascontiguousarray(ctx[b])
        cv = np.stack([c[b].reshape(8, 128).T, c_ctx.reshape(8, 128).T], axis=1)
        m["cvec"] = np.ascontiguousarray(cv.astype(f))
        maps.append(m)
    return maps


def kernel(**inputs):
    n = 8
    nc = build(NTL=32, n_layers=2)
    maps = prep_inputs(inputs, NTL=32, n_cores=n)
    res = run_bass_kernel_spmd(nc, maps, core_ids=list(range(n)))
    return np.stack([np.asarray(r["y"], np.float32) for r in res.results], axis=0)
```

```python
import numpy as np
import concourse.bass as bass
import concourse.mybir as mybir
from concourse.bass_utils import run_bass_kernel_spmd
from contextlib import ExitStack

F32 = mybir.dt.float32
BF16 = mybir.dt.bfloat16
U32 = mybir.dt.uint32
AF = mybir.ActivationFunctionType
ALU = mybir.AluOpType
AX = mybir.AxisListType

D = 1024
CTX = 256
EPS = 1e-6
NEG = -30000.0
POOL_SIZES = (2, 4, 8, 16)
ND_SEM = 40
NGB = 16


class Prog:
    def __init__(self, nc, es):
        self.nc = nc
        self.eng = {"pe": nc.tensor, "dve": nc.vector, "act": nc.scalar, "pool": nc.gpsimd, "sp": nc.sync}
        self.sem = {}
        for e in self.eng:
            self.sem[e] = es.enter_context(nc.semaphore("s_" + e))
        for i in range(ND_SEM):
            self.sem[("d", i)] = es.enter_context(nc.semaphore("s_d%d" % i))
        self.cnt = {e: 0 for e in self.eng}
        self.seen = {e: {} for e in self.eng}
        self.lw = {}
        self.rd = {}
        self.dval = [0] * ND_SEM
        self.rr_rng = {"sp": (0, 24), "pool": (24, ND_SEM)}
        self.rr = {"sp": 0, "pool": 24}
        self.out_tokens = []
        self.n_ins = 0

    def _deps(self, e, r, w, extra=()):
        deps = {}

        def add(tok):
            sk, v = tok
            if sk == "pe" and e == "pe":
                return
            if self.seen[e].get(sk, 0) < v:
                if deps.get(sk, 0) < v:
                    deps[sk] = v

        for b in r:
            if b in self.lw:
                add(self.lw[b])
        for b in w:
            if b in self.lw:
                add(self.lw[b])
            for sk, v in self.rd.get(b, {}).items():
                add((sk, v))
        for t in extra:
            add(t)
        eng = self.eng[e]
        for sk, v in deps.items():
            eng.wait_ge(self.sem[sk], v)
            self.seen[e][sk] = v
            self.n_ins += 1

    def _commit(self, tok, r, w):
        for b in w:
            self.lw[b] = tok
            self.rd[b] = {}
        for b in r:
            d = self.rd.setdefault(b, {})
            if d.get(tok[0], 0) < tok[1]:
                d[tok[0]] = tok[1]

    def op(self, e, fn, r=(), w=()):
        self._deps(e, r, w)
        ins = fn()
        self.cnt[e] += 1
        ins.then_inc(self.sem[e], 1)
        self.n_ins += 1
        tok = (e, self.cnt[e])
        self._commit(tok, r, w)
        return tok

    def dma(self, e, fn, r=(), w=(), is_out=False):
        lo_, hi_ = self.rr_rng[e]
        idx = self.rr[e]
        self.rr[e] = lo_ + (idx + 1 - lo_) % (hi_ - lo_)
        sk = ("d", idx)
        extra = [(sk, self.dval[idx])] if self.dval[idx] > 0 else []
        self._deps(e, r, w, extra)
        ins = fn()
        self.dval[idx] += 16
        ins.then_inc(self.sem[sk], 16)
        self.n_ins += 1
        tok = (sk, self.dval[idx])
        self._commit(tok, r, w)
        if is_out:
            self.out_tokens.append(tok)
        return tok

    def barrier(self):
        cur = {e: self.cnt[e] for e in self.eng}
        for i in range(ND_SEM):
            cur[("d", i)] = self.dval[i]
        for e in self.eng:
            for sk, v in cur.items():
                if v > self.seen[e].get(sk, 0):
                    self.eng[e].wait_ge(self.sem[sk], v)
                    self.seen[e][sk] = v
                    self.n_ins += 1

    def finish(self):
        e = "sp"
        for sk, v in self.out_tokens:
            if self.seen[e].get(sk, 0) < v:
                self.eng[e].wait_ge(self.sem[sk], v)
                self.seen[e][sk] = v


def build(NTL=32, n_layers=2, dbg=False, peer_slots=128):
    T = NTL * 128
    TT = T + CTX
    NTILES = NTL + 2
    NGL = NTL // 4
    L = TT + 64
    groups = [(g, list(range(4 * g, 4 * g + 4))) for g in range(NGL)] + [(NGL, [NTL, NTL + 1])]

    def ppos(tok):
        return tok + 16 if tok < T else tok + 48

    nc = bass.Bass("TRN2", target_bir_lowering=False)

    def din(name, shape, dt=F32):
        return nc.dram_tensor(name, list(shape), dt, kind="ExternalInput").ap()

    def dscr(name, shape, dt=F32):
        kind = "ExternalOutput" if (dbg and name == "xs") else "Internal"
        return nc.dram_tensor(name, list(shape), dt, kind=kind).ap()

    x_in = din("x", [T, D])
    ctx_in = din("ctx", [CTX, D])
    cvec = din("cvec", [128, 2, 8])
    w_mod = din("w_mod", [2, D, 6 * D])
    b_mod_r = din("b_mod_r", [2, 128, 6 * D])
    g_mix_r = din("g_mix_r", [2, 128, D])
    g_ffn_r = din("g_ffn_r", [2, 128, D])
    g_fin_r = din("g_fin_r", [128, D])
    w1_d = din("w1", [2, D, 640])
    w2a_d = din("w2a", [2, D, 1536])
    w2g_d = din("w2g", [2, D, 3072])
    ropeC = din("ropeC", [128, TT])
    ropeS = din("ropeS", [128, TT])
    sink_r = din("sink_r", [2, 1, 2, 512])
    pwbd_d = din("pwbd", [2, 128, 2, 128])
    pscale_d = din("pscale", [2, 128, 2])
    rc_d = din("rc", [128, 2, TT])
    sguwT_d = din("sgu_wT", [2, 128, 4, 128])
    sgub_d = din("sgu_b_r", [2, 64, 4, 128])
    wbrA_d = din("wbrA", [2, 64, 8, D])
    wbrP_d = din("wbrP", [2, 128, 2, D])
    wbrS_d = din("wbrS", [2, 64, 4, D])
    wout_d = din("wout", [2, 128, 8, D])
    wqT_d = din("wqT", [2, 128, 16, D])
    subkT_d = din("subkT", [2, 128, 16, 128])
    peer_u = [din("peer_u%d" % l, [16384, D]) for l in range(2)]
    peer_v = [din("peer_v%d" % l, [16384, D]) for l in range(2)]
    mask_d = din("maskAB", [128, 2, 512])
    ident_d = din("ident", [128, 128])
    iota_d = din("iota16", [128, 16])
    y_out = nc.dram_tensor("y", [T, D], F32, kind="ExternalOutput").ap()

    xs = dscr("xs", [TT, D])
    hTd = dscr("hTd", [NGL + 1, 128, 8, 512], BF16)
    yTd = dscr("yTd", [NGL + 1, 64, 8, 512], BF16)
    ysTd = dscr("ysTd", [NGL + 1, 64, 4, 512], BF16)
    ypTd = dscr("ypTd", [NGL + 1, 128, 2, 512], BF16)
    uvb = [dscr("uvb%d" % l, [16384, 2 * D], BF16) for l in range(2)]

    with ExitStack() as es:
        P = Prog(nc, es)
        V, A, G, PE, SP = nc.vector, nc.scalar, nc.gpsimd, nc.tensor, nc.sync

        uid = [0]

        def sb(es_, name, shape, dt=F32):
            uid[0] += 1
            return es_.enter_context(nc.sbuf_tensor("sb%d_%s" % (uid[0], name), list(shape), dt))

        PS = es.enter_context(nc.psum_tensor("ps", [128, 8, 512], F32))
        bank_ptr = [0]

        def bank(n=1):
            if n > 1 and bank_ptr[0] % n:
                bank_ptr[0] += n - bank_ptr[0] % n
            b = bank_ptr[0] % 8
            bank_ptr[0] = (bank_ptr[0] + n)
            return b

        identf = sb(es, "identf", [128, 128])
        identb = sb(es, "identb", [128, 128], BF16)
        maskb = sb(es, "maskb", [128, 2, 512], BF16)
        ones_b = sb(es, "ones_b", [128, 64], BF16)
        iota16 = sb(es, "iota16", [128, 16])
        screp = sb(es, "screp", [128, 2, 8, 128])
        with ExitStack() as e0:
            maskf = sb(e0, "maskf", [128, 2, 512])
            cv = sb(e0, "cv", [128, 2, 8])
            cs = sb(e0, "cs", [128, 2, 8])
            P.dma("sp", lambda: SP.dma_start(out=identf[:], in_=ident_d[:, :]), w=["identf"])
            P.dma("sp", lambda: SP.dma_start(out=maskf[:], in_=mask_d[:, :, :]), w=["maskf"])
            P.dma("sp", lambda: SP.dma_start(out=iota16[:], in_=iota_d[:, :]), w=["iota16"])
            P.dma("sp", lambda: SP.dma_start(out=cv[:], in_=cvec[:, :, :]), w=["cv"])
            P.dma("sp", lambda: SP.dma_start(out=xs[0:T, :], in_=x_in[:, :]), w=[("xs", t) for t in range(NTL)])
            P.dma("sp", lambda: SP.dma_start(out=xs[T:TT, :], in_=ctx_in[:, :]), w=[("xs", NTL), ("xs", NTL + 1)])
            P.op("dve", lambda: V.tensor_copy(out=identb[:], in_=identf[:]), r=["identf"], w=["identb"])
            P.op("dve", lambda: V.tensor_copy(out=maskb[:], in_=maskf[:]), r=["maskf"], w=["maskb"])
            P.op("dve", lambda: V.memset(ones_b[:], 1.0), w=["ones_b"])
            P.op("act", lambda: A.activation(out=cs[:], in_=cv[:], func=AF.Silu), r=["cv"], w=["cs"])
            P.op("dve", lambda: V.tensor_copy(out=screp[:], in_=cs[:].unsqueeze(3).broadcast_to([128, 2, 8, 128])),
                 r=["cs"], w=["screp"])
            P.barrier()

        def modulation(layer, which, mod, g_r):
            base = which * 3 * D
            with ExitStack() as em:
                wm = [sb(em, "wm%d" % i, [128, 8, 512]) for i in range(2)]
                bm = [sb(em, "bm%d" % i, [128, 512]) for i in range(2)]
                gs = [sb(em, "gs%d" % i, [128, 512]) for i in range(2)]
                tm = [sb(em, "tm%d" % i, [128, 512]) for i in range(2)]
                for ci in range(6):
                    kind, half = ci // 2, ci % 2
                    c0 = base + ci * 512
                    j = ci % 2
                    P.dma("sp", lambda: SP.dma_start(
                        out=wm[j][:], in_=w_mod[layer, :, c0:c0 + 512].rearrange("(k p) n -> p k n", p=128)),
                        w=[("wm", j)])
                    P.dma("sp", lambda: SP.dma_start(out=bm[j][:], in_=b_mod_r[layer, :, c0:c0 + 512]), w=[("bm", j)])
                    if kind == 1:
                        P.dma("sp", lambda: SP.dma_start(out=gs[j][:], in_=g_r[layer, :, half * 512:(half + 1) * 512]),
                              w=[("gs", j)])
                    for s in range(2):
                        b = bank()

                        def mm():
                            for k in range(8):
                                ins = PE.matmul(PS[:, b, :], lhsT=screp[:, s, k, :], rhs=wm[j][:, k, :],
                                                start=(k == 0), stop=(k == 7))
                            return ins
                        P.op("pe", mm, r=["screp", ("wm", j)], w=[("ps", b)])
                        dst_kind = {0: 1, 1: 0, 2: 2}[kind]
                        dst = mod[s][dst_kind][:, half * 512:(half + 1) * 512]
                        dname = ("mod", which, s, dst_kind, half)
                        if kind == 1:
                            P.op("dve", lambda: V.tensor_tensor(out=tm[s][:], in0=PS[:, b, :], in1=bm[j][:], op=ALU.add),
                                 r=[("ps", b), ("bm", j)], w=[("tm", s)])
                            P.op("dve", lambda: V.scalar_tensor_tensor(out=dst, in0=tm[s][:], scalar=1.0, in1=gs[j][:],
                                                                       op0=ALU.add, op1=ALU.mult),
                                 r=[("tm", s), ("gs", j)], w=[dname])
                        else:
                            P.op("dve", lambda: V.tensor_tensor(out=dst, in0=PS[:, b, :], in1=bm[j][:], op=ALU.add),
                                 r=[("ps", b), ("bm", j)], w=[dname])
                P.barrier()

        def modnames(which, s):
            return [("mod", which, s, k, h) for k in range(3) for h in range(2)]

        def load_cast(es_, name, dram_ap, shape, stage_bufs, dst=None, eng_cycle=("act", "pool")):
            raise NotImplementedError

        def norm_mod(xt, xname, Amod, Bmod, modr, junk, ss, hf_out, hb_out, hname, add_eng="pool"):
            P.op("dve", lambda: V.scalar_tensor_tensor(out=junk[:], in0=xt[:], scalar=1.0, in1=xt[:], op0=ALU.mult, op1=ALU.mult, accum_out=ss[:, 0:1]),
                 r=[xname], w=["junk", "ss0"])
            P.op("dve", lambda: V.tensor_scalar(out=ss[:, 1:2], in0=ss[:, 0:1], scalar1=1.0 / D, scalar2=EPS,
                                                op0=ALU.mult, op1=ALU.add), r=["ss0"], w=["ss1"])
            P.op("act", lambda: A.activation(out=ss[:, 2:3], in_=ss[:, 1:2], func=AF.Ln), r=["ss1"], w=["ss2"])
            P.op("act", lambda: A.activation(out=ss[:, 3:4], in_=ss[:, 2:3], func=AF.Exp, scale=-0.5), r=["ss2"], w=["ss3"])
            P.op("dve", lambda: V.scalar_tensor_tensor(out=junk[:], in0=xt[:], scalar=ss[:, 3:4], in1=Amod[:],
                                                       op0=ALU.mult, op1=ALU.mult),
                 r=[xname, "ss3"] + modr, w=["junk"])
            if hf_out is not None:
                P.op("dve", lambda: V.tensor_tensor(out=hf_out[:], in0=junk[:], in1=Bmod[:], op=ALU.add),
                     r=["junk"] + modr, w=[hname + "f"])
                P.op("act", lambda: A.copy(out=hb_out[:], in_=hf_out[:]), r=[hname + "f"], w=[hname])
            elif add_eng == "dve":
                P.op("dve", lambda: V.tensor_tensor(out=hb_out[:], in0=junk[:], in1=Bmod[:], op=ALU.add),
                     r=["junk"] + modr, w=[hname])
            else:
                P.op("pool", lambda: G.tensor_tensor(out=hb_out[:], in0=junk[:], in1=Bmod[:], op=ALU.add),
                     r=["junk"] + modr, w=[hname])

        def transpose8(hb, hname, dst_ap, dname, evac="act", b=None):
            if b is None:
                b = bank()
            psb = PS[:, b, :].bitcast(BF16)

            def tr():
                for k in range(8):
                    ins = PE.transpose(out=psb[:, k * 128:(k + 1) * 128], in_=hb[:, k * 128:(k + 1) * 128],
                                       identity=identb[:])
                return ins
            P.op("pe", tr, r=[hname, "identb"], w=[("ps", b)])
            src = psb.rearrange("p (k t) -> p k t", k=8)
            if evac == "act":
                P.op("act", lambda: A.copy(out=dst_ap, in_=src), r=[("ps", b)], w=[dname])
            else:
                P.op("dve", lambda: V.tensor_copy(out=dst_ap, in_=src), r=[("ps", b)], w=[dname])

        def cast_load(dst_tile_ap, dname, src_ap, stg, sname, ceng):
            P.dma("sp", lambda: SP.dma_start(out=stg, in_=src_ap), w=[sname])
            if ceng == "act":
                P.op("act", lambda: A.copy(out=dst_tile_ap, in_=stg), r=[sname], w=[dname])
            elif ceng == "pool":
                P.op("pool", lambda: G.tensor_copy(out=dst_tile_ap, in_=stg), r=[sname], w=[dname])
            else:
                P.op("dve", lambda: V.tensor_copy(out=dst_tile_ap, in_=stg), r=[sname], w=[dname])

        NQ = 16

        def tabnames(l):
            return [("uvb", l, c0, q) for c0 in (0, D) for q in range(NQ)]

        def conv_gen(l):
            rpq = 16384 // NQ
            for q in range(NQ):
                for (src_t, c0) in ((peer_u[l], 0), (peer_v[l], D)):
                    rows = slice(q * rpq, (q + 1) * rpq)
                    P.dma("pool", lambda: G.dma_start(out=uvb[l][rows, c0:c0 + D], in_=src_t[rows, :]), w=[("uvb", l, c0, q)])
                    yield
        convs = [conv_gen(l) for l in range(n_layers)]

        def pump(l, k):
            if l < n_layers:
                for _ in range(k):
                    next(convs[l], None)

        for layer in range(n_layers):
            last = layer == n_layers - 1
            with ExitStack() as eL:
                modm = [[sb(eL, "modm%d%d" % (s, k), [128, D]) for k in range(3)] for s in range(2)]
                eKV = ExitStack()
                kT = sb(eKV, "kT", [128, TT], BF16)
                vtok = sb(eKV, "vtok", [128, NTILES, 128], BF16)
                dT = sb(eKV, "dT", [128, 2, TT], BF16)
                sinkrow = sb(eKV, "sinkrow", [1, 2, 512], BF16)
                with ExitStack() as e1:
                    sk_f = sb(e1, "sk_f", [1, 2, 512])
                    sk_e = sb(e1, "sk_e", [1, 2, 512])
                    P.dma("sp", lambda: SP.dma_start(out=sk_f[:], in_=sink_r[layer, :, :, :]), w=["sk_f"])
                    P.op("act", lambda: A.activation(out=sk_e[:], in_=sk_f[:], func=AF.Exp), r=["sk_f"], w=["sk_e"])
                    P.op("act", lambda: A.copy(out=sinkrow[:], in_=sk_e[:]), r=["sk_e"], w=["sinkrow"])
                    P.barrier()
                modulation(layer, 0, modm, g_mix_r)

                with ExitStack() as e1:
                    zpad = sb(e1, "zpad", [128, 2, L])
                    e1w = ExitStack()
                    w1 = sb(e1w, "w1", [128, 8, 640], BF16)
                    stg = [sb(e1w, "stg%d" % i, [128, 2, 640]) for i in range(2)]
                    xt_ = [sb(e1w, "xt%d" % i, [128, D]) for i in range(2)]
                    junk = sb(e1w, "junk", [128, D])
                    ss = sb(e1w, "ss", [128, 4])
                    hb_ = [sb(e1w, "hb%d" % i, [128, D], BF16) for i in range(2)]
                    hTg = [sb(e1w, "hTg%d" % i, [128, 8, 512], BF16) for i in range(2)]
                    cS = [sb(e1w, "cS%d" % i, [128, 2, 512]) for i in range(2)]
                    t1 = sb(e1w, "t1", [128, 512])
                    t2 = sb(e1w, "t2", [128, 512])
                    for i in range(4):
                        cast_load(w1[:, 2 * i:2 * i + 2, :], ("w1", i),
                                  w1_d[layer, 256 * i:256 * (i + 1), :].rearrange("(k p) n -> p k n", p=128),
                                  stg[i % 2][:], ("stg", i % 2), "act")
                    P.op("dve", lambda: V.memset(zpad[:], 0.0), w=["zpad"])
                    for gi, tiles in groups:
                        j = gi % 2
                        ng = len(tiles) * 128
                        s = 0 if tiles[0] < NTL else 1
                        tok0 = tiles[0] * 128
                        for ti, tile in enumerate(tiles):
                            jj = tile % 2
                            P.dma("sp", lambda: SP.dma_start(out=xt_[jj][:], in_=xs[tile * 128:(tile + 1) * 128, :]),
                                  r=[("xs", tile)], w=[("xt", jj)])
                            norm_mod(xt_[jj], ("xt", jj), modm[s][0], modm[s][1], modnames(0, s), junk, ss, None,
                                     hb_[jj], ("hb", jj), add_eng="dve")
                            transpose8(hb_[jj], ("hb", jj), hTg[j][:, :, ti * 128:(ti + 1) * 128], ("hTg", j, ti))
                        hr = [("hTg", j, ti) for ti in range(len(tiles))]
                        P.dma("sp", lambda: SP.dma_start(out=hTd[gi, :, :, 0:ng], in_=hTg[j][:, :, 0:ng]), r=hr, w=[("hTd", gi)])
                        P.dma("sp", lambda: SP.dma_start(out=cS[j][:, 0, 0:ng], in_=ropeC[:, tok0:tok0 + ng]), w=[("cS", j, 0)])
                        P.dma("sp", lambda: SP.dma_start(out=cS[j][:, 1, 0:ng], in_=ropeS[:, tok0:tok0 + ng]), w=[("cS", j, 1)])
                        if layer == 0:
                            pump(0, 4)
                        bk, bkr = bank(), bank()
                        for (bb, c0) in ((bk, 0), (bkr, 128)):
                            def mm(bb=bb, c0=c0):
                                for k in range(8):
                                    ins = PE.matmul(PS[:, bb, 0:ng], lhsT=w1[:, k, c0:c0 + 128], rhs=hTg[j][:, k, 0:ng],
                                                    start=(k == 0), stop=(k == 7))
                                return ins
                            P.op("pe", mm, r=hr + [("w1", i_) for i_ in range(4)], w=[("ps", bb)])
                        P.op("dve", lambda: V.tensor_tensor(out=t1[:, 0:ng], in0=PS[:, bkr, 0:ng], in1=cS[j][:, 1, 0:ng], op=ALU.mult),
                             r=[("ps", bkr), ("cS", j, 1)], w=["t1"])
                        P.op("dve", lambda: V.tensor_tensor(out=t2[:, 0:ng], in0=PS[:, bk, 0:ng], in1=cS[j][:, 0, 0:ng], op=ALU.mult),
                             r=[("ps", bk), ("cS", j, 0)], w=["t2"])
                        P.op("dve", lambda: V.tensor_tensor(out=kT[:, tok0:tok0 + ng], in0=t1[:, 0:ng], in1=t2[:, 0:ng], op=ALU.add),
                             r=["t1", "t2"], w=[("kT", gi)])
                        for c in range(2):
                            bz = bank()

                            def mm(bz=bz, c=c):
                                for k in range(8):
                                    ins = PE.matmul(PS[:, bz, 0:ng], lhsT=w1[:, k, 384 + c * 128:512 + c * 128],
                                                    rhs=hTg[j][:, k, 0:ng], start=(k == 0), stop=(k == 7))
                                return ins
                            P.op("pe", mm, r=hr + [("w1", i_) for i_ in range(4)], w=[("ps", bz)])
                            p0 = ppos(tok0)
                            P.op("act", lambda: A.copy(out=zpad[:, c, p0:p0 + ng], in_=PS[:, bz, 0:ng]),
                                 r=[("ps", bz)], w=["zpad"])
                        bv = bank()

                        def mm():
                            for ti in range(len(tiles)):
                                for k in range(8):
                                    ins = PE.matmul(PS[:, bv, ti * 128:(ti + 1) * 128], lhsT=hTg[j][:, k, ti * 128:(ti + 1) * 128],
                                                    rhs=w1[:, k, 256:384], start=(k == 0), stop=(k == 7))
                            return ins
                        P.op("pe", mm, r=hr + [("w1", i_) for i_ in range(4)], w=[("ps", bv)])
                        nt = len(tiles)
                        P.op("act", lambda: A.copy(out=vtok[:, tiles[0]:tiles[0] + nt, :],
                                                   in_=PS[:, bv, 0:ng].rearrange("p (a b) -> p a b", a=nt)),
                             r=[("ps", bv)], w=[("vtok", gi)])

                    P.barrier()
                    e1w.close()
                    PA = sb(e1, "PA", [128, L])
                    PB = sb(e1, "PB", [128, L])
                    P.op("dve", lambda: V.memset(PA[:], 0.0), w=["PA"])
                    P.op("dve", lambda: V.memset(PB[:], 0.0), w=["PB"])
                    rc = sb(e1, "rc", [128, 2, TT])
                    P.dma("sp", lambda: SP.dma_start(out=rc[:], in_=rc_d[:, :, :]), w=["rc"])
                    lo, hi = 8, L - 8

                    def shadd(dst, src, s1, s2, p0=0, p1=128):
                        P.op("dve", lambda: V.tensor_tensor(out=dst[p0:p1, lo:hi], in0=src[p0:p1, lo + s1:hi + s1],
                                                            in1=src[p0:p1, lo + s2:hi + s2], op=ALU.add),
                             r=["PA", "PB", "zpad"], w=["PA", "PB"])
                    segs = [(0, T, 16), (T, TT, 48)]

                    def dfin(src, c, p0, p1):
                        for (a, b_, off) in segs:
                            P.op("dve", lambda: V.tensor_tensor(out=src[p0:p1, a + off:b_ + off], in0=src[p0:p1, a + off:b_ + off],
                                                                in1=rc[p0:p1, c, a:b_], op=ALU.mult),
                                 r=["PA", "PB", "rc"], w=["PA", "PB"])
                            P.op("dve", lambda: V.tensor_tensor(out=dT[p0:p1, c, a:b_], in0=src[p0:p1, a + off:b_ + off],
                                                                in1=zpad[p0:p1, c, a + off:b_ + off], op=ALU.subtract),
                                 r=["PA", "PB", "zpad"], w=["dT"])
                    shadd(PA, zpad[:, 0, :], -1, 0)
                    shadd(PB, PA, -1, 1, 64, 128)
                    dfin(PA, 0, 0, 64)
                    dfin(PB, 0, 64, 128)
                    shadd(PA, zpad[:, 1, :], -1, 0)
                    shadd(PB, PA, -1, 1)
                    shadd(PA, PB, -2, 2)
                    shadd(PB, PA, -4, 4, 64, 128)
                    dfin(PA, 1, 0, 64)
                    dfin(PB, 1, 64, 128)
                    P.barrier()

                allk = [("kT", gi) for gi, _ in groups]
                allv = [("vtok", gi) for gi, _ in groups]
                m2_groups = groups if not last else groups[:NGL]
                with ExitStack() as e2:
                    w2a = sb(e2, "w2a", [128, 8, 1536], BF16)
                    stg = [sb(e2, "stgA%d" % i, [128, 1, 1536]) for i in range(2)]
                    pwbd = sb(e2, "pwbd", [128, 2, 128], BF16)
                    pwf = sb(e2, "pwf", [128, 2, 128])
                    pscale = sb(e2, "pscale", [128, 2])
                    sguwT = sb(e2, "sguwT", [128, 4, 128], BF16)
                    sguf = sb(e2, "sguf", [128, 4, 128])
                    sgub = sb(e2, "sgub", [64, 4, 128])
                    junk = sb(e2, "junkA", [128, D])
                    ss = sb(e2, "ssA", [128, 4])
                    hTg = [sb(e2, "hTgA%d" % i, [128, 8, 512], BF16) for i in range(2)]
                    cS = [sb(e2, "cSA%d" % i, [128, 2, 512]) for i in range(1)] * 2
                    t1 = sb(e2, "t1A", [128, 512])
                    t2 = sb(e2, "t2A", [128, 512])
                    qT = sb(e2, "qT", [128, 4, 512], BF16)
                    pT = [sb(e2, "pT%d" % i, [128, 512], BF16) for i in range(6)]
                    lnd = sb(e2, "lnd", [64, 512])
                    rec = sb(e2, "rec", [64, 512])
                    yT = sb(e2, "yT", [64, 8, 512], BF16)
                    uT = sb(e2, "uT", [64, 4, 512], BF16)
                    vg = sb(e2, "vg", [128, 256])
                    vn = sb(e2, "vn", [128, 256], BF16)
                    sv = sb(e2, "svA", [128, 4])
                    ysT = sb(e2, "ysT", [64, 4, 512], BF16)
                    ypT = sb(e2, "ypT", [128, 2, 512], BF16)
                    tsg = sb(e2, "tsg", [64, 512])
                    for i in range(8):
                        cast_load(w2a[:, i:i + 1, :], ("w2a", i),
                                  w2a_d[layer, 128 * i:128 * (i + 1), :].rearrange("(k p) n -> p k n", p=128),
                                  stg[i % 2][:], ("stgA", i % 2), "act" if i % 2 == 0 else "pool")
                    w2r = [("w2a", i) for i in range(8)]
                    cast_load(pwbd[:], "pwbd", pwbd_d[layer, :, :, :], pwf[:], "pwf", "dve")
                    cast_load(sguwT[:], "sguwT", sguwT_d[layer, :, :, :], sguf[:], "sguf", "dve")
                    P.dma("sp", lambda: SP.dma_start(out=pscale[:], in_=pscale_d[layer, :, :]), w=["pscale"])
                    P.dma("sp", lambda: SP.dma_start(out=sgub[:], in_=sgub_d[layer, :, :, :]), w=["sgub"])
                    pti = [0]
                    def load_h(gidx):
                        gi2, tiles2 = m2_groups[gidx]
                        ng2 = len(tiles2) * 128
                        j2 = gidx % 2
                        P.dma("sp", lambda: SP.dma_start(out=hTg[j2][:, :, 0:ng2], in_=hTd[gi2, :, :, 0:ng2]),
                              r=[("hTd", gi2)], w=[("hTgA", j2)])
                    load_h(0)
                    for gidx, (gi, tiles) in enumerate(m2_groups):
                        j = gidx % 2
                        nt = len(tiles)
                        ng = nt * 128
                        s = 0 if tiles[0] < NTL else 1
                        tok0 = tiles[0] * 128
                        if gidx + 1 < len(m2_groups):
                            load_h(gidx + 1)
                        hr = [("hTgA", j)]
                        P.dma("sp", lambda: SP.dma_start(out=cS[0][:, 0, 0:ng], in_=ropeC[:, tok0:tok0 + ng]), w=[("cS", 0, 0)])
                        P.dma("sp", lambda: SP.dma_start(out=cS[0][:, 1, 0:ng], in_=ropeS[:, tok0:tok0 + ng]), w=[("cS", 0, 1)])
                        for c in range(4):
                            bq, bqr = bank(), bank()
                            for (bb, c0) in ((bq, c * 128), (bqr, 512 + c * 128)):
                                def mm(bb=bb, c0=c0):
                                    for k in range(8):
                                        ins = PE.matmul(PS[:, bb, 0:ng], lhsT=w2a[:, k, c0:c0 + 128], rhs=hTg[j][:, k, 0:ng],
                                                        start=(k == 0), stop=(k == 7))
                                    return ins
                                P.op("pe", mm, r=hr + w2r, w=[("ps", bb)])
                            P.op("dve", lambda: V.tensor_tensor(out=t1[:, 0:ng], in0=PS[:, bqr, 0:ng], in1=cS[0][:, 1, 0:ng], op=ALU.mult),
                                 r=[("ps", bqr), ("cS", 0, 1)], w=["t1"])
                            P.op("dve", lambda: V.tensor_tensor(out=t2[:, 0:ng], in0=PS[:, bq, 0:ng], in1=cS[0][:, 0, 0:ng], op=ALU.mult),
                                 r=[("ps", bq), ("cS", 0, 0)], w=["t2"])
                            P.op("dve", lambda: V.tensor_tensor(out=qT[:, c, 0:ng], in0=t1[:, 0:ng], in1=t2[:, 0:ng], op=ALU.add),
                                 r=["t1", "t2"], w=[("qT", c)])
                        qr_ = [("qT", c) for c in range(4)]
                        if layer == 0:
                            pump(1, 4)
                        for ti, tile in enumerate(tiles):
                            if tile < NTL:
                                keys = []
                                if tile - 1 >= 0:
                                    keys.append((tile - 1, 0))
                                keys.append((tile, None))
                                if tile + 1 < NTL:
                                    keys.append((tile + 1, 1))
                                keys += [(NTL, None), (NTL + 1, None)]
                            else:
                                keys = [(NTL, None), (NTL + 1, None)]
                            for hk in range(2):
                                h0, h1 = hk * 64, hk * 64 + 64
                                bnum, bden = bank(), bank()
                                sbanks = []
                                for (kt, mk) in keys:
                                    bs = bank()
                                    sbanks.append(bs)

                                    def mm(bs=bs, kt=kt, mk=mk):
                                        ins = PE.matmul(PS[:, bs, :], lhsT=kT[h0:h1, kt * 128:(kt + 1) * 128],
                                                        rhs=qT[h0:h1, :, ti * 128:(ti + 1) * 128],
                                                        start=True, stop=(mk is None))
                                        if mk is not None:
                                            ins = PE.matmul(PS[:, bs, :], lhsT=identb[:], rhs=maskb[:, mk, :], start=False, stop=True)
                                        return ins
                                    P.op("pe", mm, r=allk + qr_ + ["identb", "maskb"], w=[("ps", bs)])
                                pts = []
                                for bs in sbanks:
                                    pi = pti[0] % 6
                                    pti[0] += 1
                                    pts.append(pi)
                                    P.op("act", lambda bs=bs, pi=pi: A.activation(out=pT[pi][:], in_=PS[:, bs, :], func=AF.Exp, scale=0.125),
                                         r=[("ps", bs)], w=[("pT", pi)])
                                nk = len(keys)
                                for ki, ((kt, mk), pi) in enumerate(zip(keys, pts)):
                                    def mm(ki=ki, kt=kt, pi=pi):
                                        PE.matmul(PS[0:64, bnum, :], lhsT=vtok[:, kt, h0:h1], rhs=pT[pi][:],
                                                  start=(ki == 0), stop=(ki == nk - 1))
                                        ins = PE.matmul(PS[0:64, bden, :], lhsT=ones_b[:, 0:64], rhs=pT[pi][:],
                                                        start=(ki == 0), stop=False)
                                        if ki == nk - 1:
                                            ins = PE.matmul(PS[0:64, bden, :], lhsT=ones_b[0:1, 0:64], rhs=sinkrow[0:1, hk, :],
                                                            start=False, stop=True)
                                        return ins
                                    P.op("pe", mm, r=allv + [("pT", pi), "ones_b", "sinkrow"], w=[("ps", bnum), ("ps", bden)])
                                P.op("act", lambda: A.activation(out=lnd[:], in_=PS[0:64, bden, :], func=AF.Ln),
                                     r=[("ps", bden)], w=["lnd"])
                                P.op("act", lambda: A.activation(out=rec[:], in_=lnd[:], func=AF.Exp, scale=-1.0),
                                     r=["lnd"], w=["rec"])
                                P.op("dve", lambda: V.tensor_tensor(
                                    out=yT[:, hk * 4:hk * 4 + 4, ti * 128:(ti + 1) * 128],
                                    in0=PS[0:64, bnum, :].rearrange("p (g q) -> p g q", g=4),
                                    in1=rec[:].rearrange("p (g q) -> p g q", g=4), op=ALU.mult),
                                    r=[("ps", bnum), "rec"], w=[("yT", ti, hk)])
                        for h4 in range(4):
                            bu = bank()

                            def mm():
                                for k in range(8):
                                    ins = PE.matmul(PS[0:64, bu, 0:ng], lhsT=w2a[:, k, 1024 + h4 * 64:1088 + h4 * 64],
                                                    rhs=hTg[j][:, k, 0:ng], start=(k == 0), stop=(k == 7))
                                return ins
                            P.op("pe", mm, r=hr + w2r, w=[("ps", bu)])
                            P.op("act", lambda: A.activation(out=uT[:, h4, 0:ng], in_=PS[0:64, bu, 0:ng], func=AF.Gelu_apprx_tanh),
                                 r=[("ps", bu)], w=[("uT", h4)])
                        for ti, tile in enumerate(tiles):
                            bvs = bank()

                            def mm():
                                for k in range(8):
                                    ins = PE.matmul(PS[:, bvs, 0:256], lhsT=hTg[j][:, k, ti * 128:(ti + 1) * 128],
                                                    rhs=w2a[:, k, 1280:1536], start=(k == 0), stop=(k == 7))
                                return ins
                            P.op("pe", mm, r=hr + w2r, w=[("ps", bvs)])
                            P.op("act", lambda: A.activation(out=vg[:], in_=PS[:, bvs, 0:256], func=AF.Gelu_apprx_tanh),
                                 r=[("ps", bvs)], w=["vg"])
                            P.op("dve", lambda: V.scalar_tensor_tensor(out=junk[:, 0:256], in0=vg[:], scalar=1.0, in1=vg[:],
                                                                        op0=ALU.mult, op1=ALU.mult, accum_out=sv[:, 0:1]),
                                 r=["vg"], w=["junk", "sv0"])
                            P.op("dve", lambda: V.tensor_scalar(out=sv[:, 1:2], in0=sv[:, 0:1], scalar1=1.0 / 256, scalar2=EPS,
                                                                op0=ALU.mult, op1=ALU.add), r=["sv0"], w=["sv1"])
                            P.op("act", lambda: A.activation(out=sv[:, 2:3], in_=sv[:, 1:2], func=AF.Ln), r=["sv1"], w=["sv2"])
                            P.op("act", lambda: A.activation(out=sv[:, 3:4], in_=sv[:, 2:3], func=AF.Exp, scale=-0.5), r=["sv2"], w=["sv3"])
                            P.op("dve", lambda: V.tensor_scalar(out=vn[:], in0=vg[:], scalar1=sv[:, 3:4], scalar2=None, op0=ALU.mult),
                                 r=["vg", "sv3"], w=["vn"])
                            bm_ = bank()

                            def mm():
                                for h4 in range(4):
                                    ins = PE.matmul(PS[0:64, bm_, h4 * 128:(h4 + 1) * 128], lhsT=vn[:, h4 * 64:(h4 + 1) * 64],
                                                    rhs=sguwT[:, h4, :], start=True, stop=True)
                                return ins
                            P.op("pe", mm, r=["vn", "sguwT"], w=[("ps", bm_)])
                            P.op("dve", lambda: V.tensor_tensor(out=tsg[:], in0=PS[0:64, bm_, :],
                                                                in1=sgub[:].rearrange("p a b -> p (a b)"), op=ALU.add),
                                 r=[("ps", bm_), "sgub"], w=["tsg"])
                            P.op("dve", lambda: V.tensor_tensor(out=ysT[:, :, ti * 128:(ti + 1) * 128],
                                                                in0=tsg[:].rearrange("p (a b) -> p a b", a=4),
                                                                in1=uT[:, :, ti * 128:(ti + 1) * 128], op=ALU.mult),
                                 r=["tsg"] + [("uT", h4) for h4 in range(4)], w=[("ysT", ti)])
                        for c in range(2):
                            bp = bank()
                            P.op("pe", lambda: PE.matmul(PS[:, bp, 0:ng], lhsT=pwbd[:, c, :], rhs=dT[:, c, tok0:tok0 + ng],
                                                         start=True, stop=True), r=["pwbd", "dT"], w=[("ps", bp)])
                            P.op("dve", lambda: V.tensor_scalar(out=ypT[:, c, 0:ng], in0=PS[:, bp, 0:ng], scalar1=pscale[:, c:c + 1],
                                                                scalar2=None, op0=ALU.mult),
                                 r=[("ps", bp), "pscale"], w=[("ypT", c)])
                        yr = [("yT", ti, hk) for ti in range(nt) for hk in range(2)]
                        P.dma("sp", lambda: SP.dma_start(out=yTd[gi, :, :, 0:ng], in_=yT[:, :, 0:ng]), r=yr, w=[("yTd", gi)])
                        P.dma("sp", lambda: SP.dma_start(out=ysTd[gi, :, :, 0:ng], in_=ysT[:, :, 0:ng]),
                              r=[("ysT", ti) for ti in range(nt)], w=[("ysTd", gi)])
                        P.dma("sp", lambda: SP.dma_start(out=ypTd[gi, :, :, 0:ng], in_=ypT[:, :, 0:ng]),
                              r=[("ypT", 0), ("ypT", 1)], w=[("ypTd", gi)])
                    P.barrier()

                P.barrier()
                eKV.close()
                with ExitStack() as e3:
                    w2g = sb(e3, "w2g", [128, 8, 3072], BF16)
                    wbrA = sb(e3, "wbrA", [64, 8, D], BF16)
                    wbrP = sb(e3, "wbrP", [128, 2, D], BF16)
                    wbrS = sb(e3, "wbrS", [64, 4, D], BF16)
                    wout = sb(e3, "wout", [128, 8, D], BF16)
                    stg = [sb(e3, "stgB%d" % i, [128, 2048]) for i in range(2)]
                    hTl = [sb(e3, "hTl%d" % i, [128, 8, 512], BF16) for i in range(1)] * 2
                    yTl = sb(e3, "yTl", [64, 8, 512], BF16)
                    ysTl = sb(e3, "ysTl", [64, 4, 512], BF16)
                    ypTl = sb(e3, "ypTl", [128, 2, 512], BF16)
                    sig = [sb(e3, "sig%d" % i, [128, 512], BF16) for i in range(3)]
                    tt = [sb(e3, "tt%d" % i, [128, 512]) for i in range(4)]
                    mT = sb(e3, "mT", [128, 8, 512], BF16)
                    xt_ = [sb(e3, "xtB%d" % i, [128, D]) for i in range(1)] * 2
                    xo_ = [sb(e3, "xoB%d" % i, [128, D]) for i in range(1)] * 2
                    ci = [0]

                    def cl(dst, dname, src, width):
                        i = ci[0] % 2
                        ci[0] += 1
                        cast_load(dst, dname, src, stg[i][:, 0:width] if True else None, ("stgB", i),
                                  ("act", "pool", "dve")[ci[0] % 3])
                    for k in range(8):
                        for hh in range(2):
                            cl(w2g[:, k, hh * 1536:(hh + 1) * 1536], ("w2g", k, hh),
                               w2g_d[layer, k * 128:(k + 1) * 128, hh * 1536:(hh + 1) * 1536], 1536)
                    for h in range(0, 8, 2):
                        i = ci[0] % 2
                        ci[0] += 1
                        cast_load(wbrA[:, h:h + 2, :], ("wbrA", h), wbrA_d[layer, :, h:h + 2, :],
                                  stg[i][0:64, 0:2048].rearrange("p (a b) -> p a b", a=2), ("stgB", i), "act")
                    i = ci[0] % 2
                    ci[0] += 1
                    cast_load(wbrP[:], "wbrP", wbrP_d[layer, :, :, :], stg[i][:, 0:2048].rearrange("p (a b) -> p a b", a=2),
                              ("stgB", i), "pool")
                    for h in range(0, 4, 2):
                        i = ci[0] % 2
                        ci[0] += 1
                        cast_load(wbrS[:, h:h + 2, :], ("wbrS", h), wbrS_d[layer, :, h:h + 2, :],
                                  stg[i][0:64, 0:2048].rearrange("p (a b) -> p a b", a=2), ("stgB", i), "dve")
                    for k in range(0, 8, 2):
                        i = ci[0] % 2
                        ci[0] += 1
                        cast_load(wout[:, k:k + 2, :], ("wout", k), wout_d[layer, :, k:k + 2, :],
                                  stg[i][:, 0:2048].rearrange("p (a b) -> p a b", a=2), ("stgB", i), "act")
                    wr_g = [("w2g", k, hh) for k in range(8) for hh in range(2)]
                    wr_b = [("wbrA", h) for h in range(0, 8, 2)] + ["wbrP"] + [("wbrS", h) for h in range(0, 4, 2)]
                    wr_o = [("wout", k) for k in range(0, 8, 2)]
                    for gi, tiles in m2_groups:
                        j = 0
                        nt = len(tiles)
                        ng = nt * 128
                        s = 0 if tiles[0] < NTL else 1
                        P.dma("sp", lambda: SP.dma_start(out=hTl[j][:, :, 0:ng], in_=hTd[gi, :, :, 0:ng]), r=[("hTd", gi)], w=[("hTl", j)])
                        P.dma("sp", lambda: SP.dma_start(out=yTl[:, :, 0:ng], in_=yTd[gi, :, :, 0:ng]), r=[("yTd", gi)], w=["yTl"])
                        P.dma("sp", lambda: SP.dma_start(out=ysTl[:, :, 0:ng], in_=ysTd[gi, :, :, 0:ng]), r=[("ysTd", gi)], w=["ysTl"])
                        P.dma("sp", lambda: SP.dma_start(out=ypTl[:, :, 0:ng], in_=ypTd[gi, :, :, 0:ng]), r=[("ypTd", gi)], w=["ypTl"])
                        for m in range(8):
                            mc = slice(m * 128, (m + 1) * 128)
                            bgs = []
                            for i3 in range(3):
                                bg = bank()
                                bgs.append(bg)

                                def mmg(bg=bg, i3=i3):
                                    for k in range(8):
                                        ins = PE.matmul(PS[:, bg, 0:ng], lhsT=w2g[:, k, i3 * D + m * 128:i3 * D + (m + 1) * 128],
                                                        rhs=hTl[j][:, k, 0:ng], start=(k == 0), stop=(k == 7))
                                    return ins
                                P.op("pe", mmg, r=wr_g + [("hTl", j)], w=[("ps", bg)])
                                P.op("act", lambda bg=bg, i3=i3: A.activation(out=sig[i3][:, 0:ng], in_=PS[:, bg, 0:ng], func=AF.Sigmoid),
                                     r=[("ps", bg)], w=[("sig", i3)])
                            bA, bP, bS = bank(), bank(), bank()

                            def mmA():
                                for h in range(8):
                                    ins = PE.matmul(PS[:, bA, 0:ng], lhsT=wbrA[:, h, mc], rhs=yTl[:, h, 0:ng], start=(h == 0), stop=(h == 7))
                                return ins

                            def mmP():
                                for c in range(2):
                                    ins = PE.matmul(PS[:, bP, 0:ng], lhsT=wbrP[:, c, mc], rhs=ypTl[:, c, 0:ng], start=(c == 0), stop=(c == 1))
                                return ins

                            def mmS():
                                for h in range(4):
                                    ins = PE.matmul(PS[:, bS, 0:ng], lhsT=wbrS[:, h, mc], rhs=ysTl[:, h, 0:ng], start=(h == 0), stop=(h == 3))
                                return ins
                            P.op("pe", mmA, r=wr_b + ["yTl"], w=[("ps", bA)])
                            P.op("pe", mmP, r=wr_b + ["ypTl"], w=[("ps", bP)])
                            P.op("pe", mmS, r=wr_b + ["ysTl"], w=[("ps", bS)])
                            for i3, bb in enumerate((bA, bP, bS)):
                                P.op("dve", lambda i3=i3, bb=bb: V.tensor_tensor(out=tt[i3][:, 0:ng], in0=PS[:, bb, 0:ng], in1=sig[i3][:, 0:ng], op=ALU.mult),
                                     r=[("ps", bb), ("sig", i3)], w=[("tt", i3)])
                            P.op("pool", lambda: G.tensor_tensor(out=tt[3][:, 0:ng], in0=tt[0][:, 0:ng], in1=tt[1][:, 0:ng], op=ALU.add),
                                 r=[("tt", 0), ("tt", 1)], w=[("tt", 3)])
                            P.op("pool", lambda: G.tensor_tensor(out=mT[:, m, 0:ng], in0=tt[3][:, 0:ng], in1=tt[2][:, 0:ng], op=ALU.add),
                                 r=[("tt", 3), ("tt", 2)], w=[("mT", m)])
                        mr = [("mT", m) for m in range(8)]
                        for ti, tile in enumerate(tiles):
                            jj = 0
                            P.dma("sp", lambda: SP.dma_start(out=xt_[jj][:], in_=xs[tile * 128:(tile + 1) * 128, :]),
                                  r=[("xs", tile)], w=[("xt", jj)])
                            for half in range(2):
                                hc = slice(half * 512, (half + 1) * 512)
                                bo = bank()

                                def mmo():
                                    for k in range(8):
                                        ins = PE.matmul(PS[:, bo, :], lhsT=mT[:, k, ti * 128:(ti + 1) * 128], rhs=wout[:, k, hc],
                                                        start=(k == 0), stop=(k == 7))
                                    return ins
                                P.op("pe", mmo, r=mr + wr_o, w=[("ps", bo)])
                                P.op("dve", lambda: V.tensor_tensor(out=tt[half][:], in0=PS[:, bo, :], in1=modm[s][2][:, hc], op=ALU.mult),
                                     r=[("ps", bo)] + modnames(0, s), w=[("tt", half)])
                                P.op("pool", lambda: G.tensor_tensor(out=xo_[jj][:, hc], in0=tt[half][:], in1=xt_[jj][:, hc], op=ALU.add),
                                     r=[("tt", half), ("xt", jj)], w=[("xo", jj, half)])
                            P.dma("sp", lambda: SP.dma_start(out=xs[tile * 128:(tile + 1) * 128, :], in_=xo_[jj][:]),
                                  r=[("xo", jj, 0), ("xo", jj, 1)], w=[("xs", tile)])
                    P.barrier()
                P.barrier()

            for _ in convs[layer]:
                pass
            with ExitStack() as eP:
                modp = [[sb(eP, "modp%d%d" % (s, k), [128, D]) for k in range(3)] for s in range(2)]
                modulation(layer, 1, modp, g_ffn_r)
                wf = sb(eP, "wfold", [128, 8, 2048], BF16)
                with ExitStack() as ef:
                    wqT = sb(ef, "wqT", [128, 16, D])
                    subkT = sb(ef, "subkT", [128, 16, 128])
                    for c in range(0, 16, 4):
                        P.dma("sp", lambda: SP.dma_start(out=wqT[:, c:c + 4, :], in_=wqT_d[layer, :, c:c + 4, :]), w=[("wqT", c)])
                    P.dma("sp", lambda: SP.dma_start(out=subkT[:], in_=subkT_d[layer, :, :, :]), w=["subkT"])
                    for k in range(8):
                        for c4 in range(4):
                            b = bank()

                            def mm():
                                for cc in range(4):
                                    c = c4 * 4 + cc
                                    ins = PE.matmul(PS[:, b, cc * 128:(cc + 1) * 128], lhsT=wqT[:, c, k * 128:(k + 1) * 128],
                                                    rhs=subkT[:, c, :], start=True, stop=True)
                                return ins
                            P.op("pe", mm, r=[("wqT", c4 * 4), "subkT"], w=[("ps", b)])
                            P.op("act", lambda: A.copy(out=wf[:, k, c4 * 512:(c4 + 1) * 512], in_=PS[:, b, :]),
                                 r=[("ps", b)], w=[("wf", k, c4)])
                    P.barrier()
                wfr = [("wf", k, c4) for k in range(8) for c4 in range(4)]
                xt_ = [sb(eP, "xtP%d" % i, [128, D]) for i in range(3)]
                junk = sb(eP, "junkP", [128, D])
                ss = sb(eP, "ssP", [128, 4])
                h2b_ = [sb(eP, "h2b%d" % i, [128, D], BF16) for i in range(2)]
                h2T = sb(eP, "h2T", [128, 8, 128], BF16)
                sS = sb(eP, "sS", [128, 16, 128])
                sS2 = sb(eP, "sS2", [128, 16, 128])
                svt = sb(eP, "svt", [128, 8, 2, 16])
                sit = sb(eP, "sit", [128, 8, 2, 16], U32)
                sif = sb(eP, "sif", [128, 8, 2, 16])
                cand = sS[:].rearrange("p a b -> p (a b)").rearrange("p (h c) -> p h c", h=8)
                cand2 = sS2[:].rearrange("p a b -> p (a b)").rearrange("p (h c) -> p h c", h=8)
                ts = sb(eP, "ts", [128, 8, 16])
                tp = sb(eP, "tp", [128, 8, 16], U32)
                ta = sb(eP, "ta", [128, 8, 16], U32)
                tb_ = sb(eP, "tb", [128, 8, 16], U32)
                taf = sb(eP, "taf", [128, 8, 16])
                tbf = sb(eP, "tbf", [128, 8, 16])
                eq = sS2[:].rearrange("p a b -> p (a b)").rearrange("p (h a b) -> p h a b", h=8, a=16)
                If = sb(eP, "If", [128, 8, 16])
                Jf = sb(eP, "Jf", [128, 8, 16])
                ef_ = sb(eP, "ef", [128, 128])
                eu_ = [sb(eP, "eu%d" % i, [128, 128], U32) for i in range(2)]
                tsc = sb(eP, "tsc", [128, 8, 16])
                ex = sb(eP, "ex", [128, 8, 16])
                zz = sb(eP, "zz", [128, 8])
                rz = sb(eP, "rz", [128, 8])
                gate_ = [sb(eP, "gate%d" % i, [128, 128]) for i in range(2)]
                actv = sb(eP, "actv", [128, 128])
                gact = sb(eP, "gact", [128, 128])
                wgt = sb(eP, "wgt", [128, 128])
                guv = [sb(eP, "guv%d" % i, [128, 2 * D], BF16) for i in range(NGB)]
                prod = [sb(eP, "prod%d" % i, [128, D], BF16) for i in range(3)]
                junkb = sb(eP, "junkb", [128, D], BF16)
                GS = 4
                diag = [sb(eP, "diag%d" % i, [128, GS, 128], BF16) for i in range(3)]
                tmpo = [sb(eP, "tmpo%d" % i, [128, 512]) for i in range(2)]
                xo = sb(eP, "xoP", [128, D])
                if last:
                    gfin = sb(eP, "gfin", [128, D])
                    P.dma("sp", lambda: SP.dma_start(out=gfin[:], in_=g_fin_r[:, :]), w=["gfin"])
                cnt_u, cnt_v, cnt_p = [0], [0], [0]
                ptiles = list(range(NTILES)) if not last else list(range(NTL))
                pidx = {t_: i_ for i_, t_ in enumerate(ptiles)}

                def stage_A(tile):
                    jj = tile % 2
                    x3 = pidx[tile] % 3
                    s = 0 if tile < NTL else 1
                    P.dma("sp", lambda: SP.dma_start(out=xt_[x3][:], in_=xs[tile * 128:(tile + 1) * 128, :]),
                          r=[("xs", tile)], w=[("xt", x3)])
                    norm_mod(xt_[x3], ("xt", x3), modp[s][0], modp[s][1], modnames(1, s), junk, ss, None, h2b_[jj], ("h2", jj),
                             add_eng="dve")
                    transpose8(h2b_[jj], ("h2", jj), h2T[:], "h2T", b=0)

                    def mm():
                        for n in range(4):
                            for k in range(8):
                                ins = PE.matmul(PS[:, n, :], lhsT=h2T[:, k, :], rhs=wf[:, k, n * 512:(n + 1) * 512],
                                                start=(k == 0), stop=(k == 7))
                        return ins
                    psn = [("ps", n) for n in range(4)]
                    P.op("pe", mm, r=["h2T"] + wfr, w=psn)
                    P.op("act", lambda: A.copy(out=sS[:].rearrange("p a b -> p (a b)"),
                                               in_=PS[:, 0:4, :].rearrange("p a b -> p (a b)")), r=psn, w=["sS"])

                def stage_B(tile):
                    eu = eu_[tile % 2]
                    eun = ("eu", tile % 2)
                    gate = gate_[tile % 2]
                    gaten = ("gate", tile % 2)
                    for c in range(16):
                        h, p_ = c // 2, c % 2
                        P.op("dve", lambda: V.max(out=svt[:, h, p_, 0:8], in_=sS[:, c, :]), r=["sS"], w=[("sv", c, 0)])
                        yield
                    for c in range(16):
                        h, p_ = c // 2, c % 2
                        P.op("dve", lambda: V.max_index(out=sit[:, h, p_, 0:8], in_max=svt[:, h, p_, 0:8], in_values=sS[:, c, :]),
                             r=["sS", ("sv", c, 0)], w=[("si", c, 0)])
                        yield
                    for c in range(16):
                        h, p_ = c // 2, c % 2
                        P.op("dve", lambda: V.match_replace(out=sS2[:, c, :], in_to_replace=svt[:, h, p_, 0:8], in_values=sS[:, c, :],
                                                            imm_value=-1e30), r=["sS", ("sv", c, 0)], w=[("sS2", c)])
                        yield
                    for c in range(16):
                        h, p_ = c // 2, c % 2
                        P.op("dve", lambda: V.max(out=svt[:, h, p_, 8:16], in_=sS2[:, c, :]), r=[("sS2", c)], w=[("sv", c, 1)])
                        yield
                    for c in range(16):
                        h, p_ = c // 2, c % 2
                        P.op("dve", lambda: V.max_index(out=sit[:, h, p_, 8:16], in_max=svt[:, h, p_, 8:16], in_values=sS2[:, c, :]),
                             r=[("sS2", c), ("sv", c, 1)], w=[("si", c, 1)])
                        yield
                    svr = [("sv", c, q) for c in range(16) for q in range(2)]
                    sir = [("si", c, q) for c in range(16) for q in range(2)]
                    s2r = [("sS2", c) for c in range(16)]
                    P.op("dve", lambda: V.tensor_copy(out=sif[:], in_=sit[:]), r=sir, w=["sif"])
                    yield
                    P.op("dve", lambda: V.tensor_tensor(
                        out=cand.rearrange("p h (a b) -> p h a b", a=16),
                        in0=svt[:, :, 0, :].unsqueeze(3).broadcast_to([128, 8, 16, 16]),
                        in1=svt[:, :, 1, :].unsqueeze(2).broadcast_to([128, 8, 16, 16]), op=ALU.add), r=svr, w=["sS"])
                    yield
                    for h in range(8):
                        P.op("dve", lambda: V.max(out=ts[:, h, 0:8], in_=cand[:, h, :]), r=["sS"], w=[("ts", h, 0)])
                        yield
                    for h in range(8):
                        P.op("dve", lambda: V.max_index(out=tp[:, h, 0:8], in_max=ts[:, h, 0:8], in_values=cand[:, h, :]),
                             r=["sS", ("ts", h, 0)], w=[("tp", h, 0)])
                        yield
                    for h in range(8):
                        P.op("dve", lambda: V.match_replace(out=cand2[:, h, :], in_to_replace=ts[:, h, 0:8], in_values=cand[:, h, :],
                                                            imm_value=-1e30), r=["sS", ("ts", h, 0)] + s2r, w=[("cand2", h)])
                        yield
                    for h in range(8):
                        P.op("dve", lambda: V.max(out=ts[:, h, 8:16], in_=cand2[:, h, :]), r=[("cand2", h)], w=[("ts", h, 1)])
                        yield
                    for h in range(8):
                        P.op("dve", lambda: V.max_index(out=tp[:, h, 8:16], in_max=ts[:, h, 8:16], in_values=cand2[:, h, :]),
                             r=[("cand2", h), ("ts", h, 1)], w=[("tp", h, 1)])
                        yield
                    tsr = [("ts", h, q) for h in range(8) for q in range(2)]
                    tpr = [("tp", h, q) for h in range(8) for q in range(2)]
                    c2r = [("cand2", h) for h in range(8)]
                    P.op("dve", lambda: V.tensor_scalar(out=ta[:], in0=tp[:], scalar1=4, scalar2=None, op0=ALU.logical_shift_right),
                         r=tpr, w=["ta"])
                    yield
                    P.op("dve", lambda: V.tensor_scalar(out=tb_[:], in0=tp[:], scalar1=15, scalar2=None, op0=ALU.bitwise_and),
                         r=tpr, w=["tb"])
                    yield
                    P.op("dve", lambda: V.tensor_copy(out=taf[:], in_=ta[:]), r=["ta"], w=["taf"])
                    yield
                    P.op("dve", lambda: V.tensor_copy(out=tbf[:], in_=tb_[:]), r=["tb"], w=["tbf"])
                    yield
                    io4 = iota16[:].unsqueeze(1).unsqueeze(1).broadcast_to([128, 8, 16, 16])
                    for (src, pp, dst, dn) in ((taf, 0, If, "If"), (tbf, 1, Jf, "Jf")):
                        P.op("dve", lambda: V.tensor_tensor(out=eq, in0=src[:].unsqueeze(3).broadcast_to([128, 8, 16, 16]),
                                                            in1=io4, op=ALU.is_equal), r=["taf", "tbf", "iota16"] + c2r + s2r, w=["eqb"])
                        yield
                        P.op("dve", lambda: V.tensor_tensor(out=eq, in0=eq,
                                                            in1=sif[:, :, pp, :].unsqueeze(2).broadcast_to([128, 8, 16, 16]),
                                                            op=ALU.mult), r=["eqb", "sif"], w=["eqb"])
                        yield
                        P.op("dve", lambda: V.tensor_reduce(out=dst[:], in_=eq, axis=AX.X, op=ALU.add), r=["eqb"], w=[dn])
                        yield
                    P.op("dve", lambda: V.scalar_tensor_tensor(out=ef_[:], in0=If[:].rearrange("p a b -> p (a b)"), scalar=128.0,
                                                               in1=Jf[:].rearrange("p a b -> p (a b)"), op0=ALU.mult, op1=ALU.add),
                         r=["If", "Jf"], w=["ef"])
                    yield
                    P.op("dve", lambda: V.tensor_copy(out=eu[:], in_=ef_[:]), r=["ef"], w=[eun])
                    yield
                    P.op("dve", lambda: V.tensor_tensor(out=tsc[:], in0=ts[:], in1=ts[:, :, 0:1].broadcast_to([128, 8, 16]), op=ALU.subtract),
                         r=tsr, w=["tsc"])
                    yield
                    P.op("act", lambda: A.activation(out=ex[:], in_=tsc[:], func=AF.Exp), r=["tsc"], w=["ex"])
                    yield
                    P.op("dve", lambda: V.tensor_reduce(out=zz[:], in_=ex[:], axis=AX.X, op=ALU.add), r=["ex"], w=["zz"])
                    yield
                    P.op("dve", lambda: V.reciprocal(out=rz[:], in_=zz[:]), r=["zz"], w=["rz"])
                    yield
                    P.op("dve", lambda: V.tensor_tensor(out=gate[:].rearrange("p (a b) -> p a b", a=8), in0=ex[:],
                                                        in1=rz[:].unsqueeze(2).broadcast_to([128, 8, 16]), op=ALU.mult),
                         r=["ex", "rz"], w=[gaten])
                    yield

                def stage_CD(tile, genB, hook=None):
                    jj = tile % 2
                    eu = eu_[jj]
                    eun = ("eu", jj)
                    gate = gate_[jj]
                    h2b = h2b_[jj]
                    ngrp = peer_slots // GS
                    ab = 4 + 2 * (pidx[tile] % 2)
                    bis_of = {}
                    di_of = {}

                    def fin_act(g):
                        gs = slice(g * GS, (g + 1) * GS)
                        P.op("act", lambda: A.activation(out=gact[:, gs], in_=actv[:, gs], func=AF.Gelu_apprx_tanh),
                             r=[("actv", g * GS + q) for q in range(GS)], w=[("gact", g)])

                    def fin_rest(g):
                        gs = slice(g * GS, (g + 1) * GS)
                        di = cnt_v[0] % 3
                        cnt_v[0] += 1
                        P.op("dve", lambda: V.tensor_tensor(out=wgt[:, gs], in0=gate[:, gs], in1=gact[:, gs], op=ALU.mult),
                             r=[("gate", jj), ("gact", g)], w=[("wgt", g)])
                        P.op("dve", lambda: V.tensor_tensor(
                            out=diag[di][:],
                            in0=identb[:].unsqueeze(1).broadcast_to([128, GS, 128]),
                            in1=wgt[:, gs].unsqueeze(2).broadcast_to([128, GS, 128]), op=ALU.mult),
                            r=["identb", ("wgt", g)], w=[("diag", di)])
                        for q in range(GS):
                            sl = g * GS + q
                            bi = bis_of[g][q]

                            def mm():
                                for half in range(2):
                                    ins = PE.matmul(PS[:, ab + half, :], lhsT=diag[di][:, q, :],
                                                    rhs=guv[bi][:, D + half * 512:D + (half + 1) * 512],
                                                    start=(sl == 0), stop=(sl == peer_slots - 1))
                                return ins
                            P.op("pe", mm, r=[("guv", bi), ("diag", di)], w=[("ps", ab), ("ps", ab + 1)])

                    for g in range(ngrp):
                        if g >= 1:
                            fin_act(g - 1)
                        bis = []
                        for q in range(GS):
                            sl = g * GS + q
                            bi = cnt_u[0] % NGB
                            cnt_u[0] += 1
                            bis.append(bi)
                            pi = cnt_p[0] % 3
                            cnt_p[0] += 1
                            P.dma("pool", lambda: G.indirect_dma_start(
                                out=guv[bi][:], out_offset=None, in_=uvb[layer][:, :],
                                in_offset=bass.IndirectOffsetOnAxis(ap=eu[:, sl:sl + 1], axis=0)), r=[eun] + tabnames(layer), w=[("guv", bi)])
                            P.op("dve", lambda: V.tensor_tensor(out=prod[pi][:], in0=guv[bi][:, 0:D], in1=h2b[:], op=ALU.mult),
                                 r=[("guv", bi), ("h2", jj)], w=[("prod", pi)])
                            P.op("act", lambda: A.activation(out=junkb[:], in_=prod[pi][:], func=AF.Copy, accum_out=actv[:, sl:sl + 1]),
                                 r=[("prod", pi)], w=["junkb", ("actv", sl)])
                            if genB is not None:
                                for _ in range(2):
                                    next(genB, None)
                        bis_of[g] = bis
                        if g >= 1:
                            fin_rest(g - 1)
                        if g == 2 and hook is not None:
                            hook()
                    fin_act(ngrp - 1)
                    fin_rest(ngrp - 1)
                    if genB is not None:
                        for _ in genB:
                            pass

                def stage_E(tile):
                    jj = pidx[tile] % 3
                    ab = 4 + 2 * (pidx[tile] % 2)
                    s = 0 if tile < NTL else 1
                    for half in range(2):
                        hc = slice(half * 512, (half + 1) * 512)
                        P.op("dve", lambda: V.tensor_tensor(out=tmpo[half][:], in0=PS[:, ab + half, :], in1=modp[s][2][:, hc], op=ALU.mult),
                             r=[("ps", ab + half)] + modnames(1, s), w=[("tmpo", half)])
                        P.op("dve", lambda: V.tensor_tensor(out=xo[:, hc], in0=tmpo[half][:], in1=xt_[jj][:, hc], op=ALU.add),
                             r=[("tmpo", half), ("xt", jj)], w=[("xo", half)])
                    xor_ = [("xo", 0), ("xo", 1)]
                    if not last:
                        P.dma("sp", lambda: SP.dma_start(out=xs[tile * 128:(tile + 1) * 128, :], in_=xo[:]), r=xor_, w=[("xs", tile)],
                              is_out=dbg)
                    else:
                        P.op("dve", lambda: V.scalar_tensor_tensor(out=junk[:], in0=xo[:], scalar=1.0, in1=xo[:],
                                                                   op0=ALU.mult, op1=ALU.mult, accum_out=ss[:, 0:1]),
                             r=xor_, w=["junk", "ss0"])
                        P.op("dve", lambda: V.tensor_scalar(out=ss[:, 1:2], in0=ss[:, 0:1], scalar1=1.0 / D, scalar2=EPS,
                                                            op0=ALU.mult, op1=ALU.add), r=["ss0"], w=["ss1"])
                        P.op("act", lambda: A.activation(out=ss[:, 2:3], in_=ss[:, 1:2], func=AF.Ln), r=["ss1"], w=["ss2"])
                        P.op("act", lambda: A.activation(out=ss[:, 3:4], in_=ss[:, 2:3], func=AF.Exp, scale=-0.5), r=["ss2"], w=["ss3"])
                        P.op("dve", lambda: V.scalar_tensor_tensor(out=junk[:], in0=xo[:], scalar=ss[:, 3:4], in1=gfin[:],
                                                                   op0=ALU.mult, op1=ALU.mult),
                             r=xor_ + ["ss3", "gfin"], w=["junk"])
                        P.dma("sp", lambda: SP.dma_start(out=y_out[tile * 128:(tile + 1) * 128, :], in_=junk[:]), r=["junk"],
                              w=[("yout", tile)], is_out=True)

                stage_A(ptiles[0])
                for _ in stage_B(ptiles[0]):
                    pass
                for i_, tile in enumerate(ptiles):
                    nxt = ptiles[i_ + 1] if i_ + 1 < len(ptiles) else None
                    genB = None
                    if nxt is not None:
                        stage_A(nxt)
                        genB = stage_B(nxt)
                    prev = ptiles[i_ - 1] if i_ >= 1 else None
                    stage_CD(tile, genB, hook=(lambda: stage_E(prev)) if prev is not None else None)
                stage_E(ptiles[-1])
                P.barrier()
        P.finish()
        print("instructions:", P.n_ins)
    return nc


def prep_inputs(inp, NTL=32, n_cores=8):
    f = np.float32
    T = NTL * 128
    TT = T + CTX
    x = np.asarray(inp["x"], f)
    c = np.asarray(inp["c"], f)
    ctx = np.asarray(inp["ctx"], f)
    c_ctx = np.asarray(inp["c_ctx"], f)
    w_in = np.asarray(inp["w_in"], f)
    rp = np.concatenate([np.arange(16, 32), np.arange(0, 16), np.arange(48, 64), np.arange(32, 48)])
    d64 = np.arange(64)
    qcols = np.concatenate([np.concatenate([j * 64 + d64, (4 + j) * 64 + d64]) for j in range(4)])
    qrcols = np.concatenate([np.concatenate([j * 64 + rp, (4 + j) * 64 + rp]) for j in range(4)])
    kcols = 512 + np.arange(128)
    krcols = 512 + np.concatenate([rp, 64 + rp])
    vcols = 640 + np.arange(128)
    pcols = 768 + np.arange(256)
    ucols = 1024 + np.arange(256)
    vscols = 1280 + np.arange(256)
    w1 = np.ascontiguousarray(w_in[:, :, np.concatenate([kcols, krcols, vcols, pcols])])
    w2a = np.ascontiguousarray(w_in[:, :, np.concatenate([qcols, qrcols, ucols, vscols])])
    w2g = np.ascontiguousarray(w_in[:, :, 1536:4608])
    rows = T // 64
    row = np.repeat(np.arange(rows, dtype=f), 64)
    col = np.tile(np.arange(64, dtype=f), rows)
    inv = (np.float32(10000.0) ** (-np.arange(16, dtype=f) / np.float32(16))).astype(f)
    ar, ac = (row[:, None] * inv).astype(f), (col[:, None] * inv).astype(f)
    cos_d = np.concatenate([np.cos(ar), np.cos(ar), np.cos(ac), np.cos(ac)], axis=1).astype(f)
    sin_d = np.concatenate([-np.sin(ar), np.sin(ar), -np.sin(ac), np.sin(ac)], axis=1).astype(f)
    cos_d = np.concatenate([cos_d, np.ones((CTX, 64), f)], axis=0)
    sin_d = np.concatenate([sin_d, np.zeros((CTX, 64), f)], axis=0)
    ropeC = np.ascontiguousarray(np.concatenate([cos_d.T, cos_d.T], axis=0))
    ropeS = np.ascontiguousarray(np.concatenate([sin_d.T, sin_d.T], axis=0))
    sink = np.asarray(inp["attn_sink"], f)
    sink_r = np.ascontiguousarray(np.repeat(sink.reshape(2, 1, 2, 4, 1), 128, axis=4).reshape(2, 1, 2, 512))
    pool_w = np.asarray(inp["pool_w"], f)
    pwbd = np.zeros((2, 128, 2, 128), f)
    for g in range(4):
        o = (g % 2) * 64
        pwbd[:, o:o + 64, g // 2, o:o + 64] = pool_w[:, g]
    pscale = np.ascontiguousarray(np.asarray(inp["pool_scale"], f).reshape(2, 2, 128).transpose(0, 2, 1))
    rc = np.zeros((128, 2, TT), f)
    for g, size in enumerate(POOL_SIZES):
        for (a, l) in ((0, T), (T, CTX)):
            t = np.arange(l)
            lo = np.clip(t - size // 2, 0, l)
            hi = np.clip(t + size // 2, 0, l)
            o = (g % 2) * 64
            rc[o:o + 64, g // 2, a:a + l] = (1.0 / (hi - lo).astype(f))[None, :]
    sgu_wT = np.ascontiguousarray(np.asarray(inp["sgu_w"], f).transpose(0, 3, 1, 2))
    sgu_b_r = np.ascontiguousarray(np.broadcast_to(np.asarray(inp["sgu_b"], f)[:, None], (2, 64, 4, 128)))
    wbrA = np.ascontiguousarray(np.asarray(inp["w_br_attn"], f).reshape(2, 8, 64, D).transpose(0, 2, 1, 3))
    wbrP = np.ascontiguousarray(np.asarray(inp["w_br_pool"], f).reshape(2, 2, 128, D).transpose(0, 2, 1, 3))
    wbrS = np.ascontiguousarray(np.asarray(inp["w_br_sgu"], f).reshape(2, 4, 64, D).transpose(0, 2, 1, 3))
    wout = np.ascontiguousarray(np.asarray(inp["w_out"], f).reshape(2, 8, 128, D).transpose(0, 2, 1, 3))
    wqT = np.ascontiguousarray(np.asarray(inp["peer_wq"], f).reshape(2, D, 16, 128).transpose(0, 3, 2, 1))
    subkT = np.ascontiguousarray(np.asarray(inp["peer_subkeys"], f).reshape(2, 16, 128, 128).transpose(0, 3, 1, 2))
    jj, ii = np.meshgrid(np.arange(128), np.arange(128), indexing="ij")
    mA = np.where(jj >= ii, 0.0, NEG).astype(f)
    mB = np.where(jj <= ii, 0.0, NEG).astype(f)
    maskAB = np.ascontiguousarray(np.stack([np.tile(mA, (1, 4)), np.tile(mB, (1, 4))], axis=1))
    shared = {
        "w_mod": np.asarray(inp["w_mod"], f),
        "b_mod_r": np.ascontiguousarray(np.broadcast_to(np.asarray(inp["b_mod"], f)[:, None], (2, 128, 6 * D))),
        "g_mix_r": np.ascontiguousarray(np.broadcast_to(np.asarray(inp["g_mix"], f)[:, None], (2, 128, D))),
        "g_ffn_r": np.ascontiguousarray(np.broadcast_to(np.asarray(inp["g_ffn"], f)[:, None], (2, 128, D))),
        "g_fin_r": np.ascontiguousarray(np.broadcast_to(np.asarray(inp["g_final"], f)[None], (128, D))),
        "w1": w1, "w2a": w2a, "w2g": w2g, "ropeC": ropeC, "ropeS": ropeS, "sink_r": sink_r, "pwbd": pwbd,
        "pscale": pscale, "rc": rc, "sgu_wT": sgu_wT, "sgu_b_r": sgu_b_r, "wbrA": wbrA, "wbrP": wbrP, "wbrS": wbrS,
        "wout": wout, "wqT": wqT, "subkT": subkT,
        "peer_u0": np.ascontiguousarray(np.asarray(inp["peer_u"], f)[0]), "peer_u1": np.ascontiguousarray(np.asarray(inp["peer_u"], f)[1]),
        "peer_v0": np.ascontiguousarray(np.asarray(inp["peer_v"], f)[0]), "peer_v1": np.ascontiguousarray(np.asarray(inp["peer_v"], f)[1]),
        "maskAB": maskAB, "ident": np.eye(128, dtype=f),
        "iota16": np.ascontiguousarray(np.broadcast_to(np.arange(16, dtype=f)[None], (128, 16))),
    }
    maps = []
    for b in range(n_cores):
        m = dict(shared)
        m["x"] = np.ascontiguousarray(x[b, :T])
        m["ctx"] = np.ascontiguousarray(ctx[b])
        cv = np.stack([c[b].reshape(8, 128).T, c_ctx.reshape(8, 128).T], axis=1)
        m["cvec"] = np.ascontiguousarray(cv.astype(f))
        maps.append(m)
    return maps


def kernel(**inputs):
    n = 8
    nc = build(NTL=32, n_layers=2)
    maps = prep_inputs(inputs, NTL=32, n_cores=n)
    res = run_bass_kernel_spmd(nc, maps, core_ids=list(range(n)))
    return np.stack([np.asarray(r["y"], np.float32) for r in res.results], axis=0)
```

```python
import numpy as np
import concourse.bass as bass
import concourse.mybir as mybir
from concourse.bass_utils import run_bass_kernel_spmd
from contextlib import ExitStack

F32 = mybir.dt.float32
BF16 = mybir.dt.bfloat16
U32 = mybir.dt.uint32
AF = mybir.ActivationFunctionType
ALU = mybir.AluOpType
AX = mybir.AxisListType

D = 1024
CTX = 256
EPS = 1e-6
NEG = -30000.0
POOL_SIZES = (2, 4, 8, 16)
ND_SEM = 40
NGB = 16


class Prog:
    def __init__(self, nc, es):
        self.nc = nc
        self.eng = {"pe": nc.tensor, "dve": nc.vector, "act": nc.scalar, "pool": nc.gpsimd, "sp": nc.sync}
        self.sem = {}
        for e in self.eng:
            self.sem[e] = es.enter_context(nc.semaphore("s_" + e))
        for i in range(ND_SEM):
            self.sem[("d", i)] = es.enter_context(nc.semaphore("s_d%d" % i))
        self.cnt = {e: 0 for e in self.eng}
        self.seen = {e: {} for e in self.eng}
        self.lw = {}
        self.rd = {}
        self.dval = [0] * ND_SEM
        self.rr_rng = {"sp": (0, 24), "pool": (24, ND_SEM)}
        self.rr = {"sp": 0, "pool": 24}
        self.out_tokens = []
        self.n_ins = 0

    def _deps(self, e, r, w, extra=()):
        deps = {}

        def add(tok):
            sk, v = tok
            if sk == "pe" and e == "pe":
                return
            if self.seen[e].get(sk, 0) < v:
                if deps.get(sk, 0) < v:
                    deps[sk] = v

        for b in r:
            if b in self.lw:
                add(self.lw[b])
        for b in w:
            if b in self.lw:
                add(self.lw[b])
            for sk, v in self.rd.get(b, {}).items():
                add((sk, v))
        for t in extra:
            add(t)
        eng = self.eng[e]
        for sk, v in deps.items():
            eng.wait_ge(self.sem[sk], v)
            self.seen[e][sk] = v
            self.n_ins += 1

    def _commit(self, tok, r, w):
        for b in w:
            self.lw[b] = tok
            self.rd[b] = {}
        for b in r:
            d = self.rd.setdefault(b, {})
            if d.get(tok[0], 0) < tok[1]:
                d[tok[0]] = tok[1]

    def op(self, e, fn, r=(), w=()):
        self._deps(e, r, w)
        ins = fn()
        self.cnt[e] += 1
        ins.then_inc(self.sem[e], 1)
        self.n_ins += 1
        tok = (e, self.cnt[e])
        self._commit(tok, r, w)
        return tok

    def dma(self, e, fn, r=(), w=(), is_out=False):
        lo_, hi_ = self.rr_rng[e]
        idx = self.rr[e]
        self.rr[e] = lo_ + (idx + 1 - lo_) % (hi_ - lo_)
        sk = ("d", idx)
        extra = [(sk, self.dval[idx])] if self.dval[idx] > 0 else []
        self._deps(e, r, w, extra)
        ins = fn()
        self.dval[idx] += 16
        ins.then_inc(self.sem[sk], 16)
        self.n_ins += 1
        tok = (sk, self.dval[idx])
        self._commit(tok, r, w)
        if is_out:
            self.out_tokens.append(tok)
        return tok

    def barrier(self):
        cur = {e: self.cnt[e] for e in self.eng}
        for i in range(ND_SEM):
            cur[("d", i)] = self.dval[i]
        for e in self.eng:
            for sk, v in cur.items():
                if v > self.seen[e].get(sk, 0):
                    self.eng[e].wait_ge(self.sem[sk], v)
                    self.seen[e][sk] = v
                    self.n_ins += 1

    def finish(self):
        e = "sp"
        for sk, v in self.out_tokens:
            if self.seen[e].get(sk, 0) < v:
                self.eng[e].wait_ge(self.sem[sk], v)
                self.seen[e][sk] = v


def build(NTL=32, n_layers=2, dbg=False, peer_slots=128):
    T = NTL * 128
    TT = T + CTX
    NTILES = NTL + 2
    NGL = NTL // 4
    L = TT + 64
    groups = [(g, list(range(4 * g, 4 * g + 4))) for g in range(NGL)] + [(NGL, [NTL, NTL + 1])]

    def ppos(tok):
        return tok + 16 if tok < T else tok + 48

    nc = bass.Bass("TRN2", target_bir_lowering=False)

    def din(name, shape, dt=F32):
        return nc.dram_tensor(name, list(shape), dt, kind="ExternalInput").ap()

    def dscr(name, shape, dt=F32):
        kind = "ExternalOutput" if (dbg and name == "xs") else "Internal"
        return nc.dram_tensor(name, list(shape), dt, kind=kind).ap()

    x_in = din("x", [T, D])
    ctx_in = din("ctx", [CTX, D])
    cvec = din("cvec", [128, 2, 8])
    w_mod = din("w_mod", [2, D, 6 * D])
    b_mod_r = din("b_mod_r", [2, 128, 6 * D])
    g_mix_r = din("g_mix_r", [2, 128, D])
    g_ffn_r = din("g_ffn_r", [2, 128, D])
    g_fin_r = din("g_fin_r", [128, D])
    w1_d = din("w1", [2, D, 640])
    w2a_d = din("w2a", [2, D, 1536])
    w2g_d = din("w2g", [2, D, 3072])
    ropeC = din("ropeC", [128, TT])
    ropeS = din("ropeS", [128, TT])
    sink_r = din("sink_r", [2, 1, 2, 512])
    pwbd_d = din("pwbd", [2, 128, 2, 128])
    pscale_d = din("pscale", [2, 128, 2])
    rc_d = din("rc", [128, 2, TT])
    sguwT_d = din("sgu_wT", [2, 128, 4, 128])
    sgub_d = din("sgu_b_r", [2, 64, 4, 128])
    wbrA_d = din("wbrA", [2, 64, 8, D])
    wbrP_d = din("wbrP", [2, 128, 2, D])
    wbrS_d = din("wbrS", [2, 64, 4, D])
    wout_d = din("wout", [2, 128, 8, D])
    wqT_d = din("wqT", [2, 128, 16, D])
    subkT_d = din("subkT", [2, 128, 16, 128])
    peer_u = [din("peer_u%d" % l, [16384, D]) for l in range(2)]
    peer_v = [din("peer_v%d" % l, [16384, D]) for l in range(2)]
    mask_d = din("maskAB", [128, 2, 512])
    ident_d = din("ident", [128, 128])
    iota_d = din("iota16", [128, 16])
    y_out = nc.dram_tensor("y", [T, D], F32, kind="ExternalOutput").ap()

    xs = dscr("xs", [TT, D])
    hTd = dscr("hTd", [NGL + 1, 128, 8, 512], BF16)
    yTd = dscr("yTd", [NGL + 1, 64, 8, 512], BF16)
    ysTd = dscr("ysTd", [NGL + 1, 64, 4, 512], BF16)
    ypTd = dscr("ypTd", [NGL + 1, 128, 2, 512], BF16)
    uvb = [dscr("uvb%d" % l, [16384, 2 * D], BF16) for l in range(2)]

    with ExitStack() as es:
        P = Prog(nc, es)
        V, A, G, PE, SP = nc.vector, nc.scalar, nc.gpsimd, nc.tensor, nc.sync

        uid = [0]

        def sb(es_, name, shape, dt=F32):
            uid[0] += 1
            return es_.enter_context(nc.sbuf_tensor("sb%d_%s" % (uid[0], name), list(shape), dt))

        PS = es.enter_context(nc.psum_tensor("ps", [128, 8, 512], F32))
        bank_ptr = [0]

        def bank(n=1):
            if n > 1 and bank_ptr[0] % n:
                bank_ptr[0] += n - bank_ptr[0] % n
            b = bank_ptr[0] % 8
            bank_ptr[0] = (bank_ptr[0] + n)
            return b

        identf = sb(es, "identf", [128, 128])
        identb = sb(es, "identb", [128, 128], BF16)
        maskb = sb(es, "maskb", [128, 2, 512], BF16)
        ones_b = sb(es, "ones_b", [128, 64], BF16)
        iota16 = sb(es, "iota16", [128, 16])
        screp = sb(es, "screp", [128, 2, 8, 128])
        with ExitStack() as e0:
            maskf = sb(e0, "maskf", [128, 2, 512])
            cv = sb(e0, "cv", [128, 2, 8])
            cs = sb(e0, "cs", [128, 2, 8])
            P.dma("sp", lambda: SP.dma_start(out=identf[:], in_=ident_d[:, :]), w=["identf"])
            P.dma("sp", lambda: SP.dma_start(out=maskf[:], in_=mask_d[:, :, :]), w=["maskf"])
            P.dma("sp", lambda: SP.dma_start(out=iota16[:], in_=iota_d[:, :]), w=["iota16"])
            P.dma("sp", lambda: SP.dma_start(out=cv[:], in_=cvec[:, :, :]), w=["cv"])
            P.dma("sp", lambda: SP.dma_start(out=xs[0:T, :], in_=x_in[:, :]), w=[("xs", t) for t in range(NTL)])
            P.dma("sp", lambda: SP.dma_start(out=xs[T:TT, :], in_=ctx_in[:, :]), w=[("xs", NTL), ("xs", NTL + 1)])
            P.op("dve", lambda: V.tensor_copy(out=identb[:], in_=identf[:]), r=["identf"], w=["identb"])
            P.op("dve", lambda: V.tensor_copy(out=maskb[:], in_=maskf[:]), r=["maskf"], w=["maskb"])
            P.op("dve", lambda: V.memset(ones_b[:], 1.0), w=["ones_b"])
            P.op("act", lambda: A.activation(out=cs[:], in_=cv[:], func=AF.Silu), r=["cv"], w=["cs"])
            P.op("dve", lambda: V.tensor_copy(out=screp[:], in_=cs[:].unsqueeze(3).broadcast_to([128, 2, 8, 128])),
                 r=["cs"], w=["screp"])
            P.barrier()

        def modulation(layer, which, mod, g_r):
            base = which * 3 * D
            with ExitStack() as em:
                wm = [sb(em, "wm%d" % i, [128, 8, 512]) for i in range(2)]
                bm = [sb(em, "bm%d" % i, [128, 512]) for i in range(2)]
                gs = [sb(em, "gs%d" % i, [128, 512]) for i in range(2)]
                tm = [sb(em, "tm%d" % i, [128, 512]) for i in range(2)]
                for ci in range(6):
                    kind, half = ci // 2, ci % 2
                    c0 = base + ci * 512
                    j = ci % 2
                    P.dma("sp", lambda: SP.dma_start(
                        out=wm[j][:], in_=w_mod[layer, :, c0:c0 + 512].rearrange("(k p) n -> p k n", p=128)),
                        w=[("wm", j)])
                    P.dma("sp", lambda: SP.dma_start(out=bm[j][:], in_=b_mod_r[layer, :, c0:c0 + 512]), w=[("bm", j)])
                    if kind == 1:
                        P.dma("sp", lambda: SP.dma_start(out=gs[j][:], in_=g_r[layer, :, half * 512:(half + 1) * 512]),
                              w=[("gs", j)])
                    for s in range(2):
                        b = bank()

                        def mm():
                            for k in range(8):
                                ins = PE.matmul(PS[:, b, :], lhsT=screp[:, s, k, :], rhs=wm[j][:, k, :],
                                                start=(k == 0), stop=(k == 7))
                            return ins
                        P.op("pe", mm, r=["screp", ("wm", j)], w=[("ps", b)])
                        dst_kind = {0: 1, 1: 0, 2: 2}[kind]
                        dst = mod[s][dst_kind][:, half * 512:(half + 1) * 512]
                        dname = ("mod", which, s, dst_kind, half)
                        if kind == 1:
                            P.op("dve", lambda: V.tensor_tensor(out=tm[s][:], in0=PS[:, b, :], in1=bm[j][:], op=ALU.add),
                                 r=[("ps", b), ("bm", j)], w=[("tm", s)])
                            P.op("dve", lambda: V.scalar_tensor_tensor(out=dst, in0=tm[s][:], scalar=1.0, in1=gs[j][:],
                                                                       op0=ALU.add, op1=ALU.mult),
                                 r=[("tm", s), ("gs", j)], w=[dname])
                        else:
                            P.op("dve", lambda: V.tensor_tensor(out=dst, in0=PS[:, b, :], in1=bm[j][:], op=ALU.add),
                                 r=[("ps", b), ("bm", j)], w=[dname])
                P.barrier()

        def modnames(which, s):
            return [("mod", which, s, k, h) for k in range(3) for h in range(2)]

        def load_cast(es_, name, dram_ap, shape, stage_bufs, dst=None, eng_cycle=("act", "pool")):
            raise NotImplementedError

        def norm_mod(xt, xname, Amod, Bmod, modr, junk, ss, hf_out, hb_out, hname, add_eng="pool"):
            P.op("dve", lambda: V.scalar_tensor_tensor(out=junk[:], in0=xt[:], scalar=1.0, in1=xt[:], op0=ALU.mult, op1=ALU.mult, accum_out=ss[:, 0:1]),
                 r=[xname], w=["junk", "ss0"])
            P.op("dve", lambda: V.tensor_scalar(out=ss[:, 1:2], in0=ss[:, 0:1], scalar1=1.0 / D, scalar2=EPS,
                                                op0=ALU.mult, op1=ALU.add), r=["ss0"], w=["ss1"])
            P.op("act", lambda: A.activation(out=ss[:, 2:3], in_=ss[:, 1:2], func=AF.Ln), r=["ss1"], w=["ss2"])
            P.op("act", lambda: A.activation(out=ss[:, 3:4], in_=ss[:, 2:3], func=AF.Exp, scale=-0.5), r=["ss2"], w=["ss3"])
            P.op("dve", lambda: V.scalar_tensor_tensor(out=junk[:], in0=xt[:], scalar=ss[:, 3:4], in1=Amod[:],
                                                       op0=ALU.mult, op1=ALU.mult),
                 r=[xname, "ss3"] + modr, w=["junk"])
            if hf_out is not None:
                P.op("dve", lambda: V.tensor_tensor(out=hf_out[:], in0=junk[:], in1=Bmod[:], op=ALU.add),
                     r=["junk"] + modr, w=[hname + "f"])
                P.op("act", lambda: A.copy(out=hb_out[:], in_=hf_out[:]), r=[hname + "f"], w=[hname])
            elif add_eng == "dve":
                P.op("dve", lambda: V.tensor_tensor(out=hb_out[:], in0=junk[:], in1=Bmod[:], op=ALU.add),
                     r=["junk"] + modr, w=[hname])
            else:
                P.op("pool", lambda: G.tensor_tensor(out=hb_out[:], in0=junk[:], in1=Bmod[:], op=ALU.add),
                     r=["junk"] + modr, w=[hname])

        def transpose8(hb, hname, dst_ap, dname, evac="act", b=None):
            if b is None:
                b = bank()
            psb = PS[:, b, :].bitcast(BF16)

            def tr():
                for k in range(8):
                    ins = PE.transpose(out=psb[:, k * 128:(k + 1) * 128], in_=hb[:, k * 128:(k + 1) * 128],
                                       identity=identb[:])
                return ins
            P.op("pe", tr, r=[hname, "identb"], w=[("ps", b)])
            src = psb.rearrange("p (k t) -> p k t", k=8)
            if evac == "act":
                P.op("act", lambda: A.copy(out=dst_ap, in_=src), r=[("ps", b)], w=[dname])
            else:
                P.op("dve", lambda: V.tensor_copy(out=dst_ap, in_=src), r=[("ps", b)], w=[dname])

        def cast_load(dst_tile_ap, dname, src_ap, stg, sname, ceng):
            P.dma("sp", lambda: SP.dma_start(out=stg, in_=src_ap), w=[sname])
            if ceng == "act":
                P.op("act", lambda: A.copy(out=dst_tile_ap, in_=stg), r=[sname], w=[dname])
            elif ceng == "pool":
                P.op("pool", lambda: G.tensor_copy(out=dst_tile_ap, in_=stg), r=[sname], w=[dname])
            else:
                P.op("dve", lambda: V.tensor_copy(out=dst_tile_ap, in_=stg), r=[sname], w=[dname])

        NQ = 16

        def tabnames(l):
            return [("uvb", l, c0, q) for c0 in (0, D) for q in range(NQ)]

        def conv_gen(l):
            rpq = 16384 // NQ
            for q in range(NQ):
                for (src_t, c0) in ((peer_u[l], 0), (peer_v[l], D)):
                    rows = slice(q * rpq, (q + 1) * rpq)
                    P.dma("pool", lambda: G.dma_start(out=uvb[l][rows, c0:c0 + D], in_=src_t[rows, :]), w=[("uvb", l, c0, q)])
                    yield
        convs = [conv_gen(l) for l in range(n_layers)]

        def pump(l, k):
            if l < n_layers:
                for _ in range(k):
                    next(convs[l], None)

        for layer in range(n_layers):
            last = layer == n_layers - 1
            with ExitStack() as eL:
                modm = [[sb(eL, "modm%d%d" % (s, k), [128, D]) for k in range(3)] for s in range(2)]
                eKV = ExitStack()
                kT = sb(eKV, "kT", [128, TT], BF16)
                vtok = sb(eKV, "vtok", [128, NTILES, 128], BF16)
                dT = sb(eKV, "dT", [128, 2, TT], BF16)
                sinkrow = sb(eKV, "sinkrow", [1, 2, 512], BF16)
                with ExitStack() as e1:
                    sk_f = sb(e1, "sk_f", [1, 2, 512])
                    sk_e = sb(e1, "sk_e", [1, 2, 512])
                    P.dma("sp", lambda: SP.dma_start(out=sk_f[:], in_=sink_r[layer, :, :, :]), w=["sk_f"])
                    P.op("act", lambda: A.activation(out=sk_e[:], in_=sk_f[:], func=AF.Exp), r=["sk_f"], w=["sk_e"])
                    P.op("act", lambda: A.copy(out=sinkrow[:], in_=sk_e[:]), r=["sk_e"], w=["sinkrow"])
                    P.barrier()
                modulation(layer, 0, modm, g_mix_r)

                with ExitStack() as e1:
                    zpad = sb(e1, "zpad", [128, 2, L])
                    e1w = ExitStack()
                    w1 = sb(e1w, "w1", [128, 8, 640], BF16)
                    stg = [sb(e1w, "stg%d" % i, [128, 2, 640]) for i in range(2)]
                    xt_ = [sb(e1w, "xt%d" % i, [128, D]) for i in range(2)]
                    junk = sb(e1w, "junk", [128, D])
                    ss = sb(e1w, "ss", [128, 4])
                    hb_ = [sb(e1w, "hb%d" % i, [128, D], BF16) for i in range(2)]
                    hTg = [sb(e1w, "hTg%d" % i, [128, 8, 512], BF16) for i in range(2)]
                    cS = [sb(e1w, "cS%d" % i, [128, 2, 512]) for i in range(2)]
                    t1 = sb(e1w, "t1", [128, 512])
                    t2 = sb(e1w, "t2", [128, 512])
                    for i in range(4):
                        cast_load(w1[:, 2 * i:2 * i + 2, :], ("w1", i),
                                  w1_d[layer, 256 * i:256 * (i + 1), :].rearrange("(k p) n -> p k n", p=128),
                                  stg[i % 2][:], ("stg", i % 2), "act")
                    P.op("dve", lambda: V.memset(zpad[:], 0.0), w=["zpad"])
                    for gi, tiles in groups:
                        j = gi % 2
                        ng = len(tiles) * 128
                        s = 0 if tiles[0] < NTL else 1
                        tok0 = tiles[0] * 128
                        for ti, tile in enumerate(tiles):
                            jj = tile % 2
                            P.dma("sp", lambda: SP.dma_start(out=xt_[jj][:], in_=xs[tile * 128:(tile + 1) * 128, :]),
                                  r=[("xs", tile)], w=[("xt", jj)])
                            norm_mod(xt_[jj], ("xt", jj), modm[s][0], modm[s][1], modnames(0, s), junk, ss, None,
                                     hb_[jj], ("hb", jj), add_eng="dve")
                            transpose8(hb_[jj], ("hb", jj), hTg[j][:, :, ti * 128:(ti + 1) * 128], ("hTg", j, ti))
                        hr = [("hTg", j, ti) for ti in range(len(tiles))]
                        P.dma("sp", lambda: SP.dma_start(out=hTd[gi, :, :, 0:ng], in_=hTg[j][:, :, 0:ng]), r=hr, w=[("hTd", gi)])
                        P.dma("sp", lambda: SP.dma_start(out=cS[j][:, 0, 0:ng], in_=ropeC[:, tok0:tok0 + ng]), w=[("cS", j, 0)])
                        P.dma("sp", lambda: SP.dma_start(out=cS[j][:, 1, 0:ng], in_=ropeS[:, tok0:tok0 + ng]), w=[("cS", j, 1)])
                        if layer == 0:
                            pump(0, 2)
                        bk, bkr = bank(), bank()
                        for (bb, c0) in ((bk, 0), (bkr, 128)):
                            def mm(bb=bb, c0=c0):
                                for k in range(8):
                                    ins = PE.matmul(PS[:, bb, 0:ng], lhsT=w1[:, k, c0:c0 + 128], rhs=hTg[j][:, k, 0:ng],
                                                    start=(k == 0), stop=(k == 7))
                                return ins
                            P.op("pe", mm, r=hr + [("w1", i_) for i_ in range(4)], w=[("ps", bb)])
                        P.op("dve", lambda: V.tensor_tensor(out=t1[:, 0:ng], in0=PS[:, bkr, 0:ng], in1=cS[j][:, 1, 0:ng], op=ALU.mult),
                             r=[("ps", bkr), ("cS", j, 1)], w=["t1"])
                        P.op("dve", lambda: V.tensor_tensor(out=t2[:, 0:ng], in0=PS[:, bk, 0:ng], in1=cS[j][:, 0, 0:ng], op=ALU.mult),
                             r=[("ps", bk), ("cS", j, 0)], w=["t2"])
                        P.op("dve", lambda: V.tensor_tensor(out=kT[:, tok0:tok0 + ng], in0=t1[:, 0:ng], in1=t2[:, 0:ng], op=ALU.add),
                             r=["t1", "t2"], w=[("kT", gi)])
                        for c in range(2):
                            bz = bank()

                            def mm(bz=bz, c=c):
                                for k in range(8):
                                    ins = PE.matmul(PS[:, bz, 0:ng], lhsT=w1[:, k, 384 + c * 128:512 + c * 128],
                                                    rhs=hTg[j][:, k, 0:ng], start=(k == 0), stop=(k == 7))
                                return ins
                            P.op("pe", mm, r=hr + [("w1", i_) for i_ in range(4)], w=[("ps", bz)])
                            p0 = ppos(tok0)
                            P.op("act", lambda: A.copy(out=zpad[:, c, p0:p0 + ng], in_=PS[:, bz, 0:ng]),
                                 r=[("ps", bz)], w=["zpad"])
                        bv = bank()

                        def mm():
                            for ti in range(len(tiles)):
                                for k in range(8):
                                    ins = PE.matmul(PS[:, bv, ti * 128:(ti + 1) * 128], lhsT=hTg[j][:, k, ti * 128:(ti + 1) * 128],
                                                    rhs=w1[:, k, 256:384], start=(k == 0), stop=(k == 7))
                            return ins
                        P.op("pe", mm, r=hr + [("w1", i_) for i_ in range(4)], w=[("ps", bv)])
                        nt = len(tiles)
                        P.op("act", lambda: A.copy(out=vtok[:, tiles[0]:tiles[0] + nt, :],
                                                   in_=PS[:, bv, 0:ng].rearrange("p (a b) -> p a b", a=nt)),
                             r=[("ps", bv)], w=[("vtok", gi)])

                    P.barrier()
                    e1w.close()
                    PA = sb(e1, "PA", [128, L])
                    PB = sb(e1, "PB", [128, L])
                    P.op("dve", lambda: V.memset(PA[:], 0.0), w=["PA"])
                    P.op("dve", lambda: V.memset(PB[:], 0.0), w=["PB"])
                    rc = sb(e1, "rc", [128, 2, TT])
                    P.dma("sp", lambda: SP.dma_start(out=rc[:], in_=rc_d[:, :, :]), w=["rc"])
                    lo, hi = 8, L - 8

                    def shadd(dst, src, s1, s2, p0=0, p1=128):
                        P.op("dve", lambda: V.tensor_tensor(out=dst[p0:p1, lo:hi], in0=src[p0:p1, lo + s1:hi + s1],
                                                            in1=src[p0:p1, lo + s2:hi + s2], op=ALU.add),
                             r=["PA", "PB", "zpad"], w=["PA", "PB"])
                    segs = [(0, T, 16), (T, TT, 48)]

                    def dfin(src, c, p0, p1):
                        for (a, b_, off) in segs:
                            P.op("dve", lambda: V.tensor_tensor(out=src[p0:p1, a + off:b_ + off], in0=src[p0:p1, a + off:b_ + off],
                                                                in1=rc[p0:p1, c, a:b_], op=ALU.mult),
                                 r=["PA", "PB", "rc"], w=["PA", "PB"])
                            P.op("dve", lambda: V.tensor_tensor(out=dT[p0:p1, c, a:b_], in0=src[p0:p1, a + off:b_ + off],
                                                                in1=zpad[p0:p1, c, a + off:b_ + off], op=ALU.subtract),
                                 r=["PA", "PB", "zpad"], w=["dT"])
                    shadd(PA, zpad[:, 0, :], -1, 0)
                    shadd(PB, PA, -1, 1, 64, 128)
                    dfin(PA, 0, 0, 64)
                    dfin(PB, 0, 64, 128)
                    shadd(PA, zpad[:, 1, :], -1, 0)
                    shadd(PB, PA, -1, 1)
                    shadd(PA, PB, -2, 2)
                    shadd(PB, PA, -4, 4, 64, 128)
                    dfin(PA, 1, 0, 64)
                    dfin(PB, 1, 64, 128)
                    P.barrier()

                allk = [("kT", gi) for gi, _ in groups]
                allv = [("vtok", gi) for gi, _ in groups]
                m2_groups = groups if not last else groups[:NGL]
                with ExitStack() as e2:
                    w2a = sb(e2, "w2a", [128, 8, 1536], BF16)
                    stg = [sb(e2, "stgA%d" % i, [128, 1, 1536]) for i in range(2)]
                    pwbd = sb(e2, "pwbd", [128, 2, 128], BF16)
                    pwf = sb(e2, "pwf", [128, 2, 128])
                    pscale = sb(e2, "pscale", [128, 2])
                    sguwT = sb(e2, "sguwT", [128, 4, 128], BF16)
                    sguf = sb(e2, "sguf", [128, 4, 128])
                    sgub = sb(e2, "sgub", [64, 4, 128])
                    junk = sb(e2, "junkA", [128, D])
                    ss = sb(e2, "ssA", [128, 4])
                    hTg = [sb(e2, "hTgA%d" % i, [128, 8, 512], BF16) for i in range(2)]
                    cS = [sb(e2, "cSA%d" % i, [128, 2, 512]) for i in range(1)] * 2
                    t1 = sb(e2, "t1A", [128, 512])
                    t2 = sb(e2, "t2A", [128, 512])
                    qT = sb(e2, "qT", [128, 4, 512], BF16)
                    pT = [sb(e2, "pT%d" % i, [128, 512], BF16) for i in range(6)]
                    lnd = sb(e2, "lnd", [64, 512])
                    rec = sb(e2, "rec", [64, 512])
                    yT = sb(e2, "yT", [64, 8, 512], BF16)
                    uT = sb(e2, "uT", [64, 4, 512], BF16)
                    vg = sb(e2, "vg", [128, 256])
                    vn = sb(e2, "vn", [128, 256], BF16)
                    sv = sb(e2, "svA", [128, 4])
                    ysT = sb(e2, "ysT", [64, 4, 512], BF16)
                    ypT = sb(e2, "ypT", [128, 2, 512], BF16)
                    tsg = sb(e2, "tsg", [64, 512])
                    for i in range(8):
                        cast_load(w2a[:, i:i + 1, :], ("w2a", i),
                                  w2a_d[layer, 128 * i:128 * (i + 1), :].rearrange("(k p) n -> p k n", p=128),
                                  stg[i % 2][:], ("stgA", i % 2), "act" if i % 2 == 0 else "pool")
                    w2r = [("w2a", i) for i in range(8)]
                    cast_load(pwbd[:], "pwbd", pwbd_d[layer, :, :, :], pwf[:], "pwf", "dve")
                    cast_load(sguwT[:], "sguwT", sguwT_d[layer, :, :, :], sguf[:], "sguf", "dve")
                    P.dma("sp", lambda: SP.dma_start(out=pscale[:], in_=pscale_d[layer, :, :]), w=["pscale"])
                    P.dma("sp", lambda: SP.dma_start(out=sgub[:], in_=sgub_d[layer, :, :, :]), w=["sgub"])
                    pti = [0]
                    def load_h(gidx):
                        gi2, tiles2 = m2_groups[gidx]
                        ng2 = len(tiles2) * 128
                        j2 = gidx % 2
                        P.dma("sp", lambda: SP.dma_start(out=hTg[j2][:, :, 0:ng2], in_=hTd[gi2, :, :, 0:ng2]),
                              r=[("hTd", gi2)], w=[("hTgA", j2)])
                    load_h(0)
                    for gidx, (gi, tiles) in enumerate(m2_groups):
                        j = gidx % 2
                        nt = len(tiles)
                        ng = nt * 128
                        s = 0 if tiles[0] < NTL else 1
                        tok0 = tiles[0] * 128
                        if gidx + 1 < len(m2_groups):
                            load_h(gidx + 1)
                        hr = [("hTgA", j)]
                        P.dma("sp", lambda: SP.dma_start(out=cS[0][:, 0, 0:ng], in_=ropeC[:, tok0:tok0 + ng]), w=[("cS", 0, 0)])
                        P.dma("sp", lambda: SP.dma_start(out=cS[0][:, 1, 0:ng], in_=ropeS[:, tok0:tok0 + ng]), w=[("cS", 0, 1)])
                        for c in range(4):
                            bq, bqr = bank(), bank()
                            for (bb, c0) in ((bq, c * 128), (bqr, 512 + c * 128)):
                                def mm(bb=bb, c0=c0):
                                    for k in range(8):
                                        ins = PE.matmul(PS[:, bb, 0:ng], lhsT=w2a[:, k, c0:c0 + 128], rhs=hTg[j][:, k, 0:ng],
                                                        start=(k == 0), stop=(k == 7))
                                    return ins
                                P.op("pe", mm, r=hr + w2r, w=[("ps", bb)])
                            P.op("dve", lambda: V.tensor_tensor(out=t1[:, 0:ng], in0=PS[:, bqr, 0:ng], in1=cS[0][:, 1, 0:ng], op=ALU.mult),
                                 r=[("ps", bqr), ("cS", 0, 1)], w=["t1"])
                            P.op("dve", lambda: V.tensor_tensor(out=t2[:, 0:ng], in0=PS[:, bq, 0:ng], in1=cS[0][:, 0, 0:ng], op=ALU.mult),
                                 r=[("ps", bq), ("cS", 0, 0)], w=["t2"])
                            P.op("dve", lambda: V.tensor_tensor(out=qT[:, c, 0:ng], in0=t1[:, 0:ng], in1=t2[:, 0:ng], op=ALU.add),
                                 r=["t1", "t2"], w=[("qT", c)])
                        qr_ = [("qT", c) for c in range(4)]
                        if layer == 0:
                            pump(0, 2)
                            pump(1, 2)
                        for ti, tile in enumerate(tiles):
                            if tile < NTL:
                                keys = []
                                if tile - 1 >= 0:
                                    keys.append((tile - 1, 0))
                                keys.append((tile, None))
                                if tile + 1 < NTL:
                                    keys.append((tile + 1, 1))
                                keys += [(NTL, None), (NTL + 1, None)]
                            else:
                                keys = [(NTL, None), (NTL + 1, None)]
                            for hk in range(2):
                                h0, h1 = hk * 64, hk * 64 + 64
                                bnum, bden = bank(), bank()
                                sbanks = []
                                for (kt, mk) in keys:
                                    bs = bank()
                                    sbanks.append(bs)

                                    def mm(bs=bs, kt=kt, mk=mk):
                                        ins = PE.matmul(PS[:, bs, :], lhsT=kT[h0:h1, kt * 128:(kt + 1) * 128],
                                                        rhs=qT[h0:h1, :, ti * 128:(ti + 1) * 128],
                                                        start=True, stop=(mk is None))
                                        if mk is not None:
                                            ins = PE.matmul(PS[:, bs, :], lhsT=identb[:], rhs=maskb[:, mk, :], start=False, stop=True)
                                        return ins
                                    P.op("pe", mm, r=allk + qr_ + ["identb", "maskb"], w=[("ps", bs)])
                                pts = []
                                for bs in sbanks:
                                    pi = pti[0] % 6
                                    pti[0] += 1
                                    pts.append(pi)
                                    P.op("act", lambda bs=bs, pi=pi: A.activation(out=pT[pi][:], in_=PS[:, bs, :], func=AF.Exp, scale=0.125),
                                         r=[("ps", bs)], w=[("pT", pi)])
                                nk = len(keys)
                                for ki, ((kt, mk), pi) in enumerate(zip(keys, pts)):
                                    def mm(ki=ki, kt=kt, pi=pi):
                                        PE.matmul(PS[0:64, bnum, :], lhsT=vtok[:, kt, h0:h1], rhs=pT[pi][:],
                                                  start=(ki == 0), stop=(ki == nk - 1))
                                        ins = PE.matmul(PS[0:64, bden, :], lhsT=ones_b[:, 0:64], rhs=pT[pi][:],
                                                        start=(ki == 0), stop=False)
                                        if ki == nk - 1:
                                            ins = PE.matmul(PS[0:64, bden, :], lhsT=ones_b[0:1, 0:64], rhs=sinkrow[0:1, hk, :],
                                                            start=False, stop=True)
                                        return ins
                                    P.op("pe", mm, r=allv + [("pT", pi), "ones_b", "sinkrow"], w=[("ps", bnum), ("ps", bden)])
                                P.op("act", lambda: A.activation(out=lnd[:], in_=PS[0:64, bden, :], func=AF.Ln),
                                     r=[("ps", bden)], w=["lnd"])
                                P.op("act", lambda: A.activation(out=rec[:], in_=lnd[:], func=AF.Exp, scale=-1.0),
                                     r=["lnd"], w=["rec"])
                                P.op("dve", lambda: V.tensor_tensor(
                                    out=yT[:, hk * 4:hk * 4 + 4, ti * 128:(ti + 1) * 128],
                                    in0=PS[0:64, bnum, :].rearrange("p (g q) -> p g q", g=4),
                                    in1=rec[:].rearrange("p (g q) -> p g q", g=4), op=ALU.mult),
                                    r=[("ps", bnum), "rec"], w=[("yT", ti, hk)])
                        for h4 in range(4):
                            bu = bank()

                            def mm():
                                for k in range(8):
                                    ins = PE.matmul(PS[0:64, bu, 0:ng], lhsT=w2a[:, k, 1024 + h4 * 64:1088 + h4 * 64],
                                                    rhs=hTg[j][:, k, 0:ng], start=(k == 0), stop=(k == 7))
                                return ins
                            P.op("pe", mm, r=hr + w2r, w=[("ps", bu)])
                            P.op("act", lambda: A.activation(out=uT[:, h4, 0:ng], in_=PS[0:64, bu, 0:ng], func=AF.Gelu_apprx_tanh),
                                 r=[("ps", bu)], w=[("uT", h4)])
                        for ti, tile in enumerate(tiles):
                            bvs = bank()

                            def mm():
                                for k in range(8):
                                    ins = PE.matmul(PS[:, bvs, 0:256], lhsT=hTg[j][:, k, ti * 128:(ti + 1) * 128],
                                                    rhs=w2a[:, k, 1280:1536], start=(k == 0), stop=(k == 7))
                                return ins
                            P.op("pe", mm, r=hr + w2r, w=[("ps", bvs)])
                            P.op("act", lambda: A.activation(out=vg[:], in_=PS[:, bvs, 0:256], func=AF.Gelu_apprx_tanh),
                                 r=[("ps", bvs)], w=["vg"])
                            P.op("dve", lambda: V.scalar_tensor_tensor(out=junk[:, 0:256], in0=vg[:], scalar=1.0, in1=vg[:],
                                                                        op0=ALU.mult, op1=ALU.mult, accum_out=sv[:, 0:1]),
                                 r=["vg"], w=["junk", "sv0"])
                            P.op("dve", lambda: V.tensor_scalar(out=sv[:, 1:2], in0=sv[:, 0:1], scalar1=1.0 / 256, scalar2=EPS,
                                                                op0=ALU.mult, op1=ALU.add), r=["sv0"], w=["sv1"])
                            P.op("act", lambda: A.activation(out=sv[:, 2:3], in_=sv[:, 1:2], func=AF.Ln), r=["sv1"], w=["sv2"])
                            P.op("act", lambda: A.activation(out=sv[:, 3:4], in_=sv[:, 2:3], func=AF.Exp, scale=-0.5), r=["sv2"], w=["sv3"])
                            P.op("dve", lambda: V.tensor_scalar(out=vn[:], in0=vg[:], scalar1=sv[:, 3:4], scalar2=None, op0=ALU.mult),
                                 r=["vg", "sv3"], w=["vn"])
                            bm_ = bank()

                            def mm():
                                for h4 in range(4):
                                    ins = PE.matmul(PS[0:64, bm_, h4 * 128:(h4 + 1) * 128], lhsT=vn[:, h4 * 64:(h4 + 1) * 64],
                                                    rhs=sguwT[:, h4, :], start=True, stop=True)
                                return ins
                            P.op("pe", mm, r=["vn", "sguwT"], w=[("ps", bm_)])
                            P.op("dve", lambda: V.tensor_tensor(out=tsg[:], in0=PS[0:64, bm_, :],
                                                                in1=sgub[:].rearrange("p a b -> p (a b)"), op=ALU.add),
                                 r=[("ps", bm_), "sgub"], w=["tsg"])
                            P.op("dve", lambda: V.tensor_tensor(out=ysT[:, :, ti * 128:(ti + 1) * 128],
                                                                in0=tsg[:].rearrange("p (a b) -> p a b", a=4),
                                                                in1=uT[:, :, ti * 128:(ti + 1) * 128], op=ALU.mult),
                                 r=["tsg"] + [("uT", h4) for h4 in range(4)], w=[("ysT", ti)])
                        for c in range(2):
                            bp = bank()
                            P.op("pe", lambda: PE.matmul(PS[:, bp, 0:ng], lhsT=pwbd[:, c, :], rhs=dT[:, c, tok0:tok0 + ng],
                                                         start=True, stop=True), r=["pwbd", "dT"], w=[("ps", bp)])
                            P.op("dve", lambda: V.tensor_scalar(out=ypT[:, c, 0:ng], in0=PS[:, bp, 0:ng], scalar1=pscale[:, c:c + 1],
                                                                scalar2=None, op0=ALU.mult),
                                 r=[("ps", bp), "pscale"], w=[("ypT", c)])
                        yr = [("yT", ti, hk) for ti in range(nt) for hk in range(2)]
                        P.dma("sp", lambda: SP.dma_start(out=yTd[gi, :, :, 0:ng], in_=yT[:, :, 0:ng]), r=yr, w=[("yTd", gi)])
                        P.dma("sp", lambda: SP.dma_start(out=ysTd[gi, :, :, 0:ng], in_=ysT[:, :, 0:ng]),
                              r=[("ysT", ti) for ti in range(nt)], w=[("ysTd", gi)])
                        P.dma("sp", lambda: SP.dma_start(out=ypTd[gi, :, :, 0:ng], in_=ypT[:, :, 0:ng]),
                              r=[("ypT", 0), ("ypT", 1)], w=[("ypTd", gi)])
                    P.barrier()

                P.barrier()
                eKV.close()
                with ExitStack() as e3:
                    w2g = sb(e3, "w2g", [128, 8, 3072], BF16)
                    wbrA = sb(e3, "wbrA", [64, 8, D], BF16)
                    wbrP = sb(e3, "wbrP", [128, 2, D], BF16)
                    wbrS = sb(e3, "wbrS", [64, 4, D], BF16)
                    wout = sb(e3, "wout", [128, 8, D], BF16)
                    stg = [sb(e3, "stgB%d" % i, [128, 2048]) for i in range(2)]
                    hTl = [sb(e3, "hTl%d" % i, [128, 8, 512], BF16) for i in range(1)] * 2
                    yTl = sb(e3, "yTl", [64, 8, 512], BF16)
                    ysTl = sb(e3, "ysTl", [64, 4, 512], BF16)
                    ypTl = sb(e3, "ypTl", [128, 2, 512], BF16)
                    sig = [sb(e3, "sig%d" % i, [128, 512], BF16) for i in range(3)]
                    tt = [sb(e3, "tt%d" % i, [128, 512]) for i in range(4)]
                    mT = sb(e3, "mT", [128, 8, 512], BF16)
                    xt_ = [sb(e3, "xtB%d" % i, [128, D]) for i in range(1)] * 2
                    xo_ = [sb(e3, "xoB%d" % i, [128, D]) for i in range(1)] * 2
                    ci = [0]

                    def cl(dst, dname, src, width):
                        i = ci[0] % 2
                        ci[0] += 1
                        cast_load(dst, dname, src, stg[i][:, 0:width] if True else None, ("stgB", i),
                                  ("act", "pool", "dve")[ci[0] % 3])
                    for k in range(8):
                        for hh in range(2):
                            cl(w2g[:, k, hh * 1536:(hh + 1) * 1536], ("w2g", k, hh),
                               w2g_d[layer, k * 128:(k + 1) * 128, hh * 1536:(hh + 1) * 1536], 1536)
                    for h in range(0, 8, 2):
                        i = ci[0] % 2
                        ci[0] += 1
                        cast_load(wbrA[:, h:h + 2, :], ("wbrA", h), wbrA_d[layer, :, h:h + 2, :],
                                  stg[i][0:64, 0:2048].rearrange("p (a b) -> p a b", a=2), ("stgB", i), "act")
                    i = ci[0] % 2
                    ci[0] += 1
                    cast_load(wbrP[:], "wbrP", wbrP_d[layer, :, :, :], stg[i][:, 0:2048].rearrange("p (a b) -> p a b", a=2),
                              ("stgB", i), "pool")
                    for h in range(0, 4, 2):
                        i = ci[0] % 2
                        ci[0] += 1
                        cast_load(wbrS[:, h:h + 2, :], ("wbrS", h), wbrS_d[layer, :, h:h + 2, :],
                                  stg[i][0:64, 0:2048].rearrange("p (a b) -> p a b", a=2), ("stgB", i), "dve")
                    for k in range(0, 8, 2):
                        i = ci[0] % 2
                        ci[0] += 1
                        cast_load(wout[:, k:k + 2, :], ("wout", k), wout_d[layer, :, k:k + 2, :],
                                  stg[i][:, 0:2048].rearrange("p (a b) -> p a b", a=2), ("stgB", i), "act")
                    wr_g = [("w2g", k, hh) for k in range(8) for hh in range(2)]
                    wr_b = [("wbrA", h) for h in range(0, 8, 2)] + ["wbrP"] + [("wbrS", h) for h in range(0, 4, 2)]
                    wr_o = [("wout", k) for k in range(0, 8, 2)]
                    for gi, tiles in m2_groups:
                        j = 0
                        nt = len(tiles)
                        ng = nt * 128
                        s = 0 if tiles[0] < NTL else 1
                        if layer == 0:
                            pump(1, 2)
                        P.dma("sp", lambda: SP.dma_start(out=hTl[j][:, :, 0:ng], in_=hTd[gi, :, :, 0:ng]), r=[("hTd", gi)], w=[("hTl", j)])
                        P.dma("sp", lambda: SP.dma_start(out=yTl[:, :, 0:ng], in_=yTd[gi, :, :, 0:ng]), r=[("yTd", gi)], w=["yTl"])
                        P.dma("sp", lambda: SP.dma_start(out=ysTl[:, :, 0:ng], in_=ysTd[gi, :, :, 0:ng]), r=[("ysTd", gi)], w=["ysTl"])
                        P.dma("sp", lambda: SP.dma_start(out=ypTl[:, :, 0:ng], in_=ypTd[gi, :, :, 0:ng]), r=[("ypTd", gi)], w=["ypTl"])
                        for m in range(8):
                            mc = slice(m * 128, (m + 1) * 128)
                            bgs = []
                            for i3 in range(3):
                                bg = bank()
                                bgs.append(bg)

                                def mmg(bg=bg, i3=i3):
                                    for k in range(8):
                                        ins = PE.matmul(PS[:, bg, 0:ng], lhsT=w2g[:, k, i3 * D + m * 128:i3 * D + (m + 1) * 128],
                                                        rhs=hTl[j][:, k, 0:ng], start=(k == 0), stop=(k == 7))
                                    return ins
                                P.op("pe", mmg, r=wr_g + [("hTl", j)], w=[("ps", bg)])
                                P.op("act", lambda bg=bg, i3=i3: A.activation(out=sig[i3][:, 0:ng], in_=PS[:, bg, 0:ng], func=AF.Sigmoid),
                                     r=[("ps", bg)], w=[("sig", i3)])
                            bA, bP, bS = bank(), bank(), bank()

                            def mmA():
                                for h in range(8):
                                    ins = PE.matmul(PS[:, bA, 0:ng], lhsT=wbrA[:, h, mc], rhs=yTl[:, h, 0:ng], start=(h == 0), stop=(h == 7))
                                return ins

                            def mmP():
                                for c in range(2):
                                    ins = PE.matmul(PS[:, bP, 0:ng], lhsT=wbrP[:, c, mc], rhs=ypTl[:, c, 0:ng], start=(c == 0), stop=(c == 1))
                                return ins

                            def mmS():
                                for h in range(4):
                                    ins = PE.matmul(PS[:, bS, 0:ng], lhsT=wbrS[:, h, mc], rhs=ysTl[:, h, 0:ng], start=(h == 0), stop=(h == 3))
                                return ins
                            P.op("pe", mmA, r=wr_b + ["yTl"], w=[("ps", bA)])
                            P.op("pe", mmP, r=wr_b + ["ypTl"], w=[("ps", bP)])
                            P.op("pe", mmS, r=wr_b + ["ysTl"], w=[("ps", bS)])
                            for i3, bb in enumerate((bA, bP, bS)):
                                P.op("dve", lambda i3=i3, bb=bb: V.tensor_tensor(out=tt[i3][:, 0:ng], in0=PS[:, bb, 0:ng], in1=sig[i3][:, 0:ng], op=ALU.mult),
                                     r=[("ps", bb), ("sig", i3)], w=[("tt", i3)])
                            P.op("pool", lambda: G.tensor_tensor(out=tt[3][:, 0:ng], in0=tt[0][:, 0:ng], in1=tt[1][:, 0:ng], op=ALU.add),
                                 r=[("tt", 0), ("tt", 1)], w=[("tt", 3)])
                            P.op("pool", lambda: G.tensor_tensor(out=mT[:, m, 0:ng], in0=tt[3][:, 0:ng], in1=tt[2][:, 0:ng], op=ALU.add),
                                 r=[("tt", 3), ("tt", 2)], w=[("mT", m)])
                        mr = [("mT", m) for m in range(8)]
                        for ti, tile in enumerate(tiles):
                            jj = 0
                            P.dma("sp", lambda: SP.dma_start(out=xt_[jj][:], in_=xs[tile * 128:(tile + 1) * 128, :]),
                                  r=[("xs", tile)], w=[("xt", jj)])
                            for half in range(2):
                                hc = slice(half * 512, (half + 1) * 512)
                                bo = bank()

                                def mmo():
                                    for k in range(8):
                                        ins = PE.matmul(PS[:, bo, :], lhsT=mT[:, k, ti * 128:(ti + 1) * 128], rhs=wout[:, k, hc],
                                                        start=(k == 0), stop=(k == 7))
                                    return ins
                                P.op("pe", mmo, r=mr + wr_o, w=[("ps", bo)])
                                P.op("dve", lambda: V.tensor_tensor(out=tt[half][:], in0=PS[:, bo, :], in1=modm[s][2][:, hc], op=ALU.mult),
                                     r=[("ps", bo)] + modnames(0, s), w=[("tt", half)])
                                P.op("pool", lambda: G.tensor_tensor(out=xo_[jj][:, hc], in0=tt[half][:], in1=xt_[jj][:, hc], op=ALU.add),
                                     r=[("tt", half), ("xt", jj)], w=[("xo", jj, half)])
                            P.dma("sp", lambda: SP.dma_start(out=xs[tile * 128:(tile + 1) * 128, :], in_=xo_[jj][:]),
                                  r=[("xo", jj, 0), ("xo", jj, 1)], w=[("xs", tile)])
                    P.barrier()
                P.barrier()

            for _ in convs[layer]:
                pass
            with ExitStack() as eP:
                modp = [[sb(eP, "modp%d%d" % (s, k), [128, D]) for k in range(3)] for s in range(2)]
                modulation(layer, 1, modp, g_ffn_r)
                wf = sb(eP, "wfold", [128, 8, 2048], BF16)
                with ExitStack() as ef:
                    wqT = sb(ef, "wqT", [128, 16, D])
                    subkT = sb(ef, "subkT", [128, 16, 128])
                    for c in range(0, 16, 4):
                        P.dma("sp", lambda: SP.dma_start(out=wqT[:, c:c + 4, :], in_=wqT_d[layer, :, c:c + 4, :]), w=[("wqT", c)])
                    P.dma("sp", lambda: SP.dma_start(out=subkT[:], in_=subkT_d[layer, :, :, :]), w=["subkT"])
                    for k in range(8):
                        for c4 in range(4):
                            b = bank()

                            def mm():
                                for cc in range(4):
                                    c = c4 * 4 + cc
                                    ins = PE.matmul(PS[:, b, cc * 128:(cc + 1) * 128], lhsT=wqT[:, c, k * 128:(k + 1) * 128],
                                                    rhs=subkT[:, c, :], start=True, stop=True)
                                return ins
                            P.op("pe", mm, r=[("wqT", c4 * 4), "subkT"], w=[("ps", b)])
                            P.op("act", lambda: A.copy(out=wf[:, k, c4 * 512:(c4 + 1) * 512], in_=PS[:, b, :]),
                                 r=[("ps", b)], w=[("wf", k, c4)])
                    P.barrier()
                wfr = [("wf", k, c4) for k in range(8) for c4 in range(4)]
                xt_ = [sb(eP, "xtP%d" % i, [128, D]) for i in range(3)]
                junk = sb(eP, "junkP", [128, D])
                ss = sb(eP, "ssP", [128, 4])
                h2b_ = [sb(eP, "h2b%d" % i, [128, D], BF16) for i in range(2)]
                h2T = sb(eP, "h2T", [128, 8, 128], BF16)
                sS = sb(eP, "sS", [128, 16, 128])
                sS2 = sb(eP, "sS2", [128, 16, 128])
                svt = sb(eP, "svt", [128, 8, 2, 16])
                sit = sb(eP, "sit", [128, 8, 2, 16], U32)
                sif = sb(eP, "sif", [128, 8, 2, 16])
                cand = sS[:].rearrange("p a b -> p (a b)").rearrange("p (h c) -> p h c", h=8)
                cand2 = sS2[:].rearrange("p a b -> p (a b)").rearrange("p (h c) -> p h c", h=8)
                ts = sb(eP, "ts", [128, 8, 16])
                tp = sb(eP, "tp", [128, 8, 16], U32)
                ta = sb(eP, "ta", [128, 8, 16], U32)
                tb_ = sb(eP, "tb", [128, 8, 16], U32)
                taf = sb(eP, "taf", [128, 8, 16])
                tbf = sb(eP, "tbf", [128, 8, 16])
                eq = sS2[:].rearrange("p a b -> p (a b)").rearrange("p (h a b) -> p h a b", h=8, a=16)
                If = sb(eP, "If", [128, 8, 16])
                Jf = sb(eP, "Jf", [128, 8, 16])
                ef_ = sb(eP, "ef", [128, 128])
                eu_ = [sb(eP, "eu%d" % i, [128, 128], U32) for i in range(2)]
                tsc = sb(eP, "tsc", [128, 8, 16])
                ex = sb(eP, "ex", [128, 8, 16])
                zz = sb(eP, "zz", [128, 8])
                rz = sb(eP, "rz", [128, 8])
                gate_ = [sb(eP, "gate%d" % i, [128, 128]) for i in range(2)]
                actv = sb(eP, "actv", [128, 128])
                gact = sb(eP, "gact", [128, 128])
                wgt = sb(eP, "wgt", [128, 128])
                guv = [sb(eP, "guv%d" % i, [128, 2 * D], BF16) for i in range(NGB)]
                prod = [sb(eP, "prod%d" % i, [128, D], BF16) for i in range(3)]
                junkb = sb(eP, "junkb", [128, D], BF16)
                GS = 4
                diag = [sb(eP, "diag%d" % i, [128, GS, 128], BF16) for i in range(3)]
                tmpo = [sb(eP, "tmpo%d" % i, [128, 512]) for i in range(2)]
                xo = sb(eP, "xoP", [128, D])
                if last:
                    gfin = sb(eP, "gfin", [128, D])
                    P.dma("sp", lambda: SP.dma_start(out=gfin[:], in_=g_fin_r[:, :]), w=["gfin"])
                cnt_u, cnt_v, cnt_p = [0], [0], [0]
                ptiles = list(range(NTILES)) if not last else list(range(NTL))
                pidx = {t_: i_ for i_, t_ in enumerate(ptiles)}

                def stage_A(tile):
                    jj = tile % 2
                    x3 = pidx[tile] % 3
                    s = 0 if tile < NTL else 1
                    P.dma("sp", lambda: SP.dma_start(out=xt_[x3][:], in_=xs[tile * 128:(tile + 1) * 128, :]),
                          r=[("xs", tile)], w=[("xt", x3)])
                    norm_mod(xt_[x3], ("xt", x3), modp[s][0], modp[s][1], modnames(1, s), junk, ss, None, h2b_[jj], ("h2", jj),
                             add_eng="dve")
                    transpose8(h2b_[jj], ("h2", jj), h2T[:], "h2T", b=0)

                    def mm():
                        for n in range(4):
                            for k in range(8):
                                ins = PE.matmul(PS[:, n, :], lhsT=h2T[:, k, :], rhs=wf[:, k, n * 512:(n + 1) * 512],
                                                start=(k == 0), stop=(k == 7))
                        return ins
                    psn = [("ps", n) for n in range(4)]
                    P.op("pe", mm, r=["h2T"] + wfr, w=psn)
                    P.op("act", lambda: A.copy(out=sS[:].rearrange("p a b -> p (a b)"),
                                               in_=PS[:, 0:4, :].rearrange("p a b -> p (a b)")), r=psn, w=["sS"])

                def stage_B(tile):
                    eu = eu_[tile % 2]
                    eun = ("eu", tile % 2)
                    gate = gate_[tile % 2]
                    gaten = ("gate", tile % 2)
                    for c in range(16):
                        h, p_ = c // 2, c % 2
                        P.op("dve", lambda: V.max(out=svt[:, h, p_, 0:8], in_=sS[:, c, :]), r=["sS"], w=[("sv", c, 0)])
                        yield
                    for c in range(16):
                        h, p_ = c // 2, c % 2
                        P.op("dve", lambda: V.max_index(out=sit[:, h, p_, 0:8], in_max=svt[:, h, p_, 0:8], in_values=sS[:, c, :]),
                             r=["sS", ("sv", c, 0)], w=[("si", c, 0)])
                        yield
                    for c in range(16):
                        h, p_ = c // 2, c % 2
                        P.op("dve", lambda: V.match_replace(out=sS2[:, c, :], in_to_replace=svt[:, h, p_, 0:8], in_values=sS[:, c, :],
                                                            imm_value=-1e30), r=["sS", ("sv", c, 0)], w=[("sS2", c)])
                        yield
                    for c in range(16):
                        h, p_ = c // 2, c % 2
                        P.op("dve", lambda: V.max(out=svt[:, h, p_, 8:16], in_=sS2[:, c, :]), r=[("sS2", c)], w=[("sv", c, 1)])
                        yield
                    for c in range(16):
                        h, p_ = c // 2, c % 2
                        P.op("dve", lambda: V.max_index(out=sit[:, h, p_, 8:16], in_max=svt[:, h, p_, 8:16], in_values=sS2[:, c, :]),
                             r=[("sS2", c), ("sv", c, 1)], w=[("si", c, 1)])
                        yield
                    svr = [("sv", c, q) for c in range(16) for q in range(2)]
                    sir = [("si", c, q) for c in range(16) for q in range(2)]
                    s2r = [("sS2", c) for c in range(16)]
                    P.op("dve", lambda: V.tensor_copy(out=sif[:], in_=sit[:]), r=sir, w=["sif"])
                    yield
                    P.op("dve", lambda: V.tensor_tensor(
                        out=cand.rearrange("p h (a b) -> p h a b", a=16),
                        in0=svt[:, :, 0, :].unsqueeze(3).broadcast_to([128, 8, 16, 16]),
                        in1=svt[:, :, 1, :].unsqueeze(2).broadcast_to([128, 8, 16, 16]), op=ALU.add), r=svr, w=["sS"])
                    yield
                    for h in range(8):
                        P.op("dve", lambda: V.max(out=ts[:, h, 0:8], in_=cand[:, h, :]), r=["sS"], w=[("ts", h, 0)])
                        yield
                    for h in range(8):
                        P.op("dve", lambda: V.max_index(out=tp[:, h, 0:8], in_max=ts[:, h, 0:8], in_values=cand[:, h, :]),
                             r=["sS", ("ts", h, 0)], w=[("tp", h, 0)])
                        yield
                    for h in range(8):
                        P.op("dve", lambda: V.match_replace(out=cand2[:, h, :], in_to_replace=ts[:, h, 0:8], in_values=cand[:, h, :],
                                                            imm_value=-1e30), r=["sS", ("ts", h, 0)] + s2r, w=[("cand2", h)])
                        yield
                    for h in range(8):
                        P.op("dve", lambda: V.max(out=ts[:, h, 8:16], in_=cand2[:, h, :]), r=[("cand2", h)], w=[("ts", h, 1)])
                        yield
                    for h in range(8):
                        P.op("dve", lambda: V.max_index(out=tp[:, h, 8:16], in_max=ts[:, h, 8:16], in_values=cand2[:, h, :]),
                             r=[("cand2", h), ("ts", h, 1)], w=[("tp", h, 1)])
                        yield
                    tsr = [("ts", h, q) for h in range(8) for q in range(2)]
                    tpr = [("tp", h, q) for h in range(8) for q in range(2)]
                    c2r = [("cand2", h) for h in range(8)]
                    P.op("dve", lambda: V.tensor_scalar(out=ta[:], in0=tp[:], scalar1=4, scalar2=None, op0=ALU.logical_shift_right),
                         r=tpr, w=["ta"])
                    yield
                    P.op("dve", lambda: V.tensor_scalar(out=tb_[:], in0=tp[:], scalar1=15, scalar2=None, op0=ALU.bitwise_and),
                         r=tpr, w=["tb"])
                    yield
                    P.op("dve", lambda: V.tensor_copy(out=taf[:], in_=ta[:]), r=["ta"], w=["taf"])
                    yield
                    P.op("dve", lambda: V.tensor_copy(out=tbf[:], in_=tb_[:]), r=["tb"], w=["tbf"])
                    yield
                    io4 = iota16[:].unsqueeze(1).unsqueeze(1).broadcast_to([128, 8, 16, 16])
                    for (src, pp, dst, dn) in ((taf, 0, If, "If"), (tbf, 1, Jf, "Jf")):
                        P.op("dve", lambda: V.tensor_tensor(out=eq, in0=src[:].unsqueeze(3).broadcast_to([128, 8, 16, 16]),
                                                            in1=io4, op=ALU.is_equal), r=["taf", "tbf", "iota16"] + c2r + s2r, w=["eqb"])
                        yield
                        P.op("dve", lambda: V.tensor_tensor(out=eq, in0=eq,
                                                            in1=sif[:, :, pp, :].unsqueeze(2).broadcast_to([128, 8, 16, 16]),
                                                            op=ALU.mult), r=["eqb", "sif"], w=["eqb"])
                        yield
                        P.op("dve", lambda: V.tensor_reduce(out=dst[:], in_=eq, axis=AX.X, op=ALU.add), r=["eqb"], w=[dn])
                        yield
                    P.op("dve", lambda: V.scalar_tensor_tensor(out=ef_[:], in0=If[:].rearrange("p a b -> p (a b)"), scalar=128.0,
                                                               in1=Jf[:].rearrange("p a b -> p (a b)"), op0=ALU.mult, op1=ALU.add),
                         r=["If", "Jf"], w=["ef"])
                    yield
                    P.op("dve", lambda: V.tensor_copy(out=eu[:], in_=ef_[:]), r=["ef"], w=[eun])
                    yield
                    P.op("dve", lambda: V.tensor_tensor(out=tsc[:], in0=ts[:], in1=ts[:, :, 0:1].broadcast_to([128, 8, 16]), op=ALU.subtract),
                         r=tsr, w=["tsc"])
                    yield
                    P.op("act", lambda: A.activation(out=ex[:], in_=tsc[:], func=AF.Exp), r=["tsc"], w=["ex"])
                    yield
                    P.op("dve", lambda: V.tensor_reduce(out=zz[:], in_=ex[:], axis=AX.X, op=ALU.add), r=["ex"], w=["zz"])
                    yield
                    P.op("dve", lambda: V.reciprocal(out=rz[:], in_=zz[:]), r=["zz"], w=["rz"])
                    yield
                    P.op("dve", lambda: V.tensor_tensor(out=gate[:].rearrange("p (a b) -> p a b", a=8), in0=ex[:],
                                                        in1=rz[:].unsqueeze(2).broadcast_to([128, 8, 16]), op=ALU.mult),
                         r=["ex", "rz"], w=[gaten])
                    yield

                def stage_CD(tile, genB, hook=None):
                    jj = tile % 2
                    eu = eu_[jj]
                    eun = ("eu", jj)
                    gate = gate_[jj]
                    h2b = h2b_[jj]
                    ngrp = peer_slots // GS
                    ab = 4 + 2 * (pidx[tile] % 2)
                    bis_of = {}
                    di_of = {}

                    def fin_act(g):
                        gs = slice(g * GS, (g + 1) * GS)
                        P.op("act", lambda: A.activation(out=gact[:, gs], in_=actv[:, gs], func=AF.Gelu_apprx_tanh),
                             r=[("actv", g * GS + q) for q in range(GS)], w=[("gact", g)])

                    def fin_rest(g):
                        gs = slice(g * GS, (g + 1) * GS)
                        di = cnt_v[0] % 3
                        cnt_v[0] += 1
                        P.op("dve", lambda: V.tensor_tensor(out=wgt[:, gs], in0=gate[:, gs], in1=gact[:, gs], op=ALU.mult),
                             r=[("gate", jj), ("gact", g)], w=[("wgt", g)])
                        P.op("dve", lambda: V.tensor_tensor(
                            out=diag[di][:],
                            in0=identb[:].unsqueeze(1).broadcast_to([128, GS, 128]),
                            in1=wgt[:, gs].unsqueeze(2).broadcast_to([128, GS, 128]), op=ALU.mult),
                            r=["identb", ("wgt", g)], w=[("diag", di)])
                        for q in range(GS):
                            sl = g * GS + q
                            bi = bis_of[g][q]

                            def mm():
                                for half in range(2):
                                    ins = PE.matmul(PS[:, ab + half, :], lhsT=diag[di][:, q, :],
                                                    rhs=guv[bi][:, D + half * 512:D + (half + 1) * 512],
                                                    start=(sl == 0), stop=(sl == peer_slots - 1))
                                return ins
                            P.op("pe", mm, r=[("guv", bi), ("diag", di)], w=[("ps", ab), ("ps", ab + 1)])

                    for g in range(ngrp):
                        if g >= 1:
                            fin_act(g - 1)
                        bis = []
                        for q in range(GS):
                            sl = g * GS + q
                            bi = cnt_u[0] % NGB
                            cnt_u[0] += 1
                            bis.append(bi)
                            pi = cnt_p[0] % 3
                            cnt_p[0] += 1
                            P.dma("pool", lambda: G.indirect_dma_start(
                                out=guv[bi][:], out_offset=None, in_=uvb[layer][:, :],
                                in_offset=bass.IndirectOffsetOnAxis(ap=eu[:, sl:sl + 1], axis=0)), r=[eun] + tabnames(layer), w=[("guv", bi)])
                            P.op("dve", lambda: V.tensor_tensor(out=prod[pi][:], in0=guv[bi][:, 0:D], in1=h2b[:], op=ALU.mult),
                                 r=[("guv", bi), ("h2", jj)], w=[("prod", pi)])
                            P.op("act", lambda: A.activation(out=junkb[:], in_=prod[pi][:], func=AF.Copy, accum_out=actv[:, sl:sl + 1]),
                                 r=[("prod", pi)], w=["junkb", ("actv", sl)])
                            if genB is not None:
                                for _ in range(2):
                                    next(genB, None)
                        bis_of[g] = bis
                        if g >= 1:
                            fin_rest(g - 1)
                        if g == 2 and hook is not None:
                            hook()
                    fin_act(ngrp - 1)
                    fin_rest(ngrp - 1)
                    if genB is not None:
                        for _ in genB:
                            pass

                def stage_E(tile):
                    jj = pidx[tile] % 3
                    ab = 4 + 2 * (pidx[tile] % 2)
                    s = 0 if tile < NTL else 1
                    for half in range(2):
                        hc = slice(half * 512, (half + 1) * 512)
                        P.op("dve", lambda: V.tensor_tensor(out=tmpo[half][:], in0=PS[:, ab + half, :], in1=modp[s][2][:, hc], op=ALU.mult),
                             r=[("ps", ab + half)] + modnames(1, s), w=[("tmpo", half)])
                        P.op("dve", lambda: V.tensor_tensor(out=xo[:, hc], in0=tmpo[half][:], in1=xt_[jj][:, hc], op=ALU.add),
                             r=[("tmpo", half), ("xt", jj)], w=[("xo", half)])
                    xor_ = [("xo", 0), ("xo", 1)]
                    if not last:
                        P.dma("sp", lambda: SP.dma_start(out=xs[tile * 128:(tile + 1) * 128, :], in_=xo[:]), r=xor_, w=[("xs", tile)],
                              is_out=dbg)
                    else:
                        P.op("dve", lambda: V.scalar_tensor_tensor(out=junk[:], in0=xo[:], scalar=1.0, in1=xo[:],
                                                                   op0=ALU.mult, op1=ALU.mult, accum_out=ss[:, 0:1]),
                             r=xor_, w=["junk", "ss0"])
                        P.op("dve", lambda: V.tensor_scalar(out=ss[:, 1:2], in0=ss[:, 0:1], scalar1=1.0 / D, scalar2=EPS,
                                                            op0=ALU.mult, op1=ALU.add), r=["ss0"], w=["ss1"])
                        P.op("act", lambda: A.activation(out=ss[:, 2:3], in_=ss[:, 1:2], func=AF.Ln), r=["ss1"], w=["ss2"])
                        P.op("act", lambda: A.activation(out=ss[:, 3:4], in_=ss[:, 2:3], func=AF.Exp, scale=-0.5), r=["ss2"], w=["ss3"])
                        P.op("dve", lambda: V.scalar_tensor_tensor(out=junk[:], in0=xo[:], scalar=ss[:, 3:4], in1=gfin[:],
                                                                   op0=ALU.mult, op1=ALU.mult),
                             r=xor_ + ["ss3", "gfin"], w=["junk"])
                        P.dma("sp", lambda: SP.dma_start(out=y_out[tile * 128:(tile + 1) * 128, :], in_=junk[:]), r=["junk"],
                              w=[("yout", tile)], is_out=True)

                stage_A(ptiles[0])
                for _ in stage_B(ptiles[0]):
                    pass
                for i_, tile in enumerate(ptiles):
                    nxt = ptiles[i_ + 1] if i_ + 1 < len(ptiles) else None
                    genB = None
                    if nxt is not None:
                        stage_A(nxt)
                        genB = stage_B(nxt)
                    prev = ptiles[i_ - 1] if i_ >= 1 else None
                    stage_CD(tile, genB, hook=(lambda: stage_E(prev)) if prev is not None else None)
                stage_E(ptiles[-1])
                P.barrier()
        P.finish()
        print("instructions:", P.n_ins)
    return nc


def prep_inputs(inp, NTL=32, n_cores=8):
    f = np.float32
    T = NTL * 128
    TT = T + CTX
    x = np.asarray(inp["x"], f)
    c = np.asarray(inp["c"], f)
    ctx = np.asarray(inp["ctx"], f)
    c_ctx = np.asarray(inp["c_ctx"], f)
    w_in = np.asarray(inp["w_in"], f)
    rp = np.concatenate([np.arange(16, 32), np.arange(0, 16), np.arange(48, 64), np.arange(32, 48)])
    d64 = np.arange(64)
    qcols = np.concatenate([np.concatenate([j * 64 + d64, (4 + j) * 64 + d64]) for j in range(4)])
    qrcols = np.concatenate([np.concatenate([j * 64 + rp, (4 + j) * 64 + rp]) for j in range(4)])
    kcols = 512 + np.arange(128)
    krcols = 512 + np.concatenate([rp, 64 + rp])
    vcols = 640 + np.arange(128)
    pcols = 768 + np.arange(256)
    ucols = 1024 + np.arange(256)
    vscols = 1280 + np.arange(256)
    w1 = np.ascontiguousarray(w_in[:, :, np.concatenate([kcols, krcols, vcols, pcols])])
    w2a = np.ascontiguousarray(w_in[:, :, np.concatenate([qcols, qrcols, ucols, vscols])])
    w2g = np.ascontiguousarray(w_in[:, :, 1536:4608])
    rows = T // 64
    row = np.repeat(np.arange(rows, dtype=f), 64)
    col = np.tile(np.arange(64, dtype=f), rows)
    inv = (np.float32(10000.0) ** (-np.arange(16, dtype=f) / np.float32(16))).astype(f)
    ar, ac = (row[:, None] * inv).astype(f), (col[:, None] * inv).astype(f)
    cos_d = np.concatenate([np.cos(ar), np.cos(ar), np.cos(ac), np.cos(ac)], axis=1).astype(f)
    sin_d = np.concatenate([-np.sin(ar), np.sin(ar), -np.sin(ac), np.sin(ac)], axis=1).astype(f)
    cos_d = np.concatenate([cos_d, np.ones((CTX, 64), f)], axis=0)
    sin_d = np.concatenate([sin_d, np.zeros((CTX, 64), f)], axis=0)
    ropeC = np.ascontiguousarray(np.concatenate([cos_d.T, cos_d.T], axis=0))
    ropeS = np.ascontiguousarray(np.concatenate([sin_d.T, sin_d.T], axis=0))
    sink = np.asarray(inp["attn_sink"], f)
    sink_r = np.ascontiguousarray(np.repeat(sink.reshape(2, 1, 2, 4, 1), 128, axis=4).reshape(2, 1, 2, 512))
    pool_w = np.asarray(inp["pool_w"], f)
    pwbd = np.zeros((2, 128, 2, 128), f)
    for g in range(4):
        o = (g % 2) * 64
        pwbd[:, o:o + 64, g // 2, o:o + 64] = pool_w[:, g]
    pscale = np.ascontiguousarray(np.asarray(inp["pool_scale"], f).reshape(2, 2, 128).transpose(0, 2, 1))
    rc = np.zeros((128, 2, TT), f)
    for g, size in enumerate(POOL_SIZES):
        for (a, l) in ((0, T), (T, CTX)):
            t = np.arange(l)
            lo = np.clip(t - size // 2, 0, l)
            hi = np.clip(t + size // 2, 0, l)
            o = (g % 2) * 64
            rc[o:o + 64, g // 2, a:a + l] = (1.0 / (hi - lo).astype(f))[None, :]
    sgu_wT = np.ascontiguousarray(np.asarray(inp["sgu_w"], f).transpose(0, 3, 1, 2))
    sgu_b_r = np.ascontiguousarray(np.broadcast_to(np.asarray(inp["sgu_b"], f)[:, None], (2, 64, 4, 128)))
    wbrA = np.ascontiguousarray(np.asarray(inp["w_br_attn"], f).reshape(2, 8, 64, D).transpose(0, 2, 1, 3))
    wbrP = np.ascontiguousarray(np.asarray(inp["w_br_pool"], f).reshape(2, 2, 128, D).transpose(0, 2, 1, 3))
    wbrS = np.ascontiguousarray(np.asarray(inp["w_br_sgu"], f).reshape(2, 4, 64, D).transpose(0, 2, 1, 3))
    wout = np.ascontiguousarray(np.asarray(inp["w_out"], f).reshape(2, 8, 128, D).transpose(0, 2, 1, 3))
    wqT = np.ascontiguousarray(np.asarray(inp["peer_wq"], f).reshape(2, D, 16, 128).transpose(0, 3, 2, 1))
    subkT = np.ascontiguousarray(np.asarray(inp["peer_subkeys"], f).reshape(2, 16, 128, 128).transpose(0, 3, 1, 2))
    jj, ii = np.meshgrid(np.arange(128), np.arange(128), indexing="ij")
    mA = np.where(jj >= ii, 0.0, NEG).astype(f)
    mB = np.where(jj <= ii, 0.0, NEG).astype(f)
    maskAB = np.ascontiguousarray(np.stack([np.tile(mA, (1, 4)), np.tile(mB, (1, 4))], axis=1))
    shared = {
        "w_mod": np.asarray(inp["w_mod"], f),
        "b_mod_r": np.ascontiguousarray(np.broadcast_to(np.asarray(inp["b_mod"], f)[:, None], (2, 128, 6 * D))),
        "g_mix_r": np.ascontiguousarray(np.broadcast_to(np.asarray(inp["g_mix"], f)[:, None], (2, 128, D))),
        "g_ffn_r": np.ascontiguousarray(np.broadcast_to(np.asarray(inp["g_ffn"], f)[:, None], (2, 128, D))),
        "g_fin_r": np.ascontiguousarray(np.broadcast_to(np.asarray(inp["g_final"], f)[None], (128, D))),
        "w1": w1, "w2a": w2a, "w2g": w2g, "ropeC": ropeC, "ropeS": ropeS, "sink_r": sink_r, "pwbd": pwbd,
        "pscale": pscale, "rc": rc, "sgu_wT": sgu_wT, "sgu_b_r": sgu_b_r, "wbrA": wbrA, "wbrP": wbrP, "wbrS": wbrS,
        "wout": wout, "wqT": wqT, "subkT": subkT,
        "peer_u0": np.ascontiguousarray(np.asarray(inp["peer_u"], f)[0]), "peer_u1": np.ascontiguousarray(np.asarray(inp["peer_u"], f)[1]),
        "peer_v0": np.ascontiguousarray(np.asarray(inp["peer_v"], f)[0]), "peer_v1": np.ascontiguousarray(np.asarray(inp["peer_v"], f)[1]),
        "maskAB": maskAB, "ident": np.eye(128, dtype=f),
        "iota16": np.ascontiguousarray(np.broadcast_to(np.arange(16, dtype=f)[None], (128, 16))),
    }
    maps = []
    for b in range(n_cores):
        m = dict(shared)
        m["x"] = np.ascontiguousarray(x[b, :T])
        m["ctx"] = np.ascontiguousarray(ctx[b])
        cv = np.stack([c[b].reshape(8, 128).T, c_ctx.reshape(8, 128).T], axis=1)
        m["cvec"] = np.ascontiguousarray(cv.astype(f))
        maps.append(m)
    return maps


def kernel(**inputs):
    n = 8
    nc = build(NTL=32, n_layers=2)
    maps = prep_inputs(inputs, NTL=32, n_cores=n)
    res = run_bass_kernel_spmd(nc, maps, core_ids=list(range(n)))
    return np.stack([np.asarray(r["y"], np.float32) for r in res.results], axis=0)
```

```python
import numpy as np
import concourse.bass as bass
import concourse.mybir as mybir
from concourse.bass_utils import run_bass_kernel_spmd
from contextlib import ExitStack

F32 = mybir.dt.float32
BF16 = mybir.dt.bfloat16
U32 = mybir.dt.uint32
AF = mybir.ActivationFunctionType
ALU = mybir.AluOpType
AX = mybir.AxisListType

D = 1024
CTX = 256
EPS = 1e-6
NEG = -30000.0
POOL_SIZES = (2, 4, 8, 16)
ND_SEM = 40
NGB = 16


class Prog:
    def __init__(self, nc, es):
        self.nc = nc
        self.eng = {"pe": nc.tensor, "dve": nc.vector, "act": nc.scalar, "pool": nc.gpsimd, "sp": nc.sync}
        self.sem = {}
        for e in self.eng:
            self.sem[e] = es.enter_context(nc.semaphore("s_" + e))
        for i in range(ND_SEM):
            self.sem[("d", i)] = es.enter_context(nc.semaphore("s_d%d" % i))
        self.cnt = {e: 0 for e in self.eng}
        self.seen = {e: {} for e in self.eng}
        self.lw = {}
        self.rd = {}
        self.dval = [0] * ND_SEM
        self.rr_rng = {"sp": (0, 24), "pool": (24, ND_SEM)}
        self.rr = {"sp": 0, "pool": 24}
        self.out_tokens = []
        self.n_ins = 0

    def _deps(self, e, r, w, extra=()):
        deps = {}

        def add(tok):
            sk, v = tok
            if sk == "pe" and e == "pe":
                return
            if self.seen[e].get(sk, 0) < v:
                if deps.get(sk, 0) < v:
                    deps[sk] = v

        for b in r:
            if b in self.lw:
                add(self.lw[b])
        for b in w:
            if b in self.lw:
                add(self.lw[b])
            for sk, v in self.rd.get(b, {}).items():
                add((sk, v))
        for t in extra:
            add(t)
        eng = self.eng[e]
        for sk, v in deps.items():
            eng.wait_ge(self.sem[sk], v)
            self.seen[e][sk] = v
            self.n_ins += 1

    def _commit(self, tok, r, w):
        for b in w:
            self.lw[b] = tok
            self.rd[b] = {}
        for b in r:
            d = self.rd.setdefault(b, {})
            if d.get(tok[0], 0) < tok[1]:
                d[tok[0]] = tok[1]

    def op(self, e, fn, r=(), w=()):
        self._deps(e, r, w)
        ins = fn()
        self.cnt[e] += 1
        ins.then_inc(self.sem[e], 1)
        self.n_ins += 1
        tok = (e, self.cnt[e])
        self._commit(tok, r, w)
        return tok

    def dma(self, e, fn, r=(), w=(), is_out=False):
        lo_, hi_ = self.rr_rng[e]
        idx = self.rr[e]
        self.rr[e] = lo_ + (idx + 1 - lo_) % (hi_ - lo_)
        sk = ("d", idx)
        extra = [(sk, self.dval[idx])] if self.dval[idx] > 0 else []
        self._deps(e, r, w, extra)
        ins = fn()
        self.dval[idx] += 16
        ins.then_inc(self.sem[sk], 16)
        self.n_ins += 1
        tok = (sk, self.dval[idx])
        self._commit(tok, r, w)
        if is_out:
            self.out_tokens.append(tok)
        return tok

    def barrier(self):
        cur = {e: self.cnt[e] for e in self.eng}
        for i in range(ND_SEM):
            cur[("d", i)] = self.dval[i]
        for e in self.eng:
            for sk, v in cur.items():
                if v > self.seen[e].get(sk, 0):
                    self.eng[e].wait_ge(self.sem[sk], v)
                    self.seen[e][sk] = v
                    self.n_ins += 1

    def finish(self):
        e = "sp"
        for sk, v in self.out_tokens:
            if self.seen[e].get(sk, 0) < v:
                self.eng[e].wait_ge(self.sem[sk], v)
                self.seen[e][sk] = v


def build(NTL=32, n_layers=2, dbg=False, peer_slots=128):
    T = NTL * 128
    TT = T + CTX
    NTILES = NTL + 2
    NGL = NTL // 4
    L = TT + 64
    groups = [(g, list(range(4 * g, 4 * g + 4))) for g in range(NGL)] + [(NGL, [NTL, NTL + 1])]

    def ppos(tok):
        return tok + 16 if tok < T else tok + 48

    nc = bass.Bass("TRN2", target_bir_lowering=False)

    def din(name, shape, dt=F32):
        return nc.dram_tensor(name, list(shape), dt, kind="ExternalInput").ap()

    def dscr(name, shape, dt=F32):
        kind = "ExternalOutput" if (dbg and name == "xs") else "Internal"
        return nc.dram_tensor(name, list(shape), dt, kind=kind).ap()

    x_in = din("x", [T, D])
    ctx_in = din("ctx", [CTX, D])
    cvec = din("cvec", [128, 2, 8])
    w_mod = din("w_mod", [2, D, 6 * D])
    b_mod_r = din("b_mod_r", [2, 128, 6 * D])
    g_mix_r = din("g_mix_r", [2, 128, D])
    g_ffn_r = din("g_ffn_r", [2, 128, D])
    g_fin_r = din("g_fin_r", [128, D])
    w1_d = din("w1", [2, D, 640])
    w2a_d = din("w2a", [2, D, 1536])
    w2g_d = din("w2g", [2, D, 3072])
    ropeC = din("ropeC", [128, TT])
    ropeS = din("ropeS", [128, TT])
    sink_r = din("sink_r", [2, 1, 2, 512])
    pwbd_d = din("pwbd", [2, 128, 2, 128])
    pscale_d = din("pscale", [2, 128, 2])
    rc_d = din("rc", [128, 2, TT])
    sguwT_d = din("sgu_wT", [2, 128, 4, 128])
    sgub_d = din("sgu_b_r", [2, 64, 4, 128])
    wbrA_d = din("wbrA", [2, 64, 8, D])
    wbrP_d = din("wbrP", [2, 128, 2, D])
    wbrS_d = din("wbrS", [2, 64, 4, D])
    wout_d = din("wout", [2, 128, 8, D])
    wqT_d = din("wqT", [2, 128, 16, D])
    subkT_d = din("subkT", [2, 128, 16, 128])
    peer_u = [din("peer_u%d" % l, [16384, D]) for l in range(2)]
    peer_v = [din("peer_v%d" % l, [16384, D]) for l in range(2)]
    mask_d = din("maskAB", [128, 2, 512])
    ident_d = din("ident", [128, 128])
    iota_d = din("iota16", [128, 16])
    y_out = nc.dram_tensor("y", [T, D], F32, kind="ExternalOutput").ap()

    xs = dscr("xs", [TT, D])
    hTd = dscr("hTd", [NGL + 1, 128, 8, 512], BF16)
    yTd = dscr("yTd", [NGL + 1, 64, 8, 512], BF16)
    ysTd = dscr("ysTd", [NGL + 1, 64, 4, 512], BF16)
    ypTd = dscr("ypTd", [NGL + 1, 128, 2, 512], BF16)
    uvb = [dscr("uvb%d" % l, [16384, 2 * D], BF16) for l in range(2)]

    with ExitStack() as es:
        P = Prog(nc, es)
        V, A, G, PE, SP = nc.vector, nc.scalar, nc.gpsimd, nc.tensor, nc.sync

        uid = [0]

        def sb(es_, name, shape, dt=F32):
            uid[0] += 1
            return es_.enter_context(nc.sbuf_tensor("sb%d_%s" % (uid[0], name), list(shape), dt))

        PS = es.enter_context(nc.psum_tensor("ps", [128, 8, 512], F32))
        bank_ptr = [0]

        def bank(n=1):
            if n > 1 and bank_ptr[0] % n:
                bank_ptr[0] += n - bank_ptr[0] % n
            b = bank_ptr[0] % 8
            bank_ptr[0] = (bank_ptr[0] + n)
            return b

        identf = sb(es, "identf", [128, 128])
        identb = sb(es, "identb", [128, 128], BF16)
        maskb = sb(es, "maskb", [128, 2, 512], BF16)
        ones_b = sb(es, "ones_b", [128, 64], BF16)
        iota16 = sb(es, "iota16", [128, 16])
        screp = sb(es, "screp", [128, 2, 8, 128])
        with ExitStack() as e0:
            maskf = sb(e0, "maskf", [128, 2, 512])
            cv = sb(e0, "cv", [128, 2, 8])
            cs = sb(e0, "cs", [128, 2, 8])
            P.dma("sp", lambda: SP.dma_start(out=identf[:], in_=ident_d[:, :]), w=["identf"])
            P.dma("sp", lambda: SP.dma_start(out=maskf[:], in_=mask_d[:, :, :]), w=["maskf"])
            P.dma("sp", lambda: SP.dma_start(out=iota16[:], in_=iota_d[:, :]), w=["iota16"])
            P.dma("sp", lambda: SP.dma_start(out=cv[:], in_=cvec[:, :, :]), w=["cv"])
            P.dma("sp", lambda: SP.dma_start(out=xs[0:T, :], in_=x_in[:, :]), w=[("xs", t) for t in range(NTL)])
            P.dma("sp", lambda: SP.dma_start(out=xs[T:TT, :], in_=ctx_in[:, :]), w=[("xs", NTL), ("xs", NTL + 1)])
            P.op("dve", lambda: V.tensor_copy(out=identb[:], in_=identf[:]), r=["identf"], w=["identb"])
            P.op("dve", lambda: V.tensor_copy(out=maskb[:], in_=maskf[:]), r=["maskf"], w=["maskb"])
            P.op("dve", lambda: V.memset(ones_b[:], 1.0), w=["ones_b"])
            P.op("act", lambda: A.activation(out=cs[:], in_=cv[:], func=AF.Silu), r=["cv"], w=["cs"])
            P.op("dve", lambda: V.tensor_copy(out=screp[:], in_=cs[:].unsqueeze(3).broadcast_to([128, 2, 8, 128])),
                 r=["cs"], w=["screp"])
            P.barrier()

        def modulation(layer, which, mod, g_r):
            base = which * 3 * D
            with ExitStack() as em:
                wm = [sb(em, "wm%d" % i, [128, 8, 512]) for i in range(2)]
                bm = [sb(em, "bm%d" % i, [128, 512]) for i in range(2)]
                gs = [sb(em, "gs%d" % i, [128, 512]) for i in range(2)]
                tm = [sb(em, "tm%d" % i, [128, 512]) for i in range(2)]
                for ci in range(6):
                    kind, half = ci // 2, ci % 2
                    c0 = base + ci * 512
                    j = ci % 2
                    P.dma("sp", lambda: SP.dma_start(
                        out=wm[j][:], in_=w_mod[layer, :, c0:c0 + 512].rearrange("(k p) n -> p k n", p=128)),
                        w=[("wm", j)])
                    P.dma("sp", lambda: SP.dma_start(out=bm[j][:], in_=b_mod_r[layer, :, c0:c0 + 512]), w=[("bm", j)])
                    if kind == 1:
                        P.dma("sp", lambda: SP.dma_start(out=gs[j][:], in_=g_r[layer, :, half * 512:(half + 1) * 512]),
                              w=[("gs", j)])
                    for s in range(2):
                        b = bank()

                        def mm():
                            for k in range(8):
                                ins = PE.matmul(PS[:, b, :], lhsT=screp[:, s, k, :], rhs=wm[j][:, k, :],
                                                start=(k == 0), stop=(k == 7))
                            return ins
                        P.op("pe", mm, r=["screp", ("wm", j)], w=[("ps", b)])
                        dst_kind = {0: 1, 1: 0, 2: 2}[kind]
                        dst = mod[s][dst_kind][:, half * 512:(half + 1) * 512]
                        dname = ("mod", which, s, dst_kind, half)
                        if kind == 1:
                            P.op("dve", lambda: V.tensor_tensor(out=tm[s][:], in0=PS[:, b, :], in1=bm[j][:], op=ALU.add),
                                 r=[("ps", b), ("bm", j)], w=[("tm", s)])
                            P.op("dve", lambda: V.scalar_tensor_tensor(out=dst, in0=tm[s][:], scalar=1.0, in1=gs[j][:],
                                                                       op0=ALU.add, op1=ALU.mult),
                                 r=[("tm", s), ("gs", j)], w=[dname])
                        else:
                            P.op("dve", lambda: V.tensor_tensor(out=dst, in0=PS[:, b, :], in1=bm[j][:], op=ALU.add),
                                 r=[("ps", b), ("bm", j)], w=[dname])
                P.barrier()

        def modnames(which, s):
            return [("mod", which, s, k, h) for k in range(3) for h in range(2)]

        def load_cast(es_, name, dram_ap, shape, stage_bufs, dst=None, eng_cycle=("act", "pool")):
            raise NotImplementedError

        def norm_mod(xt, xname, Amod, Bmod, modr, junk, ss, hf_out, hb_out, hname, add_eng="pool"):
            P.op("dve", lambda: V.scalar_tensor_tensor(out=junk[:], in0=xt[:], scalar=1.0, in1=xt[:], op0=ALU.mult, op1=ALU.mult, accum_out=ss[:, 0:1]),
                 r=[xname], w=["junk", "ss0"])
            P.op("dve", lambda: V.tensor_scalar(out=ss[:, 1:2], in0=ss[:, 0:1], scalar1=1.0 / D, scalar2=EPS,
                                                op0=ALU.mult, op1=ALU.add), r=["ss0"], w=["ss1"])
            P.op("act", lambda: A.activation(out=ss[:, 2:3], in_=ss[:, 1:2], func=AF.Ln), r=["ss1"], w=["ss2"])
            P.op("act", lambda: A.activation(out=ss[:, 3:4], in_=ss[:, 2:3], func=AF.Exp, scale=-0.5), r=["ss2"], w=["ss3"])
            P.op("dve", lambda: V.scalar_tensor_tensor(out=junk[:], in0=xt[:], scalar=ss[:, 3:4], in1=Amod[:],
                                                       op0=ALU.mult, op1=ALU.mult),
                 r=[xname, "ss3"] + modr, w=["junk"])
            if hf_out is not None:
                P.op("dve", lambda: V.tensor_tensor(out=hf_out[:], in0=junk[:], in1=Bmod[:], op=ALU.add),
                     r=["junk"] + modr, w=[hname + "f"])
                P.op("act", lambda: A.copy(out=hb_out[:], in_=hf_out[:]), r=[hname + "f"], w=[hname])
            elif add_eng == "dve":
                P.op("dve", lambda: V.tensor_tensor(out=hb_out[:], in0=junk[:], in1=Bmod[:], op=ALU.add),
                     r=["junk"] + modr, w=[hname])
            else:
                P.op("pool", lambda: G.tensor_tensor(out=hb_out[:], in0=junk[:], in1=Bmod[:], op=ALU.add),
                     r=["junk"] + modr, w=[hname])

        def transpose8(hb, hname, dst_ap, dname, evac="act", b=None):
            if b is None:
                b = bank()
            psb = PS[:, b, :].bitcast(BF16)

            def tr():
                for k in range(8):
                    ins = PE.transpose(out=psb[:, k * 128:(k + 1) * 128], in_=hb[:, k * 128:(k + 1) * 128],
                                       identity=identb[:])
                return ins
            P.op("pe", tr, r=[hname, "identb"], w=[("ps", b)])
            src = psb.rearrange("p (k t) -> p k t", k=8)
            if evac == "act":
                P.op("act", lambda: A.copy(out=dst_ap, in_=src), r=[("ps", b)], w=[dname])
            else:
                P.op("dve", lambda: V.tensor_copy(out=dst_ap, in_=src), r=[("ps", b)], w=[dname])

        def cast_load(dst_tile_ap, dname, src_ap, stg, sname, ceng):
            P.dma("sp", lambda: SP.dma_start(out=stg, in_=src_ap), w=[sname])
            if ceng == "act":
                P.op("act", lambda: A.copy(out=dst_tile_ap, in_=stg), r=[sname], w=[dname])
            elif ceng == "pool":
                P.op("pool", lambda: G.tensor_copy(out=dst_tile_ap, in_=stg), r=[sname], w=[dname])
            else:
                P.op("dve", lambda: V.tensor_copy(out=dst_tile_ap, in_=stg), r=[sname], w=[dname])

        NQ = 16

        def tabnames(l):
            return [("uvb", l, c0, q) for c0 in (0, D) for q in range(NQ)]

        def conv_gen(l):
            rpq = 16384 // NQ
            for q in range(NQ):
                for (src_t, c0) in ((peer_u[l], 0), (peer_v[l], D)):
                    rows = slice(q * rpq, (q + 1) * rpq)
                    P.dma("pool", lambda: G.dma_start(out=uvb[l][rows, c0:c0 + D], in_=src_t[rows, :]), w=[("uvb", l, c0, q)])
                    yield
        convs = [conv_gen(l) for l in range(n_layers)]

        def pump(l, k):
            if l < n_layers:
                for _ in range(k):
                    next(convs[l], None)

        for layer in range(n_layers):
            last = layer == n_layers - 1
            with ExitStack() as eL:
                modm = [[sb(eL, "modm%d%d" % (s, k), [128, D]) for k in range(3)] for s in range(2)]
                eKV = ExitStack()
                kT = sb(eKV, "kT", [128, TT], BF16)
                vtok = sb(eKV, "vtok", [128, NTILES, 128], BF16)
                dT = sb(eKV, "dT", [128, 2, TT], BF16)
                sinkrow = sb(eKV, "sinkrow", [1, 2, 512], BF16)
                with ExitStack() as e1:
                    sk_f = sb(e1, "sk_f", [1, 2, 512])
                    sk_e = sb(e1, "sk_e", [1, 2, 512])
                    P.dma("sp", lambda: SP.dma_start(out=sk_f[:], in_=sink_r[layer, :, :, :]), w=["sk_f"])
                    P.op("act", lambda: A.activation(out=sk_e[:], in_=sk_f[:], func=AF.Exp), r=["sk_f"], w=["sk_e"])
                    P.op("act", lambda: A.copy(out=sinkrow[:], in_=sk_e[:]), r=["sk_e"], w=["sinkrow"])
                    P.barrier()
                modulation(layer, 0, modm, g_mix_r)

                with ExitStack() as e1:
                    zpad = sb(e1, "zpad", [128, 2, L])
                    e1w = ExitStack()
                    w1 = sb(e1w, "w1", [128, 8, 640], BF16)
                    stg = [sb(e1w, "stg%d" % i, [128, 2, 640]) for i in range(2)]
                    xt_ = [sb(e1w, "xt%d" % i, [128, D]) for i in range(2)]
                    junk = sb(e1w, "junk", [128, D])
                    ss = sb(e1w, "ss", [128, 4])
                    hb_ = [sb(e1w, "hb%d" % i, [128, D], BF16) for i in range(2)]
                    hTg = [sb(e1w, "hTg%d" % i, [128, 8, 512], BF16) for i in range(2)]
                    cS = [sb(e1w, "cS%d" % i, [128, 2, 512]) for i in range(2)]
                    t1 = sb(e1w, "t1", [128, 512])
                    t2 = sb(e1w, "t2", [128, 512])
                    for i in range(4):
                        cast_load(w1[:, 2 * i:2 * i + 2, :], ("w1", i),
                                  w1_d[layer, 256 * i:256 * (i + 1), :].rearrange("(k p) n -> p k n", p=128),
                                  stg[i % 2][:], ("stg", i % 2), "act")
                    P.op("dve", lambda: V.memset(zpad[:], 0.0), w=["zpad"])
                    for gi, tiles in groups:
                        j = gi % 2
                        ng = len(tiles) * 128
                        s = 0 if tiles[0] < NTL else 1
                        tok0 = tiles[0] * 128
                        for ti, tile in enumerate(tiles):
                            jj = tile % 2
                            P.dma("sp", lambda: SP.dma_start(out=xt_[jj][:], in_=xs[tile * 128:(tile + 1) * 128, :]),
                                  r=[("xs", tile)], w=[("xt", jj)])
                            norm_mod(xt_[jj], ("xt", jj), modm[s][0], modm[s][1], modnames(0, s), junk, ss, None,
                                     hb_[jj], ("hb", jj), add_eng="dve")
                            transpose8(hb_[jj], ("hb", jj), hTg[j][:, :, ti * 128:(ti + 1) * 128], ("hTg", j, ti))
                        hr = [("hTg", j, ti) for ti in range(len(tiles))]
                        P.dma("sp", lambda: SP.dma_start(out=hTd[gi, :, :, 0:ng], in_=hTg[j][:, :, 0:ng]), r=hr, w=[("hTd", gi)])
                        P.dma("sp", lambda: SP.dma_start(out=cS[j][:, 0, 0:ng], in_=ropeC[:, tok0:tok0 + ng]), w=[("cS", j, 0)])
                        P.dma("sp", lambda: SP.dma_start(out=cS[j][:, 1, 0:ng], in_=ropeS[:, tok0:tok0 + ng]), w=[("cS", j, 1)])
                        if layer == 0:
                            pump(0, 2)
                        bk, bkr = bank(), bank()
                        for (bb, c0) in ((bk, 0), (bkr, 128)):
                            def mm(bb=bb, c0=c0):
                                for k in range(8):
                                    ins = PE.matmul(PS[:, bb, 0:ng], lhsT=w1[:, k, c0:c0 + 128], rhs=hTg[j][:, k, 0:ng],
                                                    start=(k == 0), stop=(k == 7))
                                return ins
                            P.op("pe", mm, r=hr + [("w1", i_) for i_ in range(4)], w=[("ps", bb)])
                        P.op("dve", lambda: V.tensor_tensor(out=t1[:, 0:ng], in0=PS[:, bkr, 0:ng], in1=cS[j][:, 1, 0:ng], op=ALU.mult),
                             r=[("ps", bkr), ("cS", j, 1)], w=["t1"])
                        P.op("dve", lambda: V.tensor_tensor(out=t2[:, 0:ng], in0=PS[:, bk, 0:ng], in1=cS[j][:, 0, 0:ng], op=ALU.mult),
                             r=[("ps", bk), ("cS", j, 0)], w=["t2"])
                        P.op("dve", lambda: V.tensor_tensor(out=kT[:, tok0:tok0 + ng], in0=t1[:, 0:ng], in1=t2[:, 0:ng], op=ALU.add),
                             r=["t1", "t2"], w=[("kT", gi)])
                        for c in range(2):
                            bz = bank()

                            def mm(bz=bz, c=c):
                                for k in range(8):
                                    ins = PE.matmul(PS[:, bz, 0:ng], lhsT=w1[:, k, 384 + c * 128:512 + c * 128],
                                                    rhs=hTg[j][:, k, 0:ng], start=(k == 0), stop=(k == 7))
                                return ins
                            P.op("pe", mm, r=hr + [("w1", i_) for i_ in range(4)], w=[("ps", bz)])
                            p0 = ppos(tok0)
                            P.op("act", lambda: A.copy(out=zpad[:, c, p0:p0 + ng], in_=PS[:, bz, 0:ng]),
                                 r=[("ps", bz)], w=["zpad"])
                        bv = bank()

                        def mm():
                            for ti in range(len(tiles)):
                                for k in range(8):
                                    ins = PE.matmul(PS[:, bv, ti * 128:(ti + 1) * 128], lhsT=hTg[j][:, k, ti * 128:(ti + 1) * 128],
                                                    rhs=w1[:, k, 256:384], start=(k == 0), stop=(k == 7))
                            return ins
                        P.op("pe", mm, r=hr + [("w1", i_) for i_ in range(4)], w=[("ps", bv)])
                        nt = len(tiles)
                        P.op("act", lambda: A.copy(out=vtok[:, tiles[0]:tiles[0] + nt, :],
                                                   in_=PS[:, bv, 0:ng].rearrange("p (a b) -> p a b", a=nt)),
                             r=[("ps", bv)], w=[("vtok", gi)])

                    P.barrier()
                    e1w.close()
                    PA = sb(e1, "PA", [128, L])
                    PB = sb(e1, "PB", [128, L])
                    P.op("dve", lambda: V.memset(PA[:], 0.0), w=["PA"])
                    P.op("dve", lambda: V.memset(PB[:], 0.0), w=["PB"])
                    rc = sb(e1, "rc", [128, 2, TT])
                    P.dma("sp", lambda: SP.dma_start(out=rc[:], in_=rc_d[:, :, :]), w=["rc"])
                    lo, hi = 8, L - 8

                    def shadd(dst, src, s1, s2, p0=0, p1=128):
                        P.op("dve", lambda: V.tensor_tensor(out=dst[p0:p1, lo:hi], in0=src[p0:p1, lo + s1:hi + s1],
                                                            in1=src[p0:p1, lo + s2:hi + s2], op=ALU.add),
                             r=["PA", "PB", "zpad"], w=["PA", "PB"])
                    segs = [(0, T, 16), (T, TT, 48)]

                    def dfin(src, c, p0, p1):
                        for (a, b_, off) in segs:
                            P.op("dve", lambda: V.tensor_tensor(out=src[p0:p1, a + off:b_ + off], in0=src[p0:p1, a + off:b_ + off],
                                                                in1=rc[p0:p1, c, a:b_], op=ALU.mult),
                                 r=["PA", "PB", "rc"], w=["PA", "PB"])
                            P.op("dve", lambda: V.tensor_tensor(out=dT[p0:p1, c, a:b_], in0=src[p0:p1, a + off:b_ + off],
                                                                in1=zpad[p0:p1, c, a + off:b_ + off], op=ALU.subtract),
                                 r=["PA", "PB", "zpad"], w=["dT"])
                    shadd(PA, zpad[:, 0, :], -1, 0)
                    shadd(PB, PA, -1, 1, 64, 128)
                    dfin(PA, 0, 0, 64)
                    dfin(PB, 0, 64, 128)
                    shadd(PA, zpad[:, 1, :], -1, 0)
                    shadd(PB, PA, -1, 1)
                    shadd(PA, PB, -2, 2)
                    shadd(PB, PA, -4, 4, 64, 128)
                    dfin(PA, 1, 0, 64)
                    dfin(PB, 1, 64, 128)
                    P.barrier()

                allk = [("kT", gi) for gi, _ in groups]
                allv = [("vtok", gi) for gi, _ in groups]
                m2_groups = groups if not last else groups[:NGL]
                with ExitStack() as e2:
                    w2a = sb(e2, "w2a", [128, 8, 1536], BF16)
                    stg = [sb(e2, "stgA%d" % i, [128, 1, 1536]) for i in range(2)]
                    pwbd = sb(e2, "pwbd", [128, 2, 128], BF16)
                    pwf = sb(e2, "pwf", [128, 2, 128])
                    pscale = sb(e2, "pscale", [128, 2])
                    sguwT = sb(e2, "sguwT", [128, 4, 128], BF16)
                    sguf = sb(e2, "sguf", [128, 4, 128])
                    sgub = sb(e2, "sgub", [64, 4, 128])
                    junk = sb(e2, "junkA", [128, D])
                    ss = sb(e2, "ssA", [128, 4])
                    hTg = [sb(e2, "hTgA%d" % i, [128, 8, 512], BF16) for i in range(2)]
                    cS = [sb(e2, "cSA%d" % i, [128, 2, 512]) for i in range(1)] * 2
                    t1 = sb(e2, "t1A", [128, 512])
                    t2 = sb(e2, "t2A", [128, 512])
                    qT = sb(e2, "qT", [128, 4, 512], BF16)
                    pT = [sb(e2, "pT%d" % i, [128, 512], BF16) for i in range(6)]
                    lnd = sb(e2, "lnd", [64, 512])
                    rec = sb(e2, "rec", [64, 512])
                    yT = sb(e2, "yT", [64, 8, 512], BF16)
                    uT = sb(e2, "uT", [64, 4, 512], BF16)
                    vg = sb(e2, "vg", [128, 256])
                    vn = sb(e2, "vn", [128, 256], BF16)
                    sv = sb(e2, "svA", [128, 4])
                    ysT = sb(e2, "ysT", [64, 4, 512], BF16)
                    ypT = sb(e2, "ypT", [128, 2, 512], BF16)
                    tsg = sb(e2, "tsg", [64, 512])
                    for i in range(8):
                        cast_load(w2a[:, i:i + 1, :], ("w2a", i),
                                  w2a_d[layer, 128 * i:128 * (i + 1), :].rearrange("(k p) n -> p k n", p=128),
                                  stg[i % 2][:], ("stgA", i % 2), "act" if i % 2 == 0 else "pool")
                    w2r = [("w2a", i) for i in range(8)]
                    cast_load(pwbd[:], "pwbd", pwbd_d[layer, :, :, :], pwf[:], "pwf", "dve")
                    cast_load(sguwT[:], "sguwT", sguwT_d[layer, :, :, :], sguf[:], "sguf", "dve")
                    P.dma("sp", lambda: SP.dma_start(out=pscale[:], in_=pscale_d[layer, :, :]), w=["pscale"])
                    P.dma("sp", lambda: SP.dma_start(out=sgub[:], in_=sgub_d[layer, :, :, :]), w=["sgub"])
                    pti = [0]
                    def load_h(gidx):
                        gi2, tiles2 = m2_groups[gidx]
                        ng2 = len(tiles2) * 128
                        j2 = gidx % 2
                        P.dma("sp", lambda: SP.dma_start(out=hTg[j2][:, :, 0:ng2], in_=hTd[gi2, :, :, 0:ng2]),
                              r=[("hTd", gi2)], w=[("hTgA", j2)])
                    load_h(0)
                    for gidx, (gi, tiles) in enumerate(m2_groups):
                        j = gidx % 2
                        nt = len(tiles)
                        ng = nt * 128
                        s = 0 if tiles[0] < NTL else 1
                        tok0 = tiles[0] * 128
                        if gidx + 1 < len(m2_groups):
                            load_h(gidx + 1)
                        hr = [("hTgA", j)]
                        P.dma("sp", lambda: SP.dma_start(out=cS[0][:, 0, 0:ng], in_=ropeC[:, tok0:tok0 + ng]), w=[("cS", 0, 0)])
                        P.dma("sp", lambda: SP.dma_start(out=cS[0][:, 1, 0:ng], in_=ropeS[:, tok0:tok0 + ng]), w=[("cS", 0, 1)])
                        for c in range(4):
                            bq, bqr = bank(), bank()
                            for (bb, c0) in ((bq, c * 128), (bqr, 512 + c * 128)):
                                def mm(bb=bb, c0=c0):
                                    for k in range(8):
                                        ins = PE.matmul(PS[:, bb, 0:ng], lhsT=w2a[:, k, c0:c0 + 128], rhs=hTg[j][:, k, 0:ng],
                                                        start=(k == 0), stop=(k == 7))
                                    return ins
                                P.op("pe", mm, r=hr + w2r, w=[("ps", bb)])
                            P.op("dve", lambda: V.tensor_tensor(out=t1[:, 0:ng], in0=PS[:, bqr, 0:ng], in1=cS[0][:, 1, 0:ng], op=ALU.mult),
                                 r=[("ps", bqr), ("cS", 0, 1)], w=["t1"])
                            P.op("dve", lambda: V.tensor_tensor(out=t2[:, 0:ng], in0=PS[:, bq, 0:ng], in1=cS[0][:, 0, 0:ng], op=ALU.mult),
                                 r=[("ps", bq), ("cS", 0, 0)], w=["t2"])
                            P.op("dve", lambda: V.tensor_tensor(out=qT[:, c, 0:ng], in0=t1[:, 0:ng], in1=t2[:, 0:ng], op=ALU.add),
                                 r=["t1", "t2"], w=[("qT", c)])
                        qr_ = [("qT", c) for c in range(4)]
                        if layer == 0:
                            pump(0, 2)
                            pump(1, 2)
                        for ti, tile in enumerate(tiles):
                            if tile < NTL:
                                keys = []
                                if tile - 1 >= 0:
                                    keys.append((tile - 1, 0))
                                keys.append((tile, None))
                                if tile + 1 < NTL:
                                    keys.append((tile + 1, 1))
                                keys += [(NTL, None), (NTL + 1, None)]
                            else:
                                keys = [(NTL, None), (NTL + 1, None)]
                            for hk in range(2):
                                h0, h1 = hk * 64, hk * 64 + 64
                                bnum, bden = bank(), bank()
                                sbanks = []
                                for (kt, mk) in keys:
                                    bs = bank()
                                    sbanks.append(bs)

                                    def mm(bs=bs, kt=kt, mk=mk):
                                        ins = PE.matmul(PS[:, bs, :], lhsT=kT[h0:h1, kt * 128:(kt + 1) * 128],
                                                        rhs=qT[h0:h1, :, ti * 128:(ti + 1) * 128],
                                                        start=True, stop=(mk is None))
                                        if mk is not None:
                                            ins = PE.matmul(PS[:, bs, :], lhsT=identb[:], rhs=maskb[:, mk, :], start=False, stop=True)
                                        return ins
                                    P.op("pe", mm, r=allk + qr_ + ["identb", "maskb"], w=[("ps", bs)])
                                pts = []
                                for bs in sbanks:
                                    pi = pti[0] % 6
                                    pti[0] += 1
                                    pts.append(pi)
                                    P.op("act", lambda bs=bs, pi=pi: A.activation(out=pT[pi][:], in_=PS[:, bs, :], func=AF.Exp, scale=0.125),
                                         r=[("ps", bs)], w=[("pT", pi)])
                                nk = len(keys)
                                for ki, ((kt, mk), pi) in enumerate(zip(keys, pts)):
                                    def mm(ki=ki, kt=kt, pi=pi):
                                        PE.matmul(PS[0:64, bnum, :], lhsT=vtok[:, kt, h0:h1], rhs=pT[pi][:],
                                                  start=(ki == 0), stop=(ki == nk - 1))
                                        ins = PE.matmul(PS[0:64, bden, :], lhsT=ones_b[:, 0:64], rhs=pT[pi][:],
                                                        start=(ki == 0), stop=False)
                                        if ki == nk - 1:
                                            ins = PE.matmul(PS[0:64, bden, :], lhsT=ones_b[0:1, 0:64], rhs=sinkrow[0:1, hk, :],
                                                            start=False, stop=True)
                                        return ins
                                    P.op("pe", mm, r=allv + [("pT", pi), "ones_b", "sinkrow"], w=[("ps", bnum), ("ps", bden)])
                                P.op("act", lambda: A.activation(out=lnd[:], in_=PS[0:64, bden, :], func=AF.Ln),
                                     r=[("ps", bden)], w=["lnd"])
                                P.op("act", lambda: A.activation(out=rec[:], in_=lnd[:], func=AF.Exp, scale=-1.0),
                                     r=["lnd"], w=["rec"])
                                P.op("dve", lambda: V.tensor_tensor(
                                    out=yT[:, hk * 4:hk * 4 + 4, ti * 128:(ti + 1) * 128],
                                    in0=PS[0:64, bnum, :].rearrange("p (g q) -> p g q", g=4),
                                    in1=rec[:].rearrange("p (g q) -> p g q", g=4), op=ALU.mult),
                                    r=[("ps", bnum), "rec"], w=[("yT", ti, hk)])
                        for h4 in range(4):
                            bu = bank()

                            def mm():
                                for k in range(8):
                                    ins = PE.matmul(PS[0:64, bu, 0:ng], lhsT=w2a[:, k, 1024 + h4 * 64:1088 + h4 * 64],
                                                    rhs=hTg[j][:, k, 0:ng], start=(k == 0), stop=(k == 7))
                                return ins
                            P.op("pe", mm, r=hr + w2r, w=[("ps", bu)])
                            P.op("act", lambda: A.activation(out=uT[:, h4, 0:ng], in_=PS[0:64, bu, 0:ng], func=AF.Gelu_apprx_tanh),
                                 r=[("ps", bu)], w=[("uT", h4)])
                        for ti, tile in enumerate(tiles):
                            bvs = bank()

                            def mm():
                                for k in range(8):
                                    ins = PE.matmul(PS[:, bvs, 0:256], lhsT=hTg[j][:, k, ti * 128:(ti + 1) * 128],
                                                    rhs=w2a[:, k, 1280:1536], start=(k == 0), stop=(k == 7))
                                return ins
                            P.op("pe", mm, r=hr + w2r, w=[("ps", bvs)])
                            P.op("act", lambda: A.activation(out=vg[:], in_=PS[:, bvs, 0:256], func=AF.Gelu_apprx_tanh),
                                 r=[("ps", bvs)], w=["vg"])
                            P.op("dve", lambda: V.scalar_tensor_tensor(out=junk[:, 0:256], in0=vg[:], scalar=1.0, in1=vg[:],
                                                                        op0=ALU.mult, op1=ALU.mult, accum_out=sv[:, 0:1]),
                                 r=["vg"], w=["junk", "sv0"])
                            P.op("dve", lambda: V.tensor_scalar(out=sv[:, 1:2], in0=sv[:, 0:1], scalar1=1.0 / 256, scalar2=EPS,
                                                                op0=ALU.mult, op1=ALU.add), r=["sv0"], w=["sv1"])
                            P.op("act", lambda: A.activation(out=sv[:, 2:3], in_=sv[:, 1:2], func=AF.Ln), r=["sv1"], w=["sv2"])
                            P.op("act", lambda: A.activation(out=sv[:, 3:4], in_=sv[:, 2:3], func=AF.Exp, scale=-0.5), r=["sv2"], w=["sv3"])
                            P.op("dve", lambda: V.tensor_scalar(out=vn[:], in0=vg[:], scalar1=sv[:, 3:4], scalar2=None, op0=ALU.mult),
                                 r=["vg", "sv3"], w=["vn"])
                            bm_ = bank()

                            def mm():
                                for h4 in range(4):
                                    ins = PE.matmul(PS[0:64, bm_, h4 * 128:(h4 + 1) * 128], lhsT=vn[:, h4 * 64:(h4 + 1) * 64],
                                                    rhs=sguwT[:, h4, :], start=True, stop=True)
                                return ins
                            P.op("pe", mm, r=["vn", "sguwT"], w=[("ps", bm_)])
                            P.op("dve", lambda: V.tensor_tensor(out=tsg[:], in0=PS[0:64, bm_, :],
                                                                in1=sgub[:].rearrange("p a b -> p (a b)"), op=ALU.add),
                                 r=[("ps", bm_), "sgub"], w=["tsg"])
                            P.op("dve", lambda: V.tensor_tensor(out=ysT[:, :, ti * 128:(ti + 1) * 128],
                                                                in0=tsg[:].rearrange("p (a b) -> p a b", a=4),
                                                                in1=uT[:, :, ti * 128:(ti + 1) * 128], op=ALU.mult),
                                 r=["tsg"] + [("uT", h4) for h4 in range(4)], w=[("ysT", ti)])
                        for c in range(2):
                            bp = bank()
                            P.op("pe", lambda: PE.matmul(PS[:, bp, 0:ng], lhsT=pwbd[:, c, :], rhs=dT[:, c, tok0:tok0 + ng],
                                                         start=True, stop=True), r=["pwbd", "dT"], w=[("ps", bp)])
                            P.op("dve", lambda: V.tensor_scalar(out=ypT[:, c, 0:ng], in0=PS[:, bp, 0:ng], scalar1=pscale[:, c:c + 1],
                                                                scalar2=None, op0=ALU.mult),
                                 r=[("ps", bp), "pscale"], w=[("ypT", c)])
                        yr = [("yT", ti, hk) for ti in range(nt) for hk in range(2)]
                        P.dma("sp", lambda: SP.dma_start(out=yTd[gi, :, :, 0:ng], in_=yT[:, :, 0:ng]), r=yr, w=[("yTd", gi)])
                        P.dma("sp", lambda: SP.dma_start(out=ysTd[gi, :, :, 0:ng], in_=ysT[:, :, 0:ng]),
                              r=[("ysT", ti) for ti in range(nt)], w=[("ysTd", gi)])
                        P.dma("sp", lambda: SP.dma_start(out=ypTd[gi, :, :, 0:ng], in_=ypT[:, :, 0:ng]),
                              r=[("ypT", 0), ("ypT", 1)], w=[("ypTd", gi)])
                    P.barrier()

                P.barrier()
                eKV.close()
                with ExitStack() as e3:
                    w2g = sb(e3, "w2g", [128, 8, 3072], BF16)
                    wbrA = sb(e3, "wbrA", [64, 8, D], BF16)
                    wbrP = sb(e3, "wbrP", [128, 2, D], BF16)
                    wbrS = sb(e3, "wbrS", [64, 4, D], BF16)
                    wout = sb(e3, "wout", [128, 8, D], BF16)
                    stg = [sb(e3, "stgB%d" % i, [128, 2048]) for i in range(2)]
                    hTl = [sb(e3, "hTl%d" % i, [128, 8, 512], BF16) for i in range(2)]
                    yTl = sb(e3, "yTl", [64, 8, 512], BF16)
                    ysTl = sb(e3, "ysTl", [64, 4, 512], BF16)
                    ypTl = sb(e3, "ypTl", [128, 2, 512], BF16)
                    sig = [sb(e3, "sig%d" % i, [128, 512], BF16) for i in range(3)]
                    tt = [sb(e3, "tt%d" % i, [128, 512]) for i in range(4)]
                    mT = sb(e3, "mT", [128, 8, 512], BF16)
                    xt_ = [sb(e3, "xtB%d" % i, [128, D]) for i in range(1)] * 2
                    xo_ = [sb(e3, "xoB%d" % i, [128, D]) for i in range(1)] * 2
                    ci = [0]

                    def cl(dst, dname, src, width):
                        i = ci[0] % 2
                        ci[0] += 1
                        cast_load(dst, dname, src, stg[i][:, 0:width] if True else None, ("stgB", i),
                                  ("act", "pool", "dve")[ci[0] % 3])
                    for k in range(8):
                        for hh in range(2):
                            cl(w2g[:, k, hh * 1536:(hh + 1) * 1536], ("w2g", k, hh),
                               w2g_d[layer, k * 128:(k + 1) * 128, hh * 1536:(hh + 1) * 1536], 1536)
                    for h in range(0, 8, 2):
                        i = ci[0] % 2
                        ci[0] += 1
                        cast_load(wbrA[:, h:h + 2, :], ("wbrA", h), wbrA_d[layer, :, h:h + 2, :],
                                  stg[i][0:64, 0:2048].rearrange("p (a b) -> p a b", a=2), ("stgB", i), "act")
                    i = ci[0] % 2
                    ci[0] += 1
                    cast_load(wbrP[:], "wbrP", wbrP_d[layer, :, :, :], stg[i][:, 0:2048].rearrange("p (a b) -> p a b", a=2),
                              ("stgB", i), "pool")
                    for h in range(0, 4, 2):
                        i = ci[0] % 2
                        ci[0] += 1
                        cast_load(wbrS[:, h:h + 2, :], ("wbrS", h), wbrS_d[layer, :, h:h + 2, :],
                                  stg[i][0:64, 0:2048].rearrange("p (a b) -> p a b", a=2), ("stgB", i), "dve")
                    for k in range(0, 8, 2):
                        i = ci[0] % 2
                        ci[0] += 1
                        cast_load(wout[:, k:k + 2, :], ("wout", k), wout_d[layer, :, k:k + 2, :],
                                  stg[i][:, 0:2048].rearrange("p (a b) -> p a b", a=2), ("stgB", i), "act")
                    wr_g = [("w2g", k, hh) for k in range(8) for hh in range(2)]
                    wr_b = [("wbrA", h) for h in range(0, 8, 2)] + ["wbrP"] + [("wbrS", h) for h in range(0, 4, 2)]
                    wr_o = [("wout", k) for k in range(0, 8, 2)]
                    def load_b(gidx):
                        gi2, tiles2 = m2_groups[gidx]
                        n2 = len(tiles2) * 128
                        j2 = gidx % 2
                        P.dma("sp", lambda: SP.dma_start(out=hTl[j2][:, :, 0:n2], in_=hTd[gi2, :, :, 0:n2]), r=[("hTd", gi2)], w=[("hTl", j2)])
                    load_b(0)
                    for gidx, (gi, tiles) in enumerate(m2_groups):
                        j = gidx % 2
                        nt = len(tiles)
                        ng = nt * 128
                        s = 0 if tiles[0] < NTL else 1
                        if layer == 0:
                            pump(1, 2)
                        if gidx + 1 < len(m2_groups):
                            load_b(gidx + 1)
                        P.dma("sp", lambda: SP.dma_start(out=yTl[:, :, 0:ng], in_=yTd[gi, :, :, 0:ng]), r=[("yTd", gi)], w=["yTl"])
                        P.dma("sp", lambda: SP.dma_start(out=ysTl[:, :, 0:ng], in_=ysTd[gi, :, :, 0:ng]), r=[("ysTd", gi)], w=["ysTl"])
                        P.dma("sp", lambda: SP.dma_start(out=ypTl[:, :, 0:ng], in_=ypTd[gi, :, :, 0:ng]), r=[("ypTd", gi)], w=["ypTl"])
                        for m in range(8):
                            mc = slice(m * 128, (m + 1) * 128)
                            bgs = []
                            for i3 in range(3):
                                bg = bank()
                                bgs.append(bg)

                                def mmg(bg=bg, i3=i3):
                                    for k in range(8):
                                        ins = PE.matmul(PS[:, bg, 0:ng], lhsT=w2g[:, k, i3 * D + m * 128:i3 * D + (m + 1) * 128],
                                                        rhs=hTl[j][:, k, 0:ng], start=(k == 0), stop=(k == 7))
                                    return ins
                                P.op("pe", mmg, r=wr_g + [("hTl", j)], w=[("ps", bg)])
                                P.op("act", lambda bg=bg, i3=i3: A.activation(out=sig[i3][:, 0:ng], in_=PS[:, bg, 0:ng], func=AF.Sigmoid),
                                     r=[("ps", bg)], w=[("sig", i3)])
                            bA, bP, bS = bank(), bank(), bank()

                            def mmA():
                                for h in range(8):
                                    ins = PE.matmul(PS[:, bA, 0:ng], lhsT=wbrA[:, h, mc], rhs=yTl[:, h, 0:ng], start=(h == 0), stop=(h == 7))
                                return ins

                            def mmP():
                                for c in range(2):
                                    ins = PE.matmul(PS[:, bP, 0:ng], lhsT=wbrP[:, c, mc], rhs=ypTl[:, c, 0:ng], start=(c == 0), stop=(c == 1))
                                return ins

                            def mmS():
                                for h in range(4):
                                    ins = PE.matmul(PS[:, bS, 0:ng], lhsT=wbrS[:, h, mc], rhs=ysTl[:, h, 0:ng], start=(h == 0), stop=(h == 3))
                                return ins
                            P.op("pe", mmA, r=wr_b + ["yTl"], w=[("ps", bA)])
                            P.op("pe", mmP, r=wr_b + ["ypTl"], w=[("ps", bP)])
                            P.op("pe", mmS, r=wr_b + ["ysTl"], w=[("ps", bS)])
                            for i3, bb in enumerate((bA, bP, bS)):
                                P.op("dve", lambda i3=i3, bb=bb: V.tensor_tensor(out=tt[i3][:, 0:ng], in0=PS[:, bb, 0:ng], in1=sig[i3][:, 0:ng], op=ALU.mult),
                                     r=[("ps", bb), ("sig", i3)], w=[("tt", i3)])
                            P.op("pool", lambda: G.tensor_tensor(out=tt[3][:, 0:ng], in0=tt[0][:, 0:ng], in1=tt[1][:, 0:ng], op=ALU.add),
                                 r=[("tt", 0), ("tt", 1)], w=[("tt", 3)])
                            P.op("pool", lambda: G.tensor_tensor(out=mT[:, m, 0:ng], in0=tt[3][:, 0:ng], in1=tt[2][:, 0:ng], op=ALU.add),
                                 r=[("tt", 3), ("tt", 2)], w=[("mT", m)])
                        mr = [("mT", m) for m in range(8)]
                        for ti, tile in enumerate(tiles):
                            jj = 0
                            P.dma("sp", lambda: SP.dma_start(out=xt_[jj][:], in_=xs[tile * 128:(tile + 1) * 128, :]),
                                  r=[("xs", tile)], w=[("xt", jj)])
                            for half in range(2):
                                hc = slice(half * 512, (half + 1) * 512)
                                bo = bank()

                                def mmo():
                                    for k in range(8):
                                        ins = PE.matmul(PS[:, bo, :], lhsT=mT[:, k, ti * 128:(ti + 1) * 128], rhs=wout[:, k, hc],
                                                        start=(k == 0), stop=(k == 7))
                                    return ins
                                P.op("pe", mmo, r=mr + wr_o, w=[("ps", bo)])
                                P.op("dve", lambda: V.tensor_tensor(out=tt[half][:], in0=PS[:, bo, :], in1=modm[s][2][:, hc], op=ALU.mult),
                                     r=[("ps", bo)] + modnames(0, s), w=[("tt", half)])
                                P.op("pool", lambda: G.tensor_tensor(out=xo_[jj][:, hc], in0=tt[half][:], in1=xt_[jj][:, hc], op=ALU.add),
                                     r=[("tt", half), ("xt", jj)], w=[("xo", jj, half)])
                            P.dma("sp", lambda: SP.dma_start(out=xs[tile * 128:(tile + 1) * 128, :], in_=xo_[jj][:]),
                                  r=[("xo", jj, 0), ("xo", jj, 1)], w=[("xs", tile)])
                    P.barrier()
                P.barrier()

            for _ in convs[layer]:
                pass
            with ExitStack() as eP:
                modp = [[sb(eP, "modp%d%d" % (s, k), [128, D]) for k in range(3)] for s in range(2)]
                modulation(layer, 1, modp, g_ffn_r)
                wf = sb(eP, "wfold", [128, 8, 2048], BF16)
                with ExitStack() as ef:
                    wqT = sb(ef, "wqT", [128, 16, D])
                    subkT = sb(ef, "subkT", [128, 16, 128])
                    for c in range(0, 16, 4):
                        P.dma("sp", lambda: SP.dma_start(out=wqT[:, c:c + 4, :], in_=wqT_d[layer, :, c:c + 4, :]), w=[("wqT", c)])
                    P.dma("sp", lambda: SP.dma_start(out=subkT[:], in_=subkT_d[layer, :, :, :]), w=["subkT"])
                    for k in range(8):
                        for c4 in range(4):
                            b = bank()

                            def mm():
                                for cc in range(4):
                                    c = c4 * 4 + cc
                                    ins = PE.matmul(PS[:, b, cc * 128:(cc + 1) * 128], lhsT=wqT[:, c, k * 128:(k + 1) * 128],
                                                    rhs=subkT[:, c, :], start=True, stop=True)
                                return ins
                            P.op("pe", mm, r=[("wqT", c4 * 4), "subkT"], w=[("ps", b)])
                            P.op("act", lambda: A.copy(out=wf[:, k, c4 * 512:(c4 + 1) * 512], in_=PS[:, b, :]),
                                 r=[("ps", b)], w=[("wf", k, c4)])
                    P.barrier()
                wfr = [("wf", k, c4) for k in range(8) for c4 in range(4)]
                xt_ = [sb(eP, "xtP%d" % i, [128, D]) for i in range(3)]
                junk = sb(eP, "junkP", [128, D])
                ss = sb(eP, "ssP", [128, 4])
                h2b_ = [sb(eP, "h2b%d" % i, [128, D], BF16) for i in range(2)]
                h2T = sb(eP, "h2T", [128, 8, 128], BF16)
                sS = sb(eP, "sS", [128, 16, 128])
                sS2 = sb(eP, "sS2", [128, 16, 128])
                svt = sb(eP, "svt", [128, 8, 2, 16])
                sit = sb(eP, "sit", [128, 8, 2, 16], U32)
                sif = sb(eP, "sif", [128, 8, 2, 16])
                cand = sS[:].rearrange("p a b -> p (a b)").rearrange("p (h c) -> p h c", h=8)
                cand2 = sS2[:].rearrange("p a b -> p (a b)").rearrange("p (h c) -> p h c", h=8)
                ts = sb(eP, "ts", [128, 8, 16])
                tp = sb(eP, "tp", [128, 8, 16], U32)
                ta = sb(eP, "ta", [128, 8, 16], U32)
                tb_ = sb(eP, "tb", [128, 8, 16], U32)
                taf = sb(eP, "taf", [128, 8, 16])
                tbf = sb(eP, "tbf", [128, 8, 16])
                eq = sS2[:].rearrange("p a b -> p (a b)").rearrange("p (h a b) -> p h a b", h=8, a=16)
                If = sb(eP, "If", [128, 8, 16])
                Jf = sb(eP, "Jf", [128, 8, 16])
                ef_ = sb(eP, "ef", [128, 128])
                eu_ = [sb(eP, "eu%d" % i, [128, 128], U32) for i in range(2)]
                tsc = sb(eP, "tsc", [128, 8, 16])
                ex = sb(eP, "ex", [128, 8, 16])
                zz = sb(eP, "zz", [128, 8])
                rz = sb(eP, "rz", [128, 8])
                gate_ = [sb(eP, "gate%d" % i, [128, 128]) for i in range(2)]
                actv = sb(eP, "actv", [128, 128])
                gact = sb(eP, "gact", [128, 128])
                wgt = sb(eP, "wgt", [128, 128])
                guv = [sb(eP, "guv%d" % i, [128, 2 * D], BF16) for i in range(NGB)]
                prod = [sb(eP, "prod%d" % i, [128, D], BF16) for i in range(3)]
                junkb = sb(eP, "junkb", [128, D], BF16)
                GS = 4
                diag = [sb(eP, "diag%d" % i, [128, GS, 128], BF16) for i in range(3)]
                tmpo = [sb(eP, "tmpo%d" % i, [128, 512]) for i in range(2)]
                xo = sb(eP, "xoP", [128, D])
                if last:
                    gfin = sb(eP, "gfin", [128, D])
                    P.dma("sp", lambda: SP.dma_start(out=gfin[:], in_=g_fin_r[:, :]), w=["gfin"])
                cnt_u, cnt_v, cnt_p = [0], [0], [0]
                ptiles = list(range(NTILES)) if not last else list(range(NTL))
                pidx = {t_: i_ for i_, t_ in enumerate(ptiles)}

                def stage_A(tile):
                    jj = tile % 2
                    x3 = pidx[tile] % 3
                    s = 0 if tile < NTL else 1
                    P.dma("sp", lambda: SP.dma_start(out=xt_[x3][:], in_=xs[tile * 128:(tile + 1) * 128, :]),
                          r=[("xs", tile)], w=[("xt", x3)])
                    norm_mod(xt_[x3], ("xt", x3), modp[s][0], modp[s][1], modnames(1, s), junk, ss, None, h2b_[jj], ("h2", jj),
                             add_eng="dve")
                    transpose8(h2b_[jj], ("h2", jj), h2T[:], "h2T", b=0)

                    def mm():
                        for n in range(4):
                            for k in range(8):
                                ins = PE.matmul(PS[:, n, :], lhsT=h2T[:, k, :], rhs=wf[:, k, n * 512:(n + 1) * 512],
                                                start=(k == 0), stop=(k == 7))
                        return ins
                    psn = [("ps", n) for n in range(4)]
                    P.op("pe", mm, r=["h2T"] + wfr, w=psn)
                    P.op("act", lambda: A.copy(out=sS[:].rearrange("p a b -> p (a b)"),
                                               in_=PS[:, 0:4, :].rearrange("p a b -> p (a b)")), r=psn, w=["sS"])

                def stage_B(tile):
                    eu = eu_[tile % 2]
                    eun = ("eu", tile % 2)
                    gate = gate_[tile % 2]
                    gaten = ("gate", tile % 2)
                    for c in range(16):
                        h, p_ = c // 2, c % 2
                        P.op("dve", lambda: V.max(out=svt[:, h, p_, 0:8], in_=sS[:, c, :]), r=["sS"], w=[("sv", c, 0)])
                        yield
                    for c in range(16):
                        h, p_ = c // 2, c % 2
                        P.op("dve", lambda: V.max_index(out=sit[:, h, p_, 0:8], in_max=svt[:, h, p_, 0:8], in_values=sS[:, c, :]),
                             r=["sS", ("sv", c, 0)], w=[("si", c, 0)])
                        yield
                    for c in range(16):
                        h, p_ = c // 2, c % 2
                        P.op("dve", lambda: V.match_replace(out=sS2[:, c, :], in_to_replace=svt[:, h, p_, 0:8], in_values=sS[:, c, :],
                                                            imm_value=-1e30), r=["sS", ("sv", c, 0)], w=[("sS2", c)])
                        yield
                    for c in range(16):
                        h, p_ = c // 2, c % 2
                        P.op("dve", lambda: V.max(out=svt[:, h, p_, 8:16], in_=sS2[:, c, :]), r=[("sS2", c)], w=[("sv", c, 1)])
                        yield
                    for c in range(16):
                        h, p_ = c // 2, c % 2
                        P.op("dve", lambda: V.max_index(out=sit[:, h, p_, 8:16], in_max=svt[:, h, p_, 8:16], in_values=sS2[:, c, :]),
                             r=[("sS2", c), ("sv", c, 1)], w=[("si", c, 1)])
                        yield
                    svr = [("sv", c, q) for c in range(16) for q in range(2)]
                    sir = [("si", c, q) for c in range(16) for q in range(2)]
                    s2r = [("sS2", c) for c in range(16)]
                    P.op("dve", lambda: V.tensor_copy(out=sif[:], in_=sit[:]), r=sir, w=["sif"])
                    yield
                    P.op("dve", lambda: V.tensor_tensor(
                        out=cand.rearrange("p h (a b) -> p h a b", a=16),
                        in0=svt[:, :, 0, :].unsqueeze(3).broadcast_to([128, 8, 16, 16]),
                        in1=svt[:, :, 1, :].unsqueeze(2).broadcast_to([128, 8, 16, 16]), op=ALU.add), r=svr, w=["sS"])
                    yield
                    for h in range(8):
                        P.op("dve", lambda: V.max(out=ts[:, h, 0:8], in_=cand[:, h, :]), r=["sS"], w=[("ts", h, 0)])
                        yield
                    for h in range(8):
                        P.op("dve", lambda: V.max_index(out=tp[:, h, 0:8], in_max=ts[:, h, 0:8], in_values=cand[:, h, :]),
                             r=["sS", ("ts", h, 0)], w=[("tp", h, 0)])
                        yield
                    for h in range(8):
                        P.op("dve", lambda: V.match_replace(out=cand2[:, h, :], in_to_replace=ts[:, h, 0:8], in_values=cand[:, h, :],
                                                            imm_value=-1e30), r=["sS", ("ts", h, 0)] + s2r, w=[("cand2", h)])
                        yield
                    for h in range(8):
                        P.op("dve", lambda: V.max(out=ts[:, h, 8:16], in_=cand2[:, h, :]), r=[("cand2", h)], w=[("ts", h, 1)])
                        yield
                    for h in range(8):
                        P.op("dve", lambda: V.max_index(out=tp[:, h, 8:16], in_max=ts[:, h, 8:16], in_values=cand2[:, h, :]),
                             r=[("cand2", h), ("ts", h, 1)], w=[("tp", h, 1)])
                        yield
                    tsr = [("ts", h, q) for h in range(8) for q in range(2)]
                    tpr = [("tp", h, q) for h in range(8) for q in range(2)]
                    c2r = [("cand2", h) for h in range(8)]
                    P.op("dve", lambda: V.tensor_scalar(out=ta[:], in0=tp[:], scalar1=4, scalar2=None, op0=ALU.logical_shift_right),
                         r=tpr, w=["ta"])
                    yield
                    P.op("dve", lambda: V.tensor_scalar(out=tb_[:], in0=tp[:], scalar1=15, scalar2=None, op0=ALU.bitwise_and),
                         r=tpr, w=["tb"])
                    yield
                    P.op("dve", lambda: V.tensor_copy(out=taf[:], in_=ta[:]), r=["ta"], w=["taf"])
                    yield
                    P.op("dve", lambda: V.tensor_copy(out=tbf[:], in_=tb_[:]), r=["tb"], w=["tbf"])
                    yield
                    io4 = iota16[:].unsqueeze(1).unsqueeze(1).broadcast_to([128, 8, 16, 16])
                    for (src, pp, dst, dn) in ((taf, 0, If, "If"), (tbf, 1, Jf, "Jf")):
                        P.op("dve", lambda: V.tensor_tensor(out=eq, in0=src[:].unsqueeze(3).broadcast_to([128, 8, 16, 16]),
                                                            in1=io4, op=ALU.is_equal), r=["taf", "tbf", "iota16"] + c2r + s2r, w=["eqb"])
                        yield
                        P.op("dve", lambda: V.tensor_tensor(out=eq, in0=eq,
                                                            in1=sif[:, :, pp, :].unsqueeze(2).broadcast_to([128, 8, 16, 16]),
                                                            op=ALU.mult), r=["eqb", "sif"], w=["eqb"])
                        yield
                        P.op("dve", lambda: V.tensor_reduce(out=dst[:], in_=eq, axis=AX.X, op=ALU.add), r=["eqb"], w=[dn])
                        yield
                    P.op("dve", lambda: V.scalar_tensor_tensor(out=ef_[:], in0=If[:].rearrange("p a b -> p (a b)"), scalar=128.0,
                                                               in1=Jf[:].rearrange("p a b -> p (a b)"), op0=ALU.mult, op1=ALU.add),
                         r=["If", "Jf"], w=["ef"])
                    yield
                    P.op("dve", lambda: V.tensor_copy(out=eu[:], in_=ef_[:]), r=["ef"], w=[eun])
                    yield
                    P.op("dve", lambda: V.tensor_tensor(out=tsc[:], in0=ts[:], in1=ts[:, :, 0:1].broadcast_to([128, 8, 16]), op=ALU.subtract),
                         r=tsr, w=["tsc"])
                    yield
                    P.op("act", lambda: A.activation(out=ex[:], in_=tsc[:], func=AF.Exp), r=["tsc"], w=["ex"])
                    yield
                    P.op("dve", lambda: V.tensor_reduce(out=zz[:], in_=ex[:], axis=AX.X, op=ALU.add), r=["ex"], w=["zz"])
                    yield
                    P.op("dve", lambda: V.reciprocal(out=rz[:], in_=zz[:]), r=["zz"], w=["rz"])
                    yield
                    P.op("dve", lambda: V.tensor_tensor(out=gate[:].rearrange("p (a b) -> p a b", a=8), in0=ex[:],
                                                        in1=rz[:].unsqueeze(2).broadcast_to([128, 8, 16]), op=ALU.mult),
                         r=["ex", "rz"], w=[gaten])
                    yield

                def stage_CD(tile, genB, hook=None):
                    jj = tile % 2
                    eu = eu_[jj]
                    eun = ("eu", jj)
                    gate = gate_[jj]
                    h2b = h2b_[jj]
                    ngrp = peer_slots // GS
                    ab = 4 + 2 * (pidx[tile] % 2)
                    bis_of = {}
                    di_of = {}

                    def fin_act(g):
                        gs = slice(g * GS, (g + 1) * GS)
                        P.op("act", lambda: A.activation(out=gact[:, gs], in_=actv[:, gs], func=AF.Gelu_apprx_tanh),
                             r=[("actv", g * GS + q) for q in range(GS)], w=[("gact", g)])

                    def fin_rest(g):
                        gs = slice(g * GS, (g + 1) * GS)
                        di = cnt_v[0] % 3
                        cnt_v[0] += 1
                        P.op("dve", lambda: V.tensor_tensor(out=wgt[:, gs], in0=gate[:, gs], in1=gact[:, gs], op=ALU.mult),
                             r=[("gate", jj), ("gact", g)], w=[("wgt", g)])
                        P.op("dve", lambda: V.tensor_tensor(
                            out=diag[di][:],
                            in0=identb[:].unsqueeze(1).broadcast_to([128, GS, 128]),
                            in1=wgt[:, gs].unsqueeze(2).broadcast_to([128, GS, 128]), op=ALU.mult),
                            r=["identb", ("wgt", g)], w=[("diag", di)])
                        for q in range(GS):
                            sl = g * GS + q
                            bi = bis_of[g][q]

                            def mm():
                                for half in range(2):
                                    ins = PE.matmul(PS[:, ab + half, :], lhsT=diag[di][:, q, :],
                                                    rhs=guv[bi][:, D + half * 512:D + (half + 1) * 512],
                                                    start=(sl == 0), stop=(sl == peer_slots - 1))
                                return ins
                            P.op("pe", mm, r=[("guv", bi), ("diag", di)], w=[("ps", ab), ("ps", ab + 1)])

                    for g in range(ngrp):
                        if g >= 1:
                            fin_act(g - 1)
                        bis = []
                        for q in range(GS):
                            sl = g * GS + q
                            bi = cnt_u[0] % NGB
                            cnt_u[0] += 1
                            bis.append(bi)
                            pi = cnt_p[0] % 3
                            cnt_p[0] += 1
                            P.dma("pool", lambda: G.indirect_dma_start(
                                out=guv[bi][:], out_offset=None, in_=uvb[layer][:, :],
                                in_offset=bass.IndirectOffsetOnAxis(ap=eu[:, sl:sl + 1], axis=0)), r=[eun] + tabnames(layer), w=[("guv", bi)])
                            P.op("dve", lambda: V.tensor_tensor(out=prod[pi][:], in0=guv[bi][:, 0:D], in1=h2b[:], op=ALU.mult),
                                 r=[("guv", bi), ("h2", jj)], w=[("prod", pi)])
                            P.op("act", lambda: A.activation(out=junkb[:], in_=prod[pi][:], func=AF.Copy, accum_out=actv[:, sl:sl + 1]),
                                 r=[("prod", pi)], w=["junkb", ("actv", sl)])
                            if genB is not None:
                                for _ in range(2):
                                    next(genB, None)
                        bis_of[g] = bis
                        if g >= 1:
                            fin_rest(g - 1)
                        if g == 2 and hook is not None:
                            hook()
                    fin_act(ngrp - 1)
                    fin_rest(ngrp - 1)
                    if genB is not None:
                        for _ in genB:
                            pass

                def stage_E(tile):
                    jj = pidx[tile] % 3
                    ab = 4 + 2 * (pidx[tile] % 2)
                    s = 0 if tile < NTL else 1
                    for half in range(2):
                        hc = slice(half * 512, (half + 1) * 512)
                        P.op("dve", lambda: V.tensor_tensor(out=tmpo[half][:], in0=PS[:, ab + half, :], in1=modp[s][2][:, hc], op=ALU.mult),
                             r=[("ps", ab + half)] + modnames(1, s), w=[("tmpo", half)])
                        P.op("dve", lambda: V.tensor_tensor(out=xo[:, hc], in0=tmpo[half][:], in1=xt_[jj][:, hc], op=ALU.add),
                             r=[("tmpo", half), ("xt", jj)], w=[("xo", half)])
                    xor_ = [("xo", 0), ("xo", 1)]
                    if not last:
                        P.dma("sp", lambda: SP.dma_start(out=xs[tile * 128:(tile + 1) * 128, :], in_=xo[:]), r=xor_, w=[("xs", tile)],
                              is_out=dbg)
                    else:
                        P.op("dve", lambda: V.scalar_tensor_tensor(out=junk[:], in0=xo[:], scalar=1.0, in1=xo[:],
                                                                   op0=ALU.mult, op1=ALU.mult, accum_out=ss[:, 0:1]),
                             r=xor_, w=["junk", "ss0"])
                        P.op("dve", lambda: V.tensor_scalar(out=ss[:, 1:2], in0=ss[:, 0:1], scalar1=1.0 / D, scalar2=EPS,
                                                            op0=ALU.mult, op1=ALU.add), r=["ss0"], w=["ss1"])
                        P.op("act", lambda: A.activation(out=ss[:, 2:3], in_=ss[:, 1:2], func=AF.Ln), r=["ss1"], w=["ss2"])
                        P.op("act", lambda: A.activation(out=ss[:, 3:4], in_=ss[:, 2:3], func=AF.Exp, scale=-0.5), r=["ss2"], w=["ss3"])
                        P.op("dve", lambda: V.scalar_tensor_tensor(out=junk[:], in0=xo[:], scalar=ss[:, 3:4], in1=gfin[:],
                                                                   op0=ALU.mult, op1=ALU.mult),
                             r=xor_ + ["ss3", "gfin"], w=["junk"])
                        P.dma("sp", lambda: SP.dma_start(out=y_out[tile * 128:(tile + 1) * 128, :], in_=junk[:]), r=["junk"],
                              w=[("yout", tile)], is_out=True)

                stage_A(ptiles[0])
                for _ in stage_B(ptiles[0]):
                    pass
                for i_, tile in enumerate(ptiles):
                    nxt = ptiles[i_ + 1] if i_ + 1 < len(ptiles) else None
                    genB = None
                    if nxt is not None:
                        stage_A(nxt)
                        genB = stage_B(nxt)
                    prev = ptiles[i_ - 1] if i_ >= 1 else None
                    stage_CD(tile, genB, hook=(lambda: stage_E(prev)) if prev is not None else None)
                stage_E(ptiles[-1])
                P.barrier()
        P.finish()
        print("instructions:", P.n_ins)
    return nc


def prep_inputs(inp, NTL=32, n_cores=8):
    f = np.float32
    T = NTL * 128
    TT = T + CTX
    x = np.asarray(inp["x"], f)
    c = np.asarray(inp["c"], f)
    ctx = np.asarray(inp["ctx"], f)
    c_ctx = np.asarray(inp["c_ctx"], f)
    w_in = np.asarray(inp["w_in"], f)
    rp = np.concatenate([np.arange(16, 32), np.arange(0, 16), np.arange(48, 64), np.arange(32, 48)])
    d64 = np.arange(64)
    qcols = np.concatenate([np.concatenate([j * 64 + d64, (4 + j) * 64 + d64]) for j in range(4)])
    qrcols = np.concatenate([np.concatenate([j * 64 + rp, (4 + j) * 64 + rp]) for j in range(4)])
    kcols = 512 + np.arange(128)
    krcols = 512 + np.concatenate([rp, 64 + rp])
    vcols = 640 + np.arange(128)
    pcols = 768 + np.arange(256)
    ucols = 1024 + np.arange(256)
    vscols = 1280 + np.arange(256)
    w1 = np.ascontiguousarray(w_in[:, :, np.concatenate([kcols, krcols, vcols, pcols])])
    w2a = np.ascontiguousarray(w_in[:, :, np.concatenate([qcols, qrcols, ucols, vscols])])
    w2g = np.ascontiguousarray(w_in[:, :, 1536:4608])
    rows = T // 64
    row = np.repeat(np.arange(rows, dtype=f), 64)
    col = np.tile(np.arange(64, dtype=f), rows)
    inv = (np.float32(10000.0) ** (-np.arange(16, dtype=f) / np.float32(16))).astype(f)
    ar, ac = (row[:, None] * inv).astype(f), (col[:, None] * inv).astype(f)
    cos_d = np.concatenate([np.cos(ar), np.cos(ar), np.cos(ac), np.cos(ac)], axis=1).astype(f)
    sin_d = np.concatenate([-np.sin(ar), np.sin(ar), -np.sin(ac), np.sin(ac)], axis=1).astype(f)
    cos_d = np.concatenate([cos_d, np.ones((CTX, 64), f)], axis=0)
    sin_d = np.concatenate([sin_d, np.zeros((CTX, 64), f)], axis=0)
    ropeC = np.ascontiguousarray(np.concatenate([cos_d.T, cos_d.T], axis=0))
    ropeS = np.ascontiguousarray(np.concatenate([sin_d.T, sin_d.T], axis=0))
    sink = np.asarray(inp["attn_sink"], f)
    sink_r = np.ascontiguousarray(np.repeat(sink.reshape(2, 1, 2, 4, 1), 128, axis=4).reshape(2, 1, 2, 512))
    pool_w = np.asarray(inp["pool_w"], f)
    pwbd = np.zeros((2, 128, 2, 128), f)
    for g in range(4):
        o = (g % 2) * 64
        pwbd[:, o:o + 64, g // 2, o:o + 64] = pool_w[:, g]
    pscale = np.ascontiguousarray(np.asarray(inp["pool_scale"], f).reshape(2, 2, 128).transpose(0, 2, 1))
    rc = np.zeros((128, 2, TT), f)
    for g, size in enumerate(POOL_SIZES):
        for (a, l) in ((0, T), (T, CTX)):
            t = np.arange(l)
            lo = np.clip(t - size // 2, 0, l)
            hi = np.clip(t + size // 2, 0, l)
            o = (g % 2) * 64
            rc[o:o + 64, g // 2, a:a + l] = (1.0 / (hi - lo).astype(f))[None, :]
    sgu_wT = np.ascontiguousarray(np.asarray(inp["sgu_w"], f).transpose(0, 3, 1, 2))
    sgu_b_r = np.ascontiguousarray(np.broadcast_to(np.asarray(inp["sgu_b"], f)[:, None], (2, 64, 4, 128)))
    wbrA = np.ascontiguousarray(np.asarray(inp["w_br_attn"], f).reshape(2, 8, 64, D).transpose(0, 2, 1, 3))
    wbrP = np.ascontiguousarray(np.asarray(inp["w_br_pool"], f).reshape(2, 2, 128, D).transpose(0, 2, 1, 3))
    wbrS = np.ascontiguousarray(np.asarray(inp["w_br_sgu"], f).reshape(2, 4, 64, D).transpose(0, 2, 1, 3))
    wout = np.ascontiguousarray(np.asarray(inp["w_out"], f).reshape(2, 8, 128, D).transpose(0, 2, 1, 3))
    wqT = np.ascontiguousarray(np.asarray(inp["peer_wq"], f).reshape(2, D, 16, 128).transpose(0, 3, 2, 1))
    subkT = np.ascontiguousarray(np.asarray(inp["peer_subkeys"], f).reshape(2, 16, 128, 128).transpose(0, 3, 1, 2))
    jj, ii = np.meshgrid(np.arange(128), np.arange(128), indexing="ij")
    mA = np.where(jj >= ii, 0.0, NEG).astype(f)
    mB = np.where(jj <= ii, 0.0, NEG).astype(f)
    maskAB = np.ascontiguousarray(np.stack([np.tile(mA, (1, 4)), np.tile(mB, (1, 4))], axis=1))
    shared = {
        "w_mod": np.asarray(inp["w_mod"], f),
        "b_mod_r": np.ascontiguousarray(np.broadcast_to(np.asarray(inp["b_mod"], f)[:, None], (2, 128, 6 * D))),
        "g_mix_r": np.ascontiguousarray(np.broadcast_to(np.asarray(inp["g_mix"], f)[:, None], (2, 128, D))),
        "g_ffn_r": np.ascontiguousarray(np.broadcast_to(np.asarray(inp["g_ffn"], f)[:, None], (2, 128, D))),
        "g_fin_r": np.ascontiguousarray(np.broadcast_to(np.asarray(inp["g_final"], f)[None], (128, D))),
        "w1": w1, "w2a": w2a, "w2g": w2g, "ropeC": ropeC, "ropeS": ropeS, "sink_r": sink_r, "pwbd": pwbd,
        "pscale": pscale, "rc": rc, "sgu_wT": sgu_wT, "sgu_b_r": sgu_b_r, "wbrA": wbrA, "wbrP": wbrP, "wbrS": wbrS,
        "wout": wout, "wqT": wqT, "subkT": subkT,
        "peer_u0": np.ascontiguousarray(np.asarray(inp["peer_u"], f)[0]), "peer_u1": np.ascontiguousarray(np.asarray(inp["peer_u"], f)[1]),
        "peer_v0": np.ascontiguousarray(np.asarray(inp["peer_v"], f)[0]), "peer_v1": np.ascontiguousarray(np.asarray(inp["peer_v"], f)[1]),
        "maskAB": maskAB, "ident": np.eye(128, dtype=f),
        "iota16": np.ascontiguousarray(np.broadcast_to(np.arange(16, dtype=f)[None], (128, 16))),
    }
    maps = []
    for b in range(n_cores):
        m = dict(shared)
        m["x"] = np.ascontiguousarray(x[b, :T])
        m["ctx"] = np.ascontiguousarray(ctx[b])
        cv = np.stack([c[b].reshape(8, 128).T, c_ctx.reshape(8, 128).T], axis=1)
        m["cvec"] = np.ascontiguousarray(cv.astype(f))
        maps.append(m)
    return maps


def kernel(**inputs):
    n = 8
    nc = build(NTL=32, n_layers=2)
    maps = prep_inputs(inputs, NTL=32, n_cores=n)
    res = run_bass_kernel_spmd(nc, maps, core_ids=list(range(n)))
    return np.stack([np.asarray(r["y"], np.float32) for r in res.results], axis=0)
```

```python
import numpy as np
import concourse.bass as bass
import concourse.mybir as mybir
from concourse.bass_utils import run_bass_kernel_spmd
from contextlib import ExitStack

F32 = mybir.dt.float32
BF16 = mybir.dt.bfloat16
U32 = mybir.dt.uint32
AF = mybir.ActivationFunctionType
ALU = mybir.AluOpType
AX = mybir.AxisListType

D = 1024
CTX = 256
EPS = 1e-6
NEG = -30000.0
POOL_SIZES = (2, 4, 8, 16)
ND_SEM = 40
NGB = 16


class Prog:
    def __init__(self, nc, es):
        self.nc = nc
        self.eng = {"pe": nc.tensor, "dve": nc.vector, "act": nc.scalar, "pool": nc.gpsimd, "sp": nc.sync}
        self.sem = {}
        for e in self.eng:
            self.sem[e] = es.enter_context(nc.semaphore("s_" + e))
        for i in range(ND_SEM):
            self.sem[("d", i)] = es.enter_context(nc.semaphore("s_d%d" % i))
        self.cnt = {e: 0 for e in self.eng}
        self.seen = {e: {} for e in self.eng}
        self.lw = {}
        self.rd = {}
        self.dval = [0] * ND_SEM
        self.rr_rng = {"sp": (0, 24), "pool": (24, ND_SEM)}
        self.rr = {"sp": 0, "pool": 24}
        self.out_tokens = []
        self.n_ins = 0

    def _deps(self, e, r, w, extra=()):
        deps = {}

        def add(tok):
            sk, v = tok
            if sk == "pe" and e == "pe":
                return
            if self.seen[e].get(sk, 0) < v:
                if deps.get(sk, 0) < v:
                    deps[sk] = v

        for b in r:
            if b in self.lw:
                add(self.lw[b])
        for b in w:
            if b in self.lw:
                add(self.lw[b])
            for sk, v in self.rd.get(b, {}).items():
                add((sk, v))
        for t in extra:
            add(t)
        eng = self.eng[e]
        for sk, v in deps.items():
            eng.wait_ge(self.sem[sk], v)
            self.seen[e][sk] = v
            self.n_ins += 1

    def _commit(self, tok, r, w):
        for b in w:
            self.lw[b] = tok
            self.rd[b] = {}
        for b in r:
            d = self.rd.setdefault(b, {})
            if d.get(tok[0], 0) < tok[1]:
                d[tok[0]] = tok[1]

    def op(self, e, fn, r=(), w=()):
        self._deps(e, r, w)
        ins = fn()
        self.cnt[e] += 1
        ins.then_inc(self.sem[e], 1)
        self.n_ins += 1
        tok = (e, self.cnt[e])
        self._commit(tok, r, w)
        return tok

    def dma(self, e, fn, r=(), w=(), is_out=False):
        lo_, hi_ = self.rr_rng[e]
        idx = self.rr[e]
        self.rr[e] = lo_ + (idx + 1 - lo_) % (hi_ - lo_)
        sk = ("d", idx)
        extra = [(sk, self.dval[idx])] if self.dval[idx] > 0 else []
        self._deps(e, r, w, extra)
        ins = fn()
        self.dval[idx] += 16
        ins.then_inc(self.sem[sk], 16)
        self.n_ins += 1
        tok = (sk, self.dval[idx])
        self._commit(tok, r, w)
        if is_out:
            self.out_tokens.append(tok)
        return tok

    def barrier(self):
        cur = {e: self.cnt[e] for e in self.eng}
        for i in range(ND_SEM):
            cur[("d", i)] = self.dval[i]
        for e in self.eng:
            for sk, v in cur.items():
                if v > self.seen[e].get(sk, 0):
                    self.eng[e].wait_ge(self.sem[sk], v)
                    self.seen[e][sk] = v
                    self.n_ins += 1

    def finish(self):
        e = "sp"
        for sk, v in self.out_tokens:
            if self.seen[e].get(sk, 0) < v:
                self.eng[e].wait_ge(self.sem[sk], v)
                self.seen[e][sk] = v


def build(NTL=32, n_layers=2, dbg=False, peer_slots=128):
    T = NTL * 128
    TT = T + CTX
    NTILES = NTL + 2
    NGL = NTL // 4
    L = TT + 64
    groups = [(g, list(range(4 * g, 4 * g + 4))) for g in range(NGL)] + [(NGL, [NTL, NTL + 1])]

    def ppos(tok):
        return tok + 16 if tok < T else tok + 48

    nc = bass.Bass("TRN2", target_bir_lowering=False)

    def din(name, shape, dt=F32):
        return nc.dram_tensor(name, list(shape), dt, kind="ExternalInput").ap()

    def dscr(name, shape, dt=F32):
        kind = "ExternalOutput" if (dbg and name == "xs") else "Internal"
        return nc.dram_tensor(name, list(shape), dt, kind=kind).ap()

    x_in = din("x", [T, D])
    ctx_in = din("ctx", [CTX, D])
    cvec = din("cvec", [128, 2, 8])
    w_mod = din("w_mod", [2, D, 6 * D])
    b_mod_r = din("b_mod_r", [2, 128, 6 * D])
    g_mix_r = din("g_mix_r", [2, 128, D])
    g_ffn_r = din("g_ffn_r", [2, 128, D])
    g_fin_r = din("g_fin_r", [128, D])
    w1_d = din("w1", [2, D, 640])
    w2a_d = din("w2a", [2, D, 1536])
    w2g_d = din("w2g", [2, D, 3072])
    ropeC = din("ropeC", [128, TT])
    ropeS = din("ropeS", [128, TT])
    sink_r = din("sink_r", [2, 1, 2, 512])
    pwbd_d = din("pwbd", [2, 128, 2, 128])
    pscale_d = din("pscale", [2, 128, 2])
    rc_d = din("rc", [128, 2, TT])
    sguwT_d = din("sgu_wT", [2, 128, 4, 128])
    sgub_d = din("sgu_b_r", [2, 64, 4, 128])
    wbrA_d = din("wbrA", [2, 64, 8, D])
    wbrP_d = din("wbrP", [2, 128, 2, D])
    wbrS_d = din("wbrS", [2, 64, 4, D])
    wout_d = din("wout", [2, 128, 8, D])
    wqT_d = din("wqT", [2, 128, 16, D])
    subkT_d = din("subkT", [2, 128, 16, 128])
    peer_u = [din("peer_u%d" % l, [16384, D]) for l in range(2)]
    peer_v = [din("peer_v%d" % l, [16384, D]) for l in range(2)]
    mask_d = din("maskAB", [128, 2, 512])
    ident_d = din("ident", [128, 128])
    iota_d = din("iota16", [128, 16])
    y_out = nc.dram_tensor("y", [T, D], F32, kind="ExternalOutput").ap()

    xs = dscr("xs", [TT, D])
    hTd = dscr("hTd", [NGL + 1, 128, 8, 512], BF16)
    yTd = dscr("yTd", [NGL + 1, 64, 8, 512], BF16)
    ysTd = dscr("ysTd", [NGL + 1, 64, 4, 512], BF16)
    ypTd = dscr("ypTd", [NGL + 1, 128, 2, 512], BF16)
    uvb = [dscr("uvb%d" % l, [16384, 2 * D], BF16) for l in range(2)]

    with ExitStack() as es:
        P = Prog(nc, es)
        V, A, G, PE, SP = nc.vector, nc.scalar, nc.gpsimd, nc.tensor, nc.sync

        uid = [0]

        def sb(es_, name, shape, dt=F32):
            uid[0] += 1
            return es_.enter_context(nc.sbuf_tensor("sb%d_%s" % (uid[0], name), list(shape), dt))

        PS = es.enter_context(nc.psum_tensor("ps", [128, 8, 512], F32))
        bank_ptr = [0]

        def bank(n=1):
            if n > 1 and bank_ptr[0] % n:
                bank_ptr[0] += n - bank_ptr[0] % n
            b = bank_ptr[0] % 8
            bank_ptr[0] = (bank_ptr[0] + n)
            return b

        identf = sb(es, "identf", [128, 128])
        identb = sb(es, "identb", [128, 128], BF16)
        maskb = sb(es, "maskb", [128, 2, 512], BF16)
        ones_b = sb(es, "ones_b", [128, 64], BF16)
        iota16 = sb(es, "iota16", [128, 16])
        screp = sb(es, "screp", [128, 2, 8, 128])
        with ExitStack() as e0:
            maskf = sb(e0, "maskf", [128, 2, 512])
            cv = sb(e0, "cv", [128, 2, 8])
            cs = sb(e0, "cs", [128, 2, 8])
            P.dma("sp", lambda: SP.dma_start(out=identf[:], in_=ident_d[:, :]), w=["identf"])
            P.dma("sp", lambda: SP.dma_start(out=maskf[:], in_=mask_d[:, :, :]), w=["maskf"])
            P.dma("sp", lambda: SP.dma_start(out=iota16[:], in_=iota_d[:, :]), w=["iota16"])
            P.dma("sp", lambda: SP.dma_start(out=cv[:], in_=cvec[:, :, :]), w=["cv"])
            P.dma("sp", lambda: SP.dma_start(out=xs[0:T, :], in_=x_in[:, :]), w=[("xs", t) for t in range(NTL)])
            P.dma("sp", lambda: SP.dma_start(out=xs[T:TT, :], in_=ctx_in[:, :]), w=[("xs", NTL), ("xs", NTL + 1)])
            P.op("dve", lambda: V.tensor_copy(out=identb[:], in_=identf[:]), r=["identf"], w=["identb"])
            P.op("dve", lambda: V.tensor_copy(out=maskb[:], in_=maskf[:]), r=["maskf"], w=["maskb"])
            P.op("dve", lambda: V.memset(ones_b[:], 1.0), w=["ones_b"])
            P.op("act", lambda: A.activation(out=cs[:], in_=cv[:], func=AF.Silu), r=["cv"], w=["cs"])
            P.op("dve", lambda: V.tensor_copy(out=screp[:], in_=cs[:].unsqueeze(3).broadcast_to([128, 2, 8, 128])),
                 r=["cs"], w=["screp"])
            P.barrier()

        def modulation(layer, which, mod, g_r):
            base = which * 3 * D
            with ExitStack() as em:
                wm = [sb(em, "wm%d" % i, [128, 8, 512]) for i in range(2)]
                bm = [sb(em, "bm%d" % i, [128, 512]) for i in range(2)]
                gs = [sb(em, "gs%d" % i, [128, 512]) for i in range(2)]
                tm = [sb(em, "tm%d" % i, [128, 512]) for i in range(2)]
                for ci in range(6):
                    kind, half = ci // 2, ci % 2
                    c0 = base + ci * 512
                    j = ci % 2
                    P.dma("sp", lambda: SP.dma_start(
                        out=wm[j][:], in_=w_mod[layer, :, c0:c0 + 512].rearrange("(k p) n -> p k n", p=128)),
                        w=[("wm", j)])
                    P.dma("sp", lambda: SP.dma_start(out=bm[j][:], in_=b_mod_r[layer, :, c0:c0 + 512]), w=[("bm", j)])
                    if kind == 1:
                        P.dma("sp", lambda: SP.dma_start(out=gs[j][:], in_=g_r[layer, :, half * 512:(half + 1) * 512]),
                              w=[("gs", j)])
                    for s in range(2):
                        b = bank()

                        def mm():
                            for k in range(8):
                                ins = PE.matmul(PS[:, b, :], lhsT=screp[:, s, k, :], rhs=wm[j][:, k, :],
                                                start=(k == 0), stop=(k == 7))
                            return ins
                        P.op("pe", mm, r=["screp", ("wm", j)], w=[("ps", b)])
                        dst_kind = {0: 1, 1: 0, 2: 2}[kind]
                        dst = mod[s][dst_kind][:, half * 512:(half + 1) * 512]
                        dname = ("mod", which, s, dst_kind, half)
                        if kind == 1:
                            P.op("dve", lambda: V.tensor_tensor(out=tm[s][:], in0=PS[:, b, :], in1=bm[j][:], op=ALU.add),
                                 r=[("ps", b), ("bm", j)], w=[("tm", s)])
                            P.op("dve", lambda: V.scalar_tensor_tensor(out=dst, in0=tm[s][:], scalar=1.0, in1=gs[j][:],
                                                                       op0=ALU.add, op1=ALU.mult),
                                 r=[("tm", s), ("gs", j)], w=[dname])
                        else:
                            P.op("dve", lambda: V.tensor_tensor(out=dst, in0=PS[:, b, :], in1=bm[j][:], op=ALU.add),
                                 r=[("ps", b), ("bm", j)], w=[dname])
                P.barrier()

        def modnames(which, s):
            return [("mod", which, s, k, h) for k in range(3) for h in range(2)]

        def load_cast(es_, name, dram_ap, shape, stage_bufs, dst=None, eng_cycle=("act", "pool")):
            raise NotImplementedError

        def norm_mod(xt, xname, Amod, Bmod, modr, junk, ss, hf_out, hb_out, hname, add_eng="pool"):
            P.op("dve", lambda: V.scalar_tensor_tensor(out=junk[:], in0=xt[:], scalar=1.0, in1=xt[:], op0=ALU.mult, op1=ALU.mult, accum_out=ss[:, 0:1]),
                 r=[xname], w=["junk", "ss0"])
            P.op("dve", lambda: V.tensor_scalar(out=ss[:, 1:2], in0=ss[:, 0:1], scalar1=1.0 / D, scalar2=EPS,
                                                op0=ALU.mult, op1=ALU.add), r=["ss0"], w=["ss1"])
            P.op("act", lambda: A.activation(out=ss[:, 2:3], in_=ss[:, 1:2], func=AF.Ln), r=["ss1"], w=["ss2"])
            P.op("act", lambda: A.activation(out=ss[:, 3:4], in_=ss[:, 2:3], func=AF.Exp, scale=-0.5), r=["ss2"], w=["ss3"])
            P.op("dve", lambda: V.scalar_tensor_tensor(out=junk[:], in0=xt[:], scalar=ss[:, 3:4], in1=Amod[:],
                                                       op0=ALU.mult, op1=ALU.mult),
                 r=[xname, "ss3"] + modr, w=["junk"])
            if hf_out is not None:
                P.op("dve", lambda: V.tensor_tensor(out=hf_out[:], in0=junk[:], in1=Bmod[:], op=ALU.add),
                     r=["junk"] + modr, w=[hname + "f"])
                P.op("act", lambda: A.copy(out=hb_out[:], in_=hf_out[:]), r=[hname + "f"], w=[hname])
            elif add_eng == "dve":
                P.op("dve", lambda: V.tensor_tensor(out=hb_out[:], in0=junk[:], in1=Bmod[:], op=ALU.add),
                     r=["junk"] + modr, w=[hname])
            else:
                P.op("pool", lambda: G.tensor_tensor(out=hb_out[:], in0=junk[:], in1=Bmod[:], op=ALU.add),
                     r=["junk"] + modr, w=[hname])

        def transpose8(hb, hname, dst_ap, dname, evac="act", b=None):
            if b is None:
                b = bank()
            psb = PS[:, b, :].bitcast(BF16)

            def tr():
                for k in range(8):
                    ins = PE.transpose(out=psb[:, k * 128:(k + 1) * 128], in_=hb[:, k * 128:(k + 1) * 128],
                                       identity=identb[:])
                return ins
            P.op("pe", tr, r=[hname, "identb"], w=[("ps", b)])
            src = psb.rearrange("p (k t) -> p k t", k=8)
            if evac == "act":
                P.op("act", lambda: A.copy(out=dst_ap, in_=src), r=[("ps", b)], w=[dname])
            else:
                P.op("dve", lambda: V.tensor_copy(out=dst_ap, in_=src), r=[("ps", b)], w=[dname])

        def cast_load(dst_tile_ap, dname, src_ap, stg, sname, ceng):
            P.dma("sp", lambda: SP.dma_start(out=stg, in_=src_ap), w=[sname])
            if ceng == "act":
                P.op("act", lambda: A.copy(out=dst_tile_ap, in_=stg), r=[sname], w=[dname])
            elif ceng == "pool":
                P.op("pool", lambda: G.tensor_copy(out=dst_tile_ap, in_=stg), r=[sname], w=[dname])
            else:
                P.op("dve", lambda: V.tensor_copy(out=dst_tile_ap, in_=stg), r=[sname], w=[dname])

        NQ = 16

        def tabnames(l):
            return [("uvb", l, c0, q) for c0 in (0, D) for q in range(NQ)]

        def conv_gen(l):
            rpq = 16384 // NQ
            for q in range(NQ):
                for (src_t, c0) in ((peer_u[l], 0), (peer_v[l], D)):
                    rows = slice(q * rpq, (q + 1) * rpq)
                    P.dma("pool", lambda: G.dma_start(out=uvb[l][rows, c0:c0 + D], in_=src_t[rows, :]), w=[("uvb", l, c0, q)])
                    yield
        convs = [conv_gen(l) for l in range(n_layers)]

        def pump(l, k):
            if l < n_layers:
                for _ in range(k):
                    next(convs[l], None)

        for layer in range(n_layers):
            last = layer == n_layers - 1
            with ExitStack() as eL:
                modm = [[sb(eL, "modm%d%d" % (s, k), [128, D]) for k in range(3)] for s in range(2)]
                eKV = ExitStack()
                kT = sb(eKV, "kT", [128, TT], BF16)
                vtok = sb(eKV, "vtok", [128, NTILES, 128], BF16)
                dT = sb(eKV, "dT", [128, 2, TT], BF16)
                sinkrow = sb(eKV, "sinkrow", [1, 2, 512], BF16)
                with ExitStack() as e1:
                    sk_f = sb(e1, "sk_f", [1, 2, 512])
                    sk_e = sb(e1, "sk_e", [1, 2, 512])
                    P.dma("sp", lambda: SP.dma_start(out=sk_f[:], in_=sink_r[layer, :, :, :]), w=["sk_f"])
                    P.op("act", lambda: A.activation(out=sk_e[:], in_=sk_f[:], func=AF.Exp), r=["sk_f"], w=["sk_e"])
                    P.op("act", lambda: A.copy(out=sinkrow[:], in_=sk_e[:]), r=["sk_e"], w=["sinkrow"])
                    P.barrier()
                modulation(layer, 0, modm, g_mix_r)

                with ExitStack() as e1:
                    zpad = sb(e1, "zpad", [128, 2, L])
                    e1w = ExitStack()
                    w1 = sb(e1w, "w1", [128, 8, 640], BF16)
                    stg = [sb(e1w, "stg%d" % i, [128, 2, 640]) for i in range(2)]
                    xt_ = [sb(e1w, "xt%d" % i, [128, D]) for i in range(2)]
                    junk = sb(e1w, "junk", [128, D])
                    ss = sb(e1w, "ss", [128, 4])
                    hb_ = [sb(e1w, "hb%d" % i, [128, D], BF16) for i in range(2)]
                    hTg = [sb(e1w, "hTg%d" % i, [128, 8, 512], BF16) for i in range(2)]
                    cS = [sb(e1w, "cS%d" % i, [128, 2, 512]) for i in range(2)]
                    t1 = sb(e1w, "t1", [128, 512])
                    t2 = sb(e1w, "t2", [128, 512])
                    for i in range(4):
                        cast_load(w1[:, 2 * i:2 * i + 2, :], ("w1", i),
                                  w1_d[layer, 256 * i:256 * (i + 1), :].rearrange("(k p) n -> p k n", p=128),
                                  stg[i % 2][:], ("stg", i % 2), "act")
                    P.op("dve", lambda: V.memset(zpad[:], 0.0), w=["zpad"])
                    for gi, tiles in groups:
                        j = gi % 2
                        ng = len(tiles) * 128
                        s = 0 if tiles[0] < NTL else 1
                        tok0 = tiles[0] * 128
                        for ti, tile in enumerate(tiles):
                            jj = tile % 2
                            P.dma("sp", lambda: SP.dma_start(out=xt_[jj][:], in_=xs[tile * 128:(tile + 1) * 128, :]),
                                  r=[("xs", tile)], w=[("xt", jj)])
                            norm_mod(xt_[jj], ("xt", jj), modm[s][0], modm[s][1], modnames(0, s), junk, ss, None,
                                     hb_[jj], ("hb", jj), add_eng="dve")
                            transpose8(hb_[jj], ("hb", jj), hTg[j][:, :, ti * 128:(ti + 1) * 128], ("hTg", j, ti))
                        hr = [("hTg", j, ti) for ti in range(len(tiles))]
                        P.dma("sp", lambda: SP.dma_start(out=hTd[gi, :, :, 0:ng], in_=hTg[j][:, :, 0:ng]), r=hr, w=[("hTd", gi)])
                        P.dma("sp", lambda: SP.dma_start(out=cS[j][:, 0, 0:ng], in_=ropeC[:, tok0:tok0 + ng]), w=[("cS", j, 0)])
                        P.dma("sp", lambda: SP.dma_start(out=cS[j][:, 1, 0:ng], in_=ropeS[:, tok0:tok0 + ng]), w=[("cS", j, 1)])
                        if layer == 0:
                            pump(0, 2)
                        bk, bkr = bank(), bank()
                        for (bb, c0) in ((bk, 0), (bkr, 128)):
                            def mm(bb=bb, c0=c0):
                                for k in range(8):
                                    ins = PE.matmul(PS[:, bb, 0:ng], lhsT=w1[:, k, c0:c0 + 128], rhs=hTg[j][:, k, 0:ng],
                                                    start=(k == 0), stop=(k == 7))
                                return ins
                            P.op("pe", mm, r=hr + [("w1", i_) for i_ in range(4)], w=[("ps", bb)])
                        P.op("dve", lambda: V.tensor_tensor(out=t1[:, 0:ng], in0=PS[:, bkr, 0:ng], in1=cS[j][:, 1, 0:ng], op=ALU.mult),
                             r=[("ps", bkr), ("cS", j, 1)], w=["t1"])
                        P.op("dve", lambda: V.tensor_tensor(out=t2[:, 0:ng], in0=PS[:, bk, 0:ng], in1=cS[j][:, 0, 0:ng], op=ALU.mult),
                             r=[("ps", bk), ("cS", j, 0)], w=["t2"])
                        P.op("dve", lambda: V.tensor_tensor(out=kT[:, tok0:tok0 + ng], in0=t1[:, 0:ng], in1=t2[:, 0:ng], op=ALU.add),
                             r=["t1", "t2"], w=[("kT", gi)])
                        for c in range(2):
                            bz = bank()

                            def mm(bz=bz, c=c):
                                for k in range(8):
                                    ins = PE.matmul(PS[:, bz, 0:ng], lhsT=w1[:, k, 384 + c * 128:512 + c * 128],
                                                    rhs=hTg[j][:, k, 0:ng], start=(k == 0), stop=(k == 7))
                                return ins
                            P.op("pe", mm, r=hr + [("w1", i_) for i_ in range(4)], w=[("ps", bz)])
                            p0 = ppos(tok0)
                            P.op("act", lambda: A.copy(out=zpad[:, c, p0:p0 + ng], in_=PS[:, bz, 0:ng]),
                                 r=[("ps", bz)], w=["zpad"])
                        bv = bank()

                        def mm():
                            for ti in range(len(tiles)):
                                for k in range(8):
                                    ins = PE.matmul(PS[:, bv, ti * 128:(ti + 1) * 128], lhsT=hTg[j][:, k, ti * 128:(ti + 1) * 128],
                                                    rhs=w1[:, k, 256:384], start=(k == 0), stop=(k == 7))
                            return ins
                        P.op("pe", mm, r=hr + [("w1", i_) for i_ in range(4)], w=[("ps", bv)])
                        nt = len(tiles)
                        P.op("act", lambda: A.copy(out=vtok[:, tiles[0]:tiles[0] + nt, :],
                                                   in_=PS[:, bv, 0:ng].rearrange("p (a b) -> p a b", a=nt)),
                             r=[("ps", bv)], w=[("vtok", gi)])

                    P.barrier()
                    e1w.close()
                    PA = sb(e1, "PA", [128, L])
                    PB = sb(e1, "PB", [128, L])
                    P.op("dve", lambda: V.memset(PA[:], 0.0), w=["PA"])
                    P.op("dve", lambda: V.memset(PB[:], 0.0), w=["PB"])
                    rc = sb(e1, "rc", [128, 2, TT])
                    P.dma("sp", lambda: SP.dma_start(out=rc[:], in_=rc_d[:, :, :]), w=["rc"])
                    lo, hi = 8, L - 8

                    def shadd(dst, src, s1, s2, p0=0, p1=128):
                        P.op("dve", lambda: V.tensor_tensor(out=dst[p0:p1, lo:hi], in0=src[p0:p1, lo + s1:hi + s1],
                                                            in1=src[p0:p1, lo + s2:hi + s2], op=ALU.add),
                             r=["PA", "PB", "zpad"], w=["PA", "PB"])
                    segs = [(0, T, 16), (T, TT, 48)]

                    def dfin(src, c, p0, p1):
                        for (a, b_, off) in segs:
                            P.op("dve", lambda: V.tensor_tensor(out=src[p0:p1, a + off:b_ + off], in0=src[p0:p1, a + off:b_ + off],
                                                                in1=rc[p0:p1, c, a:b_], op=ALU.mult),
                                 r=["PA", "PB", "rc"], w=["PA", "PB"])
                            P.op("dve", lambda: V.tensor_tensor(out=dT[p0:p1, c, a:b_], in0=src[p0:p1, a + off:b_ + off],
                                                                in1=zpad[p0:p1, c, a + off:b_ + off], op=ALU.subtract),
                                 r=["PA", "PB", "zpad"], w=["dT"])
                    shadd(PA, zpad[:, 0, :], -1, 0)
                    shadd(PB, PA, -1, 1, 64, 128)
                    dfin(PA, 0, 0, 64)
                    dfin(PB, 0, 64, 128)
                    shadd(PA, zpad[:, 1, :], -1, 0)
                    shadd(PB, PA, -1, 1)
                    shadd(PA, PB, -2, 2)
                    shadd(PB, PA, -4, 4, 64, 128)
                    dfin(PA, 1, 0, 64)
                    dfin(PB, 1, 64, 128)
                    P.barrier()

                allk = [("kT", gi) for gi, _ in groups]
                allv = [("vtok", gi) for gi, _ in groups]
                m2_groups = groups if not last else groups[:NGL]
                with ExitStack() as e2:
                    w2a = sb(e2, "w2a", [128, 8, 1536], BF16)
                    stg = [sb(e2, "stgA%d" % i, [128, 1, 1536]) for i in range(2)]
                    pwbd = sb(e2, "pwbd", [128, 2, 128], BF16)
                    pwf = sb(e2, "pwf", [128, 2, 128])
                    pscale = sb(e2, "pscale", [128, 2])
                    sguwT = sb(e2, "sguwT", [128, 4, 128], BF16)
                    sguf = sb(e2, "sguf", [128, 4, 128])
                    sgub = sb(e2, "sgub", [64, 4, 128])
                    junk = sb(e2, "junkA", [128, D])
                    ss = sb(e2, "ssA", [128, 4])
                    hTg = [sb(e2, "hTgA%d" % i, [128, 8, 512], BF16) for i in range(2)]
                    cS = [sb(e2, "cSA%d" % i, [128, 2, 512]) for i in range(1)] * 2
                    t1 = sb(e2, "t1A", [128, 512])
                    t2 = sb(e2, "t2A", [128, 512])
                    qT = sb(e2, "qT", [128, 4, 512], BF16)
                    pT = [sb(e2, "pT%d" % i, [128, 512], BF16) for i in range(6)]
                    lnd = sb(e2, "lnd", [64, 512])
                    rec = sb(e2, "rec", [64, 512])
                    yT = sb(e2, "yT", [64, 8, 512], BF16)
                    uT = sb(e2, "uT", [64, 4, 512], BF16)
                    vg = sb(e2, "vg", [128, 256])
                    vn = sb(e2, "vn", [128, 256], BF16)
                    sv = sb(e2, "svA", [128, 4])
                    ysT = sb(e2, "ysT", [64, 4, 512], BF16)
                    ypT = sb(e2, "ypT", [128, 2, 512], BF16)
                    tsg = sb(e2, "tsg", [64, 512])
                    for i in range(8):
                        cast_load(w2a[:, i:i + 1, :], ("w2a", i),
                                  w2a_d[layer, 128 * i:128 * (i + 1), :].rearrange("(k p) n -> p k n", p=128),
                                  stg[i % 2][:], ("stgA", i % 2), "act" if i % 2 == 0 else "pool")
                    w2r = [("w2a", i) for i in range(8)]
                    cast_load(pwbd[:], "pwbd", pwbd_d[layer, :, :, :], pwf[:], "pwf", "dve")
                    cast_load(sguwT[:], "sguwT", sguwT_d[layer, :, :, :], sguf[:], "sguf", "dve")
                    P.dma("sp", lambda: SP.dma_start(out=pscale[:], in_=pscale_d[layer, :, :]), w=["pscale"])
                    P.dma("sp", lambda: SP.dma_start(out=sgub[:], in_=sgub_d[layer, :, :, :]), w=["sgub"])
                    pti = [0]
                    def load_h(gidx):
                        gi2, tiles2 = m2_groups[gidx]
                        ng2 = len(tiles2) * 128
                        j2 = gidx % 2
                        P.dma("sp", lambda: SP.dma_start(out=hTg[j2][:, :, 0:ng2], in_=hTd[gi2, :, :, 0:ng2]),
                              r=[("hTd", gi2)], w=[("hTgA", j2)])
                    load_h(0)
                    for gidx, (gi, tiles) in enumerate(m2_groups):
                        j = gidx % 2
                        nt = len(tiles)
                        ng = nt * 128
                        s = 0 if tiles[0] < NTL else 1
                        tok0 = tiles[0] * 128
                        if gidx + 1 < len(m2_groups):
                            load_h(gidx + 1)
                        hr = [("hTgA", j)]
                        P.dma("sp", lambda: SP.dma_start(out=cS[0][:, 0, 0:ng], in_=ropeC[:, tok0:tok0 + ng]), w=[("cS", 0, 0)])
                        P.dma("sp", lambda: SP.dma_start(out=cS[0][:, 1, 0:ng], in_=ropeS[:, tok0:tok0 + ng]), w=[("cS", 0, 1)])
                        for c in range(4):
                            bq, bqr = bank(), bank()
                            for (bb, c0) in ((bq, c * 128), (bqr, 512 + c * 128)):
                                def mm(bb=bb, c0=c0):
                                    for k in range(8):
                                        ins = PE.matmul(PS[:, bb, 0:ng], lhsT=w2a[:, k, c0:c0 + 128], rhs=hTg[j][:, k, 0:ng],
                                                        start=(k == 0), stop=(k == 7))
                                    return ins
                                P.op("pe", mm, r=hr + w2r, w=[("ps", bb)])
                            P.op("dve", lambda: V.tensor_tensor(out=t1[:, 0:ng], in0=PS[:, bqr, 0:ng], in1=cS[0][:, 1, 0:ng], op=ALU.mult),
                                 r=[("ps", bqr), ("cS", 0, 1)], w=["t1"])
                            P.op("dve", lambda: V.tensor_tensor(out=t2[:, 0:ng], in0=PS[:, bq, 0:ng], in1=cS[0][:, 0, 0:ng], op=ALU.mult),
                                 r=[("ps", bq), ("cS", 0, 0)], w=["t2"])
                            P.op("dve", lambda: V.tensor_tensor(out=qT[:, c, 0:ng], in0=t1[:, 0:ng], in1=t2[:, 0:ng], op=ALU.add),
                                 r=["t1", "t2"], w=[("qT", c)])
                        qr_ = [("qT", c) for c in range(4)]
                        if layer == 0:
                            pump(0, 2)
                            pump(1, 2)
                        for ti, tile in enumerate(tiles):
                            if tile < NTL:
                                keys = []
                                if tile - 1 >= 0:
                                    keys.append((tile - 1, 0))
                                keys.append((tile, None))
                                if tile + 1 < NTL:
                                    keys.append((tile + 1, 1))
                                keys += [(NTL, None), (NTL + 1, None)]
                            else:
                                keys = [(NTL, None), (NTL + 1, None)]
                            for hk in range(2):
                                h0, h1 = hk * 64, hk * 64 + 64
                                bnum, bden = bank(), bank()
                                sbanks = []
                                for (kt, mk) in keys:
                                    bs = bank()
                                    sbanks.append(bs)

                                    def mm(bs=bs, kt=kt, mk=mk):
                                        ins = PE.matmul(PS[:, bs, :], lhsT=kT[h0:h1, kt * 128:(kt + 1) * 128],
                                                        rhs=qT[h0:h1, :, ti * 128:(ti + 1) * 128],
                                                        start=True, stop=(mk is None))
                                        if mk is not None:
                                            ins = PE.matmul(PS[:, bs, :], lhsT=identb[:], rhs=maskb[:, mk, :], start=False, stop=True)
                                        return ins
                                    P.op("pe", mm, r=allk + qr_ + ["identb", "maskb"], w=[("ps", bs)])
                                pts = []
                                for bs in sbanks:
                                    pi = pti[0] % 6
                                    pti[0] += 1
                                    pts.append(pi)
                                    P.op("act", lambda bs=bs, pi=pi: A.activation(out=pT[pi][:], in_=PS[:, bs, :], func=AF.Exp, scale=0.125),
                                         r=[("ps", bs)], w=[("pT", pi)])
                                nk = len(keys)
                                for ki, ((kt, mk), pi) in enumerate(zip(keys, pts)):
                                    def mm(ki=ki, kt=kt, pi=pi):
                                        PE.matmul(PS[0:64, bnum, :], lhsT=vtok[:, kt, h0:h1], rhs=pT[pi][:],
                                                  start=(ki == 0), stop=(ki == nk - 1))
                                        ins = PE.matmul(PS[0:64, bden, :], lhsT=ones_b[:, 0:64], rhs=pT[pi][:],
                                                        start=(ki == 0), stop=False)
                                        if ki == nk - 1:
                                            ins = PE.matmul(PS[0:64, bden, :], lhsT=ones_b[0:1, 0:64], rhs=sinkrow[0:1, hk, :],
                                                            start=False, stop=True)
                                        return ins
                                    P.op("pe", mm, r=allv + [("pT", pi), "ones_b", "sinkrow"], w=[("ps", bnum), ("ps", bden)])
                                P.op("act", lambda: A.activation(out=lnd[:], in_=PS[0:64, bden, :], func=AF.Ln),
                                     r=[("ps", bden)], w=["lnd"])
                                P.op("act", lambda: A.activation(out=rec[:], in_=lnd[:], func=AF.Exp, scale=-1.0),
                                     r=["lnd"], w=["rec"])
                                P.op("dve", lambda: V.tensor_tensor(
                                    out=yT[:, hk * 4:hk * 4 + 4, ti * 128:(ti + 1) * 128],
                                    in0=PS[0:64, bnum, :].rearrange("p (g q) -> p g q", g=4),
                                    in1=rec[:].rearrange("p (g q) -> p g q", g=4), op=ALU.mult),
                                    r=[("ps", bnum), "rec"], w=[("yT", ti, hk)])
                        for h4 in range(4):
                            bu = bank()

                            def mm():
                                for k in range(8):
                                    ins = PE.matmul(PS[0:64, bu, 0:ng], lhsT=w2a[:, k, 1024 + h4 * 64:1088 + h4 * 64],
                                                    rhs=hTg[j][:, k, 0:ng], start=(k == 0), stop=(k == 7))
                                return ins
                            P.op("pe", mm, r=hr + w2r, w=[("ps", bu)])
                            P.op("act", lambda: A.activation(out=uT[:, h4, 0:ng], in_=PS[0:64, bu, 0:ng], func=AF.Gelu_apprx_tanh),
                                 r=[("ps", bu)], w=[("uT", h4)])
                        for ti, tile in enumerate(tiles):
                            bvs = bank()

                            def mm():
                                for k in range(8):
                                    ins = PE.matmul(PS[:, bvs, 0:256], lhsT=hTg[j][:, k, ti * 128:(ti + 1) * 128],
                                                    rhs=w2a[:, k, 1280:1536], start=(k == 0), stop=(k == 7))
                                return ins
                            P.op("pe", mm, r=hr + w2r, w=[("ps", bvs)])
                            P.op("act", lambda: A.activation(out=vg[:], in_=PS[:, bvs, 0:256], func=AF.Gelu_apprx_tanh),
                                 r=[("ps", bvs)], w=["vg"])
                            P.op("dve", lambda: V.scalar_tensor_tensor(out=junk[:, 0:256], in0=vg[:], scalar=1.0, in1=vg[:],
                                                                        op0=ALU.mult, op1=ALU.mult, accum_out=sv[:, 0:1]),
                                 r=["vg"], w=["junk", "sv0"])
                            P.op("dve", lambda: V.tensor_scalar(out=sv[:, 1:2], in0=sv[:, 0:1], scalar1=1.0 / 256, scalar2=EPS,
                                                                op0=ALU.mult, op1=ALU.add), r=["sv0"], w=["sv1"])
                            P.op("act", lambda: A.activation(out=sv[:, 2:3], in_=sv[:, 1:2], func=AF.Ln), r=["sv1"], w=["sv2"])
                            P.op("act", lambda: A.activation(out=sv[:, 3:4], in_=sv[:, 2:3], func=AF.Exp, scale=-0.5), r=["sv2"], w=["sv3"])
                            P.op("dve", lambda: V.tensor_scalar(out=vn[:], in0=vg[:], scalar1=sv[:, 3:4], scalar2=None, op0=ALU.mult),
                                 r=["vg", "sv3"], w=["vn"])
                            bm_ = bank()

                            def mm():
                                for h4 in range(4):
                                    ins = PE.matmul(PS[0:64, bm_, h4 * 128:(h4 + 1) * 128], lhsT=vn[:, h4 * 64:(h4 + 1) * 64],
                                                    rhs=sguwT[:, h4, :], start=True, stop=True)
                                return ins
                            P.op("pe", mm, r=["vn", "sguwT"], w=[("ps", bm_)])
                            P.op("dve", lambda: V.tensor_tensor(out=tsg[:], in0=PS[0:64, bm_, :],
                                                                in1=sgub[:].rearrange("p a b -> p (a b)"), op=ALU.add),
                                 r=[("ps", bm_), "sgub"], w=["tsg"])
                            P.op("dve", lambda: V.tensor_tensor(out=ysT[:, :, ti * 128:(ti + 1) * 128],
                                                                in0=tsg[:].rearrange("p (a b) -> p a b", a=4),
                                                                in1=uT[:, :, ti * 128:(ti + 1) * 128], op=ALU.mult),
                                 r=["tsg"] + [("uT", h4) for h4 in range(4)], w=[("ysT", ti)])
                        for c in range(2):
                            bp = bank()
                            P.op("pe", lambda: PE.matmul(PS[:, bp, 0:ng], lhsT=pwbd[:, c, :], rhs=dT[:, c, tok0:tok0 + ng],
                                                         start=True, stop=True), r=["pwbd", "dT"], w=[("ps", bp)])
                            P.op("dve", lambda: V.tensor_scalar(out=ypT[:, c, 0:ng], in0=PS[:, bp, 0:ng], scalar1=pscale[:, c:c + 1],
                                                                scalar2=None, op0=ALU.mult),
                                 r=[("ps", bp), "pscale"], w=[("ypT", c)])
                        yr = [("yT", ti, hk) for ti in range(nt) for hk in range(2)]
                        P.dma("sp", lambda: SP.dma_start(out=yTd[gi, :, :, 0:ng], in_=yT[:, :, 0:ng]), r=yr, w=[("yTd", gi)])
                        P.dma("sp", lambda: SP.dma_start(out=ysTd[gi, :, :, 0:ng], in_=ysT[:, :, 0:ng]),
                              r=[("ysT", ti) for ti in range(nt)], w=[("ysTd", gi)])
                        P.dma("sp", lambda: SP.dma_start(out=ypTd[gi, :, :, 0:ng], in_=ypT[:, :, 0:ng]),
                              r=[("ypT", 0), ("ypT", 1)], w=[("ypTd", gi)])
                    P.barrier()

                P.barrier()
                eKV.close()
                with ExitStack() as e3:
                    w2g = sb(e3, "w2g", [128, 8, 3072], BF16)
                    wbrA = sb(e3, "wbrA", [64, 8, D], BF16)
                    wbrP = sb(e3, "wbrP", [128, 2, D], BF16)
                    wbrS = sb(e3, "wbrS", [64, 4, D], BF16)
                    wout = sb(e3, "wout", [128, 8, D], BF16)
                    stg = [sb(e3, "stgB%d" % i, [128, 2048]) for i in range(2)]
                    hTl = [sb(e3, "hTl%d" % i, [128, 8, 512], BF16) for i in range(2)]
                    yTl = sb(e3, "yTl", [64, 8, 512], BF16)
                    ysTl = sb(e3, "ysTl", [64, 4, 512], BF16)
                    ypTl = sb(e3, "ypTl", [128, 2, 512], BF16)
                    sig = [sb(e3, "sig%d" % i, [128, 512], BF16) for i in range(3)]
                    tt = [sb(e3, "tt%d" % i, [128, 512]) for i in range(4)]
                    mT = sb(e3, "mT", [128, 8, 512], BF16)
                    xt_ = [sb(e3, "xtB%d" % i, [128, D]) for i in range(1)] * 2
                    xo_ = [sb(e3, "xoB%d" % i, [128, D]) for i in range(1)] * 2
                    ci = [0]

                    def cl(dst, dname, src, width):
                        i = ci[0] % 2
                        ci[0] += 1
                        cast_load(dst, dname, src, stg[i][:, 0:width] if True else None, ("stgB", i),
                                  ("act", "pool", "dve")[ci[0] % 3])
                    for k in range(8):
                        for hh in range(2):
                            cl(w2g[:, k, hh * 1536:(hh + 1) * 1536], ("w2g", k, hh),
                               w2g_d[layer, k * 128:(k + 1) * 128, hh * 1536:(hh + 1) * 1536], 1536)
                    for h in range(0, 8, 2):
                        i = ci[0] % 2
                        ci[0] += 1
                        cast_load(wbrA[:, h:h + 2, :], ("wbrA", h), wbrA_d[layer, :, h:h + 2, :],
                                  stg[i][0:64, 0:2048].rearrange("p (a b) -> p a b", a=2), ("stgB", i), "act")
                    i = ci[0] % 2
                    ci[0] += 1
                    cast_load(wbrP[:], "wbrP", wbrP_d[layer, :, :, :], stg[i][:, 0:2048].rearrange("p (a b) -> p a b", a=2),
                              ("stgB", i), "pool")
                    for h in range(0, 4, 2):
                        i = ci[0] % 2
                        ci[0] += 1
                        cast_load(wbrS[:, h:h + 2, :], ("wbrS", h), wbrS_d[layer, :, h:h + 2, :],
                                  stg[i][0:64, 0:2048].rearrange("p (a b) -> p a b", a=2), ("stgB", i), "dve")
                    for k in range(0, 8, 2):
                        i = ci[0] % 2
                        ci[0] += 1
                        cast_load(wout[:, k:k + 2, :], ("wout", k), wout_d[layer, :, k:k + 2, :],
                                  stg[i][:, 0:2048].rearrange("p (a b) -> p a b", a=2), ("stgB", i), "act")
                    wr_g = [("w2g", k, hh) for k in range(8) for hh in range(2)]
                    wr_b = [("wbrA", h) for h in range(0, 8, 2)] + ["wbrP"] + [("wbrS", h) for h in range(0, 4, 2)]
                    wr_o = [("wout", k) for k in range(0, 8, 2)]
                    def load_b(gidx):
                        gi2, tiles2 = m2_groups[gidx]
                        n2 = len(tiles2) * 128
                        j2 = gidx % 2
                        P.dma("sp", lambda: SP.dma_start(out=hTl[j2][:, :, 0:n2], in_=hTd[gi2, :, :, 0:n2]), r=[("hTd", gi2)], w=[("hTl", j2)])
                    load_b(0)
                    for gidx, (gi, tiles) in enumerate(m2_groups):
                        j = gidx % 2
                        nt = len(tiles)
                        ng = nt * 128
                        s = 0 if tiles[0] < NTL else 1
                        if layer == 0:
                            pump(1, 2)
                        if gidx + 1 < len(m2_groups):
                            load_b(gidx + 1)
                        P.dma("sp", lambda: SP.dma_start(out=yTl[:, :, 0:ng], in_=yTd[gi, :, :, 0:ng]), r=[("yTd", gi)], w=["yTl"])
                        P.dma("sp", lambda: SP.dma_start(out=ysTl[:, :, 0:ng], in_=ysTd[gi, :, :, 0:ng]), r=[("ysTd", gi)], w=["ysTl"])
                        P.dma("sp", lambda: SP.dma_start(out=ypTl[:, :, 0:ng], in_=ypTd[gi, :, :, 0:ng]), r=[("ypTd", gi)], w=["ypTl"])
                        for m in range(8):
                            mc = slice(m * 128, (m + 1) * 128)
                            bgs = []
                            for i3 in range(3):
                                bg = bank()
                                bgs.append(bg)

                                def mmg(bg=bg, i3=i3):
                                    for k in range(8):
                                        ins = PE.matmul(PS[:, bg, 0:ng], lhsT=w2g[:, k, i3 * D + m * 128:i3 * D + (m + 1) * 128],
                                                        rhs=hTl[j][:, k, 0:ng], start=(k == 0), stop=(k == 7))
                                    return ins
                                P.op("pe", mmg, r=wr_g + [("hTl", j)], w=[("ps", bg)])
                                P.op("act", lambda bg=bg, i3=i3: A.activation(out=sig[i3][:, 0:ng], in_=PS[:, bg, 0:ng], func=AF.Sigmoid),
                                     r=[("ps", bg)], w=[("sig", i3)])
                            bA, bP, bS = bank(), bank(), bank()

                            def mmA():
                                for h in range(8):
                                    ins = PE.matmul(PS[:, bA, 0:ng], lhsT=wbrA[:, h, mc], rhs=yTl[:, h, 0:ng], start=(h == 0), stop=(h == 7))
                                return ins

                            def mmP():
                                for c in range(2):
                                    ins = PE.matmul(PS[:, bP, 0:ng], lhsT=wbrP[:, c, mc], rhs=ypTl[:, c, 0:ng], start=(c == 0), stop=(c == 1))
                                return ins

                            def mmS():
                                for h in range(4):
                                    ins = PE.matmul(PS[:, bS, 0:ng], lhsT=wbrS[:, h, mc], rhs=ysTl[:, h, 0:ng], start=(h == 0), stop=(h == 3))
                                return ins
                            P.op("pe", mmA, r=wr_b + ["yTl"], w=[("ps", bA)])
                            P.op("pe", mmP, r=wr_b + ["ypTl"], w=[("ps", bP)])
                            P.op("pe", mmS, r=wr_b + ["ysTl"], w=[("ps", bS)])
                            for i3, bb in enumerate((bA, bP, bS)):
                                P.op("dve", lambda i3=i3, bb=bb: V.tensor_tensor(out=tt[i3][:, 0:ng], in0=PS[:, bb, 0:ng], in1=sig[i3][:, 0:ng], op=ALU.mult),
                                     r=[("ps", bb), ("sig", i3)], w=[("tt", i3)])
                            P.op("dve", lambda: V.tensor_tensor(out=tt[3][:, 0:ng], in0=tt[0][:, 0:ng], in1=tt[1][:, 0:ng], op=ALU.add),
                                 r=[("tt", 0), ("tt", 1)], w=[("tt", 3)])
                            P.op("dve", lambda: V.tensor_tensor(out=mT[:, m, 0:ng], in0=tt[3][:, 0:ng], in1=tt[2][:, 0:ng], op=ALU.add),
                                 r=[("tt", 3), ("tt", 2)], w=[("mT", m)])
                        mr = [("mT", m) for m in range(8)]
                        for ti, tile in enumerate(tiles):
                            jj = 0
                            P.dma("sp", lambda: SP.dma_start(out=xt_[jj][:], in_=xs[tile * 128:(tile + 1) * 128, :]),
                                  r=[("xs", tile)], w=[("xt", jj)])
                            for half in range(2):
                                hc = slice(half * 512, (half + 1) * 512)
                                bo = bank()

                                def mmo():
                                    for k in range(8):
                                        ins = PE.matmul(PS[:, bo, :], lhsT=mT[:, k, ti * 128:(ti + 1) * 128], rhs=wout[:, k, hc],
                                                        start=(k == 0), stop=(k == 7))
                                    return ins
                                P.op("pe", mmo, r=mr + wr_o, w=[("ps", bo)])
                                P.op("dve", lambda: V.tensor_tensor(out=tt[half][:], in0=PS[:, bo, :], in1=modm[s][2][:, hc], op=ALU.mult),
                                     r=[("ps", bo)] + modnames(0, s), w=[("tt", half)])
                                P.op("dve", lambda: V.tensor_tensor(out=xo_[jj][:, hc], in0=tt[half][:], in1=xt_[jj][:, hc], op=ALU.add),
                                     r=[("tt", half), ("xt", jj)], w=[("xo", jj, half)])
                            P.dma("sp", lambda: SP.dma_start(out=xs[tile * 128:(tile + 1) * 128, :], in_=xo_[jj][:]),
                                  r=[("xo", jj, 0), ("xo", jj, 1)], w=[("xs", tile)])
                    P.barrier()
                P.barrier()

            for _ in convs[layer]:
                pass
            with ExitStack() as eP:
                modp = [[sb(eP, "modp%d%d" % (s, k), [128, D]) for k in range(3)] for s in range(2)]
                modulation(layer, 1, modp, g_ffn_r)
                wf = sb(eP, "wfold", [128, 8, 2048], BF16)
                with ExitStack() as ef:
                    wqT = sb(ef, "wqT", [128, 16, D])
                    subkT = sb(ef, "subkT", [128, 16, 128])
                    for c in range(0, 16, 4):
                        P.dma("sp", lambda: SP.dma_start(out=wqT[:, c:c + 4, :], in_=wqT_d[layer, :, c:c + 4, :]), w=[("wqT", c)])
                    P.dma("sp", lambda: SP.dma_start(out=subkT[:], in_=subkT_d[layer, :, :, :]), w=["subkT"])
                    for k in range(8):
                        for c4 in range(4):
                            b = bank()

                            def mm():
                                for cc in range(4):
                                    c = c4 * 4 + cc
                                    ins = PE.matmul(PS[:, b, cc * 128:(cc + 1) * 128], lhsT=wqT[:, c, k * 128:(k + 1) * 128],
                                                    rhs=subkT[:, c, :], start=True, stop=True)
                                return ins
                            P.op("pe", mm, r=[("wqT", c4 * 4), "subkT"], w=[("ps", b)])
                            P.op("act", lambda: A.copy(out=wf[:, k, c4 * 512:(c4 + 1) * 512], in_=PS[:, b, :]),
                                 r=[("ps", b)], w=[("wf", k, c4)])
                    P.barrier()
                wfr = [("wf", k, c4) for k in range(8) for c4 in range(4)]
                xt_ = [sb(eP, "xtP%d" % i, [128, D]) for i in range(3)]
                junk = sb(eP, "junkP", [128, D])
                ss = sb(eP, "ssP", [128, 4])
                h2b_ = [sb(eP, "h2b%d" % i, [128, D], BF16) for i in range(2)]
                h2T = sb(eP, "h2T", [128, 8, 128], BF16)
                sS = sb(eP, "sS", [128, 16, 128])
                sS2 = sb(eP, "sS2", [128, 16, 128])
                svt = sb(eP, "svt", [128, 8, 2, 16])
                sit = sb(eP, "sit", [128, 8, 2, 16], U32)
                sif = sb(eP, "sif", [128, 8, 2, 16])
                cand = sS[:].rearrange("p a b -> p (a b)").rearrange("p (h c) -> p h c", h=8)
                cand2 = sS2[:].rearrange("p a b -> p (a b)").rearrange("p (h c) -> p h c", h=8)
                ts = sb(eP, "ts", [128, 8, 16])
                tp = sb(eP, "tp", [128, 8, 16], U32)
                ta = sb(eP, "ta", [128, 8, 16], U32)
                tb_ = sb(eP, "tb", [128, 8, 16], U32)
                taf = sb(eP, "taf", [128, 8, 16])
                tbf = sb(eP, "tbf", [128, 8, 16])
                eq = sS2[:].rearrange("p a b -> p (a b)").rearrange("p (h a b) -> p h a b", h=8, a=16)
                If = sb(eP, "If", [128, 8, 16])
                Jf = sb(eP, "Jf", [128, 8, 16])
                ef_ = sb(eP, "ef", [128, 128])
                eu_ = [sb(eP, "eu%d" % i, [128, 128], U32) for i in range(2)]
                tsc = sb(eP, "tsc", [128, 8, 16])
                ex = sb(eP, "ex", [128, 8, 16])
                zz = sb(eP, "zz", [128, 8])
                rz = sb(eP, "rz", [128, 8])
                gate_ = [sb(eP, "gate%d" % i, [128, 128]) for i in range(2)]
                actv = sb(eP, "actv", [128, 128])
                gact = sb(eP, "gact", [128, 128])
                wgt = sb(eP, "wgt", [128, 128])
                guv = [sb(eP, "guv%d" % i, [128, 2 * D], BF16) for i in range(NGB)]
                prod = [sb(eP, "prod%d" % i, [128, D], BF16) for i in range(3)]
                junkb = sb(eP, "junkb", [128, D], BF16)
                GS = 4
                diag = [sb(eP, "diag%d" % i, [128, GS, 128], BF16) for i in range(3)]
                tmpo = [sb(eP, "tmpo%d" % i, [128, 512]) for i in range(2)]
                xo = sb(eP, "xoP", [128, D])
                if last:
                    gfin = sb(eP, "gfin", [128, D])
                    P.dma("sp", lambda: SP.dma_start(out=gfin[:], in_=g_fin_r[:, :]), w=["gfin"])
                cnt_u, cnt_v, cnt_p = [0], [0], [0]
                ptiles = list(range(NTILES)) if not last else list(range(NTL))
                pidx = {t_: i_ for i_, t_ in enumerate(ptiles)}

                def stage_A(tile):
                    jj = tile % 2
                    x3 = pidx[tile] % 3
                    s = 0 if tile < NTL else 1
                    P.dma("sp", lambda: SP.dma_start(out=xt_[x3][:], in_=xs[tile * 128:(tile + 1) * 128, :]),
                          r=[("xs", tile)], w=[("xt", x3)])
                    norm_mod(xt_[x3], ("xt", x3), modp[s][0], modp[s][1], modnames(1, s), junk, ss, None, h2b_[jj], ("h2", jj),
                             add_eng="dve")
                    transpose8(h2b_[jj], ("h2", jj), h2T[:], "h2T", b=0)

                    def mm():
                        for n in range(4):
                            for k in range(8):
                                ins = PE.matmul(PS[:, n, :], lhsT=h2T[:, k, :], rhs=wf[:, k, n * 512:(n + 1) * 512],
                                                start=(k == 0), stop=(k == 7))
                        return ins
                    psn = [("ps", n) for n in range(4)]
                    P.op("pe", mm, r=["h2T"] + wfr, w=psn)
                    P.op("act", lambda: A.copy(out=sS[:].rearrange("p a b -> p (a b)"),
                                               in_=PS[:, 0:4, :].rearrange("p a b -> p (a b)")), r=psn, w=["sS"])

                def stage_B(tile):
                    eu = eu_[tile % 2]
                    eun = ("eu", tile % 2)
                    gate = gate_[tile % 2]
                    gaten = ("gate", tile % 2)
                    for c in range(16):
                        h, p_ = c // 2, c % 2
                        P.op("dve", lambda: V.max(out=svt[:, h, p_, 0:8], in_=sS[:, c, :]), r=["sS"], w=[("sv", c, 0)])
                        yield
                    for c in range(16):
                        h, p_ = c // 2, c % 2
                        P.op("dve", lambda: V.max_index(out=sit[:, h, p_, 0:8], in_max=svt[:, h, p_, 0:8], in_values=sS[:, c, :]),
                             r=["sS", ("sv", c, 0)], w=[("si", c, 0)])
                        yield
                    for c in range(16):
                        h, p_ = c // 2, c % 2
                        P.op("dve", lambda: V.match_replace(out=sS2[:, c, :], in_to_replace=svt[:, h, p_, 0:8], in_values=sS[:, c, :],
                                                            imm_value=-1e30), r=["sS", ("sv", c, 0)], w=[("sS2", c)])
                        yield
                    for c in range(16):
                        h, p_ = c // 2, c % 2
                        P.op("dve", lambda: V.max(out=svt[:, h, p_, 8:16], in_=sS2[:, c, :]), r=[("sS2", c)], w=[("sv", c, 1)])
                        yield
                    for c in range(16):
                        h, p_ = c // 2, c % 2
                        P.op("dve", lambda: V.max_index(out=sit[:, h, p_, 8:16], in_max=svt[:, h, p_, 8:16], in_values=sS2[:, c, :]),
                             r=[("sS2", c), ("sv", c, 1)], w=[("si", c, 1)])
                        yield
                    svr = [("sv", c, q) for c in range(16) for q in range(2)]
                    sir = [("si", c, q) for c in range(16) for q in range(2)]
                    s2r = [("sS2", c) for c in range(16)]
                    P.op("dve", lambda: V.tensor_copy(out=sif[:], in_=sit[:]), r=sir, w=["sif"])
                    yield
                    P.op("dve", lambda: V.tensor_tensor(
                        out=cand.rearrange("p h (a b) -> p h a b", a=16),
                        in0=svt[:, :, 0, :].unsqueeze(3).broadcast_to([128, 8, 16, 16]),
                        in1=svt[:, :, 1, :].unsqueeze(2).broadcast_to([128, 8, 16, 16]), op=ALU.add), r=svr, w=["sS"])
                    yield
                    for h in range(8):
                        P.op("dve", lambda: V.max(out=ts[:, h, 0:8], in_=cand[:, h, :]), r=["sS"], w=[("ts", h, 0)])
                        yield
                    for h in range(8):
                        P.op("dve", lambda: V.max_index(out=tp[:, h, 0:8], in_max=ts[:, h, 0:8], in_values=cand[:, h, :]),
                             r=["sS", ("ts", h, 0)], w=[("tp", h, 0)])
                        yield
                    for h in range(8):
                        P.op("dve", lambda: V.match_replace(out=cand2[:, h, :], in_to_replace=ts[:, h, 0:8], in_values=cand[:, h, :],
                                                            imm_value=-1e30), r=["sS", ("ts", h, 0)] + s2r, w=[("cand2", h)])
                        yield
                    for h in range(8):
                        P.op("dve", lambda: V.max(out=ts[:, h, 8:16], in_=cand2[:, h, :]), r=[("cand2", h)], w=[("ts", h, 1)])
                        yield
                    for h in range(8):
                        P.op("dve", lambda: V.max_index(out=tp[:, h, 8:16], in_max=ts[:, h, 8:16], in_values=cand2[:, h, :]),
                             r=[("cand2", h), ("ts", h, 1)], w=[("tp", h, 1)])
                        yield
                    tsr = [("ts", h, q) for h in range(8) for q in range(2)]
                    tpr = [("tp", h, q) for h in range(8) for q in range(2)]
                    c2r = [("cand2", h) for h in range(8)]
                    P.op("dve", lambda: V.tensor_scalar(out=ta[:], in0=tp[:], scalar1=4, scalar2=None, op0=ALU.logical_shift_right),
                         r=tpr, w=["ta"])
                    yield
                    P.op("dve", lambda: V.tensor_scalar(out=tb_[:], in0=tp[:], scalar1=15, scalar2=None, op0=ALU.bitwise_and),
                         r=tpr, w=["tb"])
                    yield
                    P.op("dve", lambda: V.tensor_copy(out=taf[:], in_=ta[:]), r=["ta"], w=["taf"])
                    yield
                    P.op("dve", lambda: V.tensor_copy(out=tbf[:], in_=tb_[:]), r=["tb"], w=["tbf"])
                    yield
                    io4 = iota16[:].unsqueeze(1).unsqueeze(1).broadcast_to([128, 8, 16, 16])
                    for (src, pp, dst, dn) in ((taf, 0, If, "If"), (tbf, 1, Jf, "Jf")):
                        P.op("dve", lambda: V.tensor_tensor(out=eq, in0=src[:].unsqueeze(3).broadcast_to([128, 8, 16, 16]),
                                                            in1=io4, op=ALU.is_equal), r=["taf", "tbf", "iota16"] + c2r + s2r, w=["eqb"])
                        yield
                        P.op("dve", lambda: V.tensor_tensor(out=eq, in0=eq,
                                                            in1=sif[:, :, pp, :].unsqueeze(2).broadcast_to([128, 8, 16, 16]),
                                                            op=ALU.mult), r=["eqb", "sif"], w=["eqb"])
                        yield
                        P.op("dve", lambda: V.tensor_reduce(out=dst[:], in_=eq, axis=AX.X, op=ALU.add), r=["eqb"], w=[dn])
                        yield
                    P.op("dve", lambda: V.scalar_tensor_tensor(out=ef_[:], in0=If[:].rearrange("p a b -> p (a b)"), scalar=128.0,
                                                               in1=Jf[:].rearrange("p a b -> p (a b)"), op0=ALU.mult, op1=ALU.add),
                         r=["If", "Jf"], w=["ef"])
                    yield
                    P.op("dve", lambda: V.tensor_copy(out=eu[:], in_=ef_[:]), r=["ef"], w=[eun])
                    yield
                    P.op("dve", lambda: V.tensor_tensor(out=tsc[:], in0=ts[:], in1=ts[:, :, 0:1].broadcast_to([128, 8, 16]), op=ALU.subtract),
                         r=tsr, w=["tsc"])
                    yield
                    P.op("act", lambda: A.activation(out=ex[:], in_=tsc[:], func=AF.Exp), r=["tsc"], w=["ex"])
                    yield
                    P.op("dve", lambda: V.tensor_reduce(out=zz[:], in_=ex[:], axis=AX.X, op=ALU.add), r=["ex"], w=["zz"])
                    yield
                    P.op("dve", lambda: V.reciprocal(out=rz[:], in_=zz[:]), r=["zz"], w=["rz"])
                    yield
                    P.op("dve", lambda: V.tensor_tensor(out=gate[:].rearrange("p (a b) -> p a b", a=8), in0=ex[:],
                                                        in1=rz[:].unsqueeze(2).broadcast_to([128, 8, 16]), op=ALU.mult),
                         r=["ex", "rz"], w=[gaten])
                    yield

                def stage_CD(tile, genB, hook=None):
                    jj = tile % 2
                    eu = eu_[jj]
                    eun = ("eu", jj)
                    gate = gate_[jj]
                    h2b = h2b_[jj]
                    ngrp = peer_slots // GS
                    ab = 4 + 2 * (pidx[tile] % 2)
                    bis_of = {}
                    di_of = {}

                    def fin_act(g):
                        gs = slice(g * GS, (g + 1) * GS)
                        P.op("act", lambda: A.activation(out=gact[:, gs], in_=actv[:, gs], func=AF.Gelu_apprx_tanh),
                             r=[("actv", g * GS + q) for q in range(GS)], w=[("gact", g)])

                    def fin_rest(g):
                        gs = slice(g * GS, (g + 1) * GS)
                        di = cnt_v[0] % 3
                        cnt_v[0] += 1
                        P.op("dve", lambda: V.tensor_tensor(out=wgt[:, gs], in0=gate[:, gs], in1=gact[:, gs], op=ALU.mult),
                             r=[("gate", jj), ("gact", g)], w=[("wgt", g)])
                        P.op("dve", lambda: V.tensor_tensor(
                            out=diag[di][:],
                            in0=identb[:].unsqueeze(1).broadcast_to([128, GS, 128]),
                            in1=wgt[:, gs].unsqueeze(2).broadcast_to([128, GS, 128]), op=ALU.mult),
                            r=["identb", ("wgt", g)], w=[("diag", di)])
                        for q in range(GS):
                            sl = g * GS + q
                            bi = bis_of[g][q]

                            def mm():
                                for half in range(2):
                                    ins = PE.matmul(PS[:, ab + half, :], lhsT=diag[di][:, q, :],
                                                    rhs=guv[bi][:, D + half * 512:D + (half + 1) * 512],
                                                    start=(sl == 0), stop=(sl == peer_slots - 1))
                                return ins
                            P.op("pe", mm, r=[("guv", bi), ("diag", di)], w=[("ps", ab), ("ps", ab + 1)])

                    for g in range(ngrp):
                        if g >= 1:
                            fin_act(g - 1)
                        bis = []
                        for q in range(GS):
                            sl = g * GS + q
                            bi = cnt_u[0] % NGB
                            cnt_u[0] += 1
                            bis.append(bi)
                            pi = cnt_p[0] % 3
                            cnt_p[0] += 1
                            P.dma("pool", lambda: G.indirect_dma_start(
                                out=guv[bi][:], out_offset=None, in_=uvb[layer][:, :],
                                in_offset=bass.IndirectOffsetOnAxis(ap=eu[:, sl:sl + 1], axis=0)), r=[eun] + tabnames(layer), w=[("guv", bi)])
                            P.op("dve", lambda: V.tensor_tensor(out=prod[pi][:], in0=guv[bi][:, 0:D], in1=h2b[:], op=ALU.mult),
                                 r=[("guv", bi), ("h2", jj)], w=[("prod", pi)])
                            P.op("act", lambda: A.activation(out=junkb[:], in_=prod[pi][:], func=AF.Copy, accum_out=actv[:, sl:sl + 1]),
                                 r=[("prod", pi)], w=["junkb", ("actv", sl)])
                            if genB is not None:
                                for _ in range(2):
                                    next(genB, None)
                        bis_of[g] = bis
                        if g >= 1:
                            fin_rest(g - 1)
                        if g == 2 and hook is not None:
                            hook()
                    fin_act(ngrp - 1)
                    fin_rest(ngrp - 1)
                    if genB is not None:
                        for _ in genB:
                            pass

                def stage_E(tile):
                    jj = pidx[tile] % 3
                    ab = 4 + 2 * (pidx[tile] % 2)
                    s = 0 if tile < NTL else 1
                    for half in range(2):
                        hc = slice(half * 512, (half + 1) * 512)
                        P.op("dve", lambda: V.tensor_tensor(out=tmpo[half][:], in0=PS[:, ab + half, :], in1=modp[s][2][:, hc], op=ALU.mult),
                             r=[("ps", ab + half)] + modnames(1, s), w=[("tmpo", half)])
                        P.op("dve", lambda: V.tensor_tensor(out=xo[:, hc], in0=tmpo[half][:], in1=xt_[jj][:, hc], op=ALU.add),
                             r=[("tmpo", half), ("xt", jj)], w=[("xo", half)])
                    xor_ = [("xo", 0), ("xo", 1)]
                    if not last:
                        P.dma("sp", lambda: SP.dma_start(out=xs[tile * 128:(tile + 1) * 128, :], in_=xo[:]), r=xor_, w=[("xs", tile)],
                              is_out=dbg)
                    else:
                        P.op("dve", lambda: V.scalar_tensor_tensor(out=junk[:], in0=xo[:], scalar=1.0, in1=xo[:],
                                                                   op0=ALU.mult, op1=ALU.mult, accum_out=ss[:, 0:1]),
                             r=xor_, w=["junk", "ss0"])
                        P.op("dve", lambda: V.tensor_scalar(out=ss[:, 1:2], in0=ss[:, 0:1], scalar1=1.0 / D, scalar2=EPS,
                                                            op0=ALU.mult, op1=ALU.add), r=["ss0"], w=["ss1"])
                        P.op("act", lambda: A.activation(out=ss[:, 2:3], in_=ss[:, 1:2], func=AF.Ln), r=["ss1"], w=["ss2"])
                        P.op("act", lambda: A.activation(out=ss[:, 3:4], in_=ss[:, 2:3], func=AF.Exp, scale=-0.5), r=["ss2"], w=["ss3"])
                        P.op("dve", lambda: V.scalar_tensor_tensor(out=junk[:], in0=xo[:], scalar=ss[:, 3:4], in1=gfin[:],
                                                                   op0=ALU.mult, op1=ALU.mult),
                             r=xor_ + ["ss3", "gfin"], w=["junk"])
                        P.dma("sp", lambda: SP.dma_start(out=y_out[tile * 128:(tile + 1) * 128, :], in_=junk[:]), r=["junk"],
                              w=[("yout", tile)], is_out=True)

                stage_A(ptiles[0])
                for _ in stage_B(ptiles[0]):
                    pass
                for i_, tile in enumerate(ptiles):
                    nxt = ptiles[i_ + 1] if i_ + 1 < len(ptiles) else None
                    genB = None
                    if nxt is not None:
                        stage_A(nxt)
                        genB = stage_B(nxt)
                    prev = ptiles[i_ - 1] if i_ >= 1 else None
                    stage_CD(tile, genB, hook=(lambda: stage_E(prev)) if prev is not None else None)
                stage_E(ptiles[-1])
                P.barrier()
        P.finish()
        print("instructions:", P.n_ins)
    return nc


def prep_inputs(inp, NTL=32, n_cores=8):
    f = np.float32
    T = NTL * 128
    TT = T + CTX
    x = np.asarray(inp["x"], f)
    c = np.asarray(inp["c"], f)
    ctx = np.asarray(inp["ctx"], f)
    c_ctx = np.asarray(inp["c_ctx"], f)
    w_in = np.asarray(inp["w_in"], f)
    rp = np.concatenate([np.arange(16, 32), np.arange(0, 16), np.arange(48, 64), np.arange(32, 48)])
    d64 = np.arange(64)
    qcols = np.concatenate([np.concatenate([j * 64 + d64, (4 + j) * 64 + d64]) for j in range(4)])
    qrcols = np.concatenate([np.concatenate([j * 64 + rp, (4 + j) * 64 + rp]) for j in range(4)])
    kcols = 512 + np.arange(128)
    krcols = 512 + np.concatenate([rp, 64 + rp])
    vcols = 640 + np.arange(128)
    pcols = 768 + np.arange(256)
    ucols = 1024 + np.arange(256)
    vscols = 1280 + np.arange(256)
    w1 = np.ascontiguousarray(w_in[:, :, np.concatenate([kcols, krcols, vcols, pcols])])
    w2a = np.ascontiguousarray(w_in[:, :, np.concatenate([qcols, qrcols, ucols, vscols])])
    w2g = np.ascontiguousarray(w_in[:, :, 1536:4608])
    rows = T // 64
    row = np.repeat(np.arange(rows, dtype=f), 64)
    col = np.tile(np.arange(64, dtype=f), rows)
    inv = (np.float32(10000.0) ** (-np.arange(16, dtype=f) / np.float32(16))).astype(f)
    ar, ac = (row[:, None] * inv).astype(f), (col[:, None] * inv).astype(f)
    cos_d = np.concatenate([np.cos(ar), np.cos(ar), np.cos(ac), np.cos(ac)], axis=1).astype(f)
    sin_d = np.concatenate([-np.sin(ar), np.sin(ar), -np.sin(ac), np.sin(ac)], axis=1).astype(f)
    cos_d = np.concatenate([cos_d, np.ones((CTX, 64), f)], axis=0)
    sin_d = np.concatenate([sin_d, np.zeros((CTX, 64), f)], axis=0)
    ropeC = np.ascontiguousarray(np.concatenate([cos_d.T, cos_d.T], axis=0))
    ropeS = np.ascontiguousarray(np.concatenate([sin_d.T, sin_d.T], axis=0))
    sink = np.asarray(inp["attn_sink"], f)
    sink_r = np.ascontiguousarray(np.repeat(sink.reshape(2, 1, 2, 4, 1), 128, axis=4).reshape(2, 1, 2, 512))
    pool_w = np.asarray(inp["pool_w"], f)
    pwbd = np.zeros((2, 128, 2, 128), f)
    for g in range(4):
        o = (g % 2) * 64
        pwbd[:, o:o + 64, g // 2, o:o + 64] = pool_w[:, g]
    pscale = np.ascontiguousarray(np.asarray(inp["pool_scale"], f).reshape(2, 2, 128).transpose(0, 2, 1))
    rc = np.zeros((128, 2, TT), f)
    for g, size in enumerate(POOL_SIZES):
        for (a, l) in ((0, T), (T, CTX)):
            t = np.arange(l)
            lo = np.clip(t - size // 2, 0, l)
            hi = np.clip(t + size // 2, 0, l)
            o = (g % 2) * 64
            rc[o:o + 64, g // 2, a:a + l] = (1.0 / (hi - lo).astype(f))[None, :]
    sgu_wT = np.ascontiguousarray(np.asarray(inp["sgu_w"], f).transpose(0, 3, 1, 2))
    sgu_b_r = np.ascontiguousarray(np.broadcast_to(np.asarray(inp["sgu_b"], f)[:, None], (2, 64, 4, 128)))
    wbrA = np.ascontiguousarray(np.asarray(inp["w_br_attn"], f).reshape(2, 8, 64, D).transpose(0, 2, 1, 3))
    wbrP = np.ascontiguousarray(np.asarray(inp["w_br_pool"], f).reshape(2, 2, 128, D).transpose(0, 2, 1, 3))
    wbrS = np.ascontiguousarray(np.asarray(inp["w_br_sgu"], f).reshape(2, 4, 64, D).transpose(0, 2, 1, 3))
    wout = np.ascontiguousarray(np.asarray(inp["w_out"], f).reshape(2, 8, 128, D).transpose(0, 2, 1, 3))
    wqT = np.ascontiguousarray(np.asarray(inp["peer_wq"], f).reshape(2, D, 16, 128).transpose(0, 3, 2, 1))
    subkT = np.ascontiguousarray(np.asarray(inp["peer_subkeys"], f).reshape(2, 16, 128, 128).transpose(0, 3, 1, 2))
    jj, ii = np.meshgrid(np.arange(128), np.arange(128), indexing="ij")
    mA = np.where(jj >= ii, 0.0, NEG).astype(f)
    mB = np.where(jj <= ii, 0.0, NEG).astype(f)
    maskAB = np.ascontiguousarray(np.stack([np.tile(mA, (1, 4)), np.tile(mB, (1, 4))], axis=1))
    shared = {
        "w_mod": np.asarray(inp["w_mod"], f),
        "b_mod_r": np.ascontiguousarray(np.broadcast_to(np.asarray(inp["b_mod"], f)[:, None], (2, 128, 6 * D))),
        "g_mix_r": np.ascontiguousarray(np.broadcast_to(np.asarray(inp["g_mix"], f)[:, None], (2, 128, D))),
        "g_ffn_r": np.ascontiguousarray(np.broadcast_to(np.asarray(inp["g_ffn"], f)[:, None], (2, 128, D))),
        "g_fin_r": np.ascontiguousarray(np.broadcast_to(np.asarray(inp["g_final"], f)[None], (128, D))),
        "w1": w1, "w2a": w2a, "w2g": w2g, "ropeC": ropeC, "ropeS": ropeS, "sink_r": sink_r, "pwbd": pwbd,
        "pscale": pscale, "rc": rc, "sgu_wT": sgu_wT, "sgu_b_r": sgu_b_r, "wbrA": wbrA, "wbrP": wbrP, "wbrS": wbrS,
        "wout": wout, "wqT": wqT, "subkT": subkT,
        "peer_u0": np.ascontiguousarray(np.asarray(inp["peer_u"], f)[0]), "peer_u1": np.ascontiguousarray(np.asarray(inp["peer_u"], f)[1]),
        "peer_v0": np.ascontiguousarray(np.asarray(inp["peer_v"], f)[0]), "peer_v1": np.ascontiguousarray(np.asarray(inp["peer_v"], f)[1]),
        "maskAB": maskAB, "ident": np.eye(128, dtype=f),
        "iota16": np.ascontiguousarray(np.broadcast_to(np.arange(16, dtype=f)[None], (128, 16))),
    }
    maps = []
    for b in range(n_cores):
        m = dict(shared)
        m["x"] = np.ascontiguousarray(x[b, :T])
        m["ctx"] = np.ascontiguousarray(ctx[b])
        cv = np.stack([c[b].reshape(8, 128).T, c_ctx.reshape(8, 128).T], axis=1)
        m["cvec"] = np.ascontiguousarray(cv.astype(f))
        maps.append(m)
    return maps


def kernel(**inputs):
    n = 8
    nc = build(NTL=32, n_layers=2)
    maps = prep_inputs(inputs, NTL=32, n_cores=n)
    res = run_bass_kernel_spmd(nc, maps, core_ids=list(range(n)))
    return np.stack([np.asarray(r["y"], np.float32) for r in res.results], axis=0)
```
